# Optimizing a Trainium2 kernel written in Bass

```python
import math
import jax, jax.numpy as jnp
from jax import lax
import numpy as np

D_MODEL = 1024
BATCH = 8
SEQ = 4096
DEPTH = 1

PLE_DIM = 256
EPS = 1e-6
NEG = -1e30
FORCE = 1e6

HG_HEADS = 4
HG_KDIM = 128
HG_VDIM = 128
HG_WIDTH = HG_HEADS * HG_VDIM
HG_CHUNK = 64

NSA_HEADS = 8
NSA_KV = 2
NSA_GROUP = NSA_HEADS // NSA_KV
HEAD_DIM = 64
NSA_WIDTH = NSA_HEADS * HEAD_DIM
KV_WIDTH = NSA_KV * HEAD_DIM
CMP_LEN = 32
CMP_STRIDE = 16
CMP_HIDDEN = 128
SEL_BLOCK = 64
SEL_TOPK = 16
WINDOW = 512
Q_BLOCK = 64

ROT_DIM = HEAD_DIM // 4
ROPE_THETA = 500000.0

MIX_WIDTH = HG_WIDTH + NSA_WIDTH

D_FF = 2816
CONV_W = 3

IN_SIZES = (HG_HEADS * HG_KDIM, HG_HEADS * HG_KDIM, HG_WIDTH, HG_WIDTH, NSA_WIDTH,
            KV_WIDTH, KV_WIDTH, KV_WIDTH, KV_WIDTH, KV_WIDTH, KV_WIDTH, 3 * NSA_HEADS)
IN_TOTAL = 2 * HG_HEADS * HG_KDIM + 2 * HG_WIDTH + NSA_WIDTH + 6 * KV_WIDTH + 3 * NSA_HEADS

kernel_name = "hymba_hgrn2_nsa_convffn_block"


def rms_norm(x, g):
    xf = x.astype(jnp.float32)
    y = xf * lax.rsqrt(jnp.mean(xf * xf, axis=-1, keepdims=True) + EPS)
    return (y * g.astype(jnp.float32)).astype(x.dtype)


def partial_rope(x, pos):
    half = ROT_DIM // 2
    inv = ROPE_THETA ** (-jnp.arange(half, dtype=jnp.float32) / half)
    ang = pos.astype(jnp.float32)[:, None] * inv[None, :]
    cos = jnp.cos(ang)[:, None, :]
    sin = jnp.sin(ang)[:, None, :]
    xr = x[..., :ROT_DIM].astype(jnp.float32)
    x1, x2 = xr[..., :half], xr[..., half:]
    rot = jnp.concatenate([x1 * cos - x2 * sin, x2 * cos + x1 * sin], axis=-1)
    return jnp.concatenate([rot.astype(x.dtype), x[..., ROT_DIM:]], axis=-1)


def hgrn2_mixer(q, f_pre, v, g, lb, norm_g):
    B, T, _ = q.shape
    dt = q.dtype
    f32 = jnp.float32
    n_ch = T // HG_CHUNK
    f = lb + (1.0 - lb) * jax.nn.sigmoid(f_pre.astype(f32))
    k = 1.0 - f
    logf = jnp.log(f)
    qf = jax.nn.silu(q.astype(f32)) * HG_KDIM ** -0.5

    def chunks(a, d):
        return a.reshape(B, n_ch, HG_CHUNK, HG_HEADS, d).transpose(1, 0, 3, 2, 4)

    qc = chunks(qf, HG_KDIM)
    kc = chunks(k, HG_KDIM)
    vc = chunks(v.astype(f32), HG_VDIM)
    bc = jnp.cumsum(chunks(logf, HG_KDIM), axis=3)
    causal = jnp.tril(jnp.ones((HG_CHUNK, HG_CHUNK), bool))[:, :, None]

    def step(S, inp):
        qt, kt, vt, bt = inp
        decay = jnp.exp(jnp.where(causal, bt[:, :, :, None, :] - bt[:, :, None, :, :], -jnp.inf))
        a = jnp.einsum('bhtk,bhsk,bhtsk->bhts', qt, kt, decay)
        o = (jnp.einsum('bhts,bhsv->bhtv', a, vt)
             + jnp.einsum('bhtk,bhkv->bhtv', qt * jnp.exp(bt), S))
        b_last = bt[:, :, -1:, :]
        S = (jnp.exp(b_last[:, :, 0, :])[..., None] * S
             + jnp.einsum('bhsk,bhsv->bhkv', kt * jnp.exp(b_last - bt), vt))
        return S, o

    S0 = jnp.zeros((B, HG_HEADS, HG_KDIM, HG_VDIM), f32)
    _, o = lax.scan(step, S0, (qc, kc, vc, bc))
    o = o.transpose(1, 0, 3, 2, 4).reshape(B, T, HG_HEADS, HG_VDIM)
    o = rms_norm(o, norm_g) * jax.nn.silu(g.astype(f32)).reshape(B, T, HG_HEADS, HG_VDIM)
    return o.reshape(B, T, HG_WIDTH).astype(dt)


def nsa_mixer(q, k_cmp, v_cmp, k_sel, v_sel, k_win, v_win, gate_pre,
              q_norm_g, k_norm_g, cmp_pe, cmp_w1, cmp_w2, out_norm_g):
    B, T, _ = q.shape
    dt = q.dtype
    f32 = jnp.float32
    pos = jnp.arange(T)
    scale = HEAD_DIM ** -0.5
    q = partial_rope(rms_norm(q.reshape(B, T, NSA_HEADS, HEAD_DIM), q_norm_g), pos)
    q = q.reshape(B, T, NSA_KV, NSA_GROUP, HEAD_DIM)

    def kv_heads(a):
        return a.reshape(B, T, NSA_KV, HEAD_DIM)

    n_cmp = (T - CMP_LEN) // CMP_STRIDE + 1
    cmp_start = jnp.arange(n_cmp) * CMP_STRIDE
    cmp_end = cmp_start + CMP_LEN - 1
    tok = cmp_start[:, None] + jnp.arange(CMP_LEN)[None, :]

    def compress(a, j):
        blk = kv_heads(a)[:, tok] + cmp_pe[j][None, None, :, None, :]
        blk = blk.transpose(0, 1, 3, 2, 4).reshape(B, n_cmp, NSA_KV, CMP_LEN * HEAD_DIM)
        return jax.nn.silu(blk @ cmp_w1[j]) @ cmp_w2[j]

    kc = partial_rope(rms_norm(compress(k_cmp, 0), k_norm_g[0]), cmp_end)
    vc = compress(v_cmp, 1)
    s = jnp.einsum('btghd,bcgd->bghtc', q, kc).astype(f32) * scale
    cmask = cmp_end[None, :] <= pos[:, None]
    p_cmp = jax.nn.softmax(jnp.where(cmask, s, NEG), axis=-1) * cmask
    o_cmp = jnp.einsum('bghtc,bcgd->btghd', p_cmp.astype(dt), vc)

    n_sel = T // SEL_BLOCK
    top = min(SEL_TOPK, n_sel)
    sel_start = jnp.arange(n_sel) * SEL_BLOCK
    overlap = jnp.clip(jnp.minimum(cmp_start[:, None] + CMP_LEN, sel_start[None, :] + SEL_BLOCK)
                       - jnp.maximum(cmp_start[:, None], sel_start[None, :]), 0, None)
    overlap = overlap.astype(f32) / CMP_LEN
    imp = jnp.einsum('bghtc,cn->btgn', p_cmp, overlap)
    cur = pos // SEL_BLOCK
    blk = jnp.arange(n_sel)
    forced = (blk[None, :] == 0) | (blk[None, :] == cur[:, None]) | (blk[None, :] == cur[:, None] - 1)
    visible = sel_start[None, :] <= pos[:, None]
    imp = jnp.where(visible[:, None, :], imp + jnp.where(forced, FORCE, 0.0)[:, None, :], NEG)
    _, sel_idx = lax.top_k(imp, top)

    def to_blocks(a):
        return a.reshape(B, n_sel, SEL_BLOCK, NSA_KV, HEAD_DIM).transpose(0, 3, 1, 2, 4)

    ks = to_blocks(partial_rope(rms_norm(kv_heads(k_sel), k_norm_g[1]), pos))
    vs = to_blocks(kv_heads(v_sel))
    pad = ((0, 0), (WINDOW, 0), (0, 0), (0, 0))
    kw = jnp.pad(partial_rope(rms_norm(kv_heads(k_win), k_norm_g[2]), pos), pad)
    vw = jnp.pad(kv_heads(v_win), pad)
    n_qb = T // Q_BLOCK
    q_blk = q.reshape(B, n_qb, Q_BLOCK, NSA_KV, NSA_GROUP, HEAD_DIM).swapaxes(0, 1)
    idx_blk = sel_idx.reshape(B, n_qb, Q_BLOCK, NSA_KV, top).swapaxes(0, 1)
    bi = jnp.arange(B)[:, None, None, None]
    gi = jnp.arange(NSA_KV)[None, None, :, None]
    span = Q_BLOCK + WINDOW

    def block_attend(args):
        j, qb, ib = args
        t = j * Q_BLOCK + jnp.arange(Q_BLOCK)
        ksb = ks[bi, gi, ib]
        vsb = vs[bi, gi, ib]
        ss = jnp.einsum('bqghd,bqgnld->bqghnl', qb, ksb).astype(f32) * scale
        kpos = ib[..., None] * SEL_BLOCK + jnp.arange(SEL_BLOCK)
        m = (kpos <= t[None, :, None, None, None])[:, :, :, None]
        ss = jnp.where(m, ss, NEG).reshape(B, Q_BLOCK, NSA_KV, NSA_GROUP, top * SEL_BLOCK)
        ps = jax.nn.softmax(ss, axis=-1).reshape(B, Q_BLOCK, NSA_KV, NSA_GROUP, top, SEL_BLOCK)
        o_s = jnp.einsum('bqghnl,bqgnld->bqghd', ps.astype(dt), vsb)
        kwb = lax.dynamic_slice_in_dim(kw, j * Q_BLOCK, span, axis=1)
        vwb = lax.dynamic_slice_in_dim(vw, j * Q_BLOCK, span, axis=1)
        spos = j * Q_BLOCK - WINDOW + jnp.arange(span)
        dist = t[:, None] - spos[None, :]
        wm = (spos[None, :] >= 0) & (dist >= 0) & (dist < WINDOW)
        sw = jnp.einsum('bqghd,bsgd->bghqs', qb, kwb).astype(f32) * scale
        pw = jax.nn.softmax(jnp.where(wm, sw, NEG), axis=-1)
        o_w = jnp.einsum('bghqs,bsgd->bqghd', pw.astype(dt), vwb)
        return o_s, o_w

    o_sel, o_win = lax.map(block_attend, (jnp.arange(n_qb), q_blk, idx_blk))
    o_sel = o_sel.swapaxes(0, 1).reshape(B, T, NSA_HEADS, HEAD_DIM)
    o_win = o_win.swapaxes(0, 1).reshape(B, T, NSA_HEADS, HEAD_DIM)
    o_cmp = o_cmp.reshape(B, T, NSA_HEADS, HEAD_DIM)

    gate = jax.nn.sigmoid(gate_pre.astype(f32)).reshape(B, T, 3, NSA_HEADS)[..., None].astype(dt)
    o = gate[:, :, 0] * o_cmp + gate[:, :, 1] * o_sel + gate[:, :, 2] * o_win
    return rms_norm(o, out_norm_g).reshape(B, T, NSA_WIDTH)


def conv_ffn(hn, w_up, conv_w, conv_b, w_down):
    u = hn @ w_up
    c = u.shape[-1]
    u = lax.conv_general_dilated(u, conv_w.reshape(CONV_W, 1, c), window_strides=(1,),
                                 padding=[(CONV_W - 1, 0)], dimension_numbers=('NWC', 'WIO', 'NWC'),
                                 feature_group_count=c) + conv_b
    gate, up = jnp.split(u, 2, axis=-1)
    return (jax.nn.silu(gate) * up) @ w_down


def setup_inputs(seed: int = 0) -> dict:
    key = jax.random.key(seed)
    ks = jax.random.split(key, 22)
    f32 = jnp.float32

    def nrm(k, shape, scale):
        return jax.random.normal(k, shape, f32) * scale

    def gain(k, shape):
        return 1.0 + 0.02 * jax.random.normal(k, shape, f32)

    return {
        "x": nrm(ks[0], (BATCH, SEQ, D_MODEL), 1.0),
        "p": nrm(ks[1], (DEPTH, BATCH, SEQ, PLE_DIM), 1.0),
        "attn_norm_g": gain(ks[2], (DEPTH, D_MODEL)),
        "w_in": nrm(ks[3], (DEPTH, D_MODEL, IN_TOTAL), D_MODEL ** -0.5),
        "hg_lb_logits": nrm(ks[4], (DEPTH + 1, HG_HEADS * HG_KDIM), 0.5),
        "hg_norm_g": gain(ks[5], (DEPTH, HG_VDIM)),
        "nsa_q_norm_g": gain(ks[6], (DEPTH, HEAD_DIM)),
        "nsa_k_norm_g": gain(ks[7], (DEPTH, 3, HEAD_DIM)),
        "cmp_pe": nrm(ks[8], (DEPTH, 2, CMP_LEN, HEAD_DIM), 0.1),
        "cmp_w1": nrm(ks[9], (DEPTH, 2, CMP_LEN * HEAD_DIM, CMP_HIDDEN), (CMP_LEN * HEAD_DIM) ** -0.5),
        "cmp_w2": nrm(ks[10], (DEPTH, 2, CMP_HIDDEN, HEAD_DIM), CMP_HIDDEN ** -0.5),
        "nsa_out_norm_g": gain(ks[11], (DEPTH, HEAD_DIM)),
        "w_out": nrm(ks[12], (DEPTH, MIX_WIDTH, D_MODEL), MIX_WIDTH ** -0.5),
        "ffn_norm_g": gain(ks[13], (DEPTH, D_MODEL)),
        "w_up": nrm(ks[14], (DEPTH, D_MODEL, 2 * D_FF), D_MODEL ** -0.5),
        "conv_w": nrm(ks[15], (DEPTH, CONV_W, 2 * D_FF), CONV_W ** -0.5),
        "conv_b": nrm(ks[16], (DEPTH, 2 * D_FF), 0.02),
        "w_down": nrm(ks[17], (DEPTH, D_FF, D_MODEL), D_FF ** -0.5),
        "ple_gate_norm_g": gain(ks[18], (DEPTH, D_MODEL)),
        "w_ple_gate": nrm(ks[19], (DEPTH, D_MODEL, D_MODEL), D_MODEL ** -0.5),
        "w_ple": nrm(ks[20], (DEPTH, PLE_DIM, D_MODEL), PLE_DIM ** -0.5),
        "ple_norm_g": gain(ks[21], (DEPTH, D_MODEL)),
    }


def reference(x, p, attn_norm_g, w_in, hg_lb_logits, hg_norm_g, nsa_q_norm_g, nsa_k_norm_g,
              cmp_pe, cmp_w1, cmp_w2, nsa_out_norm_g, w_out, ffn_norm_g, w_up, conv_w, conv_b,
              w_down, ple_gate_norm_g, w_ple_gate, w_ple, ple_norm_g):
    lb_all = jnp.cumsum(jax.nn.softmax(hg_lb_logits.astype(jnp.float32), axis=0), axis=0)
    split_points = []
    acc = 0
    for size in IN_SIZES[:-1]:
        acc += size
        split_points.append(acc)
    h = x
    for i in range(DEPTH):
        hn = rms_norm(h, attn_norm_g[i])
        proj = hn @ w_in[i]
        (hq, hf, hi, hg, nq, nkc, nvc, nks, nvs, nkw, nvw, ngate) = jnp.split(proj, split_points, axis=-1)
        o_hg = hgrn2_mixer(hq, hf, hi, hg, lb_all[i], hg_norm_g[i])
        o_nsa = nsa_mixer(nq, nkc, nvc, nks, nvs, nkw, nvw, ngate, nsa_q_norm_g[i], nsa_k_norm_g[i],
                          cmp_pe[i], cmp_w1[i], cmp_w2[i], nsa_out_norm_g[i])
        h = h + jnp.concatenate([o_hg, o_nsa], axis=-1) @ w_out[i]
        h = h + conv_ffn(rms_norm(h, ffn_norm_g[i]), w_up[i], conv_w[i], conv_b[i], w_down[i])
        e = rms_norm(p[i] @ w_ple[i], ple_norm_g[i])
        gate = jax.nn.sigmoid(rms_norm(h, ple_gate_norm_g[i]) @ w_ple_gate[i])
        h = h + gate * e
    return h
```

```python
from contextlib import ExitStack
import numpy as np
import ml_dtypes
import concourse.bass as bass
import concourse.mybir as mybir
from concourse.bass_utils import run_bass_kernel_spmd

F32 = mybir.dt.float32
BF16 = mybir.dt.bfloat16
ALU = mybir.AluOpType
AF = mybir.ActivationFunctionType
AX = mybir.AxisListType

D = 1024
IN_TOTAL = 3352
DFF = 2816
EPS = 1e-6
NEGM = -30000.0
B_STAGE = 9


class Sched:
    def __init__(self, nc, n_dma_sems=48):
        self.nc = nc
        self.engs = {"pe": nc.tensor, "act": nc.scalar, "dve": nc.vector,
                     "pool": nc.gpsimd, "sp": nc.sync}
        self.sem = {}
        self.cnt = {}
        for k in ("pe", "act", "dve", "pool"):
            self.sem[k] = nc.alloc_semaphore("s_" + k)
            self.cnt[k] = 0
        self.dma_sems = [nc.alloc_semaphore("s_dma%d" % i) for i in range(n_dma_sems)]
        self.dma_val = [0] * n_dma_sems
        self.dma_rr = 0
        self.waited = {}
        self.last_w = {}
        self.readers = {}
        self.nwaits = 0
        self.inflight = {}
        self.max_desc = 1536

    def _wait(self, eng, tok):
        sem, val, key = tok
        if key == eng and eng == "pe":
            return
        k = (eng, key)
        if self.waited.get(k, 0) >= val:
            return
        self.engs[eng].wait_ge(sem, val)
        self.nwaits += 1
        self.waited[k] = val

    def deps(self, eng, reads, writes):
        toks = []
        for r in reads:
            t = self.last_w.get(r)
            if t is not None:
                toks.append(t)
        for w in writes:
            t = self.last_w.get(w)
            if t is not None:
                toks.append(t)
            toks.extend(self.readers.get(w, ()))
        for t in toks:
            self._wait(eng, t)

    def commit(self, tok, reads, writes):
        for w in writes:
            self.last_w[w] = tok
            self.readers[w] = []
        for r in reads:
            if r in writes:
                continue
            lst = self.readers.setdefault(r, [])
            lst[:] = [t for t in lst if t[2] != tok[2]]
            lst.append(tok)

    def op(self, eng, reads, writes, fn):
        self.deps(eng, reads, writes)
        ins = fn(self.engs[eng])
        self.cnt[eng] += 1
        ins.then_inc(self.sem[eng], 1)
        tok = (self.sem[eng], self.cnt[eng], eng)
        self.commit(tok, reads, writes)
        return tok

    @staticmethod
    def _ndesc(ap):
        dims = list(ap.ap)
        total = 1
        for st, n in dims:
            total *= n
        run = 1
        for st, n in reversed(dims[1:]):
            if st == run:
                run *= n
            else:
                break
        return max(1, total // max(run, 1))

    def dma(self, out, in_, reads, writes, q="sp", **kw):
        nd = max(self._ndesc(out), self._ndesc(in_))
        fifo = self.inflight.setdefault(q, [])
        while fifo and sum(d for _, d in fifo) + nd > self.max_desc:
            tok0, _ = fifo.pop(0)
            self._wait(q, tok0)
        tok = self._dma(out, in_, reads, writes, q, **kw)
        fifo.append((tok, nd))
        return tok

    def _dma(self, out, in_, reads, writes, q="sp", **kw):
        i = self.dma_rr
        self.dma_rr = (self.dma_rr + 1) % len(self.dma_sems)
        sem = self.dma_sems[i]
        key = "dma%d" % i
        if self.dma_val[i] > 0:
            self._wait(q, (sem, self.dma_val[i], key))
        self.deps(q, reads, writes)
        self.dma_val[i] += 16
        self.engs[q].dma_start(out=out, in_=in_, **kw).then_inc(sem, 16)
        tok = (sem, self.dma_val[i], key)
        self.commit(tok, reads, writes)
        return tok


def _names(aps):
    out = []
    for a in aps:
        if a is None or isinstance(a, (int, float)):
            continue
        n = a.name
        if n not in out:
            out.append(n)
    return out


class K:
    def __init__(self, nc):
        self.nc = nc
        self.S = Sched(nc)
        self.out_toks = []
        self.es = None

    def sb(self, name, shape, dt=F32):
        return self.es.enter_context(self.nc.sbuf_tensor(name, list(shape), dt))[:]

    def ps(self, name, shape, dt=F32):
        return self.es.enter_context(self.nc.psum_tensor(name, list(shape), dt))[:]

    def barrier(self):
        S = self.S
        toks = [(S.sem[e], S.cnt[e], e) for e in ("pe", "act", "dve", "pool") if S.cnt[e] > 0]
        toks += [(S.dma_sems[i], S.dma_val[i], "dma%d" % i) for i in range(len(S.dma_sems))
                 if S.dma_val[i] > 0]
        for eng in ("sp", "pe", "act", "dve", "pool"):
            for t in toks:
                S._wait(eng, t)
        S.last_w.clear()
        S.readers.clear()

    def mm1(self, out, lhsT, rhs, start, stop):
        return self.S.op("pe", _names([lhsT, rhs]), _names([out]),
                         lambda e: e.matmul(out, lhsT=lhsT, rhs=rhs, start=start, stop=stop))

    def vmax(self, out, in_):
        return self.S.op("dve", _names([in_]), _names([out]), lambda e: e.max(out=out, in_=in_))

    def vmatch(self, out, mx, vals, imm):
        return self.S.op("dve", _names([mx, vals]), _names([out]),
                         lambda e: e.match_replace(out=out, in_to_replace=mx, in_values=vals,
                                                   imm_value=imm))

    def recip(self, out, in_):
        return self.S.op("dve", _names([in_]), _names([out]), lambda e: e.reciprocal(out=out, in_=in_))

    def act(self, out, in_, func, bias=None, scale=None, accum_out=None, eng="act"):
        kw = {}
        if bias is not None:
            kw["bias"] = bias
        if scale is not None:
            kw["scale"] = scale
        if accum_out is not None:
            kw["accum_out"] = accum_out
        rd = _names([in_, bias, scale])
        wr = _names([out, accum_out])
        return self.S.op(eng, rd, wr, lambda e: e.activation(out=out, in_=in_, func=func, **kw))

    def tt(self, out, in0, in1, op, eng="dve"):
        return self.S.op(eng, _names([in0, in1]), _names([out]),
                         lambda e: e.tensor_tensor(out=out, in0=in0, in1=in1, op=op))

    def ts(self, out, in0, s1, s2, op0, op1=None, eng="dve"):
        def f(e):
            if op1 is None:
                return e.tensor_scalar(out=out, in0=in0, scalar1=s1, scalar2=None, op0=op0)
            return e.tensor_scalar(out=out, in0=in0, scalar1=s1, scalar2=s2, op0=op0, op1=op1)
        return self.S.op(eng, _names([in0, s1, s2]), _names([out]), f)

    def stt(self, out, in0, scalar, in1, op0, op1, eng="dve"):
        return self.S.op(eng, _names([in0, scalar, in1]), _names([out]),
                         lambda e: e.scalar_tensor_tensor(out=out, in0=in0, scalar=scalar, in1=in1,
                                                          op0=op0, op1=op1))

    def cp(self, out, in_, eng="dve"):
        if eng == "act":
            return self.S.op("act", _names([in_]), _names([out]), lambda e: e.copy(out=out, in_=in_))
        return self.S.op(eng, _names([in_]), _names([out]), lambda e: e.tensor_copy(out=out, in_=in_))

    def rsum(self, out, in_, eng="dve"):
        return self.S.op(eng, _names([in_]), _names([out]),
                         lambda e: e.reduce_sum(out=out, in_=in_, axis=AX.X))

    def memset(self, out, val, eng="dve"):
        return self.S.op(eng, [], _names([out]), lambda e: e.memset(out, val))

    def mm(self, out, pairs, extra_w=()):
        rd = _names([a for p in pairs for a in p])
        n = len(pairs)

        def f(e):
            for i, (l, r) in enumerate(pairs):
                ins = e.matmul(out, lhsT=l, rhs=r, start=(i == 0), stop=(i == n - 1))
            return ins
        return self.S.op("pe", rd, _names([out]) + list(extra_w), f)

    def mms(self, groups):
        rd, wr = [], []
        for out, pairs in groups:
            wr += _names([out])
            rd += _names([a for p in pairs for a in p])

        def f(e):
            for out, pairs in groups:
                n = len(pairs)
                for i, (l, r) in enumerate(pairs):
                    ins = e.matmul(out, lhsT=l, rhs=r, start=(i == 0), stop=(i == n - 1))
            return ins
        return self.S.op("pe", list(dict.fromkeys(rd)), list(dict.fromkeys(wr)), f)

    def trs(self, items, ident):
        rd = _names([i for _, i in items] + [ident])
        wr = _names([o for o, _ in items])

        def f(e):
            for o, i in items:
                ins = e.transpose(out=o, in_=i, identity=ident)
            return ins
        return self.S.op("pe", rd, wr, f)

    def dma(self, out, in_, q="sp", **kw):
        return self.S.dma(out, in_, _names([in_]), _names([out]), q=q, **kw)

    def finish(self):
        for t in self.out_toks:
            self.S._wait("sp", t)


def rms_rstd(k, out, ss, n):
    k.act(out, ss, AF.Ln, scale=1.0 / n, bias=EPS)
    k.act(out, out, AF.Exp, scale=-0.5)


def build(T=4096, dbg=False, phases="ABCD"):
    NT = T // 128
    nc = bass.Bass("TRN2", target_bir_lowering=False)
    k = K(nc)

    def din(name, shape, dt=F32):
        return nc.dram_tensor(name, list(shape), dt, kind="ExternalInput").ap()

    def dscr(name, shape, dt):
        return nc.dram_tensor(name, list(shape), dt, kind=("ExternalOutput" if dbg else "Internal")).ap()

    x = din("x", [T, D])
    p_in = din("p", [T, 256])
    attn_g = din("attn_norm_g", [D])
    w_in = din("w_in", [D, IN_TOTAL])
    lb_logits = din("hg_lb_logits", [2, 512])
    hg_norm_g = din("hg_norm_g", [128])
    q_norm_g = din("nsa_q_norm_g", [64])
    k_norm_g = din("nsa_k_norm_g", [3, 64])
    cmp_pe = din("cmp_pe", [2, 32, 64])
    cmp_w1 = din("cmp_w1", [2, 2048, 128])
    cmp_w2 = din("cmp_w2", [2, 128, 64])
    out_norm_g = din("nsa_out_norm_g", [64])
    w_out = din("w_out", [D, D])
    ffn_g = din("ffn_norm_g", [D])
    w_up = din("w_up", [D, 2 * DFF])
    conv_w = din("conv_w", [3, 2 * DFF])
    conv_b = din("conv_b", [2 * DFF])
    w_down = din("w_down", [DFF, D])
    pleg_g = din("ple_gate_norm_g", [D])
    w_pleg = din("w_ple_gate", [D, D])
    w_ple = din("w_ple", [256, D])
    ple_g = din("ple_norm_g", [D])
    c_ident = din("c_ident", [128, 128], BF16)
    c_rope = din("c_rope", [T, 16])
    c_tri2 = din("c_tri2", [128, 128])
    c_chunk = din("c_chunk", [128, 2])

    out = nc.dram_tensor("out", [T, D], F32, kind="ExternalOutput").ap()

    FT = dscr("FT", [16, 64, T], BF16)
    VT = dscr("VT", [T, 256], BF16)
    GT = dscr("GT", [T, 24], F32)
    MIXT = dscr("MIXT", [D, T], BF16)

    c_identf = din("c_identf", [128, 128])
    c_cmask = din("c_cmask", [256, T], BF16)
    c_tri = din("c_tri", [128, 128], BF16)
    c_ntri = din("c_ntri", [128, 128], BF16)
    c_E = din("c_E", [64, T], BF16)
    c_vis = din("c_vis", [T, 64])
    c_cadd = din("c_cadd", [T, 64])
    c_ovl1 = din("c_ovl1", [256, 65], BF16)
    H2 = dscr("H2", [T, D], F32)

    with ExitStack() as es0:
        k.es = es0
        ident = k.sb("ident", [128, 128], BF16)
        k.dma(ident, c_ident)
        identf = k.sb("identf", [128, 128])
        k.dma(identf, c_identf)
        if "A" in phases:
            with ExitStack() as es:
                k.es = es
                phase_a(nc, k, T, NT, x, attn_g, w_in, lb_logits, hg_norm_g, q_norm_g, k_norm_g,
                        c_rope, c_tri2, c_chunk, ident, FT, VT, GT, MIXT)
                k.barrier()
        if "B" in phases:
            with ExitStack() as es:
                k.es = es
                phase_b(nc, k, T, NT, FT, VT, GT, MIXT, k_norm_g, cmp_pe, cmp_w1, cmp_w2, out_norm_g,
                        c_rope, c_cmask, c_tri, c_ntri, c_E, c_vis, c_cadd, c_ovl1, ident, identf)
                k.barrier()
        if "A" not in phases and dbg:
            mi = din("MIXT_in", [D, T], BF16)
            with ExitStack() as es:
                k.es = es
                tb = k.sb("dbg_mix", [128, 8, T], BF16)
                k.dma(tb, mi.rearrange("(c p) t -> p c t", p=128))
                k.dma(MIXT.rearrange("(c p) t -> p c t", p=128), tb)
                k.barrier()
        if "C" in phases:
            with ExitStack() as es:
                k.es = es
                phase_c(nc, k, T, x, MIXT, w_out, ffn_g, w_up, conv_w, conv_b, w_down, H2, ident, identf)
                k.barrier()
        if "D" in phases:
            with ExitStack() as es:
                k.es = es
                phase_d(nc, k, T, H2, p_in, pleg_g, w_pleg, w_ple, ple_g, out, ident, identf)
                k.barrier()
        k.finish()
    return nc


def phase_a(nc, k, T, NT, x, attn_g, w_in, lb_logits, hg_norm_g, q_norm_g, k_norm_g,
            c_rope, c_tri2, c_chunk, ident, FT, VT, GT, MIXT):
    S = k.S
    w_sb = k.sb("a_w", [128, 8, IN_TOTAL], BF16)
    w_v = w_in.rearrange("(c p) n -> p c n", p=128)
    for c in range(8):
        k.dma(w_sb[:, c, :], w_v[:, c, :], q="pool")
    gT = k.sb("a_gT", [128, 8])
    k.dma(gT, attn_g.rearrange("(c p) -> p c", p=128), allow_slow_non_contiguous=True)
    rope = k.sb("a_rope", [128, NT, 16])
    k.dma(rope, c_rope.rearrange("(n p) c -> p n c", p=128))
    tri2 = k.sb("a_tri2", [128, 128])
    k.dma(tri2, c_tri2)
    chunk = k.sb("a_chunk", [128, 2])
    k.dma(chunk, c_chunk)
    l0 = k.sb("a_l0", [128, 512])
    l1 = k.sb("a_l1", [128, 512])
    k.dma(l0, lb_logits[0].partition_broadcast(128))
    k.dma(l1, lb_logits[1].partition_broadcast(128))
    lb = k.sb("a_lb", [128, 512])
    oml = k.sb("a_oml", [128, 512])
    k.tt(l0, l0, l1, ALU.subtract)
    k.act(lb, l0, AF.Sigmoid)
    k.ts(oml, lb, -1.0, 1.0, ALU.mult, ALU.add)
    gq = k.sb("a_gq", [128, 12, 64])
    for h in range(12):
        src = q_norm_g if h < 8 else (k_norm_g[1] if h < 10 else k_norm_g[2])
        k.dma(gq[:, h, :], src.partition_broadcast(128))
    ghg = k.sb("a_ghg", [128, 4, 128])
    for h in range(4):
        k.dma(ghg[:, h, :], hg_norm_g.partition_broadcast(128))
    Sf = k.sb("a_Sf", [128, 4, 128])
    Sb = [k.sb("a_Sb%d" % i, [128, 4, 128], BF16) for i in range(3)]
    k.memset(Sf, 0.0)
    k.memset(Sb[0], 0.0, eng="pool")

    xt = [k.sb("a_x%d" % i, [128, D]) for i in range(2)]
    junk = k.sb("a_junk", [128, D], BF16)
    ss = k.sb("a_ss", [128, 1])
    rstd = k.sb("a_rstd", [128, 1])
    xn = k.sb("a_xn", [128, D], BF16)
    xnT = k.sb("a_xnT", [128, 8, 128], BF16)
    silq = k.sb("a_silq", [128, 512])
    sig = k.sb("a_sig", [128, 512])
    logf = k.sb("a_logf", [128, 512])
    kk = k.sb("a_kk", [128, 512])
    enb = k.sb("a_enb", [128, 512])
    epb = k.sb("a_epb", [128, 512])
    kp = k.sb("a_kp", [128, 512], BF16)
    qp = k.sb("a_qp", [128, 512], BF16)
    vb = k.sb("a_vb", [128, 512], BF16)
    sg = k.sb("a_sg", [128, 512])
    ebl = k.sb("a_ebl", [128, 4, 2])
    qkT = k.sb("a_qkT", [128, 8, 128], BF16)
    ATm = k.sb("a_ATm", [128, 4, 128], BF16)
    tmpS = k.sb("a_tmpS", [128, 4, 128])
    osb = k.sb("a_osb", [128, 4, 128])
    osq = k.sb("a_osq", [128, 4, 128])
    ss4 = k.sb("a_ss4", [128, 4])
    rstd4 = k.sb("a_rstd4", [128, 4])
    onb = k.sb("a_onb", [128, 4, 128], BF16)
    mixs = k.sb("a_mixs", [128, 4, 128], BF16)
    qk = k.sb("a_qk", [128, 12, 64])
    qsq = k.sb("a_qsq", [128, 12, 64])
    ss12 = k.sb("a_ss12", [128, 12])
    rstd12 = k.sb("a_rstd12", [128, 12])
    qkb = k.sb("a_qkb", [128, 12, 64], BF16)
    r_a = k.sb("a_ra", [128, 12, 8])
    r_b = k.sb("a_rb", [128, 12, 8])
    r_c = k.sb("a_rc", [128, 12, 8])
    r_d = k.sb("a_rd", [128, 12, 8])
    kcvc = k.sb("a_kcvc", [128, 256], BF16)
    T16 = k.sb("a_T16", [64, 16, 128], BF16)
    vv = k.sb("a_vv", [128, 256], BF16)
    gsb = k.sb("a_gsb", [128, 24])

    pA = [k.ps("a_pA%d" % i, [128, 512]) for i in range(2)]
    pB = k.ps("a_pB", [128, 512])
    pC = k.ps("a_pC", [128, 8, 128], BF16)
    pD = k.ps("a_pD", [64, 16, 128], BF16)
    pF = k.ps("a_pF", [128, 4, 128])
    pG = k.ps("a_pG", [128, 4, 128])

    cols = [(0, 512), (512, 512), (1024, 512), (1536, 512), (2048, 512), (2560, 512), (3072, 280)]
    mixv = MIXT.rearrange("(c p) t -> p c t", p=128)
    ftv = FT.rearrange("n d t -> d n t")
    pa_i = [0]

    def proj(g):
        c0, n = cols[g]
        dst = pA[pa_i[0] % 2]
        pa_i[0] += 1
        k.mm(dst[:, 0:n], [(xnT[:, c, :], w_sb[:, c, c0:c0 + n]) for c in range(8)])
        return dst

    for it in range(NT):
        t0 = it * 128
        xb = xt[it % 2]
        k.dma(xb, x[t0:t0 + 128, :])
        k.act(junk, xb, AF.Square, accum_out=ss)
        rms_rstd(k, rstd, ss, D)
        k.act(xn, xb, AF.Copy, scale=rstd)
        k.trs([(pC[:, c, :], xn[:, c * 128:(c + 1) * 128]) for c in range(8)], ident)
        k.tt(xnT, pC, gT.unsqueeze(2).to_broadcast([128, 8, 128]), ALU.mult)

        d = proj(1)
        k.act(sig, d, AF.Sigmoid)
        k.tt(sig, sig, oml, ALU.mult)
        k.tt(sig, sig, lb, ALU.add)
        k.act(logf, sig, AF.Ln)
        k.ts(kk, sig, -1.0, 1.0, ALU.mult, ALU.add)
        k.mm(pB, [(tri2, logf)])
        k.act(enb, pB, AF.Exp, scale=-1.0)
        k.act(epb, pB, AF.Exp)
        k.tt(kp, kk, enb, ALU.mult)
        k.mms([(pF[:, h, 0:2], [(logf[:, h * 128:(h + 1) * 128], chunk)]) for h in range(4)])
        k.act(ebl, pF[:, :, 0:2], AF.Exp)
        d = proj(0)
        k.act(silq, d, AF.Silu)
        k.stt(qp, silq, 128 ** -0.5, epb, ALU.mult, ALU.mult)
        d = proj(2)
        k.cp(vb, d, eng="act")
        d = proj(3)
        k.act(sg, d, AF.Silu)
        k.tt(sg, sg, ghg.rearrange("p h v -> p (h v)"), ALU.mult, eng="pool")
        k.trs([(pC[:, h, :], qp[:, h * 128:(h + 1) * 128]) for h in range(4)] +
              [(pC[:, 4 + h, :], kp[:, h * 128:(h + 1) * 128]) for h in range(4)], ident)
        k.cp(qkT, pC)
        S0, S1, S2 = Sb[(2 * it) % 3], Sb[(2 * it + 1) % 3], Sb[(2 * it + 2) % 3]
        k.mms([(pF[:, h, :], [(qkT[:, 4 + h, :], qkT[:, h, :])]) for h in range(4)])
        k.tt(ATm, pF, tri2.unsqueeze(1).to_broadcast([128, 4, 128]), ALU.mult)
        for c in range(2):
            rs = slice(c * 64, (c + 1) * 64)
            k.mms([(pF[:, h, :], [(kp[rs, h * 128:(h + 1) * 128], vb[rs, h * 128:(h + 1) * 128])])
                   for h in range(4)])
            k.tt(tmpS, pF, Sf, ALU.add)
            k.tt(Sf, tmpS, ebl[:, :, c:c + 1].to_broadcast([128, 4, 128]), ALU.mult)
            k.cp(S1 if c == 0 else S2, Sf, eng="pool")
        groups = []
        for h in range(4):
            for c in range(2):
                rs = slice(c * 64, (c + 1) * 64)
                Sc = S0 if c == 0 else S1
                groups.append((pG[rs, h, :], [(ATm[rs, h, rs], vb[rs, h * 128:(h + 1) * 128]),
                                              (qkT[:, h, rs], Sc[:, h, :])]))
        k.mms(groups)
        k.cp(osb, pG, eng="act")
        k.tt(osq, osb, osb, ALU.mult)
        k.rsum(ss4, osq)
        rms_rstd(k, rstd4, ss4, 128)
        k.tt(osb, osb, rstd4.unsqueeze(2).to_broadcast([128, 4, 128]), ALU.mult)
        k.tt(onb, osb, sg.rearrange("p (h v) -> p h v", h=4), ALU.mult)
        k.trs([(pC[:, h, :], onb[:, h, :]) for h in range(4)], ident)
        k.cp(mixs, pC[:, 0:4, :])
        k.dma(mixv[:, 0:4, t0:t0 + 128], mixs)

        d = proj(4)
        k.cp(qk[:, 0:8, :], d.rearrange("p (h d) -> p h d", d=64), eng="act")
        d = proj(5)
        k.cp(kcvc, d[:, 0:256], eng="act")
        k.cp(qk[:, 8:10, :], d[:, 256:384].rearrange("p (h d) -> p h d", d=64), eng="act")
        k.cp(vv[:, 0:128], d[:, 384:512], eng="act")
        d = proj(6)
        k.cp(qk[:, 10:12, :], d[:, 0:128].rearrange("p (h d) -> p h d", d=64), eng="act")
        k.cp(vv[:, 128:256], d[:, 128:256], eng="act")
        k.act(gsb, d[:, 256:280], AF.Sigmoid)
        k.dma(GT[t0:t0 + 128, :], gsb)
        k.dma(VT[t0:t0 + 128, :], vv)
        k.tt(qsq, qk, qk, ALU.mult, eng="pool")
        k.rsum(ss12, qsq)
        rms_rstd(k, rstd12, ss12, 64)
        k.tt(qk, qk, rstd12.unsqueeze(2).to_broadcast([128, 12, 64]), ALU.mult)
        k.tt(qk, qk, gq, ALU.mult, eng="pool")
        cosb = rope[:, it:it + 1, 0:8].to_broadcast([128, 12, 8])
        sinb = rope[:, it:it + 1, 8:16].to_broadcast([128, 12, 8])
        k.tt(r_a, qk[:, :, 0:8], cosb, ALU.mult, eng="pool")
        k.tt(r_b, qk[:, :, 8:16], sinb, ALU.mult, eng="pool")
        k.tt(r_c, qk[:, :, 8:16], cosb, ALU.mult, eng="pool")
        k.tt(r_d, qk[:, :, 0:8], sinb, ALU.mult, eng="pool")
        k.cp(qkb, qk, eng="pool")
        k.tt(qkb[:, :, 0:8], r_a, r_b, ALU.subtract, eng="pool")
        k.tt(qkb[:, :, 8:16], r_c, r_d, ALU.add, eng="pool")
        k.trs([(pD[:, n, :], qkb[:, n, :]) for n in range(12)] +
              [(pD[:, 12 + n, :], kcvc[:, n * 64:(n + 1) * 64]) for n in range(4)], ident)
        k.cp(T16, pD)
        k.dma(ftv[:, :, t0:t0 + 128], T16)


def phase_b(nc, k, T, NT, FT, VT, GT, MIXT, k_norm_g, cmp_pe, cmp_w1, cmp_w2, out_norm_g,
            c_rope, c_cmask, c_tri, c_ntri, c_E, c_vis, c_cadd, c_ovl1, ident, identf):
    NQ = T // 512
    NCMP = T // 16 - 1
    NCT = (NCMP + 127) // 128

    def crow(ct):
        return min(128, NCMP - 128 * ct)

    KA = k.sb("b_KA", [128, 2, T], BF16)
    KW = k.sb("b_KW", [64, 2, T], BF16)
    VS1 = k.sb("b_VS1", [128, NT, 2, 65], BF16)
    VW1 = k.sb("b_VW1", [128, NT, 2, 65], BF16)
    VTv = VT.rearrange("(n p) c -> p n c", p=128)
    k.memset(VS1, 1.0)
    k.memset(VW1, 1.0, eng="pool")
    for g in range(2):
        k.dma(KA[0:64, g, :], FT[8 + g])
        k.dma(KA[64:128, g, :], c_E)
        k.dma(KW[:, g, :], FT[10 + g])
        k.dma(VS1[:, :, g, 0:64], VTv[:, :, g * 64:(g + 1) * 64])
        k.dma(VW1[:, :, g, 0:64], VTv[:, :, 128 + g * 64:128 + (g + 1) * 64])
    tri = k.sb("b_tri", [128, 128], BF16)
    ntri = k.sb("b_ntri", [128, 128], BF16)
    k.dma(tri, c_tri)
    k.dma(ntri, c_ntri)
    zer = k.sb("b_zer", [128, 65], BF16)
    k.memset(zer, 0.0)
    gout = k.sb("b_gout", [128, 64])
    k.dma(gout, out_norm_g.partition_broadcast(128))

    pS = [k.ps("b_pS%d" % i, [128, 512]) for i in range(2)]
    pO = k.ps("b_pO", [128, 512])
    pCI = k.ps("b_pCI", [128, 4, 256])
    pT = k.ps("b_pT", [128, 4, 128])
    pTb = k.ps("b_pTb", [128, 4, 128], BF16)

    kcT = k.sb("b_kcT", [64, 2, NCT * 128], BF16)
    Rv = k.sb("b_R", [128, NCT, 2, 129], BF16)
    k.memset(kcT, 0.0)
    k.memset(Rv, 0.0, eng="pool")
    ovv = c_ovl1.rearrange("(ct p) c -> p ct c", p=128)
    for ct in range(NCT):
        for g in range(2):
            k.dma(Rv[:, ct, g, 64:129], ovv[:, ct, :])
    with ExitStack() as es_c:
        es_prev = k.es
        k.es = es_c
        kc2 = k.sb("b_kc2", [128, T], BF16)
        hid = k.sb("b_hid", [128, NCT * 128], BF16)
        k.memset(hid, 0.0)
        kcn = k.sb("b_kcn", [128, NCT * 2, 64])
        k.memset(kcn, 0.0)
        gk0 = k.sb("b_gk0", [128, 64])
        k.dma(gk0, k_norm_g[0].partition_broadcast(128))
        ropec = k.sb("b_ropec", [128, NCT, 16])
        k.memset(ropec, 0.0)
        rv = c_rope.rearrange("(c s) f -> c s f", s=16)
        for ct in range(NCT):
            k.dma(ropec[0:crow(ct), ct, :], rv[1 + ct * 128:1 + ct * 128 + crow(ct), 15, :])
        w1 = [k.sb("b_w1%d" % j, [128, 16, 128], BF16) for j in range(2)]
        w2 = [k.sb("b_w2%d" % j, [128, 64], BF16) for j in range(2)]
        pes = [k.sb("b_pe%d" % j, [128, 16], BF16) for j in range(2)]
        bvec = [k.sb("b_bv%d" % j, [128, 1]) for j in range(2)]
        for j in range(2):
            k.dma(w1[j], cmp_w1[j].rearrange("(l p) h -> p l h", p=128), q="pool")
            k.dma(w2[j], cmp_w2[j], q="pool")
            k.dma(pes[j], cmp_pe[j].rearrange("(l two) d -> (two d) l", two=2), q="pool",
                  allow_slow_non_contiguous=True)
        v16 = kc2.rearrange("p (c s) -> p c s", s=16)
        for j in range(2):
            k.mm(pO[:, 0:1], [(w1[j][:, l2, :], pes[j][:, l2:l2 + 1]) for l2 in range(16)])
            k.cp(bvec[j], pO[:, 0:1])
            for g in range(2):
                n = 12 + 2 * j + g
                k.dma(kc2[0:64, :], FT[n])
                k.dma(kc2[64:128, 0:T - 1], FT[n][:, 1:T])
                pairs = []
                for l2 in range(16):
                    rhs = v16[:, 0:NCMP, 2 * l2] if l2 < 8 else v16[:, 1:NCMP + 1, 2 * l2 - 16]
                    pairs.append((w1[j][:, l2, :], rhs))
                k.mm(pS[0][:, 0:NCMP], pairs)
                k.act(hid[:, 0:NCMP], pS[0][:, 0:NCMP], AF.Silu, bias=bvec[j])
                for ct in range(NCT):
                    r = crow(ct)
                    k.mm(pT[0:r, ct, 0:64], [(hid[:, ct * 128:ct * 128 + r], w2[j])])
                    if j == 0:
                        k.cp(kcn[0:r, ct * 2 + g, :], pT[0:r, ct, 0:64])
                    else:
                        k.cp(Rv[0:r, ct, g, 0:64], pT[0:r, ct, 0:64])
        NS = NCT * 2
        ksq = k.sb("b_ksq", [128, NS, 64])
        kss = k.sb("b_kss", [128, NS])
        krs = k.sb("b_krs", [128, NS])
        kcb = k.sb("b_kcb", [128, NS, 64], BF16)
        ra = k.sb("b_ra", [128, 2, 8])
        rb = k.sb("b_rb", [128, 2, 8])
        k.tt(ksq, kcn, kcn, ALU.mult)
        k.rsum(kss, ksq)
        rms_rstd(k, krs, kss, 64)
        k.tt(kcn, kcn, krs.unsqueeze(2).to_broadcast([128, NS, 64]), ALU.mult)
        k.tt(kcn, kcn, gk0.unsqueeze(1).to_broadcast([128, NS, 64]), ALU.mult)
        k.cp(kcb, kcn)
        for ct in range(NCT):
            sl = slice(ct * 2, ct * 2 + 2)
            cosb = ropec[:, ct:ct + 1, 0:8].to_broadcast([128, 2, 8])
            sinb = ropec[:, ct:ct + 1, 8:16].to_broadcast([128, 2, 8])
            k.tt(ra, kcn[:, sl, 0:8], cosb, ALU.mult)
            k.tt(rb, kcn[:, sl, 8:16], sinb, ALU.mult)
            k.tt(kcb[:, sl, 0:8], ra, rb, ALU.subtract)
            k.tt(ra, kcn[:, sl, 8:16], cosb, ALU.mult)
            k.tt(rb, kcn[:, sl, 0:8], sinb, ALU.mult)
            k.tt(kcb[:, sl, 8:16], ra, rb, ALU.add)
        for ct in range(NCT):
            r = crow(ct)
            for g in range(2):
                k.trs([(pTb[0:64, 0, 0:r], kcb[0:r, ct * 2 + g, :])], ident[0:r, 0:r])
                k.cp(kcT[:, g, ct * 128:ct * 128 + r], pTb[0:64, 0, 0:r])
        k.barrier()
        k.es = es_prev

    if B_STAGE < 1:
        return
    QA = [k.sb("b_QA%d" % i, [128, 8, 512], BF16) for i in range(2)]
    cmT = k.sb("b_cmT", [128, NCT, 512], BF16)
    gts = k.sb("b_gts", [128, 4, 24])
    vis = k.sb("b_vis", [128, 4, 64])
    cadd = k.sb("b_cadd", [128, 4, 64])
    P = [k.sb("b_P%d" % i, [128, 512], BF16) for i in range(3)]
    ocmp = k.sb("b_ocmp", [128, 4, 8, 64])
    osel = k.sb("b_osel", [128, 4, 8, 64])
    owin = k.sb("b_owin", [128, 4, 8, 64])
    oT = k.sb("b_oT", [65, 512])
    rec = k.sb("b_rec", [128, 4, 1])
    imp = k.sb("b_imp", [128, 4, 2, 64])
    itmp = k.sb("b_itmp", [128, 4, 64])
    mx = k.sb("b_mx", [128, 8])
    mx2 = k.sb("b_mx2", [128, 8])
    wk = k.sb("b_wk", [128, 64])
    mk = k.sb("b_mk", [128, 64])
    negm = k.sb("b_negm", [128, 128], BF16)
    k.memset(negm, 0.0)
    osq = k.sb("b_osq", [128, 4, 8, 64])
    oss = k.sb("b_oss", [128, 32])
    ors = k.sb("b_ors", [128, 32])
    onb = k.sb("b_onb", [128, 4, 512], BF16)
    mixs = k.sb("b_mixs", [128, 4, 128], BF16)
    ftq = FT.rearrange("n d t -> d n t")
    gtv = GT.rearrange("(s p) c -> p s c", p=128)
    visv = c_vis.rearrange("(s p) c -> p s c", p=128)
    caddv = c_cadd.rearrange("(s p) c -> p s c", p=128)
    cmv = c_cmask.rearrange("(ct p) t -> p ct t", p=128)
    mixv = MIXT.rearrange("(c p) t -> p c t", p=128)
    cnt = [0]

    def attend(Qa, h, g, kts, Ksrc, Vsrc, kdim, odst, zero_first):
        if zero_first:
            k.mm1(pO[0:65, :], zer, Qa[:, h, :], True, False)
        for idx, (kt, lo, hi, mcol, mtile) in enumerate(kts):
            i = cnt[0]
            cnt[0] += 1
            ps, Pb = pS[i % 2], P[i % 3]
            k.mm(ps[:, lo:hi], [(Ksrc[0:kdim, g, kt * 128:(kt + 1) * 128], Qa[0:kdim, h, lo:hi])])
            k.act(Pb[:, lo:hi], ps[:, lo:hi], AF.Exp, scale=0.125)
            if mtile is not None:
                k.tt(Pb[:, mcol:mcol + 128], Pb[:, mcol:mcol + 128], mtile, ALU.mult)
            k.mm1(pO[0:65, lo:hi], Vsrc[:, kt, g, :], Pb[:, lo:hi],
                  (idx == 0 and not zero_first), idx == len(kts) - 1)
        k.cp(oT, pO[0:65, :], eng="act")
        k.trs([(pT[:, s, 0:65], oT[:, s * 128:(s + 1) * 128]) for s in range(4)], identf[0:65, 0:65])
        k.recip(rec, pT[:, :, 64:65])
        k.tt(odst[:, :, h, :], pT[:, :, 0:64], rec.to_broadcast([128, 4, 64]), ALU.mult)

    for Qi in range(NQ):
        t0 = Qi * 512
        Qa = QA[Qi % 2]
        k.dma(Qa[0:64, :, :], ftq[:, 0:8, t0:t0 + 512])
        k.dma(gts, gtv[:, Qi * 4:(Qi + 1) * 4, :])
        k.dma(vis, visv[:, Qi * 4:(Qi + 1) * 4, :])
        k.dma(cadd, caddv[:, Qi * 4:(Qi + 1) * 4, :])
        k.dma(cmT, cmv[:, 0:NCT, t0:t0 + 512])
        cts = [ct for ct in range(NCT) if 16 * 128 * ct + 31 <= t0 + 511]
        for h in range(8):
            g = h // 4
            for ci, ct in enumerate(cts):
                i = cnt[0]
                cnt[0] += 1
                ps, Pb = pS[i % 2], P[i % 3]
                k.mm(ps, [(kcT[0:64, g, ct * 128:(ct + 1) * 128], Qa[0:64, h, :])])
                k.act(Pb, ps, AF.Exp, scale=0.125)
                k.tt(Pb, Pb, cmT[:, ct, :], ALU.mult)
                for s in range(4):
                    k.mm1(pCI[:, s, 0:129], Pb[:, s * 128:(s + 1) * 128], Rv[:, ct, g, :],
                          ci == 0 and s % 2 == 0, ci == len(cts) - 1)
            k.ts(rec, pCI[:, :, 64:65], 1e-30, None, ALU.add)
            k.recip(rec, rec)
            k.tt(ocmp[:, :, h, :], pCI[:, :, 0:64], rec.to_broadcast([128, 4, 64]), ALU.mult)
            if h % 4 == 0:
                k.tt(imp[:, :, g, :], pCI[:, :, 65:129], rec.to_broadcast([128, 4, 64]), ALU.mult)
            else:
                k.tt(itmp, pCI[:, :, 65:129], rec.to_broadcast([128, 4, 64]), ALU.mult)
                k.tt(imp[:, :, g, :], imp[:, :, g, :], itmp, ALU.add, eng="pool")
        if B_STAGE < 2:
            continue
        for g in range(2):
            k.tt(imp[:, :, g, :], imp[:, :, g, :], vis, ALU.mult)
            k.tt(imp[:, :, g, :], imp[:, :, g, :], cadd, ALU.add)
            for s in range(4):
                iv = imp[:, s, g, :]
                k.vmax(mx, iv)
                k.vmatch(wk, mx, iv, -3.0e38)
                k.vmax(mx2, wk)
                k.tt(mk, iv, mx2[:, 7:8].to_broadcast([128, 64]), ALU.is_ge)
                k.ts(negm[:, 64:128], mk, -NEGM, NEGM, ALU.mult, ALU.add)
                k.trs([(pTb[:, 0, :], negm)], ident)
                k.cp(Qa[64:128, 4 * g:4 * g + 4, s * 128:(s + 1) * 128],
                     pTb[64:128, 0:1, :].to_broadcast([64, 4, 128]))
        if B_STAGE < 3:
            continue
        for h in range(8):
            g = h // 4
            kts = []
            for kt in range(4 * Qi + 4):
                m = kt - 4 * Qi
                if m >= 0:
                    kts.append((kt, 128 * m, 512, 128 * m, tri))
                else:
                    kts.append((kt, 0, 512, 0, None))
            attend(Qa, h, g, kts, KA, VS1, 128, osel, False)
            kts = []
            for kt in range(max(0, 4 * Qi - 4), 4 * Qi + 4):
                m = kt - 4 * Qi
                if m >= 0:
                    kts.append((kt, 128 * m, 512, 128 * m, tri))
                else:
                    kts.append((kt, 0, 128 * (m + 5), 128 * (m + 4), ntri))
            attend(Qa, h, g, kts, KW, VW1, 64, owin, True)
        if B_STAGE < 4:
            continue
        for br, ob in enumerate((ocmp, osel, owin)):
            k.tt(ob, ob, gts[:, :, br * 8:(br + 1) * 8].unsqueeze(3).to_broadcast([128, 4, 8, 64]),
                 ALU.mult, eng=("pool" if br == 1 else "dve"))
        k.tt(ocmp, ocmp, osel, ALU.add)
        k.tt(ocmp, ocmp, owin, ALU.add, eng="pool")
        if B_STAGE < 5:
            continue
        k.tt(osq, ocmp, ocmp, ALU.mult)
        k.rsum(oss, osq.rearrange("p s h d -> p (s h) d"))
        rms_rstd(k, ors, oss, 64)
        k.tt(ocmp.rearrange("p s h d -> p (s h) d"), ocmp.rearrange("p s h d -> p (s h) d"),
             ors.unsqueeze(2).to_broadcast([128, 32, 64]), ALU.mult)
        k.tt(onb.rearrange("p s (h d) -> p (s h) d", d=64), ocmp.rearrange("p s h d -> p (s h) d"),
             gout.unsqueeze(1).to_broadcast([128, 32, 64]), ALU.mult, eng="pool")
        if B_STAGE < 6:
            continue
        for s in range(4):
            k.trs([(pTb[:, c, :], onb[:, s, c * 128:(c + 1) * 128]) for c in range(4)], ident)
            k.cp(mixs, pTb)
            if B_STAGE >= 7:
                k.dma(mixv[:, 4:8, t0 + s * 128:t0 + (s + 1) * 128], mixs)


def load_colvec(k, dst, src, n_chunks, identf, pTf, tmp):
    k.dma(tmp[0:n_chunks, :], src.rearrange("(c p) -> c p", p=128))
    k.trs([(pTf[:, 0:n_chunks], tmp[0:n_chunks, :])], identf[0:n_chunks, 0:n_chunks])
    k.cp(dst, pTf[:, 0:n_chunks])


def phase_c(nc, k, T, x, MIXT, w_out, ffn_g, w_up, conv_w, conv_b, w_down, H2, ident, identf):
    TT = 256
    NTT = T // TT
    NF = 22
    wo = k.sb("c_wo", [128, 8, D], BF16)
    wu = k.sb("c_wu", [128, 8, 2 * DFF], BF16)
    wd = k.sb("c_wd", [128, NF, D], BF16)
    wov = w_out.rearrange("(c p) n -> p c n", p=128)
    wuv = w_up.rearrange("(c p) n -> p c n", p=128)
    wdv = w_down.rearrange("(c p) n -> p c n", p=128)
    for c in range(8):
        k.dma(wo[:, c, :], wov[:, c, :], q="pool")
    for c in range(8):
        for hh in range(2):
            k.dma(wu[:, c, hh * DFF:(hh + 1) * DFF], wuv[:, c, hh * DFF:(hh + 1) * DFF], q="pool")
    for c in range(NF):
        k.dma(wd[:, c, :], wdv[:, c, :], q="pool")
    pY = [k.ps("c_pY%d" % i, [128, 512]) for i in range(2)]
    pC = k.ps("c_pC", [128, 8, 128], BF16)
    pU = k.ps("c_pU", [128, 4, 256])
    tmpv = k.sb("c_tmpv", [44, 128])
    gfT = k.sb("c_gfT", [128, 8])
    load_colvec(k, gfT, ffn_g, 8, identf, pY[0], tmpv)
    cw = k.sb("c_cw", [128, 3, 44])
    cb = k.sb("c_cb", [128, 44])
    for j in range(3):
        load_colvec(k, cw[:, j, :], conv_w[j], 44, identf, pY[0], tmpv)
    load_colvec(k, cb, conv_b, 44, identf, pY[0], tmpv)
    carry = k.sb("c_carry", [128, 44, 2])
    k.memset(carry, 0.0)

    mt = k.sb("c_mt", [128, 8, TT], BF16)
    hsb = k.sb("c_h", [128, 2, D])
    xt = k.sb("c_x", [128, 2, D])
    junk = k.sb("c_junk", [128, D], BF16)
    ss = k.sb("c_ss", [128, 1])
    rstd = k.sb("c_rstd", [128, 1])
    hn = k.sb("c_hn", [128, D], BF16)
    hnT = k.sb("c_hnT", [128, 8, TT], BF16)
    actT = k.sb("c_actT", [128, NF, TT], BF16)
    uraw = [k.sb("c_uraw%d" % i, [128, TT + 2]) for i in range(4)]
    acc = [k.sb("c_acc%d" % i, [128, TT]) for i in range(4)]
    sil = [k.sb("c_sil%d" % i, [128, TT]) for i in range(2)]
    mixv = MIXT.rearrange("(c p) t -> p c t", p=128)
    xv = x.rearrange("(n p) d -> p n d", p=128)
    h2v = H2.rearrange("(n p) d -> p n d", p=128)

    for it in range(NTT):
        t0 = it * TT
        k.dma(mt, mixv[:, :, t0:t0 + TT])
        k.dma(xt, xv[:, 2 * it:2 * it + 2, :])
        for s in range(2):
            for hh in range(2):
                py = pY[(s * 2 + hh) % 2]
                k.mm(py, [(mt[:, c, s * 128:(s + 1) * 128], wo[:, c, hh * 512:(hh + 1) * 512])
                          for c in range(8)])
                k.tt(hsb[:, s, hh * 512:(hh + 1) * 512], py, xt[:, s, hh * 512:(hh + 1) * 512], ALU.add)
            k.act(junk, hsb[:, s, :], AF.Square, accum_out=ss)
            rms_rstd(k, rstd, ss, D)
            k.act(hn, hsb[:, s, :], AF.Copy, scale=rstd)
            k.trs([(pC[:, c, :], hn[:, c * 128:(c + 1) * 128]) for c in range(8)], ident)
            k.tt(hnT[:, :, s * 128:(s + 1) * 128], pC, gfT.unsqueeze(2).to_broadcast([128, 8, 128]),
                 ALU.mult)
        for i in range(NF):
            accs = []
            for gu in range(2):
                ch = i + gu * NF
                slot = (i % 2) * 2 + gu
                pu = pU[:, slot, :]
                k.mm(pu, [(wu[:, c, ch * 128:(ch + 1) * 128], hnT[:, c, :]) for c in range(8)])
                ur, ac = uraw[slot], acc[slot]
                k.cp(ur[:, 0:2], carry[:, ch, :], eng="pool")
                k.cp(ur[:, 2:TT + 2], pu, eng="act")
                k.act(ac, pu, AF.Identity, scale=cw[:, 2, ch:ch + 1], bias=cb[:, ch:ch + 1])
                k.stt(ac, ur[:, 1:TT + 1], cw[:, 1, ch:ch + 1], ac, ALU.mult, ALU.add)
                k.stt(ac, ur[:, 0:TT], cw[:, 0, ch:ch + 1], ac, ALU.mult, ALU.add)
                k.cp(carry[:, ch, :], ur[:, TT:TT + 2], eng="pool")
                accs.append(ac)
            sl = sil[i % 2]
            k.act(sl, accs[0], AF.Silu)
            k.tt(actT[:, i, :], sl, accs[1], ALU.mult, eng="pool")
        for s in range(2):
            for hh in range(2):
                py = pY[(s * 2 + hh) % 2]
                k.mm(py, [(actT[:, i, s * 128:(s + 1) * 128], wd[:, i, hh * 512:(hh + 1) * 512])
                          for i in range(NF)])
                k.tt(hsb[:, s, hh * 512:(hh + 1) * 512], py, hsb[:, s, hh * 512:(hh + 1) * 512], ALU.add)
        k.dma(h2v[:, 2 * it:2 * it + 2, :], hsb)


def phase_d(nc, k, T, H2, p_in, pleg_g, w_pleg, w_ple, ple_g, out, ident, identf):
    NT = T // 128
    wg = k.sb("d_wg", [128, 8, D], BF16)
    wp = k.sb("d_wp", [128, 2, D], BF16)
    wgv = w_pleg.rearrange("(c p) n -> p c n", p=128)
    wpv = w_ple.rearrange("(c p) n -> p c n", p=128)
    for c in range(8):
        k.dma(wg[:, c, :], wgv[:, c, :], q="pool")
    for c in range(2):
        k.dma(wp[:, c, :], wpv[:, c, :], q="pool")
    pY = [k.ps("d_pY%d" % i, [128, 512]) for i in range(2)]
    pE = [k.ps("d_pE%d" % i, [128, 512]) for i in range(2)]
    pC = k.ps("d_pC", [128, 8, 128], BF16)
    tmpv = k.sb("d_tmpv", [8, 128])
    ggT = k.sb("d_ggT", [128, 8])
    load_colvec(k, ggT, pleg_g, 8, identf, pY[0], tmpv)
    gple = k.sb("d_gple", [128, D])
    k.dma(gple, ple_g.partition_broadcast(128))

    hb = [k.sb("d_h%d" % i, [128, D]) for i in range(2)]
    pb = [k.sb("d_p%d" % i, [128, 256]) for i in range(2)]
    junk = k.sb("d_junk", [128, D], BF16)
    ss = k.sb("d_ss", [128, 1])
    rstd = k.sb("d_rstd", [128, 1])
    ss2 = k.sb("d_ss2", [128, 2])
    rstd2 = k.sb("d_rstd2", [128, 1])
    hn = k.sb("d_hn", [128, D], BF16)
    hT = k.sb("d_hT", [128, 8, 128], BF16)
    pbf = k.sb("d_pbf", [128, 256], BF16)
    pT = k.sb("d_pT", [128, 2, 128], BF16)
    gate = k.sb("d_gate", [128, D])
    e = k.sb("d_e", [128, D])
    ob = [k.sb("d_o%d" % i, [128, D]) for i in range(2)]

    for it in range(NT):
        t0 = it * 128
        h = hb[it % 2]
        pp = pb[it % 2]
        o = ob[it % 2]
        k.dma(h, H2[t0:t0 + 128, :])
        k.dma(pp, p_in[t0:t0 + 128, :])
        k.act(junk, h, AF.Square, accum_out=ss)
        rms_rstd(k, rstd, ss, D)
        k.act(hn, h, AF.Copy, scale=rstd)
        k.trs([(pC[:, c, :], hn[:, c * 128:(c + 1) * 128]) for c in range(8)], ident)
        k.tt(hT, pC, ggT.unsqueeze(2).to_broadcast([128, 8, 128]), ALU.mult)
        for hh in range(2):
            k.mm(pY[hh], [(hT[:, c, :], wg[:, c, hh * 512:(hh + 1) * 512]) for c in range(8)])
            k.act(gate[:, hh * 512:(hh + 1) * 512], pY[hh], AF.Sigmoid)
        k.cp(pbf, pp, eng="pool")
        k.trs([(pC[:, c, :], pbf[:, c * 128:(c + 1) * 128]) for c in range(2)], ident)
        k.cp(pT, pC[:, 0:2, :])
        for hh in range(2):
            k.mm(pE[hh], [(pT[:, c, :], wp[:, c, hh * 512:(hh + 1) * 512]) for c in range(2)])
            k.act(junk[:, hh * 512:(hh + 1) * 512], pE[hh], AF.Square, accum_out=ss2[:, hh:hh + 1])
        k.tt(ss, ss2[:, 0:1], ss2[:, 1:2], ALU.add)
        rms_rstd(k, rstd2, ss, D)
        for hh in range(2):
            k.act(e[:, hh * 512:(hh + 1) * 512], pE[hh], AF.Copy, scale=rstd2)
        k.tt(e, e, gple, ALU.mult)
        k.tt(e, e, gate, ALU.mult, eng="pool")
        k.tt(o, e, h, ALU.add)
        tok = k.dma(out[t0:t0 + 128, :], o)
        k.out_toks.append(tok)


def _consts(T):
    bf = ml_dtypes.bfloat16
    c = {}
    c["c_ident"] = np.eye(128).astype(bf)
    c["c_identf"] = np.eye(128, dtype=np.float32)
    half = 8
    inv = np.float32(500000.0) ** (-np.arange(half, dtype=np.float32) / half)
    ang = np.arange(T, dtype=np.float32)[:, None] * inv[None, :].astype(np.float32)
    c["c_rope"] = np.concatenate([np.cos(ang), np.sin(ang)], 1).astype(np.float32)
    s = np.arange(128)
    c["c_tri2"] = ((s[:, None] <= s[None, :]) & (s[:, None] // 64 == s[None, :] // 64)).astype(np.float32)
    c["c_chunk"] = (s[:, None] // 64 == np.arange(2)[None, :]).astype(np.float32)
    t = np.arange(T)
    cc = np.arange(256)
    ncmp = T // 16 - 1
    c["c_cmask"] = (((16 * cc[:, None] + 31) <= t[None, :]) & (cc[:, None] < ncmp)).astype(bf)
    c["c_tri"] = (s[:, None] <= s[None, :]).astype(bf)
    c["c_ntri"] = (s[None, :] < s[:, None]).astype(bf)
    n = np.arange(64)
    c["c_E"] = ((t[None, :] // 64) == n[:, None]).astype(bf)
    cur = t // 64
    vis = (n[None, :] * 64 <= t[:, None])
    bonus = np.zeros((T, 64), np.float32)
    bonus += (n[None, :] == 0) * 1.0e6
    bonus += (n[None, :] == cur[:, None]) * 2.0e6
    bonus += (n[None, :] == cur[:, None] - 1) * 4.0e6
    c["c_vis"] = vis.astype(np.float32)
    c["c_cadd"] = np.where(vis, bonus, np.float32(-1e30)).astype(np.float32)
    cs = cc * 16
    ssb = n * 64
    ov = np.clip(np.minimum(cs[:, None] + 32, ssb[None, :] + 64)
                 - np.maximum(cs[:, None], ssb[None, :]), 0, None) / 32.0
    o1 = np.zeros((256, 65), np.float32)
    o1[:, 0] = 1.0
    o1[:, 1:] = ov
    o1[ncmp:] = 0.0
    c["c_ovl1"] = o1.astype(bf)
    return c


_W_NAMES = ["attn_norm_g", "w_in", "hg_norm_g", "nsa_q_norm_g", "nsa_k_norm_g", "cmp_pe", "cmp_w1",
            "cmp_w2", "nsa_out_norm_g", "w_out", "ffn_norm_g", "w_up", "conv_w", "conv_b", "w_down",
            "ple_gate_norm_g", "w_ple_gate", "w_ple", "ple_norm_g"]


def kernel(**inputs):
    x = np.asarray(inputs["x"], np.float32)
    p = np.asarray(inputs["p"], np.float32)
    B, T, _ = x.shape
    nc = build(T=T, dbg=False, phases="ABCD")
    shared = {n: np.ascontiguousarray(np.asarray(inputs[n], np.float32)[0]) for n in _W_NAMES}
    shared["hg_lb_logits"] = np.ascontiguousarray(np.asarray(inputs["hg_lb_logits"], np.float32))
    shared.update(_consts(T))
    in_maps = []
    for b in range(B):
        m = dict(shared)
        m["x"] = np.ascontiguousarray(x[b])
        m["p"] = np.ascontiguousarray(p[0, b])
        in_maps.append(m)
    res = run_bass_kernel_spmd(nc, in_maps, core_ids=list(range(B)))
    return np.stack([np.asarray(r["out"], np.float32) for r in res.results], axis=0)
```

```python
from contextlib import ExitStack
import numpy as np
import ml_dtypes
import concourse.bass as bass
import concourse.mybir as mybir
from concourse.bass_utils import run_bass_kernel_spmd

F32 = mybir.dt.float32
BF16 = mybir.dt.bfloat16
ALU = mybir.AluOpType
AF = mybir.ActivationFunctionType
AX = mybir.AxisListType

D = 1024
IN_TOTAL = 3352
DFF = 2816
EPS = 1e-6
NEGM = -30000.0
B_STAGE = 9
FFN_DEPTH = 2
PRO_DEPTH = 2


class Sched:
    def __init__(self, nc, n_dma_sems=48):
        self.nc = nc
        self.engs = {"pe": nc.tensor, "act": nc.scalar, "dve": nc.vector,
                     "pool": nc.gpsimd, "sp": nc.sync}
        self.sem = {}
        self.cnt = {}
        for k in ("pe", "act", "dve", "pool"):
            self.sem[k] = nc.alloc_semaphore("s_" + k)
            self.cnt[k] = 0
        self.dma_sems = [nc.alloc_semaphore("s_dma%d" % i) for i in range(n_dma_sems)]
        self.dma_val = [0] * n_dma_sems
        self.dma_rr = 0
        self.waited = {}
        self.last_w = {}
        self.readers = {}
        self.nwaits = 0
        self.inflight = {}
        self.max_desc = 1536

    def _wait(self, eng, tok):
        sem, val, key = tok
        if key == eng and eng == "pe":
            return
        k = (eng, key)
        if self.waited.get(k, 0) >= val:
            return
        self.engs[eng].wait_ge(sem, val)
        self.nwaits += 1
        self.waited[k] = val

    def deps(self, eng, reads, writes):
        toks = []
        for r in reads:
            t = self.last_w.get(r)
            if t is not None:
                toks.append(t)
        for w in writes:
            t = self.last_w.get(w)
            if t is not None:
                toks.append(t)
            toks.extend(self.readers.get(w, ()))
        for t in toks:
            self._wait(eng, t)

    def commit(self, tok, reads, writes):
        for w in writes:
            self.last_w[w] = tok
            self.readers[w] = []
        for r in reads:
            if r in writes:
                continue
            lst = self.readers.setdefault(r, [])
            lst[:] = [t for t in lst if t[2] != tok[2]]
            lst.append(tok)

    def op(self, eng, reads, writes, fn):
        self.deps(eng, reads, writes)
        ins = fn(self.engs[eng])
        self.cnt[eng] += 1
        ins.then_inc(self.sem[eng], 1)
        tok = (self.sem[eng], self.cnt[eng], eng)
        self.commit(tok, reads, writes)
        return tok

    @staticmethod
    def _ndesc(ap):
        dims = list(ap.ap)
        total = 1
        for st, n in dims:
            total *= n
        run = 1
        for st, n in reversed(dims[1:]):
            if st == run:
                run *= n
            else:
                break
        return max(1, total // max(run, 1))

    def dma(self, out, in_, reads, writes, q="sp", **kw):
        nd = max(self._ndesc(out), self._ndesc(in_))
        fifo = self.inflight.setdefault(q, [])
        while fifo and sum(d for _, d in fifo) + nd > self.max_desc:
            tok0, _ = fifo.pop(0)
            self._wait(q, tok0)
        tok = self._dma(out, in_, reads, writes, q, **kw)
        fifo.append((tok, nd))
        return tok

    def _dma(self, out, in_, reads, writes, q="sp", **kw):
        i = self.dma_rr
        self.dma_rr = (self.dma_rr + 1) % len(self.dma_sems)
        sem = self.dma_sems[i]
        key = "dma%d" % i
        if self.dma_val[i] > 0:
            self._wait(q, (sem, self.dma_val[i], key))
        self.deps(q, reads, writes)
        self.dma_val[i] += 16
        self.engs[q].dma_start(out=out, in_=in_, **kw).then_inc(sem, 16)
        tok = (sem, self.dma_val[i], key)
        self.commit(tok, reads, writes)
        return tok


_TAGS = {}
_KEEP = []


def tag(ap, name):
    _TAGS[id(ap)] = name
    _KEEP.append(ap)
    return ap


def sub(ap, fn):
    r = fn(ap)
    if id(ap) in _TAGS:
        tag(r, _TAGS[id(ap)])
    return r


def pipeline(gens, depth):
    gens = iter(gens)
    active = []
    exhausted = False
    while True:
        if not exhausted and len(active) < depth:
            g = next(gens, None)
            if g is None:
                exhausted = True
            else:
                active.append(g)
        if not active:
            if exhausted:
                break
            continue
        for g in list(active):
            try:
                next(g)
            except StopIteration:
                active.remove(g)


def _names(aps):
    out = []
    for a in aps:
        if a is None or isinstance(a, (int, float)):
            continue
        n = _TAGS.get(id(a)) or a.name
        if n not in out:
            out.append(n)
    return out


class K:
    def __init__(self, nc):
        self.nc = nc
        self.S = Sched(nc)
        self.out_toks = []
        self.es = None

    def sb(self, name, shape, dt=F32):
        return self.es.enter_context(self.nc.sbuf_tensor(name, list(shape), dt))[:]

    def ps(self, name, shape, dt=F32):
        return self.es.enter_context(self.nc.psum_tensor(name, list(shape), dt))[:]

    def barrier(self):
        S = self.S
        toks = [(S.sem[e], S.cnt[e], e) for e in ("pe", "act", "dve", "pool") if S.cnt[e] > 0]
        toks += [(S.dma_sems[i], S.dma_val[i], "dma%d" % i) for i in range(len(S.dma_sems))
                 if S.dma_val[i] > 0]
        for eng in ("sp", "pe", "act", "dve", "pool"):
            for t in toks:
                S._wait(eng, t)
        S.last_w.clear()
        S.readers.clear()

    def mm1(self, out, lhsT, rhs, start, stop):
        return self.S.op("pe", _names([lhsT, rhs]), _names([out]),
                         lambda e: e.matmul(out, lhsT=lhsT, rhs=rhs, start=start, stop=stop))

    def vmax(self, out, in_):
        return self.S.op("dve", _names([in_]), _names([out]), lambda e: e.max(out=out, in_=in_))

    def vmatch(self, out, mx, vals, imm):
        return self.S.op("dve", _names([mx, vals]), _names([out]),
                         lambda e: e.match_replace(out=out, in_to_replace=mx, in_values=vals,
                                                   imm_value=imm))

    def recip(self, out, in_):
        return self.S.op("dve", _names([in_]), _names([out]), lambda e: e.reciprocal(out=out, in_=in_))

    def act(self, out, in_, func, bias=None, scale=None, accum_out=None, eng="act"):
        kw = {}
        if bias is not None:
            kw["bias"] = bias
        if scale is not None:
            kw["scale"] = scale
        if accum_out is not None:
            kw["accum_out"] = accum_out
        rd = _names([in_, bias, scale])
        wr = _names([out, accum_out])
        return self.S.op(eng, rd, wr, lambda e: e.activation(out=out, in_=in_, func=func, **kw))

    def tt(self, out, in0, in1, op, eng="dve"):
        return self.S.op(eng, _names([in0, in1]), _names([out]),
                         lambda e: e.tensor_tensor(out=out, in0=in0, in1=in1, op=op))

    def ts(self, out, in0, s1, s2, op0, op1=None, eng="dve"):
        def f(e):
            if op1 is None:
                return e.tensor_scalar(out=out, in0=in0, scalar1=s1, scalar2=None, op0=op0)
            return e.tensor_scalar(out=out, in0=in0, scalar1=s1, scalar2=s2, op0=op0, op1=op1)
        return self.S.op(eng, _names([in0, s1, s2]), _names([out]), f)

    def stt(self, out, in0, scalar, in1, op0, op1, eng="dve"):
        return self.S.op(eng, _names([in0, scalar, in1]), _names([out]),
                         lambda e: e.scalar_tensor_tensor(out=out, in0=in0, scalar=scalar, in1=in1,
                                                          op0=op0, op1=op1))

    def cp(self, out, in_, eng="dve"):
        if eng == "act":
            return self.S.op("act", _names([in_]), _names([out]), lambda e: e.copy(out=out, in_=in_))
        return self.S.op(eng, _names([in_]), _names([out]), lambda e: e.tensor_copy(out=out, in_=in_))

    def rsum(self, out, in_, eng="dve"):
        return self.S.op(eng, _names([in_]), _names([out]),
                         lambda e: e.reduce_sum(out=out, in_=in_, axis=AX.X))

    def memset(self, out, val, eng="dve"):
        return self.S.op(eng, [], _names([out]), lambda e: e.memset(out, val))

    def mm(self, out, pairs, extra_w=()):
        rd = _names([a for p in pairs for a in p])
        n = len(pairs)

        def f(e):
            for i, (l, r) in enumerate(pairs):
                ins = e.matmul(out, lhsT=l, rhs=r, start=(i == 0), stop=(i == n - 1))
            return ins
        return self.S.op("pe", rd, _names([out]) + list(extra_w), f)

    def mms(self, groups):
        rd, wr = [], []
        for out, pairs in groups:
            wr += _names([out])
            rd += _names([a for p in pairs for a in p])

        def f(e):
            for out, pairs in groups:
                n = len(pairs)
                for i, (l, r) in enumerate(pairs):
                    ins = e.matmul(out, lhsT=l, rhs=r, start=(i == 0), stop=(i == n - 1))
            return ins
        return self.S.op("pe", list(dict.fromkeys(rd)), list(dict.fromkeys(wr)), f)

    def trs(self, items, ident):
        rd = _names([i for _, i in items] + [ident])
        wr = _names([o for o, _ in items])

        def f(e):
            for o, i in items:
                ins = e.transpose(out=o, in_=i, identity=ident)
            return ins
        return self.S.op("pe", rd, wr, f)

    def dma(self, out, in_, q="sp", **kw):
        return self.S.dma(out, in_, _names([in_]), _names([out]), q=q, **kw)

    def finish(self):
        for t in self.out_toks:
            self.S._wait("sp", t)


def rms_rstd(k, out, ss, n):
    k.act(out, ss, AF.Ln, scale=1.0 / n, bias=EPS)
    k.act(out, out, AF.Exp, scale=-0.5)


def build(T=4096, dbg=False, phases="ABCD"):
    NT = T // 128
    nc = bass.Bass("TRN2", target_bir_lowering=False)
    k = K(nc)

    def din(name, shape, dt=F32):
        return nc.dram_tensor(name, list(shape), dt, kind="ExternalInput").ap()

    def dscr(name, shape, dt):
        return nc.dram_tensor(name, list(shape), dt, kind=("ExternalOutput" if dbg else "Internal")).ap()

    x = din("x", [T, D])
    p_in = din("p", [T, 256])
    attn_g = din("attn_norm_g", [D])
    w_in = din("w_in", [D, IN_TOTAL])
    lb_logits = din("hg_lb_logits", [2, 512])
    hg_norm_g = din("hg_norm_g", [128])
    q_norm_g = din("nsa_q_norm_g", [64])
    k_norm_g = din("nsa_k_norm_g", [3, 64])
    cmp_pe = din("cmp_pe", [2, 32, 64])
    cmp_w1 = din("cmp_w1", [2, 2048, 128])
    cmp_w2 = din("cmp_w2", [2, 128, 64])
    out_norm_g = din("nsa_out_norm_g", [64])
    w_out = din("w_out", [D, D])
    ffn_g = din("ffn_norm_g", [D])
    w_up = din("w_up", [D, 2 * DFF])
    conv_w = din("conv_w", [3, 2 * DFF])
    conv_b = din("conv_b", [2 * DFF])
    w_down = din("w_down", [DFF, D])
    pleg_g = din("ple_gate_norm_g", [D])
    w_pleg = din("w_ple_gate", [D, D])
    w_ple = din("w_ple", [256, D])
    ple_g = din("ple_norm_g", [D])
    c_ident = din("c_ident", [128, 128], BF16)
    c_rope = din("c_rope", [T, 16])
    c_tri2 = din("c_tri2", [128, 128])
    c_chunk = din("c_chunk", [128, 2])

    out = nc.dram_tensor("out", [T, D], F32, kind="ExternalOutput").ap()

    FT = dscr("FT", [16, 64, T], BF16)
    VT = dscr("VT", [T, 256], BF16)
    GT = dscr("GT", [T, 24], F32)
    MIXT = dscr("MIXT", [D, T], BF16)

    c_identf = din("c_identf", [128, 128])
    c_cmask = din("c_cmask", [256, T], BF16)
    c_tri = din("c_tri", [128, 128], BF16)
    c_ntri = din("c_ntri", [128, 128], BF16)
    c_E = din("c_E", [64, T], BF16)
    c_vis = din("c_vis", [T, 64])
    c_cadd = din("c_cadd", [T, 64])
    c_ovl1 = din("c_ovl1", [256, 65], BF16)
    H2 = dscr("H2", [T, D], F32)

    with ExitStack() as es0:
        k.es = es0
        ident = k.sb("ident", [128, 128], BF16)
        k.dma(ident, c_ident)
        identf = k.sb("identf", [128, 128])
        k.dma(identf, c_identf)
        if "A" in phases:
            with ExitStack() as es:
                k.es = es
                phase_a(nc, k, T, NT, x, attn_g, w_in, lb_logits, hg_norm_g, q_norm_g, k_norm_g,
                        c_rope, c_tri2, c_chunk, ident, FT, VT, GT, MIXT)
                k.barrier()
        if "B" in phases:
            with ExitStack() as es:
                k.es = es
                phase_b(nc, k, T, NT, FT, VT, GT, MIXT, k_norm_g, cmp_pe, cmp_w1, cmp_w2, out_norm_g,
                        c_rope, c_cmask, c_tri, c_ntri, c_E, c_vis, c_cadd, c_ovl1, ident, identf)
                k.barrier()
        if "A" not in phases and dbg:
            mi = din("MIXT_in", [D, T], BF16)
            with ExitStack() as es:
                k.es = es
                tb = k.sb("dbg_mix", [128, 8, T], BF16)
                k.dma(tb, mi.rearrange("(c p) t -> p c t", p=128))
                k.dma(MIXT.rearrange("(c p) t -> p c t", p=128), tb)
                k.barrier()
        if "C" in phases:
            with ExitStack() as es:
                k.es = es
                phase_c(nc, k, T, x, MIXT, w_out, ffn_g, w_up, conv_w, conv_b, w_down, H2, ident, identf)
                k.barrier()
        if "D" in phases:
            with ExitStack() as es:
                k.es = es
                phase_d(nc, k, T, H2, p_in, pleg_g, w_pleg, w_ple, ple_g, out, ident, identf)
                k.barrier()
        k.finish()
    return nc


def phase_a(nc, k, T, NT, x, attn_g, w_in, lb_logits, hg_norm_g, q_norm_g, k_norm_g,
            c_rope, c_tri2, c_chunk, ident, FT, VT, GT, MIXT):
    S = k.S
    w_sb = k.sb("a_w", [128, 8, IN_TOTAL], BF16)
    w_v = w_in.rearrange("(c p) n -> p c n", p=128)
    for c in range(8):
        k.dma(w_sb[:, c, :], w_v[:, c, :], q="pool")
    gT = k.sb("a_gT", [128, 8])
    k.dma(gT, attn_g.rearrange("(c p) -> p c", p=128), allow_slow_non_contiguous=True)
    rope = k.sb("a_rope", [128, NT, 16])
    k.dma(rope, c_rope.rearrange("(n p) c -> p n c", p=128))
    tri2 = k.sb("a_tri2", [128, 128])
    k.dma(tri2, c_tri2)
    chunk = k.sb("a_chunk", [128, 2])
    k.dma(chunk, c_chunk)
    l0 = k.sb("a_l0", [128, 512])
    l1 = k.sb("a_l1", [128, 512])
    k.dma(l0, lb_logits[0].partition_broadcast(128))
    k.dma(l1, lb_logits[1].partition_broadcast(128))
    lb = k.sb("a_lb", [128, 512])
    oml = k.sb("a_oml", [128, 512])
    k.tt(l0, l0, l1, ALU.subtract)
    k.act(lb, l0, AF.Sigmoid)
    k.ts(oml, lb, -1.0, 1.0, ALU.mult, ALU.add)
    gq = k.sb("a_gq", [128, 12, 64])
    for h in range(12):
        src = q_norm_g if h < 8 else (k_norm_g[1] if h < 10 else k_norm_g[2])
        k.dma(gq[:, h, :], src.partition_broadcast(128))
    ghg = k.sb("a_ghg", [128, 4, 128])
    for h in range(4):
        k.dma(ghg[:, h, :], hg_norm_g.partition_broadcast(128))
    Sf = k.sb("a_Sf", [128, 4, 128])
    Sb = [k.sb("a_Sb%d" % i, [128, 4, 128], BF16) for i in range(3)]
    k.memset(Sf, 0.0)
    k.memset(Sb[0], 0.0, eng="pool")

    xt = [k.sb("a_x%d" % i, [128, D]) for i in range(2)]
    junk = k.sb("a_junk", [128, D], BF16)
    ss = k.sb("a_ss", [128, 1])
    rstd = k.sb("a_rstd", [128, 1])
    xn = k.sb("a_xn", [128, D], BF16)
    xnT = k.sb("a_xnT", [128, 8, 128], BF16)
    silq = k.sb("a_silq", [128, 512])
    sig = k.sb("a_sig", [128, 512])
    logf = k.sb("a_logf", [128, 512])
    kk = k.sb("a_kk", [128, 512])
    enb = k.sb("a_enb", [128, 512])
    epb = k.sb("a_epb", [128, 512])
    kp = k.sb("a_kp", [128, 512], BF16)
    qp = k.sb("a_qp", [128, 512], BF16)
    vb = k.sb("a_vb", [128, 512], BF16)
    sg = k.sb("a_sg", [128, 512])
    ebl = k.sb("a_ebl", [128, 4, 2])
    qkT = k.sb("a_qkT", [128, 8, 128], BF16)
    ATm = k.sb("a_ATm", [128, 4, 128], BF16)
    tmpS = k.sb("a_tmpS", [128, 4, 128])
    osb = k.sb("a_osb", [128, 4, 128])
    osq = k.sb("a_osq", [128, 4, 128])
    ss4 = k.sb("a_ss4", [128, 4])
    rstd4 = k.sb("a_rstd4", [128, 4])
    onb = k.sb("a_onb", [128, 4, 128], BF16)
    mixs = k.sb("a_mixs", [128, 4, 128], BF16)
    qk = k.sb("a_qk", [128, 12, 64])
    qsq = k.sb("a_qsq", [128, 12, 64])
    ss12 = k.sb("a_ss12", [128, 12])
    rstd12 = k.sb("a_rstd12", [128, 12])
    qkb = k.sb("a_qkb", [128, 12, 64], BF16)
    r_a = k.sb("a_ra", [128, 12, 8])
    r_b = k.sb("a_rb", [128, 12, 8])
    r_c = k.sb("a_rc", [128, 12, 8])
    r_d = k.sb("a_rd", [128, 12, 8])
    kcvc = k.sb("a_kcvc", [128, 256], BF16)
    T16 = k.sb("a_T16", [64, 16, 128], BF16)
    vv = k.sb("a_vv", [128, 256], BF16)
    gsb = k.sb("a_gsb", [128, 24])

    pA = [k.ps("a_pA%d" % i, [128, 512]) for i in range(2)]
    pB = k.ps("a_pB", [128, 512])
    pC = k.ps("a_pC", [128, 8, 128], BF16)
    pD = k.ps("a_pD", [64, 16, 128], BF16)
    pF = k.ps("a_pF", [128, 4, 128])
    pG = k.ps("a_pG", [128, 4, 128])

    cols = [(0, 512), (512, 512), (1024, 512), (1536, 512), (2048, 512), (2560, 512), (3072, 280)]
    mixv = MIXT.rearrange("(c p) t -> p c t", p=128)
    ftv = FT.rearrange("n d t -> d n t")
    pa_i = [0]

    def proj(g):
        c0, n = cols[g]
        dst = pA[pa_i[0] % 2]
        pa_i[0] += 1
        k.mm(dst[:, 0:n], [(xnT[:, c, :], w_sb[:, c, c0:c0 + n]) for c in range(8)])
        return dst

    for it in range(NT):
        t0 = it * 128
        xb = xt[it % 2]
        k.dma(xb, x[t0:t0 + 128, :])
        k.act(junk, xb, AF.Square, accum_out=ss)
        rms_rstd(k, rstd, ss, D)
        k.act(xn, xb, AF.Copy, scale=rstd)
        k.trs([(pC[:, c, :], xn[:, c * 128:(c + 1) * 128]) for c in range(8)], ident)
        k.tt(xnT, pC, gT.unsqueeze(2).to_broadcast([128, 8, 128]), ALU.mult)

        d = proj(1)
        k.act(sig, d, AF.Sigmoid)
        k.tt(sig, sig, oml, ALU.mult)
        k.tt(sig, sig, lb, ALU.add)
        k.act(logf, sig, AF.Ln)
        k.ts(kk, sig, -1.0, 1.0, ALU.mult, ALU.add)
        k.mm(pB, [(tri2, logf)])
        k.act(enb, pB, AF.Exp, scale=-1.0)
        k.act(epb, pB, AF.Exp)
        k.tt(kp, kk, enb, ALU.mult)
        k.mms([(pF[:, h, 0:2], [(logf[:, h * 128:(h + 1) * 128], chunk)]) for h in range(4)])
        k.act(ebl, pF[:, :, 0:2], AF.Exp)
        d = proj(0)
        k.act(silq, d, AF.Silu)
        k.stt(qp, silq, 128 ** -0.5, epb, ALU.mult, ALU.mult)
        d = proj(2)
        k.cp(vb, d, eng="act")
        d = proj(3)
        k.act(sg, d, AF.Silu)
        k.tt(sg, sg, ghg.rearrange("p h v -> p (h v)"), ALU.mult, eng="pool")
        k.trs([(pC[:, h, :], qp[:, h * 128:(h + 1) * 128]) for h in range(4)] +
              [(pC[:, 4 + h, :], kp[:, h * 128:(h + 1) * 128]) for h in range(4)], ident)
        k.cp(qkT, pC)
        S0, S1, S2 = Sb[(2 * it) % 3], Sb[(2 * it + 1) % 3], Sb[(2 * it + 2) % 3]
        k.mms([(pF[:, h, :], [(qkT[:, 4 + h, :], qkT[:, h, :])]) for h in range(4)])
        k.tt(ATm, pF, tri2.unsqueeze(1).to_broadcast([128, 4, 128]), ALU.mult)
        for c in range(2):
            rs = slice(c * 64, (c + 1) * 64)
            k.mms([(pF[:, h, :], [(kp[rs, h * 128:(h + 1) * 128], vb[rs, h * 128:(h + 1) * 128])])
                   for h in range(4)])
            k.tt(tmpS, pF, Sf, ALU.add)
            k.tt(Sf, tmpS, ebl[:, :, c:c + 1].to_broadcast([128, 4, 128]), ALU.mult)
            k.cp(S1 if c == 0 else S2, Sf, eng="pool")
        groups = []
        for h in range(4):
            for c in range(2):
                rs = slice(c * 64, (c + 1) * 64)
                Sc = S0 if c == 0 else S1
                groups.append((pG[rs, h, :], [(ATm[rs, h, rs], vb[rs, h * 128:(h + 1) * 128]),
                                              (qkT[:, h, rs], Sc[:, h, :])]))
        k.mms(groups)
        k.cp(osb, pG, eng="act")
        k.tt(osq, osb, osb, ALU.mult)
        k.rsum(ss4, osq)
        rms_rstd(k, rstd4, ss4, 128)
        k.tt(osb, osb, rstd4.unsqueeze(2).to_broadcast([128, 4, 128]), ALU.mult)
        k.tt(onb, osb, sg.rearrange("p (h v) -> p h v", h=4), ALU.mult)
        k.trs([(pC[:, h, :], onb[:, h, :]) for h in range(4)], ident)
        k.cp(mixs, pC[:, 0:4, :])
        k.dma(mixv[:, 0:4, t0:t0 + 128], mixs)

        d = proj(4)
        k.cp(qk[:, 0:8, :], d.rearrange("p (h d) -> p h d", d=64), eng="act")
        d = proj(5)
        k.cp(kcvc, d[:, 0:256], eng="act")
        k.cp(qk[:, 8:10, :], d[:, 256:384].rearrange("p (h d) -> p h d", d=64), eng="act")
        k.cp(vv[:, 0:128], d[:, 384:512], eng="act")
        d = proj(6)
        k.cp(qk[:, 10:12, :], d[:, 0:128].rearrange("p (h d) -> p h d", d=64), eng="act")
        k.cp(vv[:, 128:256], d[:, 128:256], eng="act")
        k.act(gsb, d[:, 256:280], AF.Sigmoid)
        k.dma(GT[t0:t0 + 128, :], gsb)
        k.dma(VT[t0:t0 + 128, :], vv)
        k.tt(qsq, qk, qk, ALU.mult, eng="pool")
        k.rsum(ss12, qsq)
        rms_rstd(k, rstd12, ss12, 64)
        k.tt(qk, qk, rstd12.unsqueeze(2).to_broadcast([128, 12, 64]), ALU.mult)
        k.tt(qk, qk, gq, ALU.mult, eng="pool")
        cosb = rope[:, it:it + 1, 0:8].to_broadcast([128, 12, 8])
        sinb = rope[:, it:it + 1, 8:16].to_broadcast([128, 12, 8])
        k.tt(r_a, qk[:, :, 0:8], cosb, ALU.mult, eng="pool")
        k.tt(r_b, qk[:, :, 8:16], sinb, ALU.mult, eng="pool")
        k.tt(r_c, qk[:, :, 8:16], cosb, ALU.mult, eng="pool")
        k.tt(r_d, qk[:, :, 0:8], sinb, ALU.mult, eng="pool")
        k.cp(qkb, qk, eng="pool")
        k.tt(qkb[:, :, 0:8], r_a, r_b, ALU.subtract, eng="pool")
        k.tt(qkb[:, :, 8:16], r_c, r_d, ALU.add, eng="pool")
        k.trs([(pD[:, n, :], qkb[:, n, :]) for n in range(12)] +
              [(pD[:, 12 + n, :], kcvc[:, n * 64:(n + 1) * 64]) for n in range(4)], ident)
        k.cp(T16, pD)
        k.dma(ftv[:, :, t0:t0 + 128], T16)


def phase_b(nc, k, T, NT, FT, VT, GT, MIXT, k_norm_g, cmp_pe, cmp_w1, cmp_w2, out_norm_g,
            c_rope, c_cmask, c_tri, c_ntri, c_E, c_vis, c_cadd, c_ovl1, ident, identf):
    NQ = T // 512
    NCMP = T // 16 - 1
    NCT = (NCMP + 127) // 128

    def crow(ct):
        return min(128, NCMP - 128 * ct)

    KA = k.sb("b_KA", [128, 2, T], BF16)
    KW = k.sb("b_KW", [64, 2, T], BF16)
    VS1 = k.sb("b_VS1", [128, NT, 2, 65], BF16)
    VW1 = k.sb("b_VW1", [128, NT, 2, 65], BF16)
    VTv = VT.rearrange("(n p) c -> p n c", p=128)
    k.memset(VS1, 1.0)
    k.memset(VW1, 1.0, eng="pool")
    for g in range(2):
        k.dma(KA[0:64, g, :], FT[8 + g])
        k.dma(KA[64:128, g, :], c_E)
        k.dma(KW[:, g, :], FT[10 + g])
        k.dma(VS1[:, :, g, 0:64], VTv[:, :, g * 64:(g + 1) * 64])
        k.dma(VW1[:, :, g, 0:64], VTv[:, :, 128 + g * 64:128 + (g + 1) * 64])
    tri = k.sb("b_tri", [128, 128], BF16)
    ntri = k.sb("b_ntri", [128, 128], BF16)
    k.dma(tri, c_tri)
    k.dma(ntri, c_ntri)
    zer = k.sb("b_zer", [128, 65], BF16)
    k.memset(zer, 0.0)
    gout = k.sb("b_gout", [128, 64])
    k.dma(gout, out_norm_g.partition_broadcast(128))

    pS = [k.ps("b_pS%d" % i, [128, 512]) for i in range(2)]
    pOs = [k.ps("b_pO%d" % i, [128, 512]) for i in range(2)]
    pO = pOs[0]
    pCIs = [k.ps("b_pCI%d" % i, [128, 4, 128]) for i in range(2)]
    pT = k.ps("b_pT", [128, 4, 128])
    pTb = k.ps("b_pTb", [128, 4, 128], BF16)

    kcT = k.sb("b_kcT", [64, 2, NCT * 128], BF16)
    Rv = k.sb("b_R", [128, NCT, 2, 128], BF16)
    k.memset(kcT, 0.0)
    k.memset(Rv, 0.0, eng="pool")
    ovv = c_ovl1.rearrange("(ct p) c -> p ct c", p=128)
    for ct in range(NCT):
        for g in range(2):
            k.dma(Rv[:, ct, g, 64:128], ovv[:, ct, 1:65])
    with ExitStack() as es_c:
        es_prev = k.es
        k.es = es_c
        kc2 = k.sb("b_kc2", [128, T], BF16)
        hid = k.sb("b_hid", [128, NCT * 128], BF16)
        k.memset(hid, 0.0)
        kcn = k.sb("b_kcn", [128, NCT * 2, 64])
        k.memset(kcn, 0.0)
        gk0 = k.sb("b_gk0", [128, 64])
        k.dma(gk0, k_norm_g[0].partition_broadcast(128))
        ropec = k.sb("b_ropec", [128, NCT, 16])
        k.memset(ropec, 0.0)
        rv = c_rope.rearrange("(c s) f -> c s f", s=16)
        for ct in range(NCT):
            k.dma(ropec[0:crow(ct), ct, :], rv[1 + ct * 128:1 + ct * 128 + crow(ct), 15, :])
        w1 = [k.sb("b_w1%d" % j, [128, 16, 128], BF16) for j in range(2)]
        w2 = [k.sb("b_w2%d" % j, [128, 64], BF16) for j in range(2)]
        pes = [k.sb("b_pe%d" % j, [128, 16], BF16) for j in range(2)]
        bvec = [k.sb("b_bv%d" % j, [128, 1]) for j in range(2)]
        for j in range(2):
            k.dma(w1[j], cmp_w1[j].rearrange("(l p) h -> p l h", p=128), q="pool")
            k.dma(w2[j], cmp_w2[j], q="pool")
            k.dma(pes[j], cmp_pe[j].rearrange("(l two) d -> (two d) l", two=2), q="pool",
                  allow_slow_non_contiguous=True)
        v16 = kc2.rearrange("p (c s) -> p c s", s=16)
        for j in range(2):
            k.mm(pO[:, 0:1], [(w1[j][:, l2, :], pes[j][:, l2:l2 + 1]) for l2 in range(16)])
            k.cp(bvec[j], pO[:, 0:1])
            for g in range(2):
                n = 12 + 2 * j + g
                k.dma(kc2[0:64, :], FT[n])
                k.dma(kc2[64:128, 0:T - 1], FT[n][:, 1:T])
                pairs = []
                for l2 in range(16):
                    rhs = v16[:, 0:NCMP, 2 * l2] if l2 < 8 else v16[:, 1:NCMP + 1, 2 * l2 - 16]
                    pairs.append((w1[j][:, l2, :], rhs))
                k.mm(pS[0][:, 0:NCMP], pairs)
                k.act(hid[:, 0:NCMP], pS[0][:, 0:NCMP], AF.Silu, bias=bvec[j])
                for ct in range(NCT):
                    r = crow(ct)
                    k.mm(pT[0:r, ct, 0:64], [(hid[:, ct * 128:ct * 128 + r], w2[j])])
                    if j == 0:
                        k.cp(kcn[0:r, ct * 2 + g, :], pT[0:r, ct, 0:64])
                    else:
                        k.cp(Rv[0:r, ct, g, 0:64], pT[0:r, ct, 0:64])
        NS = NCT * 2
        ksq = k.sb("b_ksq", [128, NS, 64])
        kss = k.sb("b_kss", [128, NS])
        krs = k.sb("b_krs", [128, NS])
        kcb = k.sb("b_kcb", [128, NS, 64], BF16)
        ra = k.sb("b_ra", [128, 2, 8])
        rb = k.sb("b_rb", [128, 2, 8])
        k.tt(ksq, kcn, kcn, ALU.mult)
        k.rsum(kss, ksq)
        rms_rstd(k, krs, kss, 64)
        k.tt(kcn, kcn, krs.unsqueeze(2).to_broadcast([128, NS, 64]), ALU.mult)
        k.tt(kcn, kcn, gk0.unsqueeze(1).to_broadcast([128, NS, 64]), ALU.mult)
        k.cp(kcb, kcn)
        for ct in range(NCT):
            sl = slice(ct * 2, ct * 2 + 2)
            cosb = ropec[:, ct:ct + 1, 0:8].to_broadcast([128, 2, 8])
            sinb = ropec[:, ct:ct + 1, 8:16].to_broadcast([128, 2, 8])
            k.tt(ra, kcn[:, sl, 0:8], cosb, ALU.mult)
            k.tt(rb, kcn[:, sl, 8:16], sinb, ALU.mult)
            k.tt(kcb[:, sl, 0:8], ra, rb, ALU.subtract)
            k.tt(ra, kcn[:, sl, 8:16], cosb, ALU.mult)
            k.tt(rb, kcn[:, sl, 0:8], sinb, ALU.mult)
            k.tt(kcb[:, sl, 8:16], ra, rb, ALU.add)
        for ct in range(NCT):
            r = crow(ct)
            for g in range(2):
                k.trs([(pTb[0:64, 0, 0:r], kcb[0:r, ct * 2 + g, :])], ident[0:r, 0:r])
                k.cp(kcT[:, g, ct * 128:ct * 128 + r], pTb[0:64, 0, 0:r])
        k.barrier()
        k.es = es_prev

    if B_STAGE < 1:
        return
    QA = [k.sb("b_QA%d" % i, [128, 8, 512], BF16) for i in range(2)]
    cmT = k.sb("b_cmT", [128, NCT, 512], BF16)
    gts = k.sb("b_gts", [128, 4, 24])
    vis = k.sb("b_vis", [128, 4, 64])
    cadd = k.sb("b_cadd", [128, 4, 64])
    NP = 5
    P = [k.sb("b_P%d" % i, [128, 512], BF16) for i in range(NP)]
    ocmp = k.sb("b_ocmp", [128, 4, 8, 64])
    osel = k.sb("b_osel", [128, 4, 8, 64])
    owin = k.sb("b_owin", [128, 4, 8, 64])
    oTs = [k.sb("b_oT%d" % i, [65, 512]) for i in range(2)]
    recs = [k.sb("b_rec%d" % i, [128, 4, 1]) for i in range(2)]
    dens = [k.sb("b_den%d" % i, [128, 4]) for i in range(2)]
    imp = k.sb("b_imp", [128, 4, 2, 64])
    itmps = [k.sb("b_itmp%d" % i, [128, 4, 64]) for i in range(2)]
    NB = 3
    mxs = [k.sb("b_mx%d" % i, [128, 8]) for i in range(NB)]
    mx2s = [k.sb("b_mx2%d" % i, [128, 8]) for i in range(NB)]
    wks = [k.sb("b_wk%d" % i, [128, 64]) for i in range(NB)]
    mks = [k.sb("b_mk%d" % i, [128, 64]) for i in range(NB)]
    negm4 = k.sb("b_negm4", [128, 4, 128], BF16)
    k.memset(negm4, 0.0)
    osq = k.sb("b_osq", [128, 4, 8, 64])
    oss = k.sb("b_oss", [128, 32])
    ors = k.sb("b_ors", [128, 32])
    onb = k.sb("b_onb", [128, 4, 512], BF16)
    mixs = k.sb("b_mixs", [128, 4, 128], BF16)
    ftq = FT.rearrange("n d t -> d n t")
    gtv = GT.rearrange("(s p) c -> p s c", p=128)
    visv = c_vis.rearrange("(s p) c -> p s c", p=128)
    caddv = c_cadd.rearrange("(s p) c -> p s c", p=128)
    cmv = c_cmask.rearrange("(ct p) t -> p ct t", p=128)
    mixv = MIXT.rearrange("(c p) t -> p c t", p=128)
    cnt = [0]
    ocnt = [0]

    def kt_gen(Qa, h, g, kt, lo, hi, mcol, mtile, Ksrc, Vsrc, kdim, pOb, first, last, zero_first, evac):
        i = cnt[0]
        cnt[0] += 1
        ps, Pb = pS[i % 2], P[i % NP]
        if first and zero_first:
            k.mm1(pOb[0:65, :], zer, Qa[:, h, :], True, False)
        k.mm(ps[:, lo:hi], [(Ksrc[0:kdim, g, kt * 128:(kt + 1) * 128], Qa[0:kdim, h, lo:hi])])
        yield
        k.act(Pb[:, lo:hi], ps[:, lo:hi], AF.Exp, scale=0.125)
        yield
        if mtile is not None:
            k.tt(Pb[:, mcol:mcol + 128], Pb[:, mcol:mcol + 128], mtile, ALU.mult)
            yield
        k.mm1(pOb[0:65, lo:hi], Vsrc[:, kt, g, :], Pb[:, lo:hi], (first and not zero_first), last)
        yield
        if last:
            yield from evac_gen(*evac)

    def evac_gen(pOb, oTb, rc, h, odst):
        k.cp(oTb, pOb[0:65, :], eng="act")
        yield
        k.trs([(pT[:, s, 0:65], oTb[:, s * 128:(s + 1) * 128]) for s in range(4)], identf[0:65, 0:65])
        yield
        k.recip(rc, pT[:, :, 64:65])
        yield
        k.tt(odst[:, :, h, :], pT[:, :, 0:64], rc.to_broadcast([128, 4, 64]), ALU.mult)
        yield

    def attend_gens(Qa, h, g, kts, Ksrc, Vsrc, kdim, odst, zero_first):
        j = ocnt[0]
        ocnt[0] += 1
        pOb, oTb, rc = pOs[j % 2], oTs[j % 2], recs[j % 2]
        n = len(kts)
        for idx, (kt, lo, hi, mcol, mtile) in enumerate(kts):
            yield kt_gen(Qa, h, g, kt, lo, hi, mcol, mtile, Ksrc, Vsrc, kdim, pOb,
                         idx == 0, idx == n - 1, zero_first, (pOb, oTb, rc, h, odst))

    def cmp_gen(Qa, h, g, cts):
        pc, rc, dn, itmp = pCIs[h % 2], recs[h % 2], dens[h % 2], itmps[h % 2]
        for ci, ct in enumerate(cts):
            i = cnt[0]
            cnt[0] += 1
            ps, Pb = pS[i % 2], P[i % NP]
            k.mm(ps, [(kcT[0:64, g, ct * 128:(ct + 1) * 128], Qa[0:64, h, :])])
            yield
            k.act(Pb, ps, AF.Exp, scale=0.125)
            yield
            k.tt(Pb, Pb, cmT[:, ct, :], ALU.mult)
            yield
            for s in range(4):
                k.mm1(pc[:, s, :], Pb[:, s * 128:(s + 1) * 128], Rv[:, ct, g, :],
                      ci == 0 and s == 0, ci == len(cts) - 1)
            yield
        k.rsum(dn, pc[:, :, 64:128])
        yield
        k.ts(rc, dn.unsqueeze(2), 1e-30, None, ALU.add)
        yield
        k.recip(rc, rc)
        yield
        k.tt(ocmp[:, :, h, :], pc[:, :, 0:64], rc.to_broadcast([128, 4, 64]), ALU.mult)
        yield
        if h % 4 == 0:
            k.tt(imp[:, :, g, :], pc[:, :, 64:128], rc.to_broadcast([128, 4, 64]), ALU.mult)
        else:
            k.tt(itmp, pc[:, :, 64:128], rc.to_broadcast([128, 4, 64]), ALU.mult)
            yield
            k.tt(imp[:, :, g, :], imp[:, :, g, :], itmp, ALU.add, eng="pool")
        yield

    def topk_gen(g, s, j):
        iv = imp[:, s, g, :]
        mx, mx2, wk, mk = mxs[j % NB], mx2s[j % NB], wks[j % NB], mks[j % NB]
        k.vmax(mx, iv)
        yield
        k.vmatch(wk, mx, iv, -3.0e38)
        yield
        k.vmax(mx2, wk)
        yield
        k.tt(mk, iv, mx2[:, 7:8].to_broadcast([128, 64]), ALU.is_ge)
        yield
        k.ts(negm4[:, s, 64:128], mk, -NEGM, NEGM, ALU.mult, ALU.add)
        yield

    for Qi in range(NQ):
        t0 = Qi * 512
        Qa = QA[Qi % 2]
        k.dma(Qa[0:64, :, :], ftq[:, 0:8, t0:t0 + 512])
        k.dma(gts, gtv[:, Qi * 4:(Qi + 1) * 4, :])
        k.dma(vis, visv[:, Qi * 4:(Qi + 1) * 4, :])
        k.dma(cadd, caddv[:, Qi * 4:(Qi + 1) * 4, :])
        k.dma(cmT, cmv[:, 0:NCT, t0:t0 + 512])
        cts = [ct for ct in range(NCT) if 16 * 128 * ct + 31 <= t0 + 511]
        pipeline((cmp_gen(Qa, h, h // 4, cts) for h in range(8)), 2)
        for g in range(2):
            k.tt(imp[:, :, g, :], imp[:, :, g, :], vis, ALU.mult)
            k.tt(imp[:, :, g, :], imp[:, :, g, :], cadd, ALU.add)
        for g in range(2):
            pipeline((topk_gen(g, s, g * 4 + s) for s in range(4)), 3)
            k.trs([(pTb[:, s, :], negm4[:, s, :]) for s in range(4)], ident)
            k.cp(Qa[64:128, 4 * g:4 * g + 4, :].rearrange("p h (s t) -> p h s t", s=4),
                 pTb[64:128, :, :].unsqueeze(1).to_broadcast([64, 4, 4, 128]))
        def all_gens():
            for h in range(8):
                g = h // 4
                kts = []
                for kt in range(4 * Qi + 4):
                    m = kt - 4 * Qi
                    if m >= 0:
                        kts.append((kt, 128 * m, 512, 128 * m, tri))
                    else:
                        kts.append((kt, 0, 512, 0, None))
                yield from attend_gens(Qa, h, g, kts, KA, VS1, 128, osel, False)
                kts = []
                for kt in range(max(0, 4 * Qi - 4), 4 * Qi + 4):
                    m = kt - 4 * Qi
                    if m >= 0:
                        kts.append((kt, 128 * m, 512, 128 * m, tri))
                    else:
                        kts.append((kt, 0, 128 * (m + 5), 128 * (m + 4), ntri))
                yield from attend_gens(Qa, h, g, kts, KW, VW1, 64, owin, True)
        pipeline(all_gens(), 3)
        if B_STAGE < 4:
            continue
        for br, ob in enumerate((ocmp, osel, owin)):
            k.tt(ob, ob, gts[:, :, br * 8:(br + 1) * 8].unsqueeze(3).to_broadcast([128, 4, 8, 64]),
                 ALU.mult, eng=("pool" if br == 1 else "dve"))
        k.tt(ocmp, ocmp, osel, ALU.add)
        k.tt(ocmp, ocmp, owin, ALU.add, eng="pool")
        if B_STAGE < 5:
            continue
        k.tt(osq, ocmp, ocmp, ALU.mult)
        k.rsum(oss, osq.rearrange("p s h d -> p (s h) d"))
        rms_rstd(k, ors, oss, 64)
        k.tt(ocmp.rearrange("p s h d -> p (s h) d"), ocmp.rearrange("p s h d -> p (s h) d"),
             ors.unsqueeze(2).to_broadcast([128, 32, 64]), ALU.mult)
        k.tt(onb.rearrange("p s (h d) -> p (s h) d", d=64), ocmp.rearrange("p s h d -> p (s h) d"),
             gout.unsqueeze(1).to_broadcast([128, 32, 64]), ALU.mult, eng="pool")
        if B_STAGE < 6:
            continue
        for s in range(4):
            k.trs([(pTb[:, c, :], onb[:, s, c * 128:(c + 1) * 128]) for c in range(4)], ident)
            k.cp(mixs, pTb)
            if B_STAGE >= 7:
                k.dma(mixv[:, 4:8, t0 + s * 128:t0 + (s + 1) * 128], mixs)


def load_colvec(k, dst, src, n_chunks, identf, pTf, tmp):
    k.dma(tmp[0:n_chunks, :], src.rearrange("(c p) -> c p", p=128))
    k.trs([(pTf[:, 0:n_chunks], tmp[0:n_chunks, :])], identf[0:n_chunks, 0:n_chunks])
    k.cp(dst, pTf[:, 0:n_chunks])


def phase_c(nc, k, T, x, MIXT, w_out, ffn_g, w_up, conv_w, conv_b, w_down, H2, ident, identf):
    TT = 256
    NTT = T // TT
    NF = 22
    wo = k.sb("c_wo", [128, 8, D], BF16)
    wu = k.sb("c_wu", [128, 8, 2 * DFF], BF16)
    wd = k.sb("c_wd", [128, NF, D], BF16)
    wov = w_out.rearrange("(c p) n -> p c n", p=128)
    wuv = w_up.rearrange("(c p) n -> p c n", p=128)
    wdv = w_down.rearrange("(c p) n -> p c n", p=128)
    for c in range(8):
        k.dma(wo[:, c, :], wov[:, c, :], q="pool")
    for c in range(8):
        for hh in range(2):
            k.dma(wu[:, c, hh * DFF:(hh + 1) * DFF], wuv[:, c, hh * DFF:(hh + 1) * DFF], q="pool")
    for c in range(NF):
        k.dma(wd[:, c, :], wdv[:, c, :], q="pool")
    pY = [k.ps("c_pY%d" % i, [128, 512]) for i in range(2)]
    pC = k.ps("c_pC", [128, 8, 128], BF16)
    pU = k.ps("c_pU", [128, 4, 256])
    tmpv = k.sb("c_tmpv", [44, 128])
    gfT = k.sb("c_gfT", [128, 8])
    load_colvec(k, gfT, ffn_g, 8, identf, pY[0], tmpv)
    cw = k.sb("c_cw", [128, 3, 44])
    cb = k.sb("c_cb", [128, 44])
    for j in range(3):
        load_colvec(k, cw[:, j, :], conv_w[j], 44, identf, pY[0], tmpv)
    load_colvec(k, cb, conv_b, 44, identf, pY[0], tmpv)
    carry = k.sb("c_carry", [128, 44, 2])
    k.memset(carry, 0.0)

    mt = k.sb("c_mt", [128, 8, TT], BF16)
    hsb = k.sb("c_h", [128, 2, D])
    xt = k.sb("c_x", [128, 2, D])
    junk = k.sb("c_junk", [128, D], BF16)
    ss = k.sb("c_ss", [128, 1])
    rstd = k.sb("c_rstd", [128, 1])
    hn = k.sb("c_hn", [128, D], BF16)
    hnT = k.sb("c_hnT", [128, 8, TT], BF16)
    actT = k.sb("c_actT", [128, NF, TT], BF16)
    uraw = [k.sb("c_uraw%d" % i, [128, TT + 2]) for i in range(4)]
    acc = [k.sb("c_acc%d" % i, [128, TT]) for i in range(4)]
    sil = [k.sb("c_sil%d" % i, [128, TT]) for i in range(2)]
    mixv = MIXT.rearrange("(c p) t -> p c t", p=128)
    xv = x.rearrange("(n p) d -> p n d", p=128)
    h2v = H2.rearrange("(n p) d -> p n d", p=128)

    for it in range(NTT):
        t0 = it * TT
        k.dma(mt, mixv[:, :, t0:t0 + TT])
        k.dma(xt, xv[:, 2 * it:2 * it + 2, :])
        for s in range(2):
            for hh in range(2):
                py = pY[(s * 2 + hh) % 2]
                k.mm(py, [(mt[:, c, s * 128:(s + 1) * 128], wo[:, c, hh * 512:(hh + 1) * 512])
                          for c in range(8)])
                k.tt(hsb[:, s, hh * 512:(hh + 1) * 512], py, xt[:, s, hh * 512:(hh + 1) * 512], ALU.add)
            k.act(junk, hsb[:, s, :], AF.Square, accum_out=ss)
            rms_rstd(k, rstd, ss, D)
            k.act(hn, hsb[:, s, :], AF.Copy, scale=rstd)
            k.trs([(pC[:, c, :], hn[:, c * 128:(c + 1) * 128]) for c in range(8)], ident)
            k.tt(hnT[:, :, s * 128:(s + 1) * 128], pC, gfT.unsqueeze(2).to_broadcast([128, 8, 128]),
                 ALU.mult)
        for i in range(NF):
            accs = []
            for gu in range(2):
                ch = i + gu * NF
                slot = (i % 2) * 2 + gu
                pu = pU[:, slot, :]
                k.mm(pu, [(wu[:, c, ch * 128:(ch + 1) * 128], hnT[:, c, :]) for c in range(8)])
                ur, ac = uraw[slot], acc[slot]
                k.cp(ur[:, 0:2], carry[:, ch, :], eng="pool")
                k.cp(ur[:, 2:TT + 2], pu, eng="act")
                k.act(ac, pu, AF.Identity, scale=cw[:, 2, ch:ch + 1], bias=cb[:, ch:ch + 1])
                k.stt(ac, ur[:, 1:TT + 1], cw[:, 1, ch:ch + 1], ac, ALU.mult, ALU.add)
                k.stt(ac, ur[:, 0:TT], cw[:, 0, ch:ch + 1], ac, ALU.mult, ALU.add)
                k.cp(carry[:, ch, :], ur[:, TT:TT + 2], eng="pool")
                accs.append(ac)
            sl = sil[i % 2]
            k.act(sl, accs[0], AF.Silu)
            k.tt(actT[:, i, :], sl, accs[1], ALU.mult, eng="pool")
        for s in range(2):
            for hh in range(2):
                py = pY[(s * 2 + hh) % 2]
                k.mm(py, [(actT[:, i, s * 128:(s + 1) * 128], wd[:, i, hh * 512:(hh + 1) * 512])
                          for i in range(NF)])
                k.tt(hsb[:, s, hh * 512:(hh + 1) * 512], py, hsb[:, s, hh * 512:(hh + 1) * 512], ALU.add)
        k.dma(h2v[:, 2 * it:2 * it + 2, :], hsb)


def phase_d(nc, k, T, H2, p_in, pleg_g, w_pleg, w_ple, ple_g, out, ident, identf):
    NT = T // 128
    wg = k.sb("d_wg", [128, 8, D], BF16)
    wp = k.sb("d_wp", [128, 2, D], BF16)
    wgv = w_pleg.rearrange("(c p) n -> p c n", p=128)
    wpv = w_ple.rearrange("(c p) n -> p c n", p=128)
    for c in range(8):
        k.dma(wg[:, c, :], wgv[:, c, :], q="pool")
    for c in range(2):
        k.dma(wp[:, c, :], wpv[:, c, :], q="pool")
    pY = [k.ps("d_pY%d" % i, [128, 512]) for i in range(2)]
    pE = [k.ps("d_pE%d" % i, [128, 512]) for i in range(2)]
    pC = k.ps("d_pC", [128, 8, 128], BF16)
    tmpv = k.sb("d_tmpv", [8, 128])
    ggT = k.sb("d_ggT", [128, 8])
    load_colvec(k, ggT, pleg_g, 8, identf, pY[0], tmpv)
    gple = k.sb("d_gple", [128, D])
    k.dma(gple, ple_g.partition_broadcast(128))

    hb = [k.sb("d_h%d" % i, [128, D]) for i in range(2)]
    pb = [k.sb("d_p%d" % i, [128, 256]) for i in range(2)]
    junk = k.sb("d_junk", [128, D], BF16)
    ss = k.sb("d_ss", [128, 1])
    rstd = k.sb("d_rstd", [128, 1])
    ss2 = k.sb("d_ss2", [128, 2])
    rstd2 = k.sb("d_rstd2", [128, 1])
    hn = k.sb("d_hn", [128, D], BF16)
    hT = k.sb("d_hT", [128, 8, 128], BF16)
    pbf = k.sb("d_pbf", [128, 256], BF16)
    pT = k.sb("d_pT", [128, 2, 128], BF16)
    gate = k.sb("d_gate", [128, D])
    e = k.sb("d_e", [128, D])
    ob = [k.sb("d_o%d" % i, [128, D]) for i in range(2)]

    for it in range(NT):
        t0 = it * 128
        h = hb[it % 2]
        pp = pb[it % 2]
        o = ob[it % 2]
        k.dma(h, H2[t0:t0 + 128, :])
        k.dma(pp, p_in[t0:t0 + 128, :])
        k.act(junk, h, AF.Square, accum_out=ss)
        rms_rstd(k, rstd, ss, D)
        k.act(hn, h, AF.Copy, scale=rstd)
        k.trs([(pC[:, c, :], hn[:, c * 128:(c + 1) * 128]) for c in range(8)], ident)
        k.tt(hT, pC, ggT.unsqueeze(2).to_broadcast([128, 8, 128]), ALU.mult)
        for hh in range(2):
            k.mm(pY[hh], [(hT[:, c, :], wg[:, c, hh * 512:(hh + 1) * 512]) for c in range(8)])
            k.act(gate[:, hh * 512:(hh + 1) * 512], pY[hh], AF.Sigmoid)
        k.cp(pbf, pp, eng="pool")
        k.trs([(pC[:, c, :], pbf[:, c * 128:(c + 1) * 128]) for c in range(2)], ident)
        k.cp(pT, pC[:, 0:2, :])
        for hh in range(2):
            k.mm(pE[hh], [(pT[:, c, :], wp[:, c, hh * 512:(hh + 1) * 512]) for c in range(2)])
            k.act(junk[:, hh * 512:(hh + 1) * 512], pE[hh], AF.Square, accum_out=ss2[:, hh:hh + 1])
        k.tt(ss, ss2[:, 0:1], ss2[:, 1:2], ALU.add)
        rms_rstd(k, rstd2, ss, D)
        for hh in range(2):
            k.act(e[:, hh * 512:(hh + 1) * 512], pE[hh], AF.Copy, scale=rstd2)
        k.tt(e, e, gple, ALU.mult)
        k.tt(e, e, gate, ALU.mult, eng="pool")
        k.tt(o, e, h, ALU.add)
        tok = k.dma(out[t0:t0 + 128, :], o)
        k.out_toks.append(tok)


def _consts(T):
    bf = ml_dtypes.bfloat16
    c = {}
    c["c_ident"] = np.eye(128).astype(bf)
    c["c_identf"] = np.eye(128, dtype=np.float32)
    half = 8
    inv = np.float32(500000.0) ** (-np.arange(half, dtype=np.float32) / half)
    ang = np.arange(T, dtype=np.float32)[:, None] * inv[None, :].astype(np.float32)
    c["c_rope"] = np.concatenate([np.cos(ang), np.sin(ang)], 1).astype(np.float32)
    s = np.arange(128)
    c["c_tri2"] = ((s[:, None] <= s[None, :]) & (s[:, None] // 64 == s[None, :] // 64)).astype(np.float32)
    c["c_chunk"] = (s[:, None] // 64 == np.arange(2)[None, :]).astype(np.float32)
    t = np.arange(T)
    cc = np.arange(256)
    ncmp = T // 16 - 1
    c["c_cmask"] = (((16 * cc[:, None] + 31) <= t[None, :]) & (cc[:, None] < ncmp)).astype(bf)
    c["c_tri"] = (s[:, None] <= s[None, :]).astype(bf)
    c["c_ntri"] = (s[None, :] < s[:, None]).astype(bf)
    n = np.arange(64)
    c["c_E"] = ((t[None, :] // 64) == n[:, None]).astype(bf)
    cur = t // 64
    vis = (n[None, :] * 64 <= t[:, None])
    bonus = np.zeros((T, 64), np.float32)
    bonus += (n[None, :] == 0) * 1.0e6
    bonus += (n[None, :] == cur[:, None]) * 2.0e6
    bonus += (n[None, :] == cur[:, None] - 1) * 4.0e6
    c["c_vis"] = vis.astype(np.float32)
    c["c_cadd"] = np.where(vis, bonus, np.float32(-1e30)).astype(np.float32)
    cs = cc * 16
    ssb = n * 64
    ov = np.clip(np.minimum(cs[:, None] + 32, ssb[None, :] + 64)
                 - np.maximum(cs[:, None], ssb[None, :]), 0, None) / 32.0
    o1 = np.zeros((256, 65), np.float32)
    o1[:, 0] = 1.0
    o1[:, 1:] = ov
    o1[ncmp:] = 0.0
    c["c_ovl1"] = o1.astype(bf)
    return c


_W_NAMES = ["attn_norm_g", "w_in", "hg_norm_g", "nsa_q_norm_g", "nsa_k_norm_g", "cmp_pe", "cmp_w1",
            "cmp_w2", "nsa_out_norm_g", "w_out", "ffn_norm_g", "w_up", "conv_w", "conv_b", "w_down",
            "ple_gate_norm_g", "w_ple_gate", "w_ple", "ple_norm_g"]


def kernel(**inputs):
    x = np.asarray(inputs["x"], np.float32)
    p = np.asarray(inputs["p"], np.float32)
    B, T, _ = x.shape
    nc = build(T=T, dbg=False, phases="ABCD")
    shared = {n: np.ascontiguousarray(np.asarray(inputs[n], np.float32)[0]) for n in _W_NAMES}
    shared["hg_lb_logits"] = np.ascontiguousarray(np.asarray(inputs["hg_lb_logits"], np.float32))
    shared.update(_consts(T))
    in_maps = []
    for b in range(B):
        m = dict(shared)
        m["x"] = np.ascontiguousarray(x[b])
        m["p"] = np.ascontiguousarray(p[0, b])
        in_maps.append(m)
    res = run_bass_kernel_spmd(nc, in_maps, core_ids=list(range(B)))
    return np.stack([np.asarray(r["out"], np.float32) for r in res.results], axis=0)
```

```python
from contextlib import ExitStack
import numpy as np
import ml_dtypes
import concourse.bass as bass
import concourse.mybir as mybir
from concourse.bass_utils import run_bass_kernel_spmd

F32 = mybir.dt.float32
BF16 = mybir.dt.bfloat16
ALU = mybir.AluOpType
AF = mybir.ActivationFunctionType
AX = mybir.AxisListType

D = 1024
IN_TOTAL = 3352
DFF = 2816
EPS = 1e-6
NEGM = -30000.0
B_STAGE = 9
FFN_DEPTH = 2
D_DEPTH = 2
PRO_DEPTH = 2


class Sched:
    def __init__(self, nc, n_dma_sems=48):
        self.nc = nc
        self.engs = {"pe": nc.tensor, "act": nc.scalar, "dve": nc.vector,
                     "pool": nc.gpsimd, "sp": nc.sync}
        self.sem = {}
        self.cnt = {}
        for k in ("pe", "act", "dve", "pool"):
            self.sem[k] = nc.alloc_semaphore("s_" + k)
            self.cnt[k] = 0
        self.dma_sems = [nc.alloc_semaphore("s_dma%d" % i) for i in range(n_dma_sems)]
        self.dma_val = [0] * n_dma_sems
        self.dma_rr = 0
        self.waited = {}
        self.last_w = {}
        self.readers = {}
        self.nwaits = 0
        self.inflight = {}
        self.max_desc = 1536

    def _wait(self, eng, tok):
        sem, val, key = tok
        if key == eng and eng == "pe":
            return
        k = (eng, key)
        if self.waited.get(k, 0) >= val:
            return
        self.engs[eng].wait_ge(sem, val)
        self.nwaits += 1
        self.waited[k] = val

    def deps(self, eng, reads, writes):
        toks = []
        for r in reads:
            t = self.last_w.get(r)
            if t is not None:
                toks.append(t)
        for w in writes:
            t = self.last_w.get(w)
            if t is not None:
                toks.append(t)
            toks.extend(self.readers.get(w, ()))
        for t in toks:
            self._wait(eng, t)

    def commit(self, tok, reads, writes):
        for w in writes:
            self.last_w[w] = tok
            self.readers[w] = []
        for r in reads:
            if r in writes:
                continue
            lst = self.readers.setdefault(r, [])
            lst[:] = [t for t in lst if t[2] != tok[2]]
            lst.append(tok)

    def op(self, eng, reads, writes, fn):
        self.deps(eng, reads, writes)
        ins = fn(self.engs[eng])
        self.cnt[eng] += 1
        ins.then_inc(self.sem[eng], 1)
        tok = (self.sem[eng], self.cnt[eng], eng)
        self.commit(tok, reads, writes)
        return tok

    @staticmethod
    def _ndesc(ap):
        dims = list(ap.ap)
        total = 1
        for st, n in dims:
            total *= n
        run = 1
        for st, n in reversed(dims[1:]):
            if st == run:
                run *= n
            else:
                break
        return max(1, total // max(run, 1))

    def dma(self, out, in_, reads, writes, q="sp", **kw):
        nd = max(self._ndesc(out), self._ndesc(in_))
        fifo = self.inflight.setdefault(q, [])
        while fifo and sum(d for _, d in fifo) + nd > self.max_desc:
            tok0, _ = fifo.pop(0)
            self._wait(q, tok0)
        tok = self._dma(out, in_, reads, writes, q, **kw)
        fifo.append((tok, nd))
        return tok

    def _dma(self, out, in_, reads, writes, q="sp", **kw):
        i = self.dma_rr
        self.dma_rr = (self.dma_rr + 1) % len(self.dma_sems)
        sem = self.dma_sems[i]
        key = "dma%d" % i
        if self.dma_val[i] > 0:
            self._wait(q, (sem, self.dma_val[i], key))
        self.deps(q, reads, writes)
        self.dma_val[i] += 16
        self.engs[q].dma_start(out=out, in_=in_, **kw).then_inc(sem, 16)
        tok = (sem, self.dma_val[i], key)
        self.commit(tok, reads, writes)
        return tok


_TAGS = {}
_KEEP = []


def tag(ap, name):
    _TAGS[id(ap)] = name
    _KEEP.append(ap)
    return ap


def sub(ap, fn):
    r = fn(ap)
    if id(ap) in _TAGS:
        tag(r, _TAGS[id(ap)])
    return r


def pipeline(gens, depth):
    gens = iter(gens)
    active = []
    exhausted = False
    while True:
        if not exhausted and len(active) < depth:
            g = next(gens, None)
            if g is None:
                exhausted = True
            else:
                active.append(g)
        if not active:
            if exhausted:
                break
            continue
        for g in list(active):
            try:
                next(g)
            except StopIteration:
                active.remove(g)


def _names(aps):
    out = []
    for a in aps:
        if a is None or isinstance(a, (int, float)):
            continue
        n = _TAGS.get(id(a)) or a.name
        if n not in out:
            out.append(n)
    return out


class K:
    def __init__(self, nc):
        self.nc = nc
        self.S = Sched(nc)
        self.out_toks = []
        self.es = None

    def sb(self, name, shape, dt=F32):
        return self.es.enter_context(self.nc.sbuf_tensor(name, list(shape), dt))[:]

    def ps(self, name, shape, dt=F32):
        return self.es.enter_context(self.nc.psum_tensor(name, list(shape), dt))[:]

    def barrier(self):
        S = self.S
        toks = [(S.sem[e], S.cnt[e], e) for e in ("pe", "act", "dve", "pool") if S.cnt[e] > 0]
        toks += [(S.dma_sems[i], S.dma_val[i], "dma%d" % i) for i in range(len(S.dma_sems))
                 if S.dma_val[i] > 0]
        for eng in ("sp", "pe", "act", "dve", "pool"):
            for t in toks:
                S._wait(eng, t)
        S.last_w.clear()
        S.readers.clear()

    def mm1(self, out, lhsT, rhs, start, stop):
        return self.S.op("pe", _names([lhsT, rhs]), _names([out]),
                         lambda e: e.matmul(out, lhsT=lhsT, rhs=rhs, start=start, stop=stop))

    def vmax(self, out, in_):
        return self.S.op("dve", _names([in_]), _names([out]), lambda e: e.max(out=out, in_=in_))

    def vmatch(self, out, mx, vals, imm):
        return self.S.op("dve", _names([mx, vals]), _names([out]),
                         lambda e: e.match_replace(out=out, in_to_replace=mx, in_values=vals,
                                                   imm_value=imm))

    def recip(self, out, in_):
        return self.S.op("dve", _names([in_]), _names([out]), lambda e: e.reciprocal(out=out, in_=in_))

    def act(self, out, in_, func, bias=None, scale=None, accum_out=None, eng="act"):
        kw = {}
        if bias is not None:
            kw["bias"] = bias
        if scale is not None:
            kw["scale"] = scale
        if accum_out is not None:
            kw["accum_out"] = accum_out
        rd = _names([in_, bias, scale])
        wr = _names([out, accum_out])
        return self.S.op(eng, rd, wr, lambda e: e.activation(out=out, in_=in_, func=func, **kw))

    def tt(self, out, in0, in1, op, eng="dve"):
        return self.S.op(eng, _names([in0, in1]), _names([out]),
                         lambda e: e.tensor_tensor(out=out, in0=in0, in1=in1, op=op))

    def ts(self, out, in0, s1, s2, op0, op1=None, eng="dve"):
        def f(e):
            if op1 is None:
                return e.tensor_scalar(out=out, in0=in0, scalar1=s1, scalar2=None, op0=op0)
            return e.tensor_scalar(out=out, in0=in0, scalar1=s1, scalar2=s2, op0=op0, op1=op1)
        return self.S.op(eng, _names([in0, s1, s2]), _names([out]), f)

    def stt(self, out, in0, scalar, in1, op0, op1, eng="dve"):
        return self.S.op(eng, _names([in0, scalar, in1]), _names([out]),
                         lambda e: e.scalar_tensor_tensor(out=out, in0=in0, scalar=scalar, in1=in1,
                                                          op0=op0, op1=op1))

    def cp(self, out, in_, eng="dve"):
        if eng == "act":
            return self.S.op("act", _names([in_]), _names([out]), lambda e: e.copy(out=out, in_=in_))
        return self.S.op(eng, _names([in_]), _names([out]), lambda e: e.tensor_copy(out=out, in_=in_))

    def rsum(self, out, in_, eng="dve"):
        return self.S.op(eng, _names([in_]), _names([out]),
                         lambda e: e.reduce_sum(out=out, in_=in_, axis=AX.X))

    def memset(self, out, val, eng="dve"):
        return self.S.op(eng, [], _names([out]), lambda e: e.memset(out, val))

    def mm(self, out, pairs, extra_w=()):
        rd = _names([a for p in pairs for a in p])
        n = len(pairs)

        def f(e):
            for i, (l, r) in enumerate(pairs):
                ins = e.matmul(out, lhsT=l, rhs=r, start=(i == 0), stop=(i == n - 1))
            return ins
        return self.S.op("pe", rd, _names([out]) + list(extra_w), f)

    def mms(self, groups):
        rd, wr = [], []
        for out, pairs in groups:
            wr += _names([out])
            rd += _names([a for p in pairs for a in p])

        def f(e):
            for out, pairs in groups:
                n = len(pairs)
                for i, (l, r) in enumerate(pairs):
                    ins = e.matmul(out, lhsT=l, rhs=r, start=(i == 0), stop=(i == n - 1))
            return ins
        return self.S.op("pe", list(dict.fromkeys(rd)), list(dict.fromkeys(wr)), f)

    def trs(self, items, ident):
        rd = _names([i for _, i in items] + [ident])
        wr = _names([o for o, _ in items])

        def f(e):
            for o, i in items:
                ins = e.transpose(out=o, in_=i, identity=ident)
            return ins
        return self.S.op("pe", rd, wr, f)

    def dma(self, out, in_, q="sp", **kw):
        return self.S.dma(out, in_, _names([in_]), _names([out]), q=q, **kw)

    def finish(self):
        for t in self.out_toks:
            self.S._wait("sp", t)


def rms_rstd(k, out, ss, n):
    k.act(out, ss, AF.Ln, scale=1.0 / n, bias=EPS)
    k.act(out, out, AF.Exp, scale=-0.5)


def build(T=4096, dbg=False, phases="ABCD"):
    NT = T // 128
    nc = bass.Bass("TRN2", target_bir_lowering=False)
    k = K(nc)

    def din(name, shape, dt=F32):
        return nc.dram_tensor(name, list(shape), dt, kind="ExternalInput").ap()

    def dscr(name, shape, dt):
        return nc.dram_tensor(name, list(shape), dt, kind=("ExternalOutput" if dbg else "Internal")).ap()

    x = din("x", [T, D])
    p_in = din("p", [T, 256])
    attn_g = din("attn_norm_g", [D])
    w_in = din("w_in", [D, IN_TOTAL])
    lb_logits = din("hg_lb_logits", [2, 512])
    hg_norm_g = din("hg_norm_g", [128])
    q_norm_g = din("nsa_q_norm_g", [64])
    k_norm_g = din("nsa_k_norm_g", [3, 64])
    cmp_pe = din("cmp_pe", [2, 32, 64])
    cmp_w1 = din("cmp_w1", [2, 2048, 128])
    cmp_w2 = din("cmp_w2", [2, 128, 64])
    out_norm_g = din("nsa_out_norm_g", [64])
    w_out = din("w_out", [D, D])
    ffn_g = din("ffn_norm_g", [D])
    w_up = din("w_up", [D, 2 * DFF])
    conv_w = din("conv_w", [3, 2 * DFF])
    conv_b = din("conv_b", [2 * DFF])
    w_down = din("w_down", [DFF, D])
    pleg_g = din("ple_gate_norm_g", [D])
    w_pleg = din("w_ple_gate", [D, D])
    w_ple = din("w_ple", [256, D])
    ple_g = din("ple_norm_g", [D])
    c_ident = din("c_ident", [128, 128], BF16)
    c_rope = din("c_rope", [T, 16])
    c_tri2 = din("c_tri2", [128, 128])
    c_chunk = din("c_chunk", [128, 2])

    out = nc.dram_tensor("out", [T, D], F32, kind="ExternalOutput").ap()

    FT = dscr("FT", [16, 64, T], BF16)
    VT = dscr("VT", [T, 256], BF16)
    GT = dscr("GT", [T, 24], F32)
    MIXT = dscr("MIXT", [D, T], BF16)

    c_identf = din("c_identf", [128, 128])
    c_cmask = din("c_cmask", [256, T], BF16)
    c_tri = din("c_tri", [128, 128], BF16)
    c_ntri = din("c_ntri", [128, 128], BF16)
    c_E = din("c_E", [64, T], BF16)
    c_vis = din("c_vis", [T, 64])
    c_cadd = din("c_cadd", [T, 64])
    c_ovl1 = din("c_ovl1", [256, 65], BF16)
    H2 = dscr("H2", [T, D], F32)

    with ExitStack() as es0:
        k.es = es0
        ident = k.sb("ident", [128, 128], BF16)
        k.dma(ident, c_ident)
        identf = k.sb("identf", [128, 128])
        k.dma(identf, c_identf)
        if "A" in phases:
            with ExitStack() as es:
                k.es = es
                phase_a(nc, k, T, NT, x, attn_g, w_in, lb_logits, hg_norm_g, q_norm_g, k_norm_g,
                        c_rope, c_tri2, c_chunk, ident, FT, VT, GT, MIXT)
                k.barrier()
        if "B" in phases:
            with ExitStack() as es:
                k.es = es
                phase_b(nc, k, T, NT, FT, VT, GT, MIXT, k_norm_g, cmp_pe, cmp_w1, cmp_w2, out_norm_g,
                        c_rope, c_cmask, c_tri, c_ntri, c_E, c_vis, c_cadd, c_ovl1, ident, identf)
                k.barrier()
        if "A" not in phases and dbg:
            mi = din("MIXT_in", [D, T], BF16)
            with ExitStack() as es:
                k.es = es
                tb = k.sb("dbg_mix", [128, 8, T], BF16)
                k.dma(tb, mi.rearrange("(c p) t -> p c t", p=128))
                k.dma(MIXT.rearrange("(c p) t -> p c t", p=128), tb)
                k.barrier()
        if "C" in phases:
            with ExitStack() as es:
                k.es = es
                phase_c(nc, k, T, x, MIXT, w_out, ffn_g, w_up, conv_w, conv_b, w_down, H2, ident, identf)
                k.barrier()
        if "D" in phases:
            with ExitStack() as es:
                k.es = es
                phase_d(nc, k, T, H2, p_in, pleg_g, w_pleg, w_ple, ple_g, out, ident, identf)
                k.barrier()
        k.finish()
    return nc


def phase_a(nc, k, T, NT, x, attn_g, w_in, lb_logits, hg_norm_g, q_norm_g, k_norm_g,
            c_rope, c_tri2, c_chunk, ident, FT, VT, GT, MIXT):
    S = k.S
    w_sb = k.sb("a_w", [128, 8, IN_TOTAL], BF16)
    w_v = w_in.rearrange("(c p) n -> p c n", p=128)
    for c in range(8):
        k.dma(w_sb[:, c, :], w_v[:, c, :], q="pool")
    gT = k.sb("a_gT", [128, 8])
    k.dma(gT, attn_g.rearrange("(c p) -> p c", p=128), allow_slow_non_contiguous=True)
    rope = k.sb("a_rope", [128, NT, 16])
    k.dma(rope, c_rope.rearrange("(n p) c -> p n c", p=128))
    tri2 = k.sb("a_tri2", [128, 128])
    k.dma(tri2, c_tri2)
    chunk = k.sb("a_chunk", [128, 2])
    k.dma(chunk, c_chunk)
    l0 = k.sb("a_l0", [128, 512])
    l1 = k.sb("a_l1", [128, 512])
    k.dma(l0, lb_logits[0].partition_broadcast(128))
    k.dma(l1, lb_logits[1].partition_broadcast(128))
    lb = k.sb("a_lb", [128, 512])
    oml = k.sb("a_oml", [128, 512])
    k.tt(l0, l0, l1, ALU.subtract)
    k.act(lb, l0, AF.Sigmoid)
    k.ts(oml, lb, -1.0, 1.0, ALU.mult, ALU.add)
    gq = k.sb("a_gq", [128, 12, 64])
    for h in range(12):
        src = q_norm_g if h < 8 else (k_norm_g[1] if h < 10 else k_norm_g[2])
        k.dma(gq[:, h, :], src.partition_broadcast(128))
    ghg = k.sb("a_ghg", [128, 4, 128])
    for h in range(4):
        k.dma(ghg[:, h, :], hg_norm_g.partition_broadcast(128))
    Sf = k.sb("a_Sf", [128, 4, 128])
    Sb = [k.sb("a_Sb%d" % i, [128, 4, 128], BF16) for i in range(3)]
    k.memset(Sf, 0.0)
    k.memset(Sb[0], 0.0, eng="pool")

    xt = [k.sb("a_x%d" % i, [128, D]) for i in range(2)]
    junk = k.sb("a_junk", [128, D], BF16)
    ss = k.sb("a_ss", [128, 1])
    rstd = k.sb("a_rstd", [128, 1])
    xn = k.sb("a_xn", [128, D], BF16)
    xnT = k.sb("a_xnT", [128, 8, 128], BF16)
    silq = k.sb("a_silq", [128, 512])
    sig = k.sb("a_sig", [128, 512])
    logf = k.sb("a_logf", [128, 512])
    kk = k.sb("a_kk", [128, 512])
    enb = k.sb("a_enb", [128, 512])
    epb = k.sb("a_epb", [128, 512])
    kp = k.sb("a_kp", [128, 512], BF16)
    qp = k.sb("a_qp", [128, 512], BF16)
    vb = k.sb("a_vb", [128, 512], BF16)
    sg = k.sb("a_sg", [128, 512])
    ebl = k.sb("a_ebl", [128, 4, 2])
    qkT = k.sb("a_qkT", [128, 8, 128], BF16)
    ATm = k.sb("a_ATm", [128, 4, 128], BF16)
    tmpS = k.sb("a_tmpS", [128, 4, 128])
    osb = k.sb("a_osb", [128, 4, 128])
    osq = k.sb("a_osq", [128, 4, 128])
    ss4 = k.sb("a_ss4", [128, 4])
    rstd4 = k.sb("a_rstd4", [128, 4])
    onb = k.sb("a_onb", [128, 4, 128], BF16)
    mixs = k.sb("a_mixs", [128, 4, 128], BF16)
    qk = k.sb("a_qk", [128, 12, 64])
    qsq = k.sb("a_qsq", [128, 12, 64])
    ss12 = k.sb("a_ss12", [128, 12])
    rstd12 = k.sb("a_rstd12", [128, 12])
    qkb = k.sb("a_qkb", [128, 12, 64], BF16)
    r_a = k.sb("a_ra", [128, 12, 8])
    r_b = k.sb("a_rb", [128, 12, 8])
    r_c = k.sb("a_rc", [128, 12, 8])
    r_d = k.sb("a_rd", [128, 12, 8])
    kcvc = k.sb("a_kcvc", [128, 256], BF16)
    T16 = k.sb("a_T16", [64, 16, 128], BF16)
    vv = k.sb("a_vv", [128, 256], BF16)
    gsb = k.sb("a_gsb", [128, 24])

    pA = [k.ps("a_pA%d" % i, [128, 512]) for i in range(2)]
    pB = k.ps("a_pB", [128, 512])
    pC = k.ps("a_pC", [128, 8, 128], BF16)
    pD = k.ps("a_pD", [64, 16, 128], BF16)
    pF = k.ps("a_pF", [128, 4, 128])
    pG = k.ps("a_pG", [128, 4, 128])

    cols = [(0, 512), (512, 512), (1024, 512), (1536, 512), (2048, 512), (2560, 512), (3072, 280)]
    mixv = MIXT.rearrange("(c p) t -> p c t", p=128)
    ftv = FT.rearrange("n d t -> d n t")
    pa_i = [0]

    def proj(g):
        c0, n = cols[g]
        dst = pA[pa_i[0] % 2]
        pa_i[0] += 1
        k.mm(dst[:, 0:n], [(xnT[:, c, :], w_sb[:, c, c0:c0 + n]) for c in range(8)])
        return dst

    for it in range(NT):
        t0 = it * 128
        xb = xt[it % 2]
        k.dma(xb, x[t0:t0 + 128, :])
        k.act(junk, xb, AF.Square, accum_out=ss)
        rms_rstd(k, rstd, ss, D)
        k.act(xn, xb, AF.Copy, scale=rstd)
        k.trs([(pC[:, c, :], xn[:, c * 128:(c + 1) * 128]) for c in range(8)], ident)
        k.tt(xnT, pC, gT.unsqueeze(2).to_broadcast([128, 8, 128]), ALU.mult)

        d = proj(1)
        k.act(sig, d, AF.Sigmoid)
        k.tt(sig, sig, oml, ALU.mult)
        k.tt(sig, sig, lb, ALU.add)
        k.act(logf, sig, AF.Ln)
        k.ts(kk, sig, -1.0, 1.0, ALU.mult, ALU.add)
        k.mm(pB, [(tri2, logf)])
        k.act(enb, pB, AF.Exp, scale=-1.0)
        k.act(epb, pB, AF.Exp)
        k.tt(kp, kk, enb, ALU.mult)
        k.mms([(pF[:, h, 0:2], [(logf[:, h * 128:(h + 1) * 128], chunk)]) for h in range(4)])
        k.act(ebl, pF[:, :, 0:2], AF.Exp)
        d = proj(0)
        k.act(silq, d, AF.Silu)
        k.stt(qp, silq, 128 ** -0.5, epb, ALU.mult, ALU.mult)
        d = proj(2)
        k.cp(vb, d, eng="act")
        d = proj(3)
        k.act(sg, d, AF.Silu)
        k.tt(sg, sg, ghg.rearrange("p h v -> p (h v)"), ALU.mult, eng="pool")
        k.trs([(pC[:, h, :], qp[:, h * 128:(h + 1) * 128]) for h in range(4)] +
              [(pC[:, 4 + h, :], kp[:, h * 128:(h + 1) * 128]) for h in range(4)], ident)
        k.cp(qkT, pC)
        S0, S1, S2 = Sb[(2 * it) % 3], Sb[(2 * it + 1) % 3], Sb[(2 * it + 2) % 3]
        k.mms([(pF[:, h, :], [(qkT[:, 4 + h, :], qkT[:, h, :])]) for h in range(4)])
        k.tt(ATm, pF, tri2.unsqueeze(1).to_broadcast([128, 4, 128]), ALU.mult)
        for c in range(2):
            rs = slice(c * 64, (c + 1) * 64)
            k.mms([(pF[:, h, :], [(kp[rs, h * 128:(h + 1) * 128], vb[rs, h * 128:(h + 1) * 128])])
                   for h in range(4)])
            k.tt(tmpS, pF, Sf, ALU.add)
            k.tt(Sf, tmpS, ebl[:, :, c:c + 1].to_broadcast([128, 4, 128]), ALU.mult)
            k.cp(S1 if c == 0 else S2, Sf, eng="pool")
        groups = []
        for h in range(4):
            for c in range(2):
                rs = slice(c * 64, (c + 1) * 64)
                Sc = S0 if c == 0 else S1
                groups.append((pG[rs, h, :], [(ATm[rs, h, rs], vb[rs, h * 128:(h + 1) * 128]),
                                              (qkT[:, h, rs], Sc[:, h, :])]))
        k.mms(groups)
        k.cp(osb, pG, eng="act")
        k.tt(osq, osb, osb, ALU.mult)
        k.rsum(ss4, osq)
        rms_rstd(k, rstd4, ss4, 128)
        k.tt(osb, osb, rstd4.unsqueeze(2).to_broadcast([128, 4, 128]), ALU.mult)
        k.tt(onb, osb, sg.rearrange("p (h v) -> p h v", h=4), ALU.mult)
        k.trs([(pC[:, h, :], onb[:, h, :]) for h in range(4)], ident)
        k.cp(mixs, pC[:, 0:4, :])
        k.dma(mixv[:, 0:4, t0:t0 + 128], mixs)

        d = proj(4)
        k.cp(qk[:, 0:8, :], d.rearrange("p (h d) -> p h d", d=64), eng="act")
        d = proj(5)
        k.cp(kcvc, d[:, 0:256], eng="act")
        k.cp(qk[:, 8:10, :], d[:, 256:384].rearrange("p (h d) -> p h d", d=64), eng="act")
        k.cp(vv[:, 0:128], d[:, 384:512], eng="act")
        d = proj(6)
        k.cp(qk[:, 10:12, :], d[:, 0:128].rearrange("p (h d) -> p h d", d=64), eng="act")
        k.cp(vv[:, 128:256], d[:, 128:256], eng="act")
        k.act(gsb, d[:, 256:280], AF.Sigmoid)
        k.dma(GT[t0:t0 + 128, :], gsb)
        k.dma(VT[t0:t0 + 128, :], vv)
        k.tt(qsq, qk, qk, ALU.mult, eng="pool")
        k.rsum(ss12, qsq)
        rms_rstd(k, rstd12, ss12, 64)
        k.tt(qk, qk, rstd12.unsqueeze(2).to_broadcast([128, 12, 64]), ALU.mult)
        k.tt(qk, qk, gq, ALU.mult, eng="pool")
        cosb = rope[:, it:it + 1, 0:8].to_broadcast([128, 12, 8])
        sinb = rope[:, it:it + 1, 8:16].to_broadcast([128, 12, 8])
        k.tt(r_a, qk[:, :, 0:8], cosb, ALU.mult, eng="pool")
        k.tt(r_b, qk[:, :, 8:16], sinb, ALU.mult, eng="pool")
        k.tt(r_c, qk[:, :, 8:16], cosb, ALU.mult, eng="pool")
        k.tt(r_d, qk[:, :, 0:8], sinb, ALU.mult, eng="pool")
        k.cp(qkb, qk, eng="pool")
        k.tt(qkb[:, :, 0:8], r_a, r_b, ALU.subtract, eng="pool")
        k.tt(qkb[:, :, 8:16], r_c, r_d, ALU.add, eng="pool")
        k.trs([(pD[:, n, :], qkb[:, n, :]) for n in range(12)] +
              [(pD[:, 12 + n, :], kcvc[:, n * 64:(n + 1) * 64]) for n in range(4)], ident)
        k.cp(T16, pD)
        k.dma(ftv[:, :, t0:t0 + 128], T16)


def phase_b(nc, k, T, NT, FT, VT, GT, MIXT, k_norm_g, cmp_pe, cmp_w1, cmp_w2, out_norm_g,
            c_rope, c_cmask, c_tri, c_ntri, c_E, c_vis, c_cadd, c_ovl1, ident, identf):
    NQ = T // 512
    NCMP = T // 16 - 1
    NCT = (NCMP + 127) // 128

    def crow(ct):
        return min(128, NCMP - 128 * ct)

    KA = k.sb("b_KA", [128, 2, T], BF16)
    KW = k.sb("b_KW", [64, 2, T], BF16)
    VS1 = k.sb("b_VS1", [128, NT, 2, 65], BF16)
    VW1 = k.sb("b_VW1", [128, NT, 2, 65], BF16)
    VTv = VT.rearrange("(n p) c -> p n c", p=128)
    k.memset(VS1, 1.0)
    k.memset(VW1, 1.0, eng="pool")
    for g in range(2):
        k.dma(KA[0:64, g, :], FT[8 + g])
        k.dma(KA[64:128, g, :], c_E)
        k.dma(KW[:, g, :], FT[10 + g])
        k.dma(VS1[:, :, g, 0:64], VTv[:, :, g * 64:(g + 1) * 64])
        k.dma(VW1[:, :, g, 0:64], VTv[:, :, 128 + g * 64:128 + (g + 1) * 64])
    tri = k.sb("b_tri", [128, 128], BF16)
    ntri = k.sb("b_ntri", [128, 128], BF16)
    k.dma(tri, c_tri)
    k.dma(ntri, c_ntri)
    zer = k.sb("b_zer", [128, 65], BF16)
    k.memset(zer, 0.0)
    gout = k.sb("b_gout", [128, 64])
    k.dma(gout, out_norm_g.partition_broadcast(128))

    pS = [k.ps("b_pS%d" % i, [128, 512]) for i in range(2)]
    pOs = [k.ps("b_pO%d" % i, [128, 512]) for i in range(2)]
    pO = pOs[0]
    pCIs = [k.ps("b_pCI%d" % i, [128, 4, 128]) for i in range(2)]
    pT = k.ps("b_pT", [128, 4, 128])
    pTb = k.ps("b_pTb", [128, 4, 128], BF16)

    kcT = k.sb("b_kcT", [64, 2, NCT * 128], BF16)
    Rv = k.sb("b_R", [128, NCT, 2, 128], BF16)
    k.memset(kcT, 0.0)
    k.memset(Rv, 0.0, eng="pool")
    ovv = c_ovl1.rearrange("(ct p) c -> p ct c", p=128)
    for ct in range(NCT):
        for g in range(2):
            k.dma(Rv[:, ct, g, 64:128], ovv[:, ct, 1:65])
    with ExitStack() as es_c:
        es_prev = k.es
        k.es = es_c
        kc2 = k.sb("b_kc2", [128, T], BF16)
        hid = k.sb("b_hid", [128, NCT * 128], BF16)
        k.memset(hid, 0.0)
        kcn = k.sb("b_kcn", [128, NCT * 2, 64])
        k.memset(kcn, 0.0)
        gk0 = k.sb("b_gk0", [128, 64])
        k.dma(gk0, k_norm_g[0].partition_broadcast(128))
        ropec = k.sb("b_ropec", [128, NCT, 16])
        k.memset(ropec, 0.0)
        rv = c_rope.rearrange("(c s) f -> c s f", s=16)
        for ct in range(NCT):
            k.dma(ropec[0:crow(ct), ct, :], rv[1 + ct * 128:1 + ct * 128 + crow(ct), 15, :])
        w1 = [k.sb("b_w1%d" % j, [128, 16, 128], BF16) for j in range(2)]
        w2 = [k.sb("b_w2%d" % j, [128, 64], BF16) for j in range(2)]
        pes = [k.sb("b_pe%d" % j, [128, 16], BF16) for j in range(2)]
        bvec = [k.sb("b_bv%d" % j, [128, 1]) for j in range(2)]
        for j in range(2):
            k.dma(w1[j], cmp_w1[j].rearrange("(l p) h -> p l h", p=128), q="pool")
            k.dma(w2[j], cmp_w2[j], q="pool")
            k.dma(pes[j], cmp_pe[j].rearrange("(l two) d -> (two d) l", two=2), q="pool",
                  allow_slow_non_contiguous=True)
        v16 = kc2.rearrange("p (c s) -> p c s", s=16)
        for j in range(2):
            k.mm(pO[:, 0:1], [(w1[j][:, l2, :], pes[j][:, l2:l2 + 1]) for l2 in range(16)])
            k.cp(bvec[j], pO[:, 0:1])
            for g in range(2):
                n = 12 + 2 * j + g
                k.dma(kc2[0:64, :], FT[n])
                k.dma(kc2[64:128, 0:T - 1], FT[n][:, 1:T])
                pairs = []
                for l2 in range(16):
                    rhs = v16[:, 0:NCMP, 2 * l2] if l2 < 8 else v16[:, 1:NCMP + 1, 2 * l2 - 16]
                    pairs.append((w1[j][:, l2, :], rhs))
                k.mm(pS[0][:, 0:NCMP], pairs)
                k.act(hid[:, 0:NCMP], pS[0][:, 0:NCMP], AF.Silu, bias=bvec[j])
                for ct in range(NCT):
                    r = crow(ct)
                    k.mm(pT[0:r, ct, 0:64], [(hid[:, ct * 128:ct * 128 + r], w2[j])])
                    if j == 0:
                        k.cp(kcn[0:r, ct * 2 + g, :], pT[0:r, ct, 0:64])
                    else:
                        k.cp(Rv[0:r, ct, g, 0:64], pT[0:r, ct, 0:64])
        NS = NCT * 2
        ksq = k.sb("b_ksq", [128, NS, 64])
        kss = k.sb("b_kss", [128, NS])
        krs = k.sb("b_krs", [128, NS])
        kcb = k.sb("b_kcb", [128, NS, 64], BF16)
        ra = k.sb("b_ra", [128, 2, 8])
        rb = k.sb("b_rb", [128, 2, 8])
        k.tt(ksq, kcn, kcn, ALU.mult)
        k.rsum(kss, ksq)
        rms_rstd(k, krs, kss, 64)
        k.tt(kcn, kcn, krs.unsqueeze(2).to_broadcast([128, NS, 64]), ALU.mult)
        k.tt(kcn, kcn, gk0.unsqueeze(1).to_broadcast([128, NS, 64]), ALU.mult)
        k.cp(kcb, kcn)
        for ct in range(NCT):
            sl = slice(ct * 2, ct * 2 + 2)
            cosb = ropec[:, ct:ct + 1, 0:8].to_broadcast([128, 2, 8])
            sinb = ropec[:, ct:ct + 1, 8:16].to_broadcast([128, 2, 8])
            k.tt(ra, kcn[:, sl, 0:8], cosb, ALU.mult)
            k.tt(rb, kcn[:, sl, 8:16], sinb, ALU.mult)
            k.tt(kcb[:, sl, 0:8], ra, rb, ALU.subtract)
            k.tt(ra, kcn[:, sl, 8:16], cosb, ALU.mult)
            k.tt(rb, kcn[:, sl, 0:8], sinb, ALU.mult)
            k.tt(kcb[:, sl, 8:16], ra, rb, ALU.add)
        for ct in range(NCT):
            r = crow(ct)
            for g in range(2):
                k.trs([(pTb[0:64, 0, 0:r], kcb[0:r, ct * 2 + g, :])], ident[0:r, 0:r])
                k.cp(kcT[:, g, ct * 128:ct * 128 + r], pTb[0:64, 0, 0:r])
        k.barrier()
        k.es = es_prev

    if B_STAGE < 1:
        return
    QA = [k.sb("b_QA%d" % i, [128, 8, 512], BF16) for i in range(2)]
    cmT = k.sb("b_cmT", [128, NCT, 512], BF16)
    gts = k.sb("b_gts", [128, 4, 24])
    vis = k.sb("b_vis", [128, 4, 64])
    cadd = k.sb("b_cadd", [128, 4, 64])
    NP = 5
    P = [k.sb("b_P%d" % i, [128, 512], BF16) for i in range(NP)]
    ocmp = k.sb("b_ocmp", [128, 4, 8, 64])
    osel = k.sb("b_osel", [128, 4, 8, 64])
    owin = k.sb("b_owin", [128, 4, 8, 64])
    oTs = [k.sb("b_oT%d" % i, [65, 512]) for i in range(2)]
    recs = [k.sb("b_rec%d" % i, [128, 4, 1]) for i in range(2)]
    dens = [k.sb("b_den%d" % i, [128, 4]) for i in range(2)]
    imp = k.sb("b_imp", [128, 4, 2, 64])
    itmps = [k.sb("b_itmp%d" % i, [128, 4, 64]) for i in range(2)]
    NB = 3
    mxs = [k.sb("b_mx%d" % i, [128, 8]) for i in range(NB)]
    mx2s = [k.sb("b_mx2%d" % i, [128, 8]) for i in range(NB)]
    wks = [k.sb("b_wk%d" % i, [128, 64]) for i in range(NB)]
    mks = [k.sb("b_mk%d" % i, [128, 64]) for i in range(NB)]
    negm4 = k.sb("b_negm4", [128, 4, 128], BF16)
    k.memset(negm4, 0.0)
    osq = k.sb("b_osq", [128, 4, 8, 64])
    oss = k.sb("b_oss", [128, 32])
    ors = k.sb("b_ors", [128, 32])
    onb = k.sb("b_onb", [128, 4, 512], BF16)
    mixs = k.sb("b_mixs", [128, 4, 128], BF16)
    ftq = FT.rearrange("n d t -> d n t")
    gtv = GT.rearrange("(s p) c -> p s c", p=128)
    visv = c_vis.rearrange("(s p) c -> p s c", p=128)
    caddv = c_cadd.rearrange("(s p) c -> p s c", p=128)
    cmv = c_cmask.rearrange("(ct p) t -> p ct t", p=128)
    mixv = MIXT.rearrange("(c p) t -> p c t", p=128)
    cnt = [0]
    ocnt = [0]

    def kt_gen(Qa, h, g, kt, lo, hi, mcol, mtile, Ksrc, Vsrc, kdim, pOb, first, last, zero_first, evac):
        i = cnt[0]
        cnt[0] += 1
        ps, Pb = pS[i % 2], P[i % NP]
        if first and zero_first:
            k.mm1(pOb[0:65, :], zer, Qa[:, h, :], True, False)
        k.mm(ps[:, lo:hi], [(Ksrc[0:kdim, g, kt * 128:(kt + 1) * 128], Qa[0:kdim, h, lo:hi])])
        yield
        k.act(Pb[:, lo:hi], ps[:, lo:hi], AF.Exp, scale=0.125)
        yield
        if mtile is not None:
            k.tt(Pb[:, mcol:mcol + 128], Pb[:, mcol:mcol + 128], mtile, ALU.mult)
            yield
        k.mm1(pOb[0:65, lo:hi], Vsrc[:, kt, g, :], Pb[:, lo:hi], (first and not zero_first), last)
        yield
        if last:
            yield from evac_gen(*evac)

    def evac_gen(pOb, oTb, rc, h, odst):
        k.cp(oTb, pOb[0:65, :], eng="act")
        yield
        k.trs([(pT[:, s, 0:65], oTb[:, s * 128:(s + 1) * 128]) for s in range(4)], identf[0:65, 0:65])
        yield
        k.recip(rc, pT[:, :, 64:65])
        yield
        k.tt(odst[:, :, h, :], pT[:, :, 0:64], rc.to_broadcast([128, 4, 64]), ALU.mult)
        yield

    def attend_gens(Qa, h, g, kts, Ksrc, Vsrc, kdim, odst, zero_first):
        j = ocnt[0]
        ocnt[0] += 1
        pOb, oTb, rc = pOs[j % 2], oTs[j % 2], recs[j % 2]
        n = len(kts)
        for idx, (kt, lo, hi, mcol, mtile) in enumerate(kts):
            yield kt_gen(Qa, h, g, kt, lo, hi, mcol, mtile, Ksrc, Vsrc, kdim, pOb,
                         idx == 0, idx == n - 1, zero_first, (pOb, oTb, rc, h, odst))

    def cmp_gen(Qa, h, g, cts):
        pc, rc, dn, itmp = pCIs[h % 2], recs[h % 2], dens[h % 2], itmps[h % 2]
        for ci, ct in enumerate(cts):
            i = cnt[0]
            cnt[0] += 1
            ps, Pb = pS[i % 2], P[i % NP]
            k.mm(ps, [(kcT[0:64, g, ct * 128:(ct + 1) * 128], Qa[0:64, h, :])])
            yield
            k.act(Pb, ps, AF.Exp, scale=0.125)
            yield
            k.tt(Pb, Pb, cmT[:, ct, :], ALU.mult)
            yield
            for s in range(4):
                k.mm1(pc[:, s, :], Pb[:, s * 128:(s + 1) * 128], Rv[:, ct, g, :],
                      ci == 0 and s == 0, ci == len(cts) - 1)
            yield
        k.rsum(dn, pc[:, :, 64:128])
        yield
        k.ts(rc, dn.unsqueeze(2), 1e-30, None, ALU.add)
        yield
        k.recip(rc, rc)
        yield
        k.tt(ocmp[:, :, h, :], pc[:, :, 0:64], rc.to_broadcast([128, 4, 64]), ALU.mult)
        yield
        if h % 4 == 0:
            k.tt(imp[:, :, g, :], pc[:, :, 64:128], rc.to_broadcast([128, 4, 64]), ALU.mult)
        else:
            k.tt(itmp, pc[:, :, 64:128], rc.to_broadcast([128, 4, 64]), ALU.mult)
            yield
            k.tt(imp[:, :, g, :], imp[:, :, g, :], itmp, ALU.add, eng="pool")
        yield

    def topk_gen(g, s, j):
        iv = imp[:, s, g, :]
        mx, mx2, wk, mk = mxs[j % NB], mx2s[j % NB], wks[j % NB], mks[j % NB]
        k.vmax(mx, iv)
        yield
        k.vmatch(wk, mx, iv, -3.0e38)
        yield
        k.vmax(mx2, wk)
        yield
        k.tt(mk, iv, mx2[:, 7:8].to_broadcast([128, 64]), ALU.is_ge)
        yield
        k.ts(negm4[:, s, 64:128], mk, -NEGM, NEGM, ALU.mult, ALU.add)
        yield

    for Qi in range(NQ):
        t0 = Qi * 512
        Qa = QA[Qi % 2]
        k.dma(Qa[0:64, :, :], ftq[:, 0:8, t0:t0 + 512])
        k.dma(gts, gtv[:, Qi * 4:(Qi + 1) * 4, :])
        k.dma(vis, visv[:, Qi * 4:(Qi + 1) * 4, :])
        k.dma(cadd, caddv[:, Qi * 4:(Qi + 1) * 4, :])
        k.dma(cmT, cmv[:, 0:NCT, t0:t0 + 512])
        cts = [ct for ct in range(NCT) if 16 * 128 * ct + 31 <= t0 + 511]
        pipeline((cmp_gen(Qa, h, h // 4, cts) for h in range(8)), 2)
        for g in range(2):
            k.tt(imp[:, :, g, :], imp[:, :, g, :], vis, ALU.mult)
            k.tt(imp[:, :, g, :], imp[:, :, g, :], cadd, ALU.add)
        for g in range(2):
            pipeline((topk_gen(g, s, g * 4 + s) for s in range(4)), 3)
            k.trs([(pTb[:, s, :], negm4[:, s, :]) for s in range(4)], ident)
            k.cp(Qa[64:128, 4 * g:4 * g + 4, :].rearrange("p h (s t) -> p h s t", s=4),
                 pTb[64:128, :, :].unsqueeze(1).to_broadcast([64, 4, 4, 128]))
        def all_gens():
            for h in range(8):
                g = h // 4
                kts = []
                for kt in range(4 * Qi + 4):
                    m = kt - 4 * Qi
                    if m >= 0:
                        kts.append((kt, 128 * m, 512, 128 * m, tri))
                    else:
                        kts.append((kt, 0, 512, 0, None))
                yield from attend_gens(Qa, h, g, kts, KA, VS1, 128, osel, False)
                kts = []
                for kt in range(max(0, 4 * Qi - 4), 4 * Qi + 4):
                    m = kt - 4 * Qi
                    if m >= 0:
                        kts.append((kt, 128 * m, 512, 128 * m, tri))
                    else:
                        kts.append((kt, 0, 128 * (m + 5), 128 * (m + 4), ntri))
                yield from attend_gens(Qa, h, g, kts, KW, VW1, 64, owin, True)
        pipeline(all_gens(), 3)
        if B_STAGE < 4:
            continue
        for br, ob in enumerate((ocmp, osel, owin)):
            k.tt(ob, ob, gts[:, :, br * 8:(br + 1) * 8].unsqueeze(3).to_broadcast([128, 4, 8, 64]),
                 ALU.mult, eng=("pool" if br == 1 else "dve"))
        k.tt(ocmp, ocmp, osel, ALU.add)
        k.tt(ocmp, ocmp, owin, ALU.add, eng="pool")
        if B_STAGE < 5:
            continue
        k.tt(osq, ocmp, ocmp, ALU.mult)
        k.rsum(oss, osq.rearrange("p s h d -> p (s h) d"))
        rms_rstd(k, ors, oss, 64)
        k.tt(ocmp.rearrange("p s h d -> p (s h) d"), ocmp.rearrange("p s h d -> p (s h) d"),
             ors.unsqueeze(2).to_broadcast([128, 32, 64]), ALU.mult)
        k.tt(onb.rearrange("p s (h d) -> p (s h) d", d=64), ocmp.rearrange("p s h d -> p (s h) d"),
             gout.unsqueeze(1).to_broadcast([128, 32, 64]), ALU.mult, eng="pool")
        if B_STAGE < 6:
            continue
        for s in range(4):
            k.trs([(pTb[:, c, :], onb[:, s, c * 128:(c + 1) * 128]) for c in range(4)], ident)
            k.cp(mixs, pTb)
            if B_STAGE >= 7:
                k.dma(mixv[:, 4:8, t0 + s * 128:t0 + (s + 1) * 128], mixs)


def load_colvec(k, dst, src, n_chunks, identf, pTf, tmp):
    k.dma(tmp[0:n_chunks, :], src.rearrange("(c p) -> c p", p=128))
    k.trs([(pTf[:, 0:n_chunks], tmp[0:n_chunks, :])], identf[0:n_chunks, 0:n_chunks])
    k.cp(dst, pTf[:, 0:n_chunks])


def phase_c(nc, k, T, x, MIXT, w_out, ffn_g, w_up, conv_w, conv_b, w_down, H2, ident, identf):
    TT = 256
    NTT = T // TT
    NF = 22
    wo = k.sb("c_wo", [128, 8, D], BF16)
    wu = k.sb("c_wu", [128, 8, 2 * DFF], BF16)
    wd = k.sb("c_wd", [128, NF, D], BF16)
    wov = w_out.rearrange("(c p) n -> p c n", p=128)
    wuv = w_up.rearrange("(c p) n -> p c n", p=128)
    wdv = w_down.rearrange("(c p) n -> p c n", p=128)
    for c in range(8):
        k.dma(wo[:, c, :], wov[:, c, :], q="pool")
    for c in range(8):
        for hh in range(2):
            k.dma(wu[:, c, hh * DFF:(hh + 1) * DFF], wuv[:, c, hh * DFF:(hh + 1) * DFF], q="pool")
    for c in range(NF):
        k.dma(wd[:, c, :], wdv[:, c, :], q="pool")
    pY = [k.ps("c_pY%d" % i, [128, 512]) for i in range(2)]
    pC = k.ps("c_pC", [128, 8, 128], BF16)
    pUs = [k.ps("c_pU%d" % i, [128, 256]) for i in range(4)]
    tmpv = k.sb("c_tmpv", [44, 128])
    gfT = k.sb("c_gfT", [128, 8])
    load_colvec(k, gfT, ffn_g, 8, identf, pY[0], tmpv)
    cw = k.sb("c_cw", [128, 3, 44])
    cb = k.sb("c_cb", [128, 44])
    for j in range(3):
        load_colvec(k, cw[:, j, :], conv_w[j], 44, identf, pY[0], tmpv)
    load_colvec(k, cb, conv_b, 44, identf, pY[0], tmpv)
    carry = k.sb("c_carry", [128, 44, 2])
    k.memset(carry, 0.0)

    mt = k.sb("c_mt", [128, 8, TT], BF16)
    hsb = k.sb("c_h", [128, 2, D])
    xt = k.sb("c_x", [128, 2, D])
    junk = k.sb("c_junk", [128, D], BF16)
    ss = k.sb("c_ss", [128, 1])
    rstd = k.sb("c_rstd", [128, 1])
    hn = k.sb("c_hn", [128, D], BF16)
    hnT = k.sb("c_hnT", [128, 8, TT], BF16)
    actT = k.sb("c_actT", [128, NF, TT], BF16)
    uraw = [k.sb("c_uraw%d" % i, [128, TT + 2]) for i in range(4)]
    acc = [k.sb("c_acc%d" % i, [128, TT]) for i in range(4)]
    sil = [k.sb("c_sil%d" % i, [128, TT]) for i in range(2)]
    mixv = MIXT.rearrange("(c p) t -> p c t", p=128)
    xv = x.rearrange("(n p) d -> p n d", p=128)
    h2v = H2.rearrange("(n p) d -> p n d", p=128)

    for it in range(NTT):
        t0 = it * TT
        k.dma(mt, mixv[:, :, t0:t0 + TT])
        k.dma(xt, xv[:, 2 * it:2 * it + 2, :])
        for s in range(2):
            for hh in range(2):
                py = pY[(s * 2 + hh) % 2]
                k.mm(py, [(mt[:, c, s * 128:(s + 1) * 128], wo[:, c, hh * 512:(hh + 1) * 512])
                          for c in range(8)])
                k.tt(hsb[:, s, hh * 512:(hh + 1) * 512], py, xt[:, s, hh * 512:(hh + 1) * 512], ALU.add)
            k.act(junk, hsb[:, s, :], AF.Square, accum_out=ss)
            rms_rstd(k, rstd, ss, D)
            k.act(hn, hsb[:, s, :], AF.Copy, scale=rstd)
            k.trs([(pC[:, c, :], hn[:, c * 128:(c + 1) * 128]) for c in range(8)], ident)
            k.tt(hnT[:, :, s * 128:(s + 1) * 128], pC, gfT.unsqueeze(2).to_broadcast([128, 8, 128]),
                 ALU.mult)
        def ffn_gen(i):
            accs = []
            for gu in range(2):
                ch = i + gu * NF
                slot = (i % 2) * 2 + gu
                pu = pUs[slot]
                k.mm(pu, [(wu[:, c, ch * 128:(ch + 1) * 128], hnT[:, c, :]) for c in range(8)])
                yield
                ur, ac = uraw[slot], acc[slot]
                k.cp(ur[:, 0:2], carry[:, ch, :], eng="pool")
                k.cp(ur[:, 2:TT + 2], pu, eng="act")
                yield
                k.act(ac, pu, AF.Identity, scale=cw[:, 2, ch:ch + 1], bias=cb[:, ch:ch + 1])
                yield
                k.stt(ac, ur[:, 1:TT + 1], cw[:, 1, ch:ch + 1], ac, ALU.mult, ALU.add)
                yield
                k.stt(ac, ur[:, 0:TT], cw[:, 0, ch:ch + 1], ac, ALU.mult, ALU.add)
                k.cp(carry[:, ch, :], ur[:, TT:TT + 2], eng="pool")
                yield
                accs.append(ac)
            sl = sil[i % 2]
            k.act(sl, accs[0], AF.Silu)
            yield
            k.tt(actT[:, i, :], sl, accs[1], ALU.mult, eng="pool")
            yield
        pipeline((ffn_gen(i) for i in range(NF)), FFN_DEPTH)
        for s in range(2):
            for hh in range(2):
                py = pY[(s * 2 + hh) % 2]
                k.mm(py, [(actT[:, i, s * 128:(s + 1) * 128], wd[:, i, hh * 512:(hh + 1) * 512])
                          for i in range(NF)])
                k.tt(hsb[:, s, hh * 512:(hh + 1) * 512], py, hsb[:, s, hh * 512:(hh + 1) * 512], ALU.add)
        k.dma(h2v[:, 2 * it:2 * it + 2, :], hsb)


def phase_d(nc, k, T, H2, p_in, pleg_g, w_pleg, w_ple, ple_g, out, ident, identf):
    NT = T // 128
    wg = k.sb("d_wg", [128, 8, D], BF16)
    wp = k.sb("d_wp", [128, 2, D], BF16)
    wgv = w_pleg.rearrange("(c p) n -> p c n", p=128)
    wpv = w_ple.rearrange("(c p) n -> p c n", p=128)
    for c in range(8):
        k.dma(wg[:, c, :], wgv[:, c, :], q="pool")
    for c in range(2):
        k.dma(wp[:, c, :], wpv[:, c, :], q="pool")
    pY = [k.ps("d_pY%d" % i, [128, 512]) for i in range(2)]
    pE4 = [k.ps("d_pE%d" % i, [128, 512]) for i in range(4)]
    pC = k.ps("d_pC", [128, 8, 128], BF16)
    tmpv = k.sb("d_tmpv", [8, 128])
    ggT = k.sb("d_ggT", [128, 8])
    load_colvec(k, ggT, pleg_g, 8, identf, pY[0], tmpv)
    gple = k.sb("d_gple", [128, D])
    k.dma(gple, ple_g.partition_broadcast(128))

    hb = [k.sb("d_h%d" % i, [128, D]) for i in range(2)]
    pb = [k.sb("d_p%d" % i, [128, 256]) for i in range(2)]
    junks = [k.sb("d_junk%d" % i, [128, D], BF16) for i in range(2)]
    sss = [k.sb("d_ss%d" % i, [128, 1]) for i in range(2)]
    rstds = [k.sb("d_rstd%d" % i, [128, 1]) for i in range(2)]
    ss2s = [k.sb("d_ss2%d" % i, [128, 2]) for i in range(2)]
    rstd2s = [k.sb("d_rstd2%d" % i, [128, 1]) for i in range(2)]
    hns = [k.sb("d_hn%d" % i, [128, D], BF16) for i in range(2)]
    hTs = [k.sb("d_hT%d" % i, [128, 8, 128], BF16) for i in range(2)]
    pbfs = [k.sb("d_pbf%d" % i, [128, 256], BF16) for i in range(2)]
    pTs = [k.sb("d_pT%d" % i, [128, 2, 128], BF16) for i in range(2)]
    gates = [k.sb("d_gate%d" % i, [128, D]) for i in range(2)]
    es = [k.sb("d_e%d" % i, [128, D]) for i in range(2)]
    ob = [k.sb("d_o%d" % i, [128, D]) for i in range(2)]

    def d_gen(it):
        t0 = it * 128
        b = it % 2
        h, pp, o = hb[b], pb[b], ob[b]
        junk, ss, rstd, ss2, rstd2 = junks[b], sss[b], rstds[b], ss2s[b], rstd2s[b]
        hn, hT, pbf, pT, gate, e = hns[b], hTs[b], pbfs[b], pTs[b], gates[b], es[b]
        pE = pE4[2 * b:2 * b + 2]
        k.dma(h, H2[t0:t0 + 128, :])
        k.dma(pp, p_in[t0:t0 + 128, :])
        yield
        k.act(junk, h, AF.Square, accum_out=ss)
        yield
        k.act(rstd, ss, AF.Ln, scale=1.0 / D, bias=EPS)
        yield
        k.act(rstd, rstd, AF.Exp, scale=-0.5)
        yield
        k.act(hn, h, AF.Copy, scale=rstd)
        yield
        k.trs([(pC[:, c, :], hn[:, c * 128:(c + 1) * 128]) for c in range(8)], ident)
        yield
        k.tt(hT, pC, ggT.unsqueeze(2).to_broadcast([128, 8, 128]), ALU.mult)
        yield
        k.cp(pbf, pp, eng="pool")
        yield
        k.trs([(pC[:, c, :], pbf[:, c * 128:(c + 1) * 128]) for c in range(2)], ident)
        yield
        k.cp(pT, pC[:, 0:2, :])
        yield
        for hh in range(2):
            k.mm(pY[hh], [(hT[:, c, :], wg[:, c, hh * 512:(hh + 1) * 512]) for c in range(8)])
            yield
            k.act(gate[:, hh * 512:(hh + 1) * 512], pY[hh], AF.Sigmoid)
            yield
        for hh in range(2):
            k.mm(pE[hh], [(pT[:, c, :], wp[:, c, hh * 512:(hh + 1) * 512]) for c in range(2)])
            yield
            k.act(junk[:, hh * 512:(hh + 1) * 512], pE[hh], AF.Square, accum_out=ss2[:, hh:hh + 1])
            yield
        k.tt(ss, ss2[:, 0:1], ss2[:, 1:2], ALU.add)
        yield
        k.act(rstd2, ss, AF.Ln, scale=1.0 / D, bias=EPS)
        yield
        k.act(rstd2, rstd2, AF.Exp, scale=-0.5)
        yield
        for hh in range(2):
            k.act(e[:, hh * 512:(hh + 1) * 512], pE[hh], AF.Copy, scale=rstd2)
            yield
        k.tt(e, e, gple, ALU.mult)
        yield
        k.tt(e, e, gate, ALU.mult, eng="pool")
        yield
        k.tt(o, e, h, ALU.add)
        yield
        tok = k.dma(out[t0:t0 + 128, :], o)
        k.out_toks.append(tok)
        yield

    pipeline((d_gen(it) for it in range(NT)), D_DEPTH)


def _consts(T):
    bf = ml_dtypes.bfloat16
    c = {}
    c["c_ident"] = np.eye(128).astype(bf)
    c["c_identf"] = np.eye(128, dtype=np.float32)
    half = 8
    inv = np.float32(500000.0) ** (-np.arange(half, dtype=np.float32) / half)
    ang = np.arange(T, dtype=np.float32)[:, None] * inv[None, :].astype(np.float32)
    c["c_rope"] = np.concatenate([np.cos(ang), np.sin(ang)], 1).astype(np.float32)
    s = np.arange(128)
    c["c_tri2"] = ((s[:, None] <= s[None, :]) & (s[:, None] // 64 == s[None, :] // 64)).astype(np.float32)
    c["c_chunk"] = (s[:, None] // 64 == np.arange(2)[None, :]).astype(np.float32)
    t = np.arange(T)
    cc = np.arange(256)
    ncmp = T // 16 - 1
    c["c_cmask"] = (((16 * cc[:, None] + 31) <= t[None, :]) & (cc[:, None] < ncmp)).astype(bf)
    c["c_tri"] = (s[:, None] <= s[None, :]).astype(bf)
    c["c_ntri"] = (s[None, :] < s[:, None]).astype(bf)
    n = np.arange(64)
    c["c_E"] = ((t[None, :] // 64) == n[:, None]).astype(bf)
    cur = t // 64
    vis = (n[None, :] * 64 <= t[:, None])
    bonus = np.zeros((T, 64), np.float32)
    bonus += (n[None, :] == 0) * 1.0e6
    bonus += (n[None, :] == cur[:, None]) * 2.0e6
    bonus += (n[None, :] == cur[:, None] - 1) * 4.0e6
    c["c_vis"] = vis.astype(np.float32)
    c["c_cadd"] = np.where(vis, bonus, np.float32(-1e30)).astype(np.float32)
    cs = cc * 16
    ssb = n * 64
    ov = np.clip(np.minimum(cs[:, None] + 32, ssb[None, :] + 64)
                 - np.maximum(cs[:, None], ssb[None, :]), 0, None) / 32.0
    o1 = np.zeros((256, 65), np.float32)
    o1[:, 0] = 1.0
    o1[:, 1:] = ov
    o1[ncmp:] = 0.0
    c["c_ovl1"] = o1.astype(bf)
    return c


_W_NAMES = ["attn_norm_g", "w_in", "hg_norm_g", "nsa_q_norm_g", "nsa_k_norm_g", "cmp_pe", "cmp_w1",
            "cmp_w2", "nsa_out_norm_g", "w_out", "ffn_norm_g", "w_up", "conv_w", "conv_b", "w_down",
            "ple_gate_norm_g", "w_ple_gate", "w_ple", "ple_norm_g"]


def kernel(**inputs):
    x = np.asarray(inputs["x"], np.float32)
    p = np.asarray(inputs["p"], np.float32)
    B, T, _ = x.shape
    nc = build(T=T, dbg=False, phases="ABCD")
    shared = {n: np.ascontiguousarray(np.asarray(inputs[n], np.float32)[0]) for n in _W_NAMES}
    shared["hg_lb_logits"] = np.ascontiguousarray(np.asarray(inputs["hg_lb_logits"], np.float32))
    shared.update(_consts(T))
    in_maps = []
    for b in range(B):
        m = dict(shared)
        m["x"] = np.ascontiguousarray(x[b])
        m["p"] = np.ascontiguousarray(p[0, b])
        in_maps.append(m)
    res = run_bass_kernel_spmd(nc, in_maps, core_ids=list(range(B)))
    return np.stack([np.asarray(r["out"], np.float32) for r in res.results], axis=0)
```

```python
from contextlib import ExitStack
import numpy as np
import ml_dtypes
import concourse.bass as bass
import concourse.mybir as mybir
from concourse.bass_utils import run_bass_kernel_spmd

F32 = mybir.dt.float32
BF16 = mybir.dt.bfloat16
ALU = mybir.AluOpType
AF = mybir.ActivationFunctionType
AX = mybir.AxisListType

D = 1024
IN_TOTAL = 3352
DFF = 2816
EPS = 1e-6
NEGM = -30000.0
B_STAGE = 9
FFN_DEPTH = 2
D_DEPTH = 2
PRO_DEPTH = 2


class Sched:
    def __init__(self, nc, n_dma_sems=48):
        self.nc = nc
        self.engs = {"pe": nc.tensor, "act": nc.scalar, "dve": nc.vector,
                     "pool": nc.gpsimd, "sp": nc.sync}
        self.sem = {}
        self.cnt = {}
        for k in ("pe", "act", "dve", "pool"):
            self.sem[k] = nc.alloc_semaphore("s_" + k)
            self.cnt[k] = 0
        self.dma_sems = [nc.alloc_semaphore("s_dma%d" % i) for i in range(n_dma_sems)]
        self.dma_val = [0] * n_dma_sems
        self.dma_rr = 0
        self.waited = {}
        self.last_w = {}
        self.readers = {}
        self.nwaits = 0
        self.inflight = {}
        self.max_desc = 600

    def _wait(self, eng, tok):
        sem, val, key = tok
        if key == eng and eng == "pe":
            return
        k = (eng, key)
        if self.waited.get(k, 0) >= val:
            return
        self.engs[eng].wait_ge(sem, val)
        self.nwaits += 1
        self.waited[k] = val

    def deps(self, eng, reads, writes):
        toks = []
        for r in reads:
            t = self.last_w.get(r)
            if t is not None:
                toks.append(t)
        for w in writes:
            t = self.last_w.get(w)
            if t is not None:
                toks.append(t)
            toks.extend(self.readers.get(w, ()))
        for t in toks:
            self._wait(eng, t)

    def commit(self, tok, reads, writes):
        for w in writes:
            self.last_w[w] = tok
            self.readers[w] = []
        for r in reads:
            if r in writes:
                continue
            lst = self.readers.setdefault(r, [])
            lst[:] = [t for t in lst if t[2] != tok[2]]
            lst.append(tok)

    def op(self, eng, reads, writes, fn):
        self.deps(eng, reads, writes)
        ins = fn(self.engs[eng])
        self.cnt[eng] += 1
        ins.then_inc(self.sem[eng], 1)
        tok = (self.sem[eng], self.cnt[eng], eng)
        self.commit(tok, reads, writes)
        return tok

    @staticmethod
    def _ndesc(ap):
        dims = list(ap.ap)
        total = 1
        for st, n in dims:
            total *= n
        run = 1
        for st, n in reversed(dims[1:]):
            if st == run:
                run *= n
            else:
                break
        return max(1, total // max(run, 1))

    def dma(self, out, in_, reads, writes, q="sp", **kw):
        nd = max(self._ndesc(out), self._ndesc(in_))
        fifo = self.inflight.setdefault(q, [])
        while fifo and sum(d for _, d in fifo) + nd > self.max_desc:
            tok0, _ = fifo.pop(0)
            self._wait(q, tok0)
        tok = self._dma(out, in_, reads, writes, q, **kw)
        fifo.append((tok, nd))
        return tok

    def _dma(self, out, in_, reads, writes, q="sp", **kw):
        i = self.dma_rr
        self.dma_rr = (self.dma_rr + 1) % len(self.dma_sems)
        sem = self.dma_sems[i]
        key = "dma%d" % i
        if self.dma_val[i] > 0:
            self._wait(q, (sem, self.dma_val[i], key))
        self.deps(q, reads, writes)
        self.dma_val[i] += 16
        self.engs[q].dma_start(out=out, in_=in_, **kw).then_inc(sem, 16)
        tok = (sem, self.dma_val[i], key)
        self.commit(tok, reads, writes)
        return tok


_TAGS = {}
_KEEP = []


def tag(ap, name):
    _TAGS[id(ap)] = name
    _KEEP.append(ap)
    return ap


def sub(ap, fn):
    r = fn(ap)
    if id(ap) in _TAGS:
        tag(r, _TAGS[id(ap)])
    return r


def pipeline(gens, depth):
    gens = iter(gens)
    active = []
    exhausted = False
    while True:
        if not exhausted and len(active) < depth:
            g = next(gens, None)
            if g is None:
                exhausted = True
            else:
                active.append(g)
        if not active:
            if exhausted:
                break
            continue
        for g in list(active):
            try:
                next(g)
            except StopIteration:
                active.remove(g)


def _names(aps):
    out = []
    for a in aps:
        if a is None or isinstance(a, (int, float)):
            continue
        n = _TAGS.get(id(a)) or a.name
        if n not in out:
            out.append(n)
    return out


class K:
    def __init__(self, nc):
        self.nc = nc
        self.S = Sched(nc)
        self.out_toks = []
        self.es = None

    def sb(self, name, shape, dt=F32):
        return self.es.enter_context(self.nc.sbuf_tensor(name, list(shape), dt))[:]

    def ps(self, name, shape, dt=F32):
        return self.es.enter_context(self.nc.psum_tensor(name, list(shape), dt))[:]

    def barrier(self):
        S = self.S
        toks = [(S.sem[e], S.cnt[e], e) for e in ("pe", "act", "dve", "pool") if S.cnt[e] > 0]
        toks += [(S.dma_sems[i], S.dma_val[i], "dma%d" % i) for i in range(len(S.dma_sems))
                 if S.dma_val[i] > 0]
        for eng in ("sp", "pe", "act", "dve", "pool"):
            for t in toks:
                S._wait(eng, t)
        S.last_w.clear()
        S.readers.clear()

    def mm1(self, out, lhsT, rhs, start, stop):
        return self.S.op("pe", _names([lhsT, rhs]), _names([out]),
                         lambda e: e.matmul(out, lhsT=lhsT, rhs=rhs, start=start, stop=stop))

    def vmax(self, out, in_):
        return self.S.op("dve", _names([in_]), _names([out]), lambda e: e.max(out=out, in_=in_))

    def vmatch(self, out, mx, vals, imm):
        return self.S.op("dve", _names([mx, vals]), _names([out]),
                         lambda e: e.match_replace(out=out, in_to_replace=mx, in_values=vals,
                                                   imm_value=imm))

    def recip(self, out, in_):
        return self.S.op("dve", _names([in_]), _names([out]), lambda e: e.reciprocal(out=out, in_=in_))

    def act(self, out, in_, func, bias=None, scale=None, accum_out=None, eng="act"):
        kw = {}
        if bias is not None:
            kw["bias"] = bias
        if scale is not None:
            kw["scale"] = scale
        if accum_out is not None:
            kw["accum_out"] = accum_out
        rd = _names([in_, bias, scale])
        wr = _names([out, accum_out])
        return self.S.op(eng, rd, wr, lambda e: e.activation(out=out, in_=in_, func=func, **kw))

    def tt(self, out, in0, in1, op, eng="dve"):
        return self.S.op(eng, _names([in0, in1]), _names([out]),
                         lambda e: e.tensor_tensor(out=out, in0=in0, in1=in1, op=op))

    def ts(self, out, in0, s1, s2, op0, op1=None, eng="dve"):
        def f(e):
            if op1 is None:
                return e.tensor_scalar(out=out, in0=in0, scalar1=s1, scalar2=None, op0=op0)
            return e.tensor_scalar(out=out, in0=in0, scalar1=s1, scalar2=s2, op0=op0, op1=op1)
        return self.S.op(eng, _names([in0, s1, s2]), _names([out]), f)

    def stt(self, out, in0, scalar, in1, op0, op1, eng="dve"):
        return self.S.op(eng, _names([in0, scalar, in1]), _names([out]),
                         lambda e: e.scalar_tensor_tensor(out=out, in0=in0, scalar=scalar, in1=in1,
                                                          op0=op0, op1=op1))

    def cp(self, out, in_, eng="dve"):
        if eng == "act":
            return self.S.op("act", _names([in_]), _names([out]), lambda e: e.copy(out=out, in_=in_))
        return self.S.op(eng, _names([in_]), _names([out]), lambda e: e.tensor_copy(out=out, in_=in_))

    def rsum(self, out, in_, eng="dve"):
        return self.S.op(eng, _names([in_]), _names([out]),
                         lambda e: e.reduce_sum(out=out, in_=in_, axis=AX.X))

    def memset(self, out, val, eng="dve"):
        return self.S.op(eng, [], _names([out]), lambda e: e.memset(out, val))

    def mm(self, out, pairs, extra_w=()):
        rd = _names([a for p in pairs for a in p])
        n = len(pairs)

        def f(e):
            for i, (l, r) in enumerate(pairs):
                ins = e.matmul(out, lhsT=l, rhs=r, start=(i == 0), stop=(i == n - 1))
            return ins
        return self.S.op("pe", rd, _names([out]) + list(extra_w), f)

    def mms(self, groups):
        rd, wr = [], []
        for out, pairs in groups:
            wr += _names([out])
            rd += _names([a for p in pairs for a in p])

        def f(e):
            for out, pairs in groups:
                n = len(pairs)
                for i, (l, r) in enumerate(pairs):
                    ins = e.matmul(out, lhsT=l, rhs=r, start=(i == 0), stop=(i == n - 1))
            return ins
        return self.S.op("pe", list(dict.fromkeys(rd)), list(dict.fromkeys(wr)), f)

    def trs(self, items, ident):
        rd = _names([i for _, i in items] + [ident])
        wr = _names([o for o, _ in items])

        def f(e):
            for o, i in items:
                ins = e.transpose(out=o, in_=i, identity=ident)
            return ins
        return self.S.op("pe", rd, wr, f)

    def dma(self, out, in_, q="sp", **kw):
        return self.S.dma(out, in_, _names([in_]), _names([out]), q=q, **kw)

    def finish(self):
        for t in self.out_toks:
            self.S._wait("sp", t)


def rms_rstd(k, out, ss, n):
    k.act(out, ss, AF.Ln, scale=1.0 / n, bias=EPS)
    k.act(out, out, AF.Exp, scale=-0.5)


def build(T=4096, dbg=False, phases="ABCD"):
    NT = T // 128
    nc = bass.Bass("TRN2", target_bir_lowering=False)
    k = K(nc)

    def din(name, shape, dt=F32):
        return nc.dram_tensor(name, list(shape), dt, kind="ExternalInput").ap()

    def dscr(name, shape, dt):
        return nc.dram_tensor(name, list(shape), dt, kind=("ExternalOutput" if dbg else "Internal")).ap()

    x = din("x", [T, D])
    p_in = din("p", [T, 256])
    attn_g = din("attn_norm_g", [D])
    w_in = din("w_in", [D, IN_TOTAL])
    lb_logits = din("hg_lb_logits", [2, 512])
    hg_norm_g = din("hg_norm_g", [128])
    q_norm_g = din("nsa_q_norm_g", [64])
    k_norm_g = din("nsa_k_norm_g", [3, 64])
    cmp_pe = din("cmp_pe", [2, 32, 64])
    cmp_w1 = din("cmp_w1", [2, 2048, 128])
    cmp_w2 = din("cmp_w2", [2, 128, 64])
    out_norm_g = din("nsa_out_norm_g", [64])
    w_out = din("w_out", [D, D])
    ffn_g = din("ffn_norm_g", [D])
    w_up = din("w_up", [D, 2 * DFF])
    conv_w = din("conv_w", [3, 2 * DFF])
    conv_b = din("conv_b", [2 * DFF])
    w_down = din("w_down", [DFF, D])
    pleg_g = din("ple_gate_norm_g", [D])
    w_pleg = din("w_ple_gate", [D, D])
    w_ple = din("w_ple", [256, D])
    ple_g = din("ple_norm_g", [D])
    c_ident = din("c_ident", [128, 128], BF16)
    c_rope = din("c_rope", [T, 16])
    c_tri2 = din("c_tri2", [128, 128])
    c_chunk = din("c_chunk", [128, 2])

    out = nc.dram_tensor("out", [T, D], F32, kind="ExternalOutput").ap()

    FT = dscr("FT", [16, 64, T], BF16)
    VT = dscr("VT", [T, 256], BF16)
    GT = dscr("GT", [T, 24], F32)
    MIXT = dscr("MIXT", [D, T], BF16)

    c_identf = din("c_identf", [128, 128])
    c_cmask = din("c_cmask", [256, T], BF16)
    c_tri = din("c_tri", [128, 128], BF16)
    c_ntri = din("c_ntri", [128, 128], BF16)
    c_E = din("c_E", [64, T], BF16)
    c_vis = din("c_vis", [T, 64])
    c_cadd = din("c_cadd", [T, 64])
    c_ovl1 = din("c_ovl1", [256, 65], BF16)
    H2 = dscr("H2", [T, D], F32)

    with ExitStack() as es0:
        k.es = es0
        ident = k.sb("ident", [128, 128], BF16)
        k.dma(ident, c_ident)
        identf = k.sb("identf", [128, 128])
        k.dma(identf, c_identf)
        if "A" in phases:
            with ExitStack() as es:
                k.es = es
                phase_a(nc, k, T, NT, x, attn_g, w_in, lb_logits, hg_norm_g, q_norm_g, k_norm_g,
                        c_rope, c_tri2, c_chunk, ident, FT, VT, GT, MIXT)
                k.barrier()
        if "B" in phases:
            with ExitStack() as es:
                k.es = es
                phase_b(nc, k, T, NT, FT, VT, GT, MIXT, k_norm_g, cmp_pe, cmp_w1, cmp_w2, out_norm_g,
                        c_rope, c_cmask, c_tri, c_ntri, c_E, c_vis, c_cadd, c_ovl1, ident, identf)
                k.barrier()
        if "A" not in phases and dbg:
            mi = din("MIXT_in", [D, T], BF16)
            with ExitStack() as es:
                k.es = es
                tb = k.sb("dbg_mix", [128, 8, T], BF16)
                k.dma(tb, mi.rearrange("(c p) t -> p c t", p=128))
                k.dma(MIXT.rearrange("(c p) t -> p c t", p=128), tb)
                k.barrier()
        if "C" in phases:
            with ExitStack() as es:
                k.es = es
                phase_c(nc, k, T, x, MIXT, w_out, ffn_g, w_up, conv_w, conv_b, w_down, H2, ident, identf)
                k.barrier()
        if "D" in phases:
            with ExitStack() as es:
                k.es = es
                phase_d(nc, k, T, H2, p_in, pleg_g, w_pleg, w_ple, ple_g, out, ident, identf)
                k.barrier()
        k.finish()
    return nc


def phase_a(nc, k, T, NT, x, attn_g, w_in, lb_logits, hg_norm_g, q_norm_g, k_norm_g,
            c_rope, c_tri2, c_chunk, ident, FT, VT, GT, MIXT):
    S = k.S
    w_sb = k.sb("a_w", [128, 8, IN_TOTAL], BF16)
    w_v = w_in.rearrange("(c p) n -> p c n", p=128)
    for c in range(8):
        k.dma(w_sb[:, c, :], w_v[:, c, :], q="pool")
    gT = k.sb("a_gT", [128, 8])
    k.dma(gT, attn_g.rearrange("(c p) -> p c", p=128), allow_slow_non_contiguous=True)
    rope = k.sb("a_rope", [128, NT, 16])
    k.dma(rope, c_rope.rearrange("(n p) c -> p n c", p=128))
    tri2 = k.sb("a_tri2", [128, 128])
    k.dma(tri2, c_tri2)
    chunk = k.sb("a_chunk", [128, 2])
    k.dma(chunk, c_chunk)
    l0 = k.sb("a_l0", [128, 512])
    l1 = k.sb("a_l1", [128, 512])
    k.dma(l0, lb_logits[0].partition_broadcast(128))
    k.dma(l1, lb_logits[1].partition_broadcast(128))
    lb = k.sb("a_lb", [128, 512])
    oml = k.sb("a_oml", [128, 512])
    k.tt(l0, l0, l1, ALU.subtract)
    k.act(lb, l0, AF.Sigmoid)
    k.ts(oml, lb, -1.0, 1.0, ALU.mult, ALU.add)
    gq = k.sb("a_gq", [128, 12, 64])
    for h in range(12):
        src = q_norm_g if h < 8 else (k_norm_g[1] if h < 10 else k_norm_g[2])
        k.dma(gq[:, h, :], src.partition_broadcast(128))
    ghg = k.sb("a_ghg", [128, 4, 128])
    for h in range(4):
        k.dma(ghg[:, h, :], hg_norm_g.partition_broadcast(128))
    Sf = k.sb("a_Sf", [128, 4, 128])
    Sb = [k.sb("a_Sb%d" % i, [128, 4, 128], BF16) for i in range(3)]
    k.memset(Sf, 0.0)
    k.memset(Sb[0], 0.0, eng="pool")

    xt = [k.sb("a_x%d" % i, [128, D]) for i in range(2)]
    junk = k.sb("a_junk", [128, D], BF16)
    ss = k.sb("a_ss", [128, 1])
    rstd = k.sb("a_rstd", [128, 1])
    xn = k.sb("a_xn", [128, D], BF16)
    xnT = k.sb("a_xnT", [128, 8, 128], BF16)
    silq = k.sb("a_silq", [128, 512])
    sig = k.sb("a_sig", [128, 512])
    logf = k.sb("a_logf", [128, 512])
    kk = k.sb("a_kk", [128, 512])
    enb = k.sb("a_enb", [128, 512])
    epb = k.sb("a_epb", [128, 512])
    kp = k.sb("a_kp", [128, 512], BF16)
    qp = k.sb("a_qp", [128, 512], BF16)
    vb = k.sb("a_vb", [128, 512], BF16)
    sg = k.sb("a_sg", [128, 512])
    ebl = k.sb("a_ebl", [128, 4, 2])
    qkT = k.sb("a_qkT", [128, 8, 128], BF16)
    ATm = k.sb("a_ATm", [128, 4, 128], BF16)
    tmpS = k.sb("a_tmpS", [128, 4, 128])
    osb = k.sb("a_osb", [128, 4, 128])
    osq = k.sb("a_osq", [128, 4, 128])
    ss4 = k.sb("a_ss4", [128, 4])
    rstd4 = k.sb("a_rstd4", [128, 4])
    onb = k.sb("a_onb", [128, 4, 128], BF16)
    mixs = k.sb("a_mixs", [128, 4, 128], BF16)
    qk = k.sb("a_qk", [128, 12, 64])
    qsq = k.sb("a_qsq", [128, 12, 64])
    ss12 = k.sb("a_ss12", [128, 12])
    rstd12 = k.sb("a_rstd12", [128, 12])
    qkb = k.sb("a_qkb", [128, 12, 64], BF16)
    r_a = k.sb("a_ra", [128, 12, 8])
    r_b = k.sb("a_rb", [128, 12, 8])
    r_c = k.sb("a_rc", [128, 12, 8])
    r_d = k.sb("a_rd", [128, 12, 8])
    kcvc = k.sb("a_kcvc", [128, 256], BF16)
    T16 = k.sb("a_T16", [64, 16, 128], BF16)
    vv = k.sb("a_vv", [128, 256], BF16)
    gsb = k.sb("a_gsb", [128, 24])

    pA = [k.ps("a_pA%d" % i, [128, 512]) for i in range(2)]
    pB = k.ps("a_pB", [128, 512])
    pC = k.ps("a_pC", [128, 8, 128], BF16)
    pD = k.ps("a_pD", [64, 16, 128], BF16)
    pF = k.ps("a_pF", [128, 4, 128])
    pG = k.ps("a_pG", [128, 4, 128])

    cols = [(0, 512), (512, 512), (1024, 512), (1536, 512), (2048, 512), (2560, 512), (3072, 280)]
    mixv = MIXT.rearrange("(c p) t -> p c t", p=128)
    ftv = FT.rearrange("n d t -> d n t")
    xnT2 = [xnT, k.sb("a_xnT1", [128, 8, 128], BF16)]

    def proj(g, dst, xT):
        c0, n = cols[g]
        k.mm(dst[:, 0:n], [(xT[:, c, :], w_sb[:, c, c0:c0 + n]) for c in range(8)])
        return dst

    def genH(it):
        t0 = it * 128
        xb = xt[it % 2]
        xT = xnT2[it % 2]
        pAh = pA[0]
        if it == 0:
            k.dma(xb, x[t0:t0 + 128, :])
        if it + 1 < NT:
            k.dma(xt[(it + 1) % 2], x[t0 + 128:t0 + 256, :])
        yield
        k.act(junk, xb, AF.Square, accum_out=ss)
        yield
        k.act(rstd, ss, AF.Ln, scale=1.0 / D, bias=EPS)
        yield
        k.act(rstd, rstd, AF.Exp, scale=-0.5)
        yield
        k.act(xn, xb, AF.Copy, scale=rstd)
        yield
        k.trs([(pC[:, c, :], xn[:, c * 128:(c + 1) * 128]) for c in range(8)], ident)
        yield
        k.tt(xT, pC, gT.unsqueeze(2).to_broadcast([128, 8, 128]), ALU.mult)
        yield
        d = proj(1, pAh, xT)
        yield
        k.act(sig, d, AF.Sigmoid)
        yield
        k.tt(sig, sig, oml, ALU.mult)
        yield
        k.tt(sig, sig, lb, ALU.add)
        yield
        k.act(logf, sig, AF.Ln)
        yield
        k.ts(kk, sig, -1.0, 1.0, ALU.mult, ALU.add)
        yield
        k.mm(pB, [(tri2, logf)])
        yield
        k.act(enb, pB, AF.Exp, scale=-1.0)
        yield
        k.act(epb, pB, AF.Exp)
        yield
        k.tt(kp, kk, enb, ALU.mult)
        yield
        k.mms([(pF[:, h, 0:2], [(logf[:, h * 128:(h + 1) * 128], chunk)]) for h in range(4)])
        yield
        k.act(ebl, pF[:, :, 0:2], AF.Exp)
        yield
        d = proj(0, pAh, xT)
        yield
        k.act(silq, d, AF.Silu)
        yield
        k.stt(qp, silq, 128 ** -0.5, epb, ALU.mult, ALU.mult)
        yield
        d = proj(2, pAh, xT)
        yield
        k.cp(vb, d, eng="act")
        yield
        d = proj(3, pAh, xT)
        yield
        k.act(sg, d, AF.Silu)
        yield
        k.tt(sg, sg, ghg.rearrange("p h v -> p (h v)"), ALU.mult, eng="pool")
        yield
        k.trs([(pC[:, h, :], qp[:, h * 128:(h + 1) * 128]) for h in range(4)] +
              [(pC[:, 4 + h, :], kp[:, h * 128:(h + 1) * 128]) for h in range(4)], ident)
        yield
        k.cp(qkT, pC)
        yield
        S0, S1, S2 = Sb[(2 * it) % 3], Sb[(2 * it + 1) % 3], Sb[(2 * it + 2) % 3]
        k.mms([(pF[:, h, :], [(qkT[:, 4 + h, :], qkT[:, h, :])]) for h in range(4)])
        yield
        k.tt(ATm, pF, tri2.unsqueeze(1).to_broadcast([128, 4, 128]), ALU.mult)
        yield
        for c in range(2):
            rs = slice(c * 64, (c + 1) * 64)
            k.mms([(pF[:, h, :], [(kp[rs, h * 128:(h + 1) * 128], vb[rs, h * 128:(h + 1) * 128])])
                   for h in range(4)])
            yield
            k.tt(tmpS, pF, Sf, ALU.add)
            yield
            k.tt(Sf, tmpS, ebl[:, :, c:c + 1].to_broadcast([128, 4, 128]), ALU.mult)
            yield
            k.cp(S1 if c == 0 else S2, Sf, eng="pool")
            yield
        groups = []
        for h in range(4):
            for c in range(2):
                rs = slice(c * 64, (c + 1) * 64)
                Sc = S0 if c == 0 else S1
                groups.append((pG[rs, h, :], [(ATm[rs, h, rs], vb[rs, h * 128:(h + 1) * 128]),
                                              (qkT[:, h, rs], Sc[:, h, :])]))
        k.mms(groups)
        yield
        k.cp(osb, pG, eng="act")
        yield
        k.tt(osq, osb, osb, ALU.mult)
        yield
        k.rsum(ss4, osq)
        yield
        k.act(rstd4, ss4, AF.Ln, scale=1.0 / 128, bias=EPS)
        yield
        k.act(rstd4, rstd4, AF.Exp, scale=-0.5)
        yield
        k.tt(osb, osb, rstd4.unsqueeze(2).to_broadcast([128, 4, 128]), ALU.mult)
        yield
        k.tt(onb, osb, sg.rearrange("p (h v) -> p h v", h=4), ALU.mult)
        yield
        k.trs([(pC[:, h, :], onb[:, h, :]) for h in range(4)], ident)
        yield
        k.cp(mixs, pC[:, 0:4, :])
        yield
        k.dma(mixv[:, 0:4, t0:t0 + 128], mixs)
        yield

    def genN(it):
        t0 = it * 128
        xT = xnT2[it % 2]
        pAn = pA[1]
        d = proj(4, pAn, xT)
        yield
        k.cp(qk[:, 0:8, :], d.rearrange("p (h d) -> p h d", d=64), eng="act")
        yield
        d = proj(5, pAn, xT)
        yield
        k.cp(kcvc, d[:, 0:256], eng="act")
        yield
        k.cp(qk[:, 8:10, :], d[:, 256:384].rearrange("p (h d) -> p h d", d=64), eng="act")
        yield
        k.cp(vv[:, 0:128], d[:, 384:512], eng="act")
        yield
        d = proj(6, pAn, xT)
        yield
        k.cp(qk[:, 10:12, :], d[:, 0:128].rearrange("p (h d) -> p h d", d=64), eng="act")
        yield
        k.cp(vv[:, 128:256], d[:, 128:256], eng="act")
        yield
        k.act(gsb, d[:, 256:280], AF.Sigmoid)
        yield
        k.dma(GT[t0:t0 + 128, :], gsb)
        k.dma(VT[t0:t0 + 128, :], vv)
        yield
        k.tt(qsq, qk, qk, ALU.mult, eng="pool")
        yield
        k.rsum(ss12, qsq)
        yield
        k.act(rstd12, ss12, AF.Ln, scale=1.0 / 64, bias=EPS)
        yield
        k.act(rstd12, rstd12, AF.Exp, scale=-0.5)
        yield
        k.tt(qk, qk, rstd12.unsqueeze(2).to_broadcast([128, 12, 64]), ALU.mult)
        yield
        k.tt(qk, qk, gq, ALU.mult, eng="pool")
        yield
        cosb = rope[:, it:it + 1, 0:8].to_broadcast([128, 12, 8])
        sinb = rope[:, it:it + 1, 8:16].to_broadcast([128, 12, 8])
        k.tt(r_a, qk[:, :, 0:8], cosb, ALU.mult, eng="pool")
        yield
        k.tt(r_b, qk[:, :, 8:16], sinb, ALU.mult, eng="pool")
        yield
        k.tt(r_c, qk[:, :, 8:16], cosb, ALU.mult, eng="pool")
        yield
        k.tt(r_d, qk[:, :, 0:8], sinb, ALU.mult, eng="pool")
        yield
        k.cp(qkb, qk, eng="pool")
        yield
        k.tt(qkb[:, :, 0:8], r_a, r_b, ALU.subtract, eng="pool")
        yield
        k.tt(qkb[:, :, 8:16], r_c, r_d, ALU.add, eng="pool")
        yield
        k.trs([(pD[:, n, :], qkb[:, n, :]) for n in range(12)] +
              [(pD[:, 12 + n, :], kcvc[:, n * 64:(n + 1) * 64]) for n in range(4)], ident)
        yield
        k.cp(T16, pD)
        yield
        k.dma(ftv[:, :, t0:t0 + 128], T16)
        yield

    for it in range(NT + 1):
        gens = []
        if it < NT:
            gens.append(genH(it))
        if it >= 1:
            gens.append(genN(it - 1))
        pipeline(iter(gens), 2)


def phase_b(nc, k, T, NT, FT, VT, GT, MIXT, k_norm_g, cmp_pe, cmp_w1, cmp_w2, out_norm_g,
            c_rope, c_cmask, c_tri, c_ntri, c_E, c_vis, c_cadd, c_ovl1, ident, identf):
    NQ = T // 512
    NCMP = T // 16 - 1
    NCT = (NCMP + 127) // 128

    def crow(ct):
        return min(128, NCMP - 128 * ct)

    KA = k.sb("b_KA", [128, 2, T], BF16)
    KW = k.sb("b_KW", [64, 2, T], BF16)
    VS1 = k.sb("b_VS1", [128, NT, 2, 65], BF16)
    VW1 = k.sb("b_VW1", [128, NT, 2, 65], BF16)
    VTv = VT.rearrange("(n p) c -> p n c", p=128)
    k.memset(VS1, 1.0)
    k.memset(VW1, 1.0, eng="pool")
    for g in range(2):
        k.dma(KA[0:64, g, :], FT[8 + g])
        k.dma(KA[64:128, g, :], c_E)
        k.dma(KW[:, g, :], FT[10 + g])
        k.dma(VS1[:, :, g, 0:64], VTv[:, :, g * 64:(g + 1) * 64])
        k.dma(VW1[:, :, g, 0:64], VTv[:, :, 128 + g * 64:128 + (g + 1) * 64])
    tri = k.sb("b_tri", [128, 128], BF16)
    ntri = k.sb("b_ntri", [128, 128], BF16)
    k.dma(tri, c_tri)
    k.dma(ntri, c_ntri)
    zer = k.sb("b_zer", [128, 65], BF16)
    k.memset(zer, 0.0)
    gout = k.sb("b_gout", [128, 64])
    k.dma(gout, out_norm_g.partition_broadcast(128))

    pS = [k.ps("b_pS%d" % i, [128, 512]) for i in range(2)]
    pOs = [k.ps("b_pO%d" % i, [128, 512]) for i in range(2)]
    pO = pOs[0]
    pCIs = [k.ps("b_pCI%d" % i, [128, 4, 128]) for i in range(2)]
    pT = k.ps("b_pT", [128, 4, 128])
    pTb = k.ps("b_pTb", [128, 4, 128], BF16)

    kcT = k.sb("b_kcT", [64, 2, NCT * 128], BF16)
    Rv = k.sb("b_R", [128, NCT, 2, 128], BF16)
    k.memset(kcT, 0.0)
    k.memset(Rv, 0.0, eng="pool")
    ovv = c_ovl1.rearrange("(ct p) c -> p ct c", p=128)
    for ct in range(NCT):
        for g in range(2):
            k.dma(Rv[:, ct, g, 64:128], ovv[:, ct, 1:65])
    with ExitStack() as es_c:
        es_prev = k.es
        k.es = es_c
        kc2 = k.sb("b_kc2", [128, T], BF16)
        hid = k.sb("b_hid", [128, NCT * 128], BF16)
        k.memset(hid, 0.0)
        kcn = k.sb("b_kcn", [128, NCT * 2, 64])
        k.memset(kcn, 0.0)
        gk0 = k.sb("b_gk0", [128, 64])
        k.dma(gk0, k_norm_g[0].partition_broadcast(128))
        ropec = k.sb("b_ropec", [128, NCT, 16])
        k.memset(ropec, 0.0)
        rv = c_rope.rearrange("(c s) f -> c s f", s=16)
        for ct in range(NCT):
            k.dma(ropec[0:crow(ct), ct, :], rv[1 + ct * 128:1 + ct * 128 + crow(ct), 15, :])
        w1 = [k.sb("b_w1%d" % j, [128, 16, 128], BF16) for j in range(2)]
        w2 = [k.sb("b_w2%d" % j, [128, 64], BF16) for j in range(2)]
        pes = [k.sb("b_pe%d" % j, [128, 16], BF16) for j in range(2)]
        bvec = [k.sb("b_bv%d" % j, [128, 1]) for j in range(2)]
        for j in range(2):
            k.dma(w1[j], cmp_w1[j].rearrange("(l p) h -> p l h", p=128), q="pool")
            k.dma(w2[j], cmp_w2[j], q="pool")
            k.dma(pes[j], cmp_pe[j].rearrange("(l two) d -> (two d) l", two=2), q="pool",
                  allow_slow_non_contiguous=True)
        v16 = kc2.rearrange("p (c s) -> p c s", s=16)
        for j in range(2):
            k.mm(pO[:, 0:1], [(w1[j][:, l2, :], pes[j][:, l2:l2 + 1]) for l2 in range(16)])
            k.cp(bvec[j], pO[:, 0:1])
            for g in range(2):
                n = 12 + 2 * j + g
                k.dma(kc2[0:64, :], FT[n])
                k.dma(kc2[64:128, 0:T - 1], FT[n][:, 1:T])
                pairs = []
                for l2 in range(16):
                    rhs = v16[:, 0:NCMP, 2 * l2] if l2 < 8 else v16[:, 1:NCMP + 1, 2 * l2 - 16]
                    pairs.append((w1[j][:, l2, :], rhs))
                k.mm(pS[0][:, 0:NCMP], pairs)
                k.act(hid[:, 0:NCMP], pS[0][:, 0:NCMP], AF.Silu, bias=bvec[j])
                for ct in range(NCT):
                    r = crow(ct)
                    k.mm(pT[0:r, ct, 0:64], [(hid[:, ct * 128:ct * 128 + r], w2[j])])
                    if j == 0:
                        k.cp(kcn[0:r, ct * 2 + g, :], pT[0:r, ct, 0:64])
                    else:
                        k.cp(Rv[0:r, ct, g, 0:64], pT[0:r, ct, 0:64])
        NS = NCT * 2
        ksq = k.sb("b_ksq", [128, NS, 64])
        kss = k.sb("b_kss", [128, NS])
        krs = k.sb("b_krs", [128, NS])
        kcb = k.sb("b_kcb", [128, NS, 64], BF16)
        ra = k.sb("b_ra", [128, 2, 8])
        rb = k.sb("b_rb", [128, 2, 8])
        k.tt(ksq, kcn, kcn, ALU.mult)
        k.rsum(kss, ksq)
        rms_rstd(k, krs, kss, 64)
        k.tt(kcn, kcn, krs.unsqueeze(2).to_broadcast([128, NS, 64]), ALU.mult)
        k.tt(kcn, kcn, gk0.unsqueeze(1).to_broadcast([128, NS, 64]), ALU.mult)
        k.cp(kcb, kcn)
        for ct in range(NCT):
            sl = slice(ct * 2, ct * 2 + 2)
            cosb = ropec[:, ct:ct + 1, 0:8].to_broadcast([128, 2, 8])
            sinb = ropec[:, ct:ct + 1, 8:16].to_broadcast([128, 2, 8])
            k.tt(ra, kcn[:, sl, 0:8], cosb, ALU.mult)
            k.tt(rb, kcn[:, sl, 8:16], sinb, ALU.mult)
            k.tt(kcb[:, sl, 0:8], ra, rb, ALU.subtract)
            k.tt(ra, kcn[:, sl, 8:16], cosb, ALU.mult)
            k.tt(rb, kcn[:, sl, 0:8], sinb, ALU.mult)
            k.tt(kcb[:, sl, 8:16], ra, rb, ALU.add)
        for ct in range(NCT):
            r = crow(ct)
            for g in range(2):
                k.trs([(pTb[0:64, 0, 0:r], kcb[0:r, ct * 2 + g, :])], ident[0:r, 0:r])
                k.cp(kcT[:, g, ct * 128:ct * 128 + r], pTb[0:64, 0, 0:r])
        k.barrier()
        k.es = es_prev

    if B_STAGE < 1:
        return
    QA = [k.sb("b_QA%d" % i, [128, 8, 512], BF16) for i in range(2)]
    cmT = k.sb("b_cmT", [128, NCT, 512], BF16)
    gts = k.sb("b_gts", [128, 4, 24])
    vis = k.sb("b_vis", [128, 4, 64])
    cadd = k.sb("b_cadd", [128, 4, 64])
    NP = 5
    P = [k.sb("b_P%d" % i, [128, 512], BF16) for i in range(NP)]
    ocmp = k.sb("b_ocmp", [128, 4, 8, 64])
    osel = k.sb("b_osel", [128, 4, 8, 64])
    owin = k.sb("b_owin", [128, 4, 8, 64])
    oTs = [k.sb("b_oT%d" % i, [65, 512]) for i in range(2)]
    recs = [k.sb("b_rec%d" % i, [128, 4, 1]) for i in range(2)]
    dens = [k.sb("b_den%d" % i, [128, 4]) for i in range(2)]
    imp = k.sb("b_imp", [128, 4, 2, 64])
    itmps = [k.sb("b_itmp%d" % i, [128, 4, 64]) for i in range(2)]
    NB = 3
    mxs = [k.sb("b_mx%d" % i, [128, 8]) for i in range(NB)]
    mx2s = [k.sb("b_mx2%d" % i, [128, 8]) for i in range(NB)]
    wks = [k.sb("b_wk%d" % i, [128, 64]) for i in range(NB)]
    mks = [k.sb("b_mk%d" % i, [128, 64]) for i in range(NB)]
    negm4 = k.sb("b_negm4", [128, 4, 128], BF16)
    k.memset(negm4, 0.0)
    osq = k.sb("b_osq", [128, 4, 8, 64])
    oss = k.sb("b_oss", [128, 32])
    ors = k.sb("b_ors", [128, 32])
    onb = k.sb("b_onb", [128, 4, 512], BF16)
    mixs = k.sb("b_mixs", [128, 4, 128], BF16)
    ftq = FT.rearrange("n d t -> d n t")
    gtv = GT.rearrange("(s p) c -> p s c", p=128)
    visv = c_vis.rearrange("(s p) c -> p s c", p=128)
    caddv = c_cadd.rearrange("(s p) c -> p s c", p=128)
    cmv = c_cmask.rearrange("(ct p) t -> p ct t", p=128)
    mixv = MIXT.rearrange("(c p) t -> p c t", p=128)
    cnt = [0]
    ocnt = [0]

    def kt_gen(Qa, h, g, kt, lo, hi, mcol, mtile, Ksrc, Vsrc, kdim, pOb, first, last, zero_first, evac):
        i = cnt[0]
        cnt[0] += 1
        ps, Pb = pS[i % 2], P[i % NP]
        if first and zero_first:
            k.mm1(pOb[0:65, :], zer, Qa[:, h, :], True, False)
        k.mm(ps[:, lo:hi], [(Ksrc[0:kdim, g, kt * 128:(kt + 1) * 128], Qa[0:kdim, h, lo:hi])])
        yield
        k.act(Pb[:, lo:hi], ps[:, lo:hi], AF.Exp, scale=0.125)
        yield
        if mtile is not None:
            k.tt(Pb[:, mcol:mcol + 128], Pb[:, mcol:mcol + 128], mtile, ALU.mult)
            yield
        k.mm1(pOb[0:65, lo:hi], Vsrc[:, kt, g, :], Pb[:, lo:hi], (first and not zero_first), last)
        yield
        if last:
            yield from evac_gen(*evac)

    def evac_gen(pOb, oTb, rc, h, odst):
        k.cp(oTb, pOb[0:65, :], eng="act")
        yield
        k.trs([(pT[:, s, 0:65], oTb[:, s * 128:(s + 1) * 128]) for s in range(4)], identf[0:65, 0:65])
        yield
        k.recip(rc, pT[:, :, 64:65])
        yield
        k.tt(odst[:, :, h, :], pT[:, :, 0:64], rc.to_broadcast([128, 4, 64]), ALU.mult)
        yield

    def attend_gens(Qa, h, g, kts, Ksrc, Vsrc, kdim, odst, zero_first):
        j = ocnt[0]
        ocnt[0] += 1
        pOb, oTb, rc = pOs[j % 2], oTs[j % 2], recs[j % 2]
        n = len(kts)
        for idx, (kt, lo, hi, mcol, mtile) in enumerate(kts):
            yield kt_gen(Qa, h, g, kt, lo, hi, mcol, mtile, Ksrc, Vsrc, kdim, pOb,
                         idx == 0, idx == n - 1, zero_first, (pOb, oTb, rc, h, odst))

    def cmp_gen(Qa, h, g, cts):
        pc, rc, dn, itmp = pCIs[h % 2], recs[h % 2], dens[h % 2], itmps[h % 2]
        for ci, ct in enumerate(cts):
            i = cnt[0]
            cnt[0] += 1
            ps, Pb = pS[i % 2], P[i % NP]
            k.mm(ps, [(kcT[0:64, g, ct * 128:(ct + 1) * 128], Qa[0:64, h, :])])
            yield
            k.act(Pb, ps, AF.Exp, scale=0.125)
            yield
            k.tt(Pb, Pb, cmT[:, ct, :], ALU.mult)
            yield
            for s in range(4):
                k.mm1(pc[:, s, :], Pb[:, s * 128:(s + 1) * 128], Rv[:, ct, g, :],
                      ci == 0 and s == 0, ci == len(cts) - 1)
            yield
        k.rsum(dn, pc[:, :, 64:128])
        yield
        k.ts(rc, dn.unsqueeze(2), 1e-30, None, ALU.add)
        yield
        k.recip(rc, rc)
        yield
        k.tt(ocmp[:, :, h, :], pc[:, :, 0:64], rc.to_broadcast([128, 4, 64]), ALU.mult)
        yield
        if h % 4 == 0:
            k.tt(imp[:, :, g, :], pc[:, :, 64:128], rc.to_broadcast([128, 4, 64]), ALU.mult)
        else:
            k.tt(itmp, pc[:, :, 64:128], rc.to_broadcast([128, 4, 64]), ALU.mult)
            yield
            k.tt(imp[:, :, g, :], imp[:, :, g, :], itmp, ALU.add, eng="pool")
        yield

    def topk_gen(g, s, j):
        iv = imp[:, s, g, :]
        mx, mx2, wk, mk = mxs[j % NB], mx2s[j % NB], wks[j % NB], mks[j % NB]
        k.vmax(mx, iv)
        yield
        k.vmatch(wk, mx, iv, -3.0e38)
        yield
        k.vmax(mx2, wk)
        yield
        k.tt(mk, iv, mx2[:, 7:8].to_broadcast([128, 64]), ALU.is_ge)
        yield
        k.ts(negm4[:, s, 64:128], mk, -NEGM, NEGM, ALU.mult, ALU.add)
        yield

    for Qi in range(NQ):
        t0 = Qi * 512
        Qa = QA[Qi % 2]
        k.dma(Qa[0:64, :, :], ftq[:, 0:8, t0:t0 + 512])
        k.dma(gts, gtv[:, Qi * 4:(Qi + 1) * 4, :])
        k.dma(vis, visv[:, Qi * 4:(Qi + 1) * 4, :])
        k.dma(cadd, caddv[:, Qi * 4:(Qi + 1) * 4, :])
        k.dma(cmT, cmv[:, 0:NCT, t0:t0 + 512])
        cts = [ct for ct in range(NCT) if 16 * 128 * ct + 31 <= t0 + 511]
        pipeline((cmp_gen(Qa, h, h // 4, cts) for h in range(8)), 2)
        for g in range(2):
            k.tt(imp[:, :, g, :], imp[:, :, g, :], vis, ALU.mult)
            k.tt(imp[:, :, g, :], imp[:, :, g, :], cadd, ALU.add)
        for g in range(2):
            pipeline((topk_gen(g, s, g * 4 + s) for s in range(4)), 3)
            k.trs([(pTb[:, s, :], negm4[:, s, :]) for s in range(4)], ident)
            k.cp(Qa[64:128, 4 * g:4 * g + 4, :].rearrange("p h (s t) -> p h s t", s=4),
                 pTb[64:128, :, :].unsqueeze(1).to_broadcast([64, 4, 4, 128]))
        def all_gens():
            for h in range(8):
                g = h // 4
                kts = []
                for kt in range(4 * Qi + 4):
                    m = kt - 4 * Qi
                    if m >= 0:
                        kts.append((kt, 128 * m, 512, 128 * m, tri))
                    else:
                        kts.append((kt, 0, 512, 0, None))
                yield from attend_gens(Qa, h, g, kts, KA, VS1, 128, osel, False)
                kts = []
                for kt in range(max(0, 4 * Qi - 4), 4 * Qi + 4):
                    m = kt - 4 * Qi
                    if m >= 0:
                        kts.append((kt, 128 * m, 512, 128 * m, tri))
                    else:
                        kts.append((kt, 0, 128 * (m + 5), 128 * (m + 4), ntri))
                yield from attend_gens(Qa, h, g, kts, KW, VW1, 64, owin, True)
        pipeline(all_gens(), 5)
        if B_STAGE < 4:
            continue
        for br, ob in enumerate((ocmp, osel, owin)):
            k.tt(ob, ob, gts[:, :, br * 8:(br + 1) * 8].unsqueeze(3).to_broadcast([128, 4, 8, 64]),
                 ALU.mult, eng=("pool" if br == 1 else "dve"))
        k.tt(ocmp, ocmp, osel, ALU.add)
        k.tt(ocmp, ocmp, owin, ALU.add, eng="pool")
        if B_STAGE < 5:
            continue
        k.tt(osq, ocmp, ocmp, ALU.mult)
        k.rsum(oss, osq.rearrange("p s h d -> p (s h) d"))
        rms_rstd(k, ors, oss, 64)
        k.tt(ocmp.rearrange("p s h d -> p (s h) d"), ocmp.rearrange("p s h d -> p (s h) d"),
             ors.unsqueeze(2).to_broadcast([128, 32, 64]), ALU.mult)
        k.tt(onb.rearrange("p s (h d) -> p (s h) d", d=64), ocmp.rearrange("p s h d -> p (s h) d"),
             gout.unsqueeze(1).to_broadcast([128, 32, 64]), ALU.mult, eng="pool")
        if B_STAGE < 6:
            continue
        for s in range(4):
            k.trs([(pTb[:, c, :], onb[:, s, c * 128:(c + 1) * 128]) for c in range(4)], ident)
            k.cp(mixs, pTb)
            if B_STAGE >= 7:
                k.dma(mixv[:, 4:8, t0 + s * 128:t0 + (s + 1) * 128], mixs)


def load_colvec(k, dst, src, n_chunks, identf, pTf, tmp):
    k.dma(tmp[0:n_chunks, :], src.rearrange("(c p) -> c p", p=128))
    k.trs([(pTf[:, 0:n_chunks], tmp[0:n_chunks, :])], identf[0:n_chunks, 0:n_chunks])
    k.cp(dst, pTf[:, 0:n_chunks])


def phase_c(nc, k, T, x, MIXT, w_out, ffn_g, w_up, conv_w, conv_b, w_down, H2, ident, identf):
    TT = 256
    NTT = T // TT
    NF = 22
    wo = k.sb("c_wo", [128, 8, D], BF16)
    wu = k.sb("c_wu", [128, 8, 2 * DFF], BF16)
    wd = k.sb("c_wd", [128, NF, D], BF16)
    wov = w_out.rearrange("(c p) n -> p c n", p=128)
    wuv = w_up.rearrange("(c p) n -> p c n", p=128)
    wdv = w_down.rearrange("(c p) n -> p c n", p=128)
    for c in range(8):
        k.dma(wo[:, c, :], wov[:, c, :], q="pool")
    for c in range(8):
        for hh in range(2):
            k.dma(wu[:, c, hh * DFF:(hh + 1) * DFF], wuv[:, c, hh * DFF:(hh + 1) * DFF], q="pool")
    for c in range(NF):
        k.dma(wd[:, c, :], wdv[:, c, :], q="pool")
    pY = [k.ps("c_pY%d" % i, [128, 512]) for i in range(2)]
    pC = k.ps("c_pC", [128, 8, 128], BF16)
    pUs = [k.ps("c_pU%d" % i, [128, 256]) for i in range(4)]
    tmpv = k.sb("c_tmpv", [44, 128])
    gfT = k.sb("c_gfT", [128, 8])
    load_colvec(k, gfT, ffn_g, 8, identf, pY[0], tmpv)
    cw = k.sb("c_cw", [128, 3, 44])
    cb = k.sb("c_cb", [128, 44])
    for j in range(3):
        load_colvec(k, cw[:, j, :], conv_w[j], 44, identf, pY[0], tmpv)
    load_colvec(k, cb, conv_b, 44, identf, pY[0], tmpv)
    carry = k.sb("c_carry", [128, 44, 2])
    k.memset(carry, 0.0)

    mt = k.sb("c_mt", [128, 8, TT], BF16)
    hsb = k.sb("c_h", [128, 2, D])
    xt = k.sb("c_x", [128, 2, D])
    junk = k.sb("c_junk", [128, D], BF16)
    ss = k.sb("c_ss", [128, 1])
    rstd = k.sb("c_rstd", [128, 1])
    hn = k.sb("c_hn", [128, D], BF16)
    hnT = k.sb("c_hnT", [128, 8, TT], BF16)
    actT = k.sb("c_actT", [128, NF, TT], BF16)
    uraw = [k.sb("c_uraw%d" % i, [128, TT + 2]) for i in range(4)]
    acc = [k.sb("c_acc%d" % i, [128, TT]) for i in range(4)]
    sil = [k.sb("c_sil%d" % i, [128, TT]) for i in range(2)]
    mixv = MIXT.rearrange("(c p) t -> p c t", p=128)
    xv = x.rearrange("(n p) d -> p n d", p=128)
    h2v = H2.rearrange("(n p) d -> p n d", p=128)

    for it in range(NTT):
        t0 = it * TT
        k.dma(mt, mixv[:, :, t0:t0 + TT])
        k.dma(xt, xv[:, 2 * it:2 * it + 2, :])
        for s in range(2):
            for hh in range(2):
                py = pY[(s * 2 + hh) % 2]
                k.mm(py, [(mt[:, c, s * 128:(s + 1) * 128], wo[:, c, hh * 512:(hh + 1) * 512])
                          for c in range(8)])
                k.tt(hsb[:, s, hh * 512:(hh + 1) * 512], py, xt[:, s, hh * 512:(hh + 1) * 512], ALU.add)
            k.act(junk, hsb[:, s, :], AF.Square, accum_out=ss)
            rms_rstd(k, rstd, ss, D)
            k.act(hn, hsb[:, s, :], AF.Copy, scale=rstd)
            k.trs([(pC[:, c, :], hn[:, c * 128:(c + 1) * 128]) for c in range(8)], ident)
            k.tt(hnT[:, :, s * 128:(s + 1) * 128], pC, gfT.unsqueeze(2).to_broadcast([128, 8, 128]),
                 ALU.mult)
        def ffn_gen(i):
            accs = []
            for gu in range(2):
                ch = i + gu * NF
                slot = (i % 2) * 2 + gu
                pu = pUs[slot]
                k.mm(pu, [(wu[:, c, ch * 128:(ch + 1) * 128], hnT[:, c, :]) for c in range(8)])
                yield
                ur, ac = uraw[slot], acc[slot]
                k.cp(ur[:, 0:2], carry[:, ch, :], eng="pool")
                k.cp(ur[:, 2:TT + 2], pu, eng="act")
                yield
                k.act(ac, pu, AF.Identity, scale=cw[:, 2, ch:ch + 1], bias=cb[:, ch:ch + 1])
                yield
                k.stt(ac, ur[:, 1:TT + 1], cw[:, 1, ch:ch + 1], ac, ALU.mult, ALU.add)
                yield
                k.stt(ac, ur[:, 0:TT], cw[:, 0, ch:ch + 1], ac, ALU.mult, ALU.add)
                k.cp(carry[:, ch, :], ur[:, TT:TT + 2], eng="pool")
                yield
                accs.append(ac)
            sl = sil[i % 2]
            k.act(sl, accs[0], AF.Silu)
            yield
            k.tt(actT[:, i, :], sl, accs[1], ALU.mult, eng="pool")
            yield
        pipeline((ffn_gen(i) for i in range(NF)), FFN_DEPTH)
        for s in range(2):
            for hh in range(2):
                py = pY[(s * 2 + hh) % 2]
                k.mm(py, [(actT[:, i, s * 128:(s + 1) * 128], wd[:, i, hh * 512:(hh + 1) * 512])
                          for i in range(NF)])
                k.tt(hsb[:, s, hh * 512:(hh + 1) * 512], py, hsb[:, s, hh * 512:(hh + 1) * 512], ALU.add)
        k.dma(h2v[:, 2 * it:2 * it + 2, :], hsb)


def phase_d(nc, k, T, H2, p_in, pleg_g, w_pleg, w_ple, ple_g, out, ident, identf):
    NT = T // 128
    wg = k.sb("d_wg", [128, 8, D], BF16)
    wp = k.sb("d_wp", [128, 2, D], BF16)
    wgv = w_pleg.rearrange("(c p) n -> p c n", p=128)
    wpv = w_ple.rearrange("(c p) n -> p c n", p=128)
    for c in range(8):
        k.dma(wg[:, c, :], wgv[:, c, :], q="pool")
    for c in range(2):
        k.dma(wp[:, c, :], wpv[:, c, :], q="pool")
    pY = [k.ps("d_pY%d" % i, [128, 512]) for i in range(2)]
    pE4 = [k.ps("d_pE%d" % i, [128, 512]) for i in range(4)]
    pC = k.ps("d_pC", [128, 8, 128], BF16)
    tmpv = k.sb("d_tmpv", [8, 128])
    ggT = k.sb("d_ggT", [128, 8])
    load_colvec(k, ggT, pleg_g, 8, identf, pY[0], tmpv)
    gple = k.sb("d_gple", [128, D])
    k.dma(gple, ple_g.partition_broadcast(128))

    hb = [k.sb("d_h%d" % i, [128, D]) for i in range(2)]
    pb = [k.sb("d_p%d" % i, [128, 256]) for i in range(2)]
    junks = [k.sb("d_junk%d" % i, [128, D], BF16) for i in range(2)]
    sss = [k.sb("d_ss%d" % i, [128, 1]) for i in range(2)]
    rstds = [k.sb("d_rstd%d" % i, [128, 1]) for i in range(2)]
    ss2s = [k.sb("d_ss2%d" % i, [128, 2]) for i in range(2)]
    rstd2s = [k.sb("d_rstd2%d" % i, [128, 1]) for i in range(2)]
    hns = [k.sb("d_hn%d" % i, [128, D], BF16) for i in range(2)]
    hTs = [k.sb("d_hT%d" % i, [128, 8, 128], BF16) for i in range(2)]
    pbfs = [k.sb("d_pbf%d" % i, [128, 256], BF16) for i in range(2)]
    pTs = [k.sb("d_pT%d" % i, [128, 2, 128], BF16) for i in range(2)]
    gates = [k.sb("d_gate%d" % i, [128, D]) for i in range(2)]
    es = [k.sb("d_e%d" % i, [128, D]) for i in range(2)]
    ob = [k.sb("d_o%d" % i, [128, D]) for i in range(2)]

    def d_gen(it):
        t0 = it * 128
        b = it % 2
        h, pp, o = hb[b], pb[b], ob[b]
        junk, ss, rstd, ss2, rstd2 = junks[b], sss[b], rstds[b], ss2s[b], rstd2s[b]
        hn, hT, pbf, pT, gate, e = hns[b], hTs[b], pbfs[b], pTs[b], gates[b], es[b]
        pE = pE4[2 * b:2 * b + 2]
        k.dma(h, H2[t0:t0 + 128, :])
        k.dma(pp, p_in[t0:t0 + 128, :])
        yield
        k.act(junk, h, AF.Square, accum_out=ss)
        yield
        k.act(rstd, ss, AF.Ln, scale=1.0 / D, bias=EPS)
        yield
        k.act(rstd, rstd, AF.Exp, scale=-0.5)
        yield
        k.act(hn, h, AF.Copy, scale=rstd)
        yield
        k.trs([(pC[:, c, :], hn[:, c * 128:(c + 1) * 128]) for c in range(8)], ident)
        yield
        k.tt(hT, pC, ggT.unsqueeze(2).to_broadcast([128, 8, 128]), ALU.mult)
        yield
        k.cp(pbf, pp, eng="pool")
        yield
        k.trs([(pC[:, c, :], pbf[:, c * 128:(c + 1) * 128]) for c in range(2)], ident)
        yield
        k.cp(pT, pC[:, 0:2, :])
        yield
        for hh in range(2):
            k.mm(pY[hh], [(hT[:, c, :], wg[:, c, hh * 512:(hh + 1) * 512]) for c in range(8)])
            yield
            k.act(gate[:, hh * 512:(hh + 1) * 512], pY[hh], AF.Sigmoid)
            yield
        for hh in range(2):
            k.mm(pE[hh], [(pT[:, c, :], wp[:, c, hh * 512:(hh + 1) * 512]) for c in range(2)])
            yield
            k.act(junk[:, hh * 512:(hh + 1) * 512], pE[hh], AF.Square, accum_out=ss2[:, hh:hh + 1])
            yield
        k.tt(ss, ss2[:, 0:1], ss2[:, 1:2], ALU.add)
        yield
        k.act(rstd2, ss, AF.Ln, scale=1.0 / D, bias=EPS)
        yield
        k.act(rstd2, rstd2, AF.Exp, scale=-0.5)
        yield
        for hh in range(2):
            k.act(e[:, hh * 512:(hh + 1) * 512], pE[hh], AF.Copy, scale=rstd2)
            yield
        k.tt(e, e, gple, ALU.mult)
        yield
        k.tt(e, e, gate, ALU.mult, eng="pool")
        yield
        k.tt(o, e, h, ALU.add)
        yield
        tok = k.dma(out[t0:t0 + 128, :], o)
        k.out_toks.append(tok)
        yield

    pipeline((d_gen(it) for it in range(NT)), D_DEPTH)


def _consts(T):
    bf = ml_dtypes.bfloat16
    c = {}
    c["c_ident"] = np.eye(128).astype(bf)
    c["c_identf"] = np.eye(128, dtype=np.float32)
    half = 8
    inv = np.float32(500000.0) ** (-np.arange(half, dtype=np.float32) / half)
    ang = np.arange(T, dtype=np.float32)[:, None] * inv[None, :].astype(np.float32)
    c["c_rope"] = np.concatenate([np.cos(ang), np.sin(ang)], 1).astype(np.float32)
    s = np.arange(128)
    c["c_tri2"] = ((s[:, None] <= s[None, :]) & (s[:, None] // 64 == s[None, :] // 64)).astype(np.float32)
    c["c_chunk"] = (s[:, None] // 64 == np.arange(2)[None, :]).astype(np.float32)
    t = np.arange(T)
    cc = np.arange(256)
    ncmp = T // 16 - 1
    c["c_cmask"] = (((16 * cc[:, None] + 31) <= t[None, :]) & (cc[:, None] < ncmp)).astype(bf)
    c["c_tri"] = (s[:, None] <= s[None, :]).astype(bf)
    c["c_ntri"] = (s[None, :] < s[:, None]).astype(bf)
    n = np.arange(64)
    c["c_E"] = ((t[None, :] // 64) == n[:, None]).astype(bf)
    cur = t // 64
    vis = (n[None, :] * 64 <= t[:, None])
    bonus = np.zeros((T, 64), np.float32)
    bonus += (n[None, :] == 0) * 1.0e6
    bonus += (n[None, :] == cur[:, None]) * 2.0e6
    bonus += (n[None, :] == cur[:, None] - 1) * 4.0e6
    c["c_vis"] = vis.astype(np.float32)
    c["c_cadd"] = np.where(vis, bonus, np.float32(-1e30)).astype(np.float32)
    cs = cc * 16
    ssb = n * 64
    ov = np.clip(np.minimum(cs[:, None] + 32, ssb[None, :] + 64)
                 - np.maximum(cs[:, None], ssb[None, :]), 0, None) / 32.0
    o1 = np.zeros((256, 65), np.float32)
    o1[:, 0] = 1.0
    o1[:, 1:] = ov
    o1[ncmp:] = 0.0
    c["c_ovl1"] = o1.astype(bf)
    return c


_W_NAMES = ["attn_norm_g", "w_in", "hg_norm_g", "nsa_q_norm_g", "nsa_k_norm_g", "cmp_pe", "cmp_w1",
            "cmp_w2", "nsa_out_norm_g", "w_out", "ffn_norm_g", "w_up", "conv_w", "conv_b", "w_down",
            "ple_gate_norm_g", "w_ple_gate", "w_ple", "ple_norm_g"]


def kernel(**inputs):
    x = np.asarray(inputs["x"], np.float32)
    p = np.asarray(inputs["p"], np.float32)
    B, T, _ = x.shape
    nc = build(T=T, dbg=False, phases="ABCD")
    shared = {n: np.ascontiguousarray(np.asarray(inputs[n], np.float32)[0]) for n in _W_NAMES}
    shared["hg_lb_logits"] = np.ascontiguousarray(np.asarray(inputs["hg_lb_logits"], np.float32))
    shared.update(_consts(T))
    in_maps = []
    for b in range(B):
        m = dict(shared)
        m["x"] = np.ascontiguousarray(x[b])
        m["p"] = np.ascontiguousarray(p[0, b])
        in_maps.append(m)
    res = run_bass_kernel_spmd(nc, in_maps, core_ids=list(range(B)))
    return np.stack([np.asarray(r["out"], np.float32) for r in res.results], axis=0)
```

```python
from contextlib import ExitStack
import numpy as np
import ml_dtypes
import concourse.bass as bass
import concourse.mybir as mybir
from concourse.bass_utils import run_bass_kernel_spmd

F32 = mybir.dt.float32
BF16 = mybir.dt.bfloat16
ALU = mybir.AluOpType
AF = mybir.ActivationFunctionType
AX = mybir.AxisListType

D = 1024
IN_TOTAL = 3352
DFF = 2816
EPS = 1e-6
NEGM = -30000.0
B_STAGE = 9
FFN_DEPTH = 2
D_DEPTH = 2
PRO_DEPTH = 2


class Sched:
    def __init__(self, nc, n_dma_sems=48):
        self.nc = nc
        self.engs = {"pe": nc.tensor, "act": nc.scalar, "dve": nc.vector,
                     "pool": nc.gpsimd, "sp": nc.sync}
        self.sem = {}
        self.cnt = {}
        for k in ("pe", "act", "dve", "pool"):
            self.sem[k] = nc.alloc_semaphore("s_" + k)
            self.cnt[k] = 0
        self.dma_sems = [nc.alloc_semaphore("s_dma%d" % i) for i in range(n_dma_sems)]
        self.dma_val = [0] * n_dma_sems
        self.dma_rr = 0
        self.waited = {}
        self.last_w = {}
        self.readers = {}
        self.nwaits = 0
        self.inflight = {}
        self.max_desc = 600

    def _wait(self, eng, tok):
        sem, val, key = tok
        if key == eng and eng == "pe":
            return
        k = (eng, key)
        if self.waited.get(k, 0) >= val:
            return
        self.engs[eng].wait_ge(sem, val)
        self.nwaits += 1
        self.waited[k] = val

    def deps(self, eng, reads, writes):
        toks = []
        for r in reads:
            t = self.last_w.get(r)
            if t is not None:
                toks.append(t)
        for w in writes:
            t = self.last_w.get(w)
            if t is not None:
                toks.append(t)
            toks.extend(self.readers.get(w, ()))
        for t in toks:
            self._wait(eng, t)

    def commit(self, tok, reads, writes):
        for w in writes:
            self.last_w[w] = tok
            self.readers[w] = []
        for r in reads:
            if r in writes:
                continue
            lst = self.readers.setdefault(r, [])
            lst[:] = [t for t in lst if t[2] != tok[2]]
            lst.append(tok)

    def op(self, eng, reads, writes, fn):
        self.deps(eng, reads, writes)
        ins = fn(self.engs[eng])
        self.cnt[eng] += 1
        ins.then_inc(self.sem[eng], 1)
        tok = (self.sem[eng], self.cnt[eng], eng)
        self.commit(tok, reads, writes)
        return tok

    @staticmethod
    def _ndesc(ap):
        dims = list(ap.ap)
        total = 1
        for st, n in dims:
            total *= n
        run = 1
        for st, n in reversed(dims[1:]):
            if st == run:
                run *= n
            else:
                break
        return max(1, total // max(run, 1))

    def dma(self, out, in_, reads, writes, q="sp", **kw):
        nd = max(self._ndesc(out), self._ndesc(in_))
        fifo = self.inflight.setdefault(q, [])
        while fifo and sum(d for _, d in fifo) + nd > self.max_desc:
            tok0, _ = fifo.pop(0)
            self._wait(q, tok0)
        tok = self._dma(out, in_, reads, writes, q, **kw)
        fifo.append((tok, nd))
        return tok

    def _dma(self, out, in_, reads, writes, q="sp", **kw):
        i = self.dma_rr
        self.dma_rr = (self.dma_rr + 1) % len(self.dma_sems)
        sem = self.dma_sems[i]
        key = "dma%d" % i
        if self.dma_val[i] > 0:
            self._wait(q, (sem, self.dma_val[i], key))
        self.deps(q, reads, writes)
        self.dma_val[i] += 16
        self.engs[q].dma_start(out=out, in_=in_, **kw).then_inc(sem, 16)
        tok = (sem, self.dma_val[i], key)
        self.commit(tok, reads, writes)
        return tok


_TAGS = {}
_KEEP = []


def tag(ap, name):
    _TAGS[id(ap)] = name
    _KEEP.append(ap)
    return ap


def sub(ap, fn):
    r = fn(ap)
    if id(ap) in _TAGS:
        tag(r, _TAGS[id(ap)])
    return r


def pipeline(gens, depth):
    gens = iter(gens)
    active = []
    exhausted = False
    while True:
        if not exhausted and len(active) < depth:
            g = next(gens, None)
            if g is None:
                exhausted = True
            else:
                active.append(g)
        if not active:
            if exhausted:
                break
            continue
        for g in list(active):
            try:
                next(g)
            except StopIteration:
                active.remove(g)


def _names(aps):
    out = []
    for a in aps:
        if a is None or isinstance(a, (int, float)):
            continue
        n = _TAGS.get(id(a)) or a.name
        if n not in out:
            out.append(n)
    return out


class K:
    def __init__(self, nc):
        self.nc = nc
        self.S = Sched(nc)
        self.out_toks = []
        self.es = None

    def sb(self, name, shape, dt=F32):
        return self.es.enter_context(self.nc.sbuf_tensor(name, list(shape), dt))[:]

    def ps(self, name, shape, dt=F32):
        return self.es.enter_context(self.nc.psum_tensor(name, list(shape), dt))[:]

    def barrier(self):
        S = self.S
        toks = [(S.sem[e], S.cnt[e], e) for e in ("pe", "act", "dve", "pool") if S.cnt[e] > 0]
        toks += [(S.dma_sems[i], S.dma_val[i], "dma%d" % i) for i in range(len(S.dma_sems))
                 if S.dma_val[i] > 0]
        for eng in ("sp", "pe", "act", "dve", "pool"):
            for t in toks:
                S._wait(eng, t)
        S.last_w.clear()
        S.readers.clear()

    def mm1(self, out, lhsT, rhs, start, stop):
        return self.S.op("pe", _names([lhsT, rhs]), _names([out]),
                         lambda e: e.matmul(out, lhsT=lhsT, rhs=rhs, start=start, stop=stop))

    def vmax(self, out, in_):
        return self.S.op("dve", _names([in_]), _names([out]), lambda e: e.max(out=out, in_=in_))

    def vmatch(self, out, mx, vals, imm):
        return self.S.op("dve", _names([mx, vals]), _names([out]),
                         lambda e: e.match_replace(out=out, in_to_replace=mx, in_values=vals,
                                                   imm_value=imm))

    def recip(self, out, in_):
        return self.S.op("dve", _names([in_]), _names([out]), lambda e: e.reciprocal(out=out, in_=in_))

    def act(self, out, in_, func, bias=None, scale=None, accum_out=None, eng="act"):
        kw = {}
        if bias is not None:
            kw["bias"] = bias
        if scale is not None:
            kw["scale"] = scale
        if accum_out is not None:
            kw["accum_out"] = accum_out
        rd = _names([in_, bias, scale])
        wr = _names([out, accum_out])
        return self.S.op(eng, rd, wr, lambda e: e.activation(out=out, in_=in_, func=func, **kw))

    def tt(self, out, in0, in1, op, eng="dve"):
        return self.S.op(eng, _names([in0, in1]), _names([out]),
                         lambda e: e.tensor_tensor(out=out, in0=in0, in1=in1, op=op))

    def ts(self, out, in0, s1, s2, op0, op1=None, eng="dve"):
        def f(e):
            if op1 is None:
                return e.tensor_scalar(out=out, in0=in0, scalar1=s1, scalar2=None, op0=op0)
            return e.tensor_scalar(out=out, in0=in0, scalar1=s1, scalar2=s2, op0=op0, op1=op1)
        return self.S.op(eng, _names([in0, s1, s2]), _names([out]), f)

    def stt(self, out, in0, scalar, in1, op0, op1, eng="dve"):
        return self.S.op(eng, _names([in0, scalar, in1]), _names([out]),
                         lambda e: e.scalar_tensor_tensor(out=out, in0=in0, scalar=scalar, in1=in1,
                                                          op0=op0, op1=op1))

    def cp(self, out, in_, eng="dve"):
        if eng == "act":
            return self.S.op("act", _names([in_]), _names([out]), lambda e: e.copy(out=out, in_=in_))
        return self.S.op(eng, _names([in_]), _names([out]), lambda e: e.tensor_copy(out=out, in_=in_))

    def rsum(self, out, in_, eng="dve"):
        return self.S.op(eng, _names([in_]), _names([out]),
                         lambda e: e.reduce_sum(out=out, in_=in_, axis=AX.X))

    def memset(self, out, val, eng="dve"):
        return self.S.op(eng, [], _names([out]), lambda e: e.memset(out, val))

    def mm(self, out, pairs, extra_w=()):
        rd = _names([a for p in pairs for a in p])
        n = len(pairs)

        def f(e):
            for i, (l, r) in enumerate(pairs):
                ins = e.matmul(out, lhsT=l, rhs=r, start=(i == 0), stop=(i == n - 1))
            return ins
        return self.S.op("pe", rd, _names([out]) + list(extra_w), f)

    def mms(self, groups):
        rd, wr = [], []
        for out, pairs in groups:
            wr += _names([out])
            rd += _names([a for p in pairs for a in p])

        def f(e):
            for out, pairs in groups:
                n = len(pairs)
                for i, (l, r) in enumerate(pairs):
                    ins = e.matmul(out, lhsT=l, rhs=r, start=(i == 0), stop=(i == n - 1))
            return ins
        return self.S.op("pe", list(dict.fromkeys(rd)), list(dict.fromkeys(wr)), f)

    def trs(self, items, ident):
        rd = _names([i for _, i in items] + [ident])
        wr = _names([o for o, _ in items])

        def f(e):
            for o, i in items:
                ins = e.transpose(out=o, in_=i, identity=ident)
            return ins
        return self.S.op("pe", rd, wr, f)

    def dma(self, out, in_, q="sp", **kw):
        return self.S.dma(out, in_, _names([in_]), _names([out]), q=q, **kw)

    def finish(self):
        for t in self.out_toks:
            self.S._wait("sp", t)


def rms_rstd(k, out, ss, n):
    k.act(out, ss, AF.Ln, scale=1.0 / n, bias=EPS)
    k.act(out, out, AF.Exp, scale=-0.5)


def build(T=4096, dbg=False, phases="ABCD"):
    NT = T // 128
    nc = bass.Bass("TRN2", target_bir_lowering=False)
    k = K(nc)

    def din(name, shape, dt=F32):
        return nc.dram_tensor(name, list(shape), dt, kind="ExternalInput").ap()

    def dscr(name, shape, dt):
        return nc.dram_tensor(name, list(shape), dt, kind=("ExternalOutput" if dbg else "Internal")).ap()

    x = din("x", [T, D])
    p_in = din("p", [T, 256])
    attn_g = din("attn_norm_g", [D])
    w_in = din("w_in", [D, IN_TOTAL])
    lb_logits = din("hg_lb_logits", [2, 512])
    hg_norm_g = din("hg_norm_g", [128])
    q_norm_g = din("nsa_q_norm_g", [64])
    k_norm_g = din("nsa_k_norm_g", [3, 64])
    cmp_pe = din("cmp_pe", [2, 32, 64])
    cmp_w1 = din("cmp_w1", [2, 2048, 128])
    cmp_w2 = din("cmp_w2", [2, 128, 64])
    out_norm_g = din("nsa_out_norm_g", [64])
    w_out = din("w_out", [D, D])
    ffn_g = din("ffn_norm_g", [D])
    w_up = din("w_up", [D, 2 * DFF])
    conv_w = din("conv_w", [3, 2 * DFF])
    conv_b = din("conv_b", [2 * DFF])
    w_down = din("w_down", [DFF, D])
    pleg_g = din("ple_gate_norm_g", [D])
    w_pleg = din("w_ple_gate", [D, D])
    w_ple = din("w_ple", [256, D])
    ple_g = din("ple_norm_g", [D])
    c_ident = din("c_ident", [128, 128], BF16)
    c_rope = din("c_rope", [T, 16])
    c_tri2 = din("c_tri2", [128, 128])
    c_chunk = din("c_chunk", [128, 2])

    out = nc.dram_tensor("out", [T, D], F32, kind="ExternalOutput").ap()

    FT = dscr("FT", [16, 64, T], BF16)
    VT = dscr("VT", [T, 256], BF16)
    GT = dscr("GT", [T, 24], F32)
    MIXT = dscr("MIXT", [D, T], BF16)

    c_identf = din("c_identf", [128, 128])
    c_cmask = din("c_cmask", [256, T], BF16)
    c_tri = din("c_tri", [128, 128], BF16)
    c_ntri = din("c_ntri", [128, 128], BF16)
    c_E = din("c_E", [64, T], BF16)
    c_vis = din("c_vis", [T, 64])
    c_cadd = din("c_cadd", [T, 64])
    c_ovl1 = din("c_ovl1", [256, 65], BF16)
    H2 = dscr("H2", [T, D], F32)

    with ExitStack() as es0:
        k.es = es0
        ident = k.sb("ident", [128, 128], BF16)
        k.dma(ident, c_ident)
        identf = k.sb("identf", [128, 128])
        k.dma(identf, c_identf)
        if "A" in phases:
            with ExitStack() as es:
                k.es = es
                phase_a(nc, k, T, NT, x, attn_g, w_in, lb_logits, hg_norm_g, q_norm_g, k_norm_g,
                        c_rope, c_tri2, c_chunk, ident, FT, VT, GT, MIXT)
                k.barrier()
        if "B" in phases:
            with ExitStack() as es:
                k.es = es
                phase_b(nc, k, T, NT, FT, VT, GT, MIXT, k_norm_g, cmp_pe, cmp_w1, cmp_w2, out_norm_g,
                        c_rope, c_cmask, c_tri, c_ntri, c_E, c_vis, c_cadd, c_ovl1, ident, identf)
                k.barrier()
        if "A" not in phases and dbg:
            mi = din("MIXT_in", [D, T], BF16)
            with ExitStack() as es:
                k.es = es
                tb = k.sb("dbg_mix", [128, 8, T], BF16)
                k.dma(tb, mi.rearrange("(c p) t -> p c t", p=128))
                k.dma(MIXT.rearrange("(c p) t -> p c t", p=128), tb)
                k.barrier()
        if "C" in phases:
            with ExitStack() as es:
                k.es = es
                phase_c(nc, k, T, x, MIXT, w_out, ffn_g, w_up, conv_w, conv_b, w_down, H2, ident, identf)
                k.barrier()
        if "D" in phases:
            with ExitStack() as es:
                k.es = es
                phase_d(nc, k, T, H2, p_in, pleg_g, w_pleg, w_ple, ple_g, out, ident, identf)
                k.barrier()
        k.finish()
    return nc


def phase_a(nc, k, T, NT, x, attn_g, w_in, lb_logits, hg_norm_g, q_norm_g, k_norm_g,
            c_rope, c_tri2, c_chunk, ident, FT, VT, GT, MIXT):
    S = k.S
    w_sb = k.sb("a_w", [128, 8, IN_TOTAL], BF16)
    w_v = w_in.rearrange("(c p) n -> p c n", p=128)
    for c in range(8):
        k.dma(w_sb[:, c, :], w_v[:, c, :], q="pool")
    gT = k.sb("a_gT", [128, 8])
    k.dma(gT, attn_g.rearrange("(c p) -> p c", p=128), allow_slow_non_contiguous=True)
    rope = k.sb("a_rope", [128, NT, 16])
    k.dma(rope, c_rope.rearrange("(n p) c -> p n c", p=128))
    tri2 = k.sb("a_tri2", [128, 128])
    k.dma(tri2, c_tri2)
    chunk = k.sb("a_chunk", [128, 2])
    k.dma(chunk, c_chunk)
    l0 = k.sb("a_l0", [128, 512])
    l1 = k.sb("a_l1", [128, 512])
    k.dma(l0, lb_logits[0].partition_broadcast(128))
    k.dma(l1, lb_logits[1].partition_broadcast(128))
    lb = k.sb("a_lb", [128, 512])
    oml = k.sb("a_oml", [128, 512])
    k.tt(l0, l0, l1, ALU.subtract)
    k.act(lb, l0, AF.Sigmoid)
    k.ts(oml, lb, -1.0, 1.0, ALU.mult, ALU.add)
    gq = k.sb("a_gq", [128, 12, 64])
    for h in range(12):
        src = q_norm_g if h < 8 else (k_norm_g[1] if h < 10 else k_norm_g[2])
        k.dma(gq[:, h, :], src.partition_broadcast(128))
    ghg = k.sb("a_ghg", [128, 4, 128])
    for h in range(4):
        k.dma(ghg[:, h, :], hg_norm_g.partition_broadcast(128))
    Sf = k.sb("a_Sf", [128, 4, 128])
    Sb = [k.sb("a_Sb%d" % i, [128, 4, 128], BF16) for i in range(3)]
    k.memset(Sf, 0.0)
    k.memset(Sb[0], 0.0, eng="pool")

    xt = [k.sb("a_x%d" % i, [128, D]) for i in range(2)]
    junk = k.sb("a_junk", [128, D], BF16)
    ss = k.sb("a_ss", [128, 1])
    rstd = k.sb("a_rstd", [128, 1])
    xn = k.sb("a_xn", [128, D], BF16)
    xnT = k.sb("a_xnT", [128, 8, 128], BF16)
    silq = k.sb("a_silq", [128, 512])
    sig = k.sb("a_sig", [128, 512])
    logf = k.sb("a_logf", [128, 512])
    kk = k.sb("a_kk", [128, 512])
    enb = k.sb("a_enb", [128, 512])
    epb = k.sb("a_epb", [128, 512])
    kp = k.sb("a_kp", [128, 512], BF16)
    qp = k.sb("a_qp", [128, 512], BF16)
    vb = k.sb("a_vb", [128, 512], BF16)
    sg = k.sb("a_sg", [128, 512])
    ebl = k.sb("a_ebl", [128, 4, 2])
    qkT = k.sb("a_qkT", [128, 8, 128], BF16)
    ATm = k.sb("a_ATm", [128, 4, 128], BF16)
    tmpS = k.sb("a_tmpS", [128, 4, 128])
    osb = k.sb("a_osb", [128, 4, 128])
    osq = k.sb("a_osq", [128, 4, 128])
    ss4 = k.sb("a_ss4", [128, 4])
    rstd4 = k.sb("a_rstd4", [128, 4])
    onb = k.sb("a_onb", [128, 4, 128], BF16)
    mixs = k.sb("a_mixs", [128, 4, 128], BF16)
    qk = k.sb("a_qk", [128, 12, 64])
    qsq = k.sb("a_qsq", [128, 12, 64])
    ss12 = k.sb("a_ss12", [128, 12])
    rstd12 = k.sb("a_rstd12", [128, 12])
    qkb = k.sb("a_qkb", [128, 12, 64], BF16)
    r_a = k.sb("a_ra", [128, 12, 8])
    r_b = k.sb("a_rb", [128, 12, 8])
    r_c = k.sb("a_rc", [128, 12, 8])
    r_d = k.sb("a_rd", [128, 12, 8])
    kcvc = k.sb("a_kcvc", [128, 256], BF16)
    T16 = k.sb("a_T16", [64, 16, 128], BF16)
    vv = k.sb("a_vv", [128, 256], BF16)
    gsb = k.sb("a_gsb", [128, 24])

    pA = [k.ps("a_pA%d" % i, [128, 512]) for i in range(2)]
    pB = k.ps("a_pB", [128, 512])
    pC = k.ps("a_pC", [128, 8, 128], BF16)
    pD = k.ps("a_pD", [64, 16, 128], BF16)
    pF = k.ps("a_pF", [128, 4, 128])
    pG = k.ps("a_pG", [128, 4, 128])

    cols = [(0, 512), (512, 512), (1024, 512), (1536, 512), (2048, 512), (2560, 512), (3072, 280)]
    mixv = MIXT.rearrange("(c p) t -> p c t", p=128)
    ftv = FT.rearrange("n d t -> d n t")
    xnT2 = [xnT, k.sb("a_xnT1", [128, 8, 128], BF16)]

    def proj(g, dst, xT):
        c0, n = cols[g]
        k.mm(dst[:, 0:n], [(xT[:, c, :], w_sb[:, c, c0:c0 + n]) for c in range(8)])
        return dst

    def genH(it):
        t0 = it * 128
        xb = xt[it % 2]
        xT = xnT2[it % 2]
        pAh = pA[0]
        if it == 0:
            k.dma(xb, x[t0:t0 + 128, :])
        if it + 1 < NT:
            k.dma(xt[(it + 1) % 2], x[t0 + 128:t0 + 256, :])
        yield
        k.act(junk, xb, AF.Square, accum_out=ss)
        yield
        k.act(rstd, ss, AF.Ln, scale=1.0 / D, bias=EPS)
        yield
        k.act(rstd, rstd, AF.Exp, scale=-0.5)
        yield
        k.act(xn, xb, AF.Copy, scale=rstd)
        yield
        k.trs([(pC[:, c, :], xn[:, c * 128:(c + 1) * 128]) for c in range(8)], ident)
        yield
        k.tt(xT, pC, gT.unsqueeze(2).to_broadcast([128, 8, 128]), ALU.mult)
        yield
        d = proj(1, pAh, xT)
        yield
        k.act(sig, d, AF.Sigmoid)
        yield
        k.tt(sig, sig, oml, ALU.mult)
        yield
        k.tt(sig, sig, lb, ALU.add)
        yield
        k.act(logf, sig, AF.Ln)
        yield
        k.ts(kk, sig, -1.0, 1.0, ALU.mult, ALU.add)
        yield
        k.mm(pB, [(tri2, logf)])
        yield
        k.act(enb, pB, AF.Exp, scale=-1.0)
        yield
        k.act(epb, pB, AF.Exp)
        yield
        k.tt(kp, kk, enb, ALU.mult)
        yield
        k.mms([(pF[:, h, 0:2], [(logf[:, h * 128:(h + 1) * 128], chunk)]) for h in range(4)])
        yield
        k.act(ebl, pF[:, :, 0:2], AF.Exp)
        yield
        d = proj(0, pAh, xT)
        yield
        k.act(silq, d, AF.Silu)
        yield
        k.stt(qp, silq, 128 ** -0.5, epb, ALU.mult, ALU.mult)
        yield
        d = proj(2, pAh, xT)
        yield
        k.cp(vb, d, eng="act")
        yield
        d = proj(3, pAh, xT)
        yield
        k.act(sg, d, AF.Silu)
        yield
        k.tt(sg, sg, ghg.rearrange("p h v -> p (h v)"), ALU.mult, eng="pool")
        yield
        k.trs([(pC[:, h, :], qp[:, h * 128:(h + 1) * 128]) for h in range(4)] +
              [(pC[:, 4 + h, :], kp[:, h * 128:(h + 1) * 128]) for h in range(4)], ident)
        yield
        k.cp(qkT, pC)
        yield
        S0, S1, S2 = Sb[(2 * it) % 3], Sb[(2 * it + 1) % 3], Sb[(2 * it + 2) % 3]
        k.mms([(pF[:, h, :], [(qkT[:, 4 + h, :], qkT[:, h, :])]) for h in range(4)])
        yield
        k.tt(ATm, pF, tri2.unsqueeze(1).to_broadcast([128, 4, 128]), ALU.mult)
        yield
        for c in range(2):
            rs = slice(c * 64, (c + 1) * 64)
            k.mms([(pF[:, h, :], [(kp[rs, h * 128:(h + 1) * 128], vb[rs, h * 128:(h + 1) * 128])])
                   for h in range(4)])
            yield
            k.tt(tmpS, pF, Sf, ALU.add)
            yield
            k.tt(Sf, tmpS, ebl[:, :, c:c + 1].to_broadcast([128, 4, 128]), ALU.mult)
            yield
            k.cp(S1 if c == 0 else S2, Sf, eng="pool")
            yield
        groups = []
        for h in range(4):
            for c in range(2):
                rs = slice(c * 64, (c + 1) * 64)
                Sc = S0 if c == 0 else S1
                groups.append((pG[rs, h, :], [(ATm[rs, h, rs], vb[rs, h * 128:(h + 1) * 128]),
                                              (qkT[:, h, rs], Sc[:, h, :])]))
        k.mms(groups)
        yield
        k.cp(osb, pG, eng="act")
        yield
        k.tt(osq, osb, osb, ALU.mult)
        yield
        k.rsum(ss4, osq)
        yield
        k.act(rstd4, ss4, AF.Ln, scale=1.0 / 128, bias=EPS)
        yield
        k.act(rstd4, rstd4, AF.Exp, scale=-0.5)
        yield
        k.tt(osb, osb, rstd4.unsqueeze(2).to_broadcast([128, 4, 128]), ALU.mult)
        yield
        k.tt(onb, osb, sg.rearrange("p (h v) -> p h v", h=4), ALU.mult)
        yield
        k.trs([(pC[:, h, :], onb[:, h, :]) for h in range(4)], ident)
        yield
        k.cp(mixs, pC[:, 0:4, :])
        yield
        k.dma(mixv[:, 0:4, t0:t0 + 128], mixs)
        yield

    def genN(it):
        t0 = it * 128
        xT = xnT2[it % 2]
        pAn = pA[1]
        d = proj(4, pAn, xT)
        yield
        k.cp(qk[:, 0:8, :], d.rearrange("p (h d) -> p h d", d=64), eng="act")
        yield
        d = proj(5, pAn, xT)
        yield
        k.cp(kcvc, d[:, 0:256], eng="act")
        yield
        k.cp(qk[:, 8:10, :], d[:, 256:384].rearrange("p (h d) -> p h d", d=64), eng="act")
        yield
        k.cp(vv[:, 0:128], d[:, 384:512], eng="act")
        yield
        d = proj(6, pAn, xT)
        yield
        k.cp(qk[:, 10:12, :], d[:, 0:128].rearrange("p (h d) -> p h d", d=64), eng="act")
        yield
        k.cp(vv[:, 128:256], d[:, 128:256], eng="act")
        yield
        k.act(gsb, d[:, 256:280], AF.Sigmoid)
        yield
        k.dma(GT[t0:t0 + 128, :], gsb)
        k.dma(VT[t0:t0 + 128, :], vv)
        yield
        k.tt(qsq, qk, qk, ALU.mult, eng="pool")
        yield
        k.rsum(ss12, qsq)
        yield
        k.act(rstd12, ss12, AF.Ln, scale=1.0 / 64, bias=EPS)
        yield
        k.act(rstd12, rstd12, AF.Exp, scale=-0.5)
        yield
        k.tt(qk, qk, rstd12.unsqueeze(2).to_broadcast([128, 12, 64]), ALU.mult)
        yield
        k.tt(qk, qk, gq, ALU.mult, eng="pool")
        yield
        cosb = rope[:, it:it + 1, 0:8].to_broadcast([128, 12, 8])
        sinb = rope[:, it:it + 1, 8:16].to_broadcast([128, 12, 8])
        k.tt(r_a, qk[:, :, 0:8], cosb, ALU.mult, eng="pool")
        yield
        k.tt(r_b, qk[:, :, 8:16], sinb, ALU.mult, eng="pool")
        yield
        k.tt(r_c, qk[:, :, 8:16], cosb, ALU.mult, eng="pool")
        yield
        k.tt(r_d, qk[:, :, 0:8], sinb, ALU.mult, eng="pool")
        yield
        k.cp(qkb, qk, eng="pool")
        yield
        k.tt(qkb[:, :, 0:8], r_a, r_b, ALU.subtract, eng="pool")
        yield
        k.tt(qkb[:, :, 8:16], r_c, r_d, ALU.add, eng="pool")
        yield
        k.trs([(pD[:, n, :], qkb[:, n, :]) for n in range(12)] +
              [(pD[:, 12 + n, :], kcvc[:, n * 64:(n + 1) * 64]) for n in range(4)], ident)
        yield
        k.cp(T16, pD)
        yield
        k.dma(ftv[:, :, t0:t0 + 128], T16)
        yield

    for it in range(NT + 1):
        gens = []
        if it < NT:
            gens.append(genH(it))
        if it >= 1:
            gens.append(genN(it - 1))
        pipeline(iter(gens), 2)


def phase_b(nc, k, T, NT, FT, VT, GT, MIXT, k_norm_g, cmp_pe, cmp_w1, cmp_w2, out_norm_g,
            c_rope, c_cmask, c_tri, c_ntri, c_E, c_vis, c_cadd, c_ovl1, ident, identf):
    NQ = T // 512
    NCMP = T // 16 - 1
    NCT = (NCMP + 127) // 128

    def crow(ct):
        return min(128, NCMP - 128 * ct)

    KA = k.sb("b_KA", [128, 2, T], BF16)
    KW = k.sb("b_KW", [64, 2, T], BF16)
    VS1 = k.sb("b_VS1", [128, NT, 2, 65], BF16)
    VW1 = k.sb("b_VW1", [128, NT, 2, 65], BF16)
    VTv = VT.rearrange("(n p) c -> p n c", p=128)
    k.memset(VS1, 1.0)
    k.memset(VW1, 1.0, eng="pool")
    for g in range(2):
        k.dma(KA[0:64, g, :], FT[8 + g])
        k.dma(KA[64:128, g, :], c_E)
        k.dma(KW[:, g, :], FT[10 + g])
        k.dma(VS1[:, :, g, 0:64], VTv[:, :, g * 64:(g + 1) * 64])
        k.dma(VW1[:, :, g, 0:64], VTv[:, :, 128 + g * 64:128 + (g + 1) * 64])
    tri = k.sb("b_tri", [128, 128], BF16)
    ntri = k.sb("b_ntri", [128, 128], BF16)
    k.dma(tri, c_tri)
    k.dma(ntri, c_ntri)
    zer = k.sb("b_zer", [128, 65], BF16)
    k.memset(zer, 0.0)
    gout = k.sb("b_gout", [128, 64])
    k.dma(gout, out_norm_g.partition_broadcast(128))

    pS = [k.ps("b_pS%d" % i, [128, 512]) for i in range(2)]
    pOs = [k.ps("b_pO%d" % i, [128, 512]) for i in range(2)]
    pO = pOs[0]
    pCIs = [k.ps("b_pCI%d" % i, [128, 4, 128]) for i in range(2)]
    pT = k.ps("b_pT", [128, 4, 128])
    pTb = k.ps("b_pTb", [128, 4, 128], BF16)

    kcT = k.sb("b_kcT", [64, 2, NCT * 128], BF16)
    Rv = k.sb("b_R", [128, NCT, 2, 128], BF16)
    k.memset(kcT, 0.0)
    k.memset(Rv, 0.0, eng="pool")
    ovv = c_ovl1.rearrange("(ct p) c -> p ct c", p=128)
    for ct in range(NCT):
        for g in range(2):
            k.dma(Rv[:, ct, g, 64:128], ovv[:, ct, 1:65])
    with ExitStack() as es_c:
        es_prev = k.es
        k.es = es_c
        kc2 = k.sb("b_kc2", [128, T], BF16)
        hid = k.sb("b_hid", [128, NCT * 128], BF16)
        k.memset(hid, 0.0)
        kcn = k.sb("b_kcn", [128, NCT * 2, 64])
        k.memset(kcn, 0.0)
        gk0 = k.sb("b_gk0", [128, 64])
        k.dma(gk0, k_norm_g[0].partition_broadcast(128))
        ropec = k.sb("b_ropec", [128, NCT, 16])
        k.memset(ropec, 0.0)
        rv = c_rope.rearrange("(c s) f -> c s f", s=16)
        for ct in range(NCT):
            k.dma(ropec[0:crow(ct), ct, :], rv[1 + ct * 128:1 + ct * 128 + crow(ct), 15, :])
        w1 = [k.sb("b_w1%d" % j, [128, 16, 128], BF16) for j in range(2)]
        w2 = [k.sb("b_w2%d" % j, [128, 64], BF16) for j in range(2)]
        pes = [k.sb("b_pe%d" % j, [128, 16], BF16) for j in range(2)]
        bvec = [k.sb("b_bv%d" % j, [128, 1]) for j in range(2)]
        for j in range(2):
            k.dma(w1[j], cmp_w1[j].rearrange("(l p) h -> p l h", p=128), q="pool")
            k.dma(w2[j], cmp_w2[j], q="pool")
            k.dma(pes[j], cmp_pe[j].rearrange("(l two) d -> (two d) l", two=2), q="pool",
                  allow_slow_non_contiguous=True)
        v16 = kc2.rearrange("p (c s) -> p c s", s=16)
        for j in range(2):
            k.mm(pO[:, 0:1], [(w1[j][:, l2, :], pes[j][:, l2:l2 + 1]) for l2 in range(16)])
            k.cp(bvec[j], pO[:, 0:1])
            for g in range(2):
                n = 12 + 2 * j + g
                k.dma(kc2[0:64, :], FT[n])
                k.dma(kc2[64:128, 0:T - 1], FT[n][:, 1:T])
                pairs = []
                for l2 in range(16):
                    rhs = v16[:, 0:NCMP, 2 * l2] if l2 < 8 else v16[:, 1:NCMP + 1, 2 * l2 - 16]
                    pairs.append((w1[j][:, l2, :], rhs))
                k.mm(pS[0][:, 0:NCMP], pairs)
                k.act(hid[:, 0:NCMP], pS[0][:, 0:NCMP], AF.Silu, bias=bvec[j])
                for ct in range(NCT):
                    r = crow(ct)
                    k.mm(pT[0:r, ct, 0:64], [(hid[:, ct * 128:ct * 128 + r], w2[j])])
                    if j == 0:
                        k.cp(kcn[0:r, ct * 2 + g, :], pT[0:r, ct, 0:64])
                    else:
                        k.cp(Rv[0:r, ct, g, 0:64], pT[0:r, ct, 0:64])
        NS = NCT * 2
        ksq = k.sb("b_ksq", [128, NS, 64])
        kss = k.sb("b_kss", [128, NS])
        krs = k.sb("b_krs", [128, NS])
        kcb = k.sb("b_kcb", [128, NS, 64], BF16)
        ra = k.sb("b_ra", [128, 2, 8])
        rb = k.sb("b_rb", [128, 2, 8])
        k.tt(ksq, kcn, kcn, ALU.mult)
        k.rsum(kss, ksq)
        rms_rstd(k, krs, kss, 64)
        k.tt(kcn, kcn, krs.unsqueeze(2).to_broadcast([128, NS, 64]), ALU.mult)
        k.tt(kcn, kcn, gk0.unsqueeze(1).to_broadcast([128, NS, 64]), ALU.mult)
        k.cp(kcb, kcn)
        for ct in range(NCT):
            sl = slice(ct * 2, ct * 2 + 2)
            cosb = ropec[:, ct:ct + 1, 0:8].to_broadcast([128, 2, 8])
            sinb = ropec[:, ct:ct + 1, 8:16].to_broadcast([128, 2, 8])
            k.tt(ra, kcn[:, sl, 0:8], cosb, ALU.mult)
            k.tt(rb, kcn[:, sl, 8:16], sinb, ALU.mult)
            k.tt(kcb[:, sl, 0:8], ra, rb, ALU.subtract)
            k.tt(ra, kcn[:, sl, 8:16], cosb, ALU.mult)
            k.tt(rb, kcn[:, sl, 0:8], sinb, ALU.mult)
            k.tt(kcb[:, sl, 8:16], ra, rb, ALU.add)
        for ct in range(NCT):
            r = crow(ct)
            for g in range(2):
                k.trs([(pTb[0:64, 0, 0:r], kcb[0:r, ct * 2 + g, :])], ident[0:r, 0:r])
                k.cp(kcT[:, g, ct * 128:ct * 128 + r], pTb[0:64, 0, 0:r])
        k.barrier()
        k.es = es_prev

    if B_STAGE < 1:
        return
    QA = [k.sb("b_QA%d" % i, [128, 8, 512], BF16) for i in range(2)]
    cmT = k.sb("b_cmT", [128, NCT, 512], BF16)
    gts = k.sb("b_gts", [128, 4, 24])
    vis = k.sb("b_vis", [128, 4, 64])
    cadd = k.sb("b_cadd", [128, 4, 64])
    NP = 5
    P = [k.sb("b_P%d" % i, [128, 512], BF16) for i in range(NP)]
    ocmp = k.sb("b_ocmp", [128, 4, 8, 64])
    osel = k.sb("b_osel", [128, 4, 8, 64])
    owin = k.sb("b_owin", [128, 4, 8, 64])
    oTs = [k.sb("b_oT%d" % i, [65, 512]) for i in range(2)]
    recs = [k.sb("b_rec%d" % i, [128, 4, 1]) for i in range(2)]
    dens = [k.sb("b_den%d" % i, [128, 4]) for i in range(2)]
    imp = k.sb("b_imp", [128, 4, 2, 64])
    itmps = [k.sb("b_itmp%d" % i, [128, 4, 64]) for i in range(2)]
    NB = 3
    mxs = [k.sb("b_mx%d" % i, [128, 8]) for i in range(NB)]
    mx2s = [k.sb("b_mx2%d" % i, [128, 8]) for i in range(NB)]
    wks = [k.sb("b_wk%d" % i, [128, 64]) for i in range(NB)]
    mks = [k.sb("b_mk%d" % i, [128, 64]) for i in range(NB)]
    negm4 = k.sb("b_negm4", [128, 4, 128], BF16)
    k.memset(negm4, 0.0)
    osq = k.sb("b_osq", [128, 4, 8, 64])
    oss = k.sb("b_oss", [128, 32])
    ors = k.sb("b_ors", [128, 32])
    onb = k.sb("b_onb", [128, 4, 512], BF16)
    mixs = k.sb("b_mixs", [128, 4, 128], BF16)
    ftq = FT.rearrange("n d t -> d n t")
    gtv = GT.rearrange("(s p) c -> p s c", p=128)
    visv = c_vis.rearrange("(s p) c -> p s c", p=128)
    caddv = c_cadd.rearrange("(s p) c -> p s c", p=128)
    cmv = c_cmask.rearrange("(ct p) t -> p ct t", p=128)
    mixv = MIXT.rearrange("(c p) t -> p c t", p=128)
    cnt = [0]
    ocnt = [0]

    def kt_gen(Qa, h, g, kt, lo, hi, mcol, mtile, Ksrc, Vsrc, kdim, pOb, first, last, zero_first, evac):
        i = cnt[0]
        cnt[0] += 1
        ps, Pb = pS[i % 2], P[i % NP]
        if first and zero_first:
            k.mm1(pOb[0:65, :], zer, Qa[:, h, :], True, False)
        k.mm(ps[:, lo:hi], [(Ksrc[0:kdim, g, kt * 128:(kt + 1) * 128], Qa[0:kdim, h, lo:hi])])
        yield
        k.act(Pb[:, lo:hi], ps[:, lo:hi], AF.Exp, scale=0.125)
        yield
        if mtile is not None:
            k.tt(Pb[:, mcol:mcol + 128], Pb[:, mcol:mcol + 128], mtile, ALU.mult)
            yield
        k.mm1(pOb[0:65, lo:hi], Vsrc[:, kt, g, :], Pb[:, lo:hi], (first and not zero_first), last)
        yield
        if last:
            yield from evac_gen(*evac)

    def evac_gen(pOb, oTb, rc, h, odst):
        k.cp(oTb, pOb[0:65, :], eng="act")
        yield
        k.trs([(pT[:, s, 0:65], oTb[:, s * 128:(s + 1) * 128]) for s in range(4)], identf[0:65, 0:65])
        yield
        k.recip(rc, pT[:, :, 64:65])
        yield
        k.tt(odst[:, :, h, :], pT[:, :, 0:64], rc.to_broadcast([128, 4, 64]), ALU.mult)
        yield

    def attend_gens(Qa, h, g, kts, Ksrc, Vsrc, kdim, odst, zero_first):
        j = ocnt[0]
        ocnt[0] += 1
        pOb, oTb, rc = pOs[j % 2], oTs[j % 2], recs[j % 2]
        n = len(kts)
        for idx, (kt, lo, hi, mcol, mtile) in enumerate(kts):
            yield kt_gen(Qa, h, g, kt, lo, hi, mcol, mtile, Ksrc, Vsrc, kdim, pOb,
                         idx == 0, idx == n - 1, zero_first, (pOb, oTb, rc, h, odst))

    def cmp_gen(Qa, h, g, cts):
        pc, rc, dn, itmp = pCIs[h % 2], recs[h % 2], dens[h % 2], itmps[h % 2]
        for ci, ct in enumerate(cts):
            i = cnt[0]
            cnt[0] += 1
            ps, Pb = pS[i % 2], P[i % NP]
            k.mm(ps, [(kcT[0:64, g, ct * 128:(ct + 1) * 128], Qa[0:64, h, :])])
            yield
            k.act(Pb, ps, AF.Exp, scale=0.125)
            yield
            k.tt(Pb, Pb, cmT[:, ct, :], ALU.mult)
            yield
            for s in range(4):
                k.mm1(pc[:, s, :], Pb[:, s * 128:(s + 1) * 128], Rv[:, ct, g, :],
                      ci == 0 and s == 0, ci == len(cts) - 1)
            yield
        k.rsum(dn, pc[:, :, 64:128])
        yield
        k.ts(rc, dn.unsqueeze(2), 1e-30, None, ALU.add)
        yield
        k.recip(rc, rc)
        yield
        k.tt(ocmp[:, :, h, :], pc[:, :, 0:64], rc.to_broadcast([128, 4, 64]), ALU.mult)
        yield
        if h % 4 == 0:
            k.tt(imp[:, :, g, :], pc[:, :, 64:128], rc.to_broadcast([128, 4, 64]), ALU.mult)
        else:
            k.tt(itmp, pc[:, :, 64:128], rc.to_broadcast([128, 4, 64]), ALU.mult)
            yield
            k.tt(imp[:, :, g, :], imp[:, :, g, :], itmp, ALU.add, eng="pool")
        yield

    def topk_gen(g, s, j):
        iv = imp[:, s, g, :]
        mx, mx2, wk, mk = mxs[j % NB], mx2s[j % NB], wks[j % NB], mks[j % NB]
        k.vmax(mx, iv)
        yield
        k.vmatch(wk, mx, iv, -3.0e38)
        yield
        k.vmax(mx2, wk)
        yield
        k.tt(mk, iv, mx2[:, 7:8].to_broadcast([128, 64]), ALU.is_ge)
        yield
        k.ts(negm4[:, s, 64:128], mk, -NEGM, NEGM, ALU.mult, ALU.add)
        yield

    for Qi in range(NQ):
        t0 = Qi * 512
        Qa = QA[Qi % 2]
        k.dma(Qa[0:64, :, :], ftq[:, 0:8, t0:t0 + 512])
        k.dma(gts, gtv[:, Qi * 4:(Qi + 1) * 4, :])
        k.dma(vis, visv[:, Qi * 4:(Qi + 1) * 4, :])
        k.dma(cadd, caddv[:, Qi * 4:(Qi + 1) * 4, :])
        k.dma(cmT, cmv[:, 0:NCT, t0:t0 + 512])
        cts = [ct for ct in range(NCT) if 16 * 128 * ct + 31 <= t0 + 511]
        pipeline((cmp_gen(Qa, h, h // 4, cts) for h in range(8)), 2)
        for g in range(2):
            k.tt(imp[:, :, g, :], imp[:, :, g, :], vis, ALU.mult)
            k.tt(imp[:, :, g, :], imp[:, :, g, :], cadd, ALU.add)
        for g in range(2):
            pipeline((topk_gen(g, s, g * 4 + s) for s in range(4)), 3)
            k.trs([(pTb[:, s, :], negm4[:, s, :]) for s in range(4)], ident)
            k.cp(Qa[64:128, 4 * g:4 * g + 4, :].rearrange("p h (s t) -> p h s t", s=4),
                 pTb[64:128, :, :].unsqueeze(1).to_broadcast([64, 4, 4, 128]))
        def all_gens():
            for h in range(8):
                g = h // 4
                kts = []
                for kt in range(4 * Qi + 4):
                    m = kt - 4 * Qi
                    if m >= 0:
                        kts.append((kt, 128 * m, 512, 128 * m, tri))
                    else:
                        kts.append((kt, 0, 512, 0, None))
                yield from attend_gens(Qa, h, g, kts, KA, VS1, 128, osel, False)
                kts = []
                for kt in range(max(0, 4 * Qi - 4), 4 * Qi + 4):
                    m = kt - 4 * Qi
                    if m >= 0:
                        kts.append((kt, 128 * m, 512, 128 * m, tri))
                    else:
                        kts.append((kt, 0, 128 * (m + 5), 128 * (m + 4), ntri))
                yield from attend_gens(Qa, h, g, kts, KW, VW1, 64, owin, True)
        pipeline(all_gens(), 5)
        if B_STAGE < 4:
            continue
        for br, ob in enumerate((ocmp, osel, owin)):
            k.tt(ob, ob, gts[:, :, br * 8:(br + 1) * 8].unsqueeze(3).to_broadcast([128, 4, 8, 64]),
                 ALU.mult, eng=("pool" if br == 1 else "dve"))
        k.tt(ocmp, ocmp, osel, ALU.add)
        k.tt(ocmp, ocmp, owin, ALU.add, eng="pool")
        if B_STAGE < 5:
            continue
        k.tt(osq, ocmp, ocmp, ALU.mult)
        k.rsum(oss, osq.rearrange("p s h d -> p (s h) d"))
        rms_rstd(k, ors, oss, 64)
        k.tt(ocmp.rearrange("p s h d -> p (s h) d"), ocmp.rearrange("p s h d -> p (s h) d"),
             ors.unsqueeze(2).to_broadcast([128, 32, 64]), ALU.mult)
        k.tt(onb.rearrange("p s (h d) -> p (s h) d", d=64), ocmp.rearrange("p s h d -> p (s h) d"),
             gout.unsqueeze(1).to_broadcast([128, 32, 64]), ALU.mult, eng="pool")
        if B_STAGE < 6:
            continue
        for s in range(4):
            k.trs([(pTb[:, c, :], onb[:, s, c * 128:(c + 1) * 128]) for c in range(4)], ident)
            k.cp(mixs, pTb)
            if B_STAGE >= 7:
                k.dma(mixv[:, 4:8, t0 + s * 128:t0 + (s + 1) * 128], mixs)


def load_colvec(k, dst, src, n_chunks, identf, pTf, tmp):
    k.dma(tmp[0:n_chunks, :], src.rearrange("(c p) -> c p", p=128))
    k.trs([(pTf[:, 0:n_chunks], tmp[0:n_chunks, :])], identf[0:n_chunks, 0:n_chunks])
    k.cp(dst, pTf[:, 0:n_chunks])


def phase_c(nc, k, T, x, MIXT, w_out, ffn_g, w_up, conv_w, conv_b, w_down, H2, ident, identf):
    TT = 256
    NTT = T // TT
    NF = 22
    wo = k.sb("c_wo", [128, 8, D], BF16)
    wu = k.sb("c_wu", [128, 8, 2 * DFF], BF16)
    wd = k.sb("c_wd", [128, NF, D], BF16)
    wov = w_out.rearrange("(c p) n -> p c n", p=128)
    wuv = w_up.rearrange("(c p) n -> p c n", p=128)
    wdv = w_down.rearrange("(c p) n -> p c n", p=128)
    for c in range(8):
        k.dma(wo[:, c, :], wov[:, c, :], q="pool")
    for c in range(8):
        for hh in range(2):
            k.dma(wu[:, c, hh * DFF:(hh + 1) * DFF], wuv[:, c, hh * DFF:(hh + 1) * DFF], q="pool")
    for c in range(NF):
        k.dma(wd[:, c, :], wdv[:, c, :], q="pool")
    pY = [k.ps("c_pY%d" % i, [128, 512]) for i in range(2)]
    pC = k.ps("c_pC", [128, 8, 128], BF16)
    pUs = [k.ps("c_pU%d" % i, [128, 256]) for i in range(4)]
    tmpv = k.sb("c_tmpv", [44, 128])
    gfT = k.sb("c_gfT", [128, 8])
    load_colvec(k, gfT, ffn_g, 8, identf, pY[0], tmpv)
    cw = k.sb("c_cw", [128, 3, 44])
    cb = k.sb("c_cb", [128, 44])
    for j in range(3):
        load_colvec(k, cw[:, j, :], conv_w[j], 44, identf, pY[0], tmpv)
    load_colvec(k, cb, conv_b, 44, identf, pY[0], tmpv)
    carry = k.sb("c_carry", [128, 44, 2])
    k.memset(carry, 0.0)

    mt = k.sb("c_mt", [128, 8, TT], BF16)
    hsb = k.sb("c_h", [128, 2, D])
    xt = k.sb("c_x", [128, 2, D])
    junk = k.sb("c_junk", [128, D], BF16)
    ss = k.sb("c_ss", [128, 1])
    rstd = k.sb("c_rstd", [128, 1])
    hn = k.sb("c_hn", [128, D], BF16)
    hnT = k.sb("c_hnT", [128, 8, TT], BF16)
    actT = k.sb("c_actT", [128, NF, TT], BF16)
    NU = 7
    uraw = [k.sb("c_uraw%d" % i, [128, TT + 2]) for i in range(NU)]
    acc = [k.sb("c_acc%d" % i, [128, TT]) for i in range(NU)]
    sil = [k.sb("c_sil%d" % i, [128, TT]) for i in range(4)]
    psl = pUs + [pY[0][:, 0:256], pY[1][:, 0:256]]
    mixv = MIXT.rearrange("(c p) t -> p c t", p=128)
    xv = x.rearrange("(n p) d -> p n d", p=128)
    h2v = H2.rearrange("(n p) d -> p n d", p=128)

    for it in range(NTT):
        t0 = it * TT
        k.dma(mt, mixv[:, :, t0:t0 + TT])
        k.dma(xt, xv[:, 2 * it:2 * it + 2, :])
        for s in range(2):
            for hh in range(2):
                py = pY[(s * 2 + hh) % 2]
                k.mm(py, [(mt[:, c, s * 128:(s + 1) * 128], wo[:, c, hh * 512:(hh + 1) * 512])
                          for c in range(8)])
                k.tt(hsb[:, s, hh * 512:(hh + 1) * 512], py, xt[:, s, hh * 512:(hh + 1) * 512], ALU.add)
            k.act(junk, hsb[:, s, :], AF.Square, accum_out=ss)
            rms_rstd(k, rstd, ss, D)
            k.act(hn, hsb[:, s, :], AF.Copy, scale=rstd)
            k.trs([(pC[:, c, :], hn[:, c * 128:(c + 1) * 128]) for c in range(8)], ident)
            k.tt(hnT[:, :, s * 128:(s + 1) * 128], pC, gfT.unsqueeze(2).to_broadcast([128, 8, 128]),
                 ALU.mult)
        def half_gen(n):
            i, gu = divmod(n, 2)
            ch = i + gu * NF
            pu = psl[n % 6]
            ur, ac = uraw[n % NU], acc[n % NU]
            k.mm(pu, [(wu[:, c, ch * 128:(ch + 1) * 128], hnT[:, c, :]) for c in range(8)])
            yield
            k.cp(ur[:, 0:2], carry[:, ch, :], eng="pool")
            k.cp(ur[:, 2:TT + 2], pu, eng="act")
            yield
            k.act(ac, pu, AF.Identity, scale=cw[:, 2, ch:ch + 1], bias=cb[:, ch:ch + 1])
            yield
            k.stt(ac, ur[:, 1:TT + 1], cw[:, 1, ch:ch + 1], ac, ALU.mult, ALU.add)
            yield
            k.stt(ac, ur[:, 0:TT], cw[:, 0, ch:ch + 1], ac, ALU.mult, ALU.add)
            k.cp(carry[:, ch, :], ur[:, TT:TT + 2], eng="pool")
            yield
            if gu == 1:
                sl = sil[i % 4]
                k.act(sl, acc[(n - 1) % NU], AF.Silu)
                yield
                k.tt(actT[:, i, :], sl, ac, ALU.mult, eng="pool")
                yield
        pipeline((half_gen(n) for n in range(2 * NF)), 6)
        for s in range(2):
            for hh in range(2):
                py = pY[(s * 2 + hh) % 2]
                k.mm(py, [(actT[:, i, s * 128:(s + 1) * 128], wd[:, i, hh * 512:(hh + 1) * 512])
                          for i in range(NF)])
                k.tt(hsb[:, s, hh * 512:(hh + 1) * 512], py, hsb[:, s, hh * 512:(hh + 1) * 512], ALU.add)
        k.dma(h2v[:, 2 * it:2 * it + 2, :], hsb)


def phase_d(nc, k, T, H2, p_in, pleg_g, w_pleg, w_ple, ple_g, out, ident, identf):
    NT = T // 128
    wg = k.sb("d_wg", [128, 8, D], BF16)
    wp = k.sb("d_wp", [128, 2, D], BF16)
    wgv = w_pleg.rearrange("(c p) n -> p c n", p=128)
    wpv = w_ple.rearrange("(c p) n -> p c n", p=128)
    for c in range(8):
        k.dma(wg[:, c, :], wgv[:, c, :], q="pool")
    for c in range(2):
        k.dma(wp[:, c, :], wpv[:, c, :], q="pool")
    pY = [k.ps("d_pY%d" % i, [128, 512]) for i in range(2)]
    pE4 = [k.ps("d_pE%d" % i, [128, 512]) for i in range(4)]
    pC = k.ps("d_pC", [128, 8, 128], BF16)
    tmpv = k.sb("d_tmpv", [8, 128])
    ggT = k.sb("d_ggT", [128, 8])
    load_colvec(k, ggT, pleg_g, 8, identf, pY[0], tmpv)
    gple = k.sb("d_gple", [128, D])
    k.dma(gple, ple_g.partition_broadcast(128))

    hb = [k.sb("d_h%d" % i, [128, D]) for i in range(2)]
    pb = [k.sb("d_p%d" % i, [128, 256]) for i in range(2)]
    junks = [k.sb("d_junk%d" % i, [128, D], BF16) for i in range(2)]
    sss = [k.sb("d_ss%d" % i, [128, 1]) for i in range(2)]
    rstds = [k.sb("d_rstd%d" % i, [128, 1]) for i in range(2)]
    ss2s = [k.sb("d_ss2%d" % i, [128, 2]) for i in range(2)]
    rstd2s = [k.sb("d_rstd2%d" % i, [128, 1]) for i in range(2)]
    hns = [k.sb("d_hn%d" % i, [128, D], BF16) for i in range(2)]
    hTs = [k.sb("d_hT%d" % i, [128, 8, 128], BF16) for i in range(2)]
    pbfs = [k.sb("d_pbf%d" % i, [128, 256], BF16) for i in range(2)]
    pTs = [k.sb("d_pT%d" % i, [128, 2, 128], BF16) for i in range(2)]
    gates = [k.sb("d_gate%d" % i, [128, D]) for i in range(2)]
    es = [k.sb("d_e%d" % i, [128, D]) for i in range(2)]
    ob = [k.sb("d_o%d" % i, [128, D]) for i in range(2)]

    def d_gen(it):
        t0 = it * 128
        b = it % 2
        h, pp, o = hb[b], pb[b], ob[b]
        junk, ss, rstd, ss2, rstd2 = junks[b], sss[b], rstds[b], ss2s[b], rstd2s[b]
        hn, hT, pbf, pT, gate, e = hns[b], hTs[b], pbfs[b], pTs[b], gates[b], es[b]
        pE = pE4[2 * b:2 * b + 2]
        k.dma(h, H2[t0:t0 + 128, :])
        k.dma(pp, p_in[t0:t0 + 128, :])
        yield
        k.act(junk, h, AF.Square, accum_out=ss)
        yield
        k.act(rstd, ss, AF.Ln, scale=1.0 / D, bias=EPS)
        yield
        k.act(rstd, rstd, AF.Exp, scale=-0.5)
        yield
        k.act(hn, h, AF.Copy, scale=rstd)
        yield
        k.trs([(pC[:, c, :], hn[:, c * 128:(c + 1) * 128]) for c in range(8)], ident)
        yield
        k.tt(hT, pC, ggT.unsqueeze(2).to_broadcast([128, 8, 128]), ALU.mult)
        yield
        k.cp(pbf, pp, eng="pool")
        yield
        k.trs([(pC[:, c, :], pbf[:, c * 128:(c + 1) * 128]) for c in range(2)], ident)
        yield
        k.cp(pT, pC[:, 0:2, :])
        yield
        for hh in range(2):
            k.mm(pY[hh], [(hT[:, c, :], wg[:, c, hh * 512:(hh + 1) * 512]) for c in range(8)])
            yield
            k.act(gate[:, hh * 512:(hh + 1) * 512], pY[hh], AF.Sigmoid)
            yield
        for hh in range(2):
            k.mm(pE[hh], [(pT[:, c, :], wp[:, c, hh * 512:(hh + 1) * 512]) for c in range(2)])
            yield
            k.act(junk[:, hh * 512:(hh + 1) * 512], pE[hh], AF.Square, accum_out=ss2[:, hh:hh + 1])
            yield
        k.tt(ss, ss2[:, 0:1], ss2[:, 1:2], ALU.add)
        yield
        k.act(rstd2, ss, AF.Ln, scale=1.0 / D, bias=EPS)
        yield
        k.act(rstd2, rstd2, AF.Exp, scale=-0.5)
        yield
        for hh in range(2):
            k.act(e[:, hh * 512:(hh + 1) * 512], pE[hh], AF.Copy, scale=rstd2)
            yield
        k.tt(e, e, gple, ALU.mult)
        yield
        k.tt(e, e, gate, ALU.mult, eng="pool")
        yield
        k.tt(o, e, h, ALU.add)
        yield
        tok = k.dma(out[t0:t0 + 128, :], o)
        k.out_toks.append(tok)
        yield

    pipeline((d_gen(it) for it in range(NT)), D_DEPTH)


def _consts(T):
    bf = ml_dtypes.bfloat16
    c = {}
    c["c_ident"] = np.eye(128).astype(bf)
    c["c_identf"] = np.eye(128, dtype=np.float32)
    half = 8
    inv = np.float32(500000.0) ** (-np.arange(half, dtype=np.float32) / half)
    ang = np.arange(T, dtype=np.float32)[:, None] * inv[None, :].astype(np.float32)
    c["c_rope"] = np.concatenate([np.cos(ang), np.sin(ang)], 1).astype(np.float32)
    s = np.arange(128)
    c["c_tri2"] = ((s[:, None] <= s[None, :]) & (s[:, None] // 64 == s[None, :] // 64)).astype(np.float32)
    c["c_chunk"] = (s[:, None] // 64 == np.arange(2)[None, :]).astype(np.float32)
    t = np.arange(T)
    cc = np.arange(256)
    ncmp = T // 16 - 1
    c["c_cmask"] = (((16 * cc[:, None] + 31) <= t[None, :]) & (cc[:, None] < ncmp)).astype(bf)
    c["c_tri"] = (s[:, None] <= s[None, :]).astype(bf)
    c["c_ntri"] = (s[None, :] < s[:, None]).astype(bf)
    n = np.arange(64)
    c["c_E"] = ((t[None, :] // 64) == n[:, None]).astype(bf)
    cur = t // 64
    vis = (n[None, :] * 64 <= t[:, None])
    bonus = np.zeros((T, 64), np.float32)
    bonus += (n[None, :] == 0) * 1.0e6
    bonus += (n[None, :] == cur[:, None]) * 2.0e6
    bonus += (n[None, :] == cur[:, None] - 1) * 4.0e6
    c["c_vis"] = vis.astype(np.float32)
    c["c_cadd"] = np.where(vis, bonus, np.float32(-1e30)).astype(np.float32)
    cs = cc * 16
    ssb = n * 64
    ov = np.clip(np.minimum(cs[:, None] + 32, ssb[None, :] + 64)
                 - np.maximum(cs[:, None], ssb[None, :]), 0, None) / 32.0
    o1 = np.zeros((256, 65), np.float32)
    o1[:, 0] = 1.0
    o1[:, 1:] = ov
    o1[ncmp:] = 0.0
    c["c_ovl1"] = o1.astype(bf)
    return c


_W_NAMES = ["attn_norm_g", "w_in", "hg_norm_g", "nsa_q_norm_g", "nsa_k_norm_g", "cmp_pe", "cmp_w1",
            "cmp_w2", "nsa_out_norm_g", "w_out", "ffn_norm_g", "w_up", "conv_w", "conv_b", "w_down",
            "ple_gate_norm_g", "w_ple_gate", "w_ple", "ple_norm_g"]


def kernel(**inputs):
    x = np.asarray(inputs["x"], np.float32)
    p = np.asarray(inputs["p"], np.float32)
    B, T, _ = x.shape
    nc = build(T=T, dbg=False, phases="ABCD")
    shared = {n: np.ascontiguousarray(np.asarray(inputs[n], np.float32)[0]) for n in _W_NAMES}
    shared["hg_lb_logits"] = np.ascontiguousarray(np.asarray(inputs["hg_lb_logits"], np.float32))
    shared.update(_consts(T))
    in_maps = []
    for b in range(B):
        m = dict(shared)
        m["x"] = np.ascontiguousarray(x[b])
        m["p"] = np.ascontiguousarray(p[0, b])
        in_maps.append(m)
    res = run_bass_kernel_spmd(nc, in_maps, core_ids=list(range(B)))
    return np.stack([np.asarray(r["out"], np.float32) for r in res.results], axis=0)
```

```python
from contextlib import ExitStack
import numpy as np
import ml_dtypes
import concourse.bass as bass
import concourse.mybir as mybir
from concourse.bass_utils import run_bass_kernel_spmd

F32 = mybir.dt.float32
BF16 = mybir.dt.bfloat16
ALU = mybir.AluOpType
AF = mybir.ActivationFunctionType
AX = mybir.AxisListType

D = 1024
IN_TOTAL = 3352
DFF = 2816
EPS = 1e-6
NEGM = -30000.0
B_STAGE = 9
FFN_DEPTH = 2
D_DEPTH = 2
PRO_DEPTH = 2


class Sched:
    def __init__(self, nc, n_dma_sems=48):
        self.nc = nc
        self.engs = {"pe": nc.tensor, "act": nc.scalar, "dve": nc.vector,
                     "pool": nc.gpsimd, "sp": nc.sync}
        self.sem = {}
        self.cnt = {}
        for k in ("pe", "act", "dve", "pool"):
            self.sem[k] = nc.alloc_semaphore("s_" + k)
            self.cnt[k] = 0
        self.dma_sems = [nc.alloc_semaphore("s_dma%d" % i) for i in range(n_dma_sems)]
        self.dma_val = [0] * n_dma_sems
        self.dma_rr = 0
        self.waited = {}
        self.last_w = {}
        self.readers = {}
        self.nwaits = 0
        self.inflight = {}
        self.max_desc = 600

    def _wait(self, eng, tok):
        sem, val, key = tok
        if key == eng and eng == "pe":
            return
        k = (eng, key)
        if self.waited.get(k, 0) >= val:
            return
        self.engs[eng].wait_ge(sem, val)
        self.nwaits += 1
        self.waited[k] = val

    def deps(self, eng, reads, writes):
        toks = []
        for r in reads:
            t = self.last_w.get(r)
            if t is not None:
                toks.append(t)
        for w in writes:
            t = self.last_w.get(w)
            if t is not None:
                toks.append(t)
            toks.extend(self.readers.get(w, ()))
        for t in toks:
            self._wait(eng, t)

    def commit(self, tok, reads, writes):
        for w in writes:
            self.last_w[w] = tok
            self.readers[w] = []
        for r in reads:
            if r in writes:
                continue
            lst = self.readers.setdefault(r, [])
            lst[:] = [t for t in lst if t[2] != tok[2]]
            lst.append(tok)

    def op(self, eng, reads, writes, fn):
        self.deps(eng, reads, writes)
        ins = fn(self.engs[eng])
        self.cnt[eng] += 1
        ins.then_inc(self.sem[eng], 1)
        tok = (self.sem[eng], self.cnt[eng], eng)
        self.commit(tok, reads, writes)
        return tok

    @staticmethod
    def _ndesc(ap):
        dims = list(ap.ap)
        total = 1
        for st, n in dims:
            total *= n
        run = 1
        for st, n in reversed(dims[1:]):
            if st == run:
                run *= n
            else:
                break
        return max(1, total // max(run, 1))

    def dma(self, out, in_, reads, writes, q="sp", **kw):
        nd = max(self._ndesc(out), self._ndesc(in_))
        fifo = self.inflight.setdefault(q, [])
        while fifo and sum(d for _, d in fifo) + nd > self.max_desc:
            tok0, _ = fifo.pop(0)
            self._wait(q, tok0)
        tok = self._dma(out, in_, reads, writes, q, **kw)
        fifo.append((tok, nd))
        return tok

    def _dma(self, out, in_, reads, writes, q="sp", **kw):
        i = self.dma_rr
        self.dma_rr = (self.dma_rr + 1) % len(self.dma_sems)
        sem = self.dma_sems[i]
        key = "dma%d" % i
        if self.dma_val[i] > 0:
            self._wait(q, (sem, self.dma_val[i], key))
        self.deps(q, reads, writes)
        self.dma_val[i] += 16
        self.engs[q].dma_start(out=out, in_=in_, **kw).then_inc(sem, 16)
        tok = (sem, self.dma_val[i], key)
        self.commit(tok, reads, writes)
        return tok


_TAGS = {}
_KEEP = []


def tag(ap, name):
    _TAGS[id(ap)] = name
    _KEEP.append(ap)
    return ap


def sub(ap, fn):
    r = fn(ap)
    if id(ap) in _TAGS:
        tag(r, _TAGS[id(ap)])
    return r


def pipeline(gens, depth):
    gens = iter(gens)
    active = []
    exhausted = False
    while True:
        if not exhausted and len(active) < depth:
            g = next(gens, None)
            if g is None:
                exhausted = True
            else:
                active.append(g)
        if not active:
            if exhausted:
                break
            continue
        for g in list(active):
            try:
                next(g)
            except StopIteration:
                active.remove(g)


def _names(aps):
    out = []
    for a in aps:
        if a is None or isinstance(a, (int, float)):
            continue
        n = _TAGS.get(id(a)) or a.name
        if n not in out:
            out.append(n)
    return out


class K:
    def __init__(self, nc):
        self.nc = nc
        self.S = Sched(nc)
        self.out_toks = []
        self.es = None

    def sb(self, name, shape, dt=F32):
        return self.es.enter_context(self.nc.sbuf_tensor(name, list(shape), dt))[:]

    def ps(self, name, shape, dt=F32):
        return self.es.enter_context(self.nc.psum_tensor(name, list(shape), dt))[:]

    def barrier(self):
        S = self.S
        toks = [(S.sem[e], S.cnt[e], e) for e in ("pe", "act", "dve", "pool") if S.cnt[e] > 0]
        toks += [(S.dma_sems[i], S.dma_val[i], "dma%d" % i) for i in range(len(S.dma_sems))
                 if S.dma_val[i] > 0]
        for eng in ("sp", "pe", "act", "dve", "pool"):
            for t in toks:
                S._wait(eng, t)
        S.last_w.clear()
        S.readers.clear()

    def mm1(self, out, lhsT, rhs, start, stop):
        return self.S.op("pe", _names([lhsT, rhs]), _names([out]),
                         lambda e: e.matmul(out, lhsT=lhsT, rhs=rhs, start=start, stop=stop))

    def vmax(self, out, in_):
        return self.S.op("dve", _names([in_]), _names([out]), lambda e: e.max(out=out, in_=in_))

    def vmatch(self, out, mx, vals, imm):
        return self.S.op("dve", _names([mx, vals]), _names([out]),
                         lambda e: e.match_replace(out=out, in_to_replace=mx, in_values=vals,
                                                   imm_value=imm))

    def recip(self, out, in_):
        return self.S.op("dve", _names([in_]), _names([out]), lambda e: e.reciprocal(out=out, in_=in_))

    def act(self, out, in_, func, bias=None, scale=None, accum_out=None, eng="act"):
        kw = {}
        if bias is not None:
            kw["bias"] = bias
        if scale is not None:
            kw["scale"] = scale
        if accum_out is not None:
            kw["accum_out"] = accum_out
        rd = _names([in_, bias, scale])
        wr = _names([out, accum_out])
        return self.S.op(eng, rd, wr, lambda e: e.activation(out=out, in_=in_, func=func, **kw))

    def tt(self, out, in0, in1, op, eng="dve"):
        return self.S.op(eng, _names([in0, in1]), _names([out]),
                         lambda e: e.tensor_tensor(out=out, in0=in0, in1=in1, op=op))

    def ts(self, out, in0, s1, s2, op0, op1=None, eng="dve"):
        def f(e):
            if op1 is None:
                return e.tensor_scalar(out=out, in0=in0, scalar1=s1, scalar2=None, op0=op0)
            return e.tensor_scalar(out=out, in0=in0, scalar1=s1, scalar2=s2, op0=op0, op1=op1)
        return self.S.op(eng, _names([in0, s1, s2]), _names([out]), f)

    def stt(self, out, in0, scalar, in1, op0, op1, eng="dve"):
        return self.S.op(eng, _names([in0, scalar, in1]), _names([out]),
                         lambda e: e.scalar_tensor_tensor(out=out, in0=in0, scalar=scalar, in1=in1,
                                                          op0=op0, op1=op1))

    def cp(self, out, in_, eng="dve"):
        if eng == "act":
            return self.S.op("act", _names([in_]), _names([out]), lambda e: e.copy(out=out, in_=in_))
        return self.S.op(eng, _names([in_]), _names([out]), lambda e: e.tensor_copy(out=out, in_=in_))

    def rsum(self, out, in_, eng="dve"):
        return self.S.op(eng, _names([in_]), _names([out]),
                         lambda e: e.reduce_sum(out=out, in_=in_, axis=AX.X))

    def memset(self, out, val, eng="dve"):
        return self.S.op(eng, [], _names([out]), lambda e: e.memset(out, val))

    def mm(self, out, pairs, extra_w=()):
        rd = _names([a for p in pairs for a in p])
        n = len(pairs)

        def f(e):
            for i, (l, r) in enumerate(pairs):
                ins = e.matmul(out, lhsT=l, rhs=r, start=(i == 0), stop=(i == n - 1))
            return ins
        return self.S.op("pe", rd, _names([out]) + list(extra_w), f)

    def mms(self, groups):
        rd, wr = [], []
        for out, pairs in groups:
            wr += _names([out])
            rd += _names([a for p in pairs for a in p])

        def f(e):
            for out, pairs in groups:
                n = len(pairs)
                for i, (l, r) in enumerate(pairs):
                    ins = e.matmul(out, lhsT=l, rhs=r, start=(i == 0), stop=(i == n - 1))
            return ins
        return self.S.op("pe", list(dict.fromkeys(rd)), list(dict.fromkeys(wr)), f)

    def trs(self, items, ident):
        rd = _names([i for _, i in items] + [ident])
        wr = _names([o for o, _ in items])

        def f(e):
            for o, i in items:
                ins = e.transpose(out=o, in_=i, identity=ident)
            return ins
        return self.S.op("pe", rd, wr, f)

    def dma(self, out, in_, q="sp", **kw):
        return self.S.dma(out, in_, _names([in_]), _names([out]), q=q, **kw)

    def finish(self):
        for t in self.out_toks:
            self.S._wait("sp", t)


def rms_rstd(k, out, ss, n):
    k.act(out, ss, AF.Ln, scale=1.0 / n, bias=EPS)
    k.act(out, out, AF.Exp, scale=-0.5)


def build(T=4096, dbg=False, phases="ABCD"):
    NT = T // 128
    nc = bass.Bass("TRN2", target_bir_lowering=False)
    k = K(nc)

    def din(name, shape, dt=F32):
        return nc.dram_tensor(name, list(shape), dt, kind="ExternalInput").ap()

    def dscr(name, shape, dt):
        return nc.dram_tensor(name, list(shape), dt, kind=("ExternalOutput" if dbg else "Internal")).ap()

    x = din("x", [T, D])
    p_in = din("p", [T, 256])
    attn_g = din("attn_norm_g", [D])
    w_in = din("w_in", [D, IN_TOTAL])
    lb_logits = din("hg_lb_logits", [2, 512])
    hg_norm_g = din("hg_norm_g", [128])
    q_norm_g = din("nsa_q_norm_g", [64])
    k_norm_g = din("nsa_k_norm_g", [3, 64])
    cmp_pe = din("cmp_pe", [2, 32, 64])
    cmp_w1 = din("cmp_w1", [2, 2048, 128])
    cmp_w2 = din("cmp_w2", [2, 128, 64])
    out_norm_g = din("nsa_out_norm_g", [64])
    w_out = din("w_out", [D, D])
    ffn_g = din("ffn_norm_g", [D])
    w_up = din("w_up", [D, 2 * DFF])
    conv_w = din("conv_w", [3, 2 * DFF])
    conv_b = din("conv_b", [2 * DFF])
    w_down = din("w_down", [DFF, D])
    pleg_g = din("ple_gate_norm_g", [D])
    w_pleg = din("w_ple_gate", [D, D])
    w_ple = din("w_ple", [256, D])
    ple_g = din("ple_norm_g", [D])
    c_ident = din("c_ident", [128, 128], BF16)
    c_rope = din("c_rope", [T, 16])
    c_tri2 = din("c_tri2", [128, 128])
    c_chunk = din("c_chunk", [128, 2])

    out = nc.dram_tensor("out", [T, D], F32, kind="ExternalOutput").ap()

    FT = dscr("FT", [16, 64, T], BF16)
    VT = dscr("VT", [T, 256], BF16)
    GT = dscr("GT", [T, 24], F32)
    MIXT = dscr("MIXT", [D, T], BF16)

    c_identf = din("c_identf", [128, 128])
    c_cmask = din("c_cmask", [256, T], BF16)
    c_tri = din("c_tri", [128, 128], BF16)
    c_ntri = din("c_ntri", [128, 128], BF16)
    c_E = din("c_E", [64, T], BF16)
    c_vis = din("c_vis", [T, 64])
    c_cadd = din("c_cadd", [T, 64])
    c_ovl1 = din("c_ovl1", [256, 65], BF16)
    H2 = dscr("H2", [T, D], F32)

    with ExitStack() as es0:
        k.es = es0
        ident = k.sb("ident", [128, 128], BF16)
        k.dma(ident, c_ident)
        identf = k.sb("identf", [128, 128])
        k.dma(identf, c_identf)
        if "A" in phases:
            with ExitStack() as es:
                k.es = es
                phase_a(nc, k, T, NT, x, attn_g, w_in, lb_logits, hg_norm_g, q_norm_g, k_norm_g,
                        c_rope, c_tri2, c_chunk, ident, FT, VT, GT, MIXT)
                k.barrier()
        if "B" in phases:
            with ExitStack() as es:
                k.es = es
                phase_b(nc, k, T, NT, FT, VT, GT, MIXT, k_norm_g, cmp_pe, cmp_w1, cmp_w2, out_norm_g,
                        c_rope, c_cmask, c_tri, c_ntri, c_E, c_vis, c_cadd, c_ovl1, ident, identf)
                k.barrier()
        if "A" not in phases and dbg:
            mi = din("MIXT_in", [D, T], BF16)
            with ExitStack() as es:
                k.es = es
                tb = k.sb("dbg_mix", [128, 8, T], BF16)
                k.dma(tb, mi.rearrange("(c p) t -> p c t", p=128))
                k.dma(MIXT.rearrange("(c p) t -> p c t", p=128), tb)
                k.barrier()
        if "C" in phases:
            with ExitStack() as es:
                k.es = es
                phase_c(nc, k, T, x, MIXT, w_out, ffn_g, w_up, conv_w, conv_b, w_down, H2, ident, identf)
                k.barrier()
        if "D" in phases:
            with ExitStack() as es:
                k.es = es
                phase_d(nc, k, T, H2, p_in, pleg_g, w_pleg, w_ple, ple_g, out, ident, identf)
                k.barrier()
        k.finish()
    return nc


def phase_a(nc, k, T, NT, x, attn_g, w_in, lb_logits, hg_norm_g, q_norm_g, k_norm_g,
            c_rope, c_tri2, c_chunk, ident, FT, VT, GT, MIXT):
    S = k.S
    w_sb = k.sb("a_w", [128, 8, IN_TOTAL], BF16)
    w_v = w_in.rearrange("(c p) n -> p c n", p=128)
    for c in range(8):
        k.dma(w_sb[:, c, :], w_v[:, c, :], q="pool")
    gT = k.sb("a_gT", [128, 8])
    k.dma(gT, attn_g.rearrange("(c p) -> p c", p=128), allow_slow_non_contiguous=True)
    rope = k.sb("a_rope", [128, NT, 16])
    k.dma(rope, c_rope.rearrange("(n p) c -> p n c", p=128))
    tri2 = k.sb("a_tri2", [128, 128])
    k.dma(tri2, c_tri2)
    chunk = k.sb("a_chunk", [128, 2])
    k.dma(chunk, c_chunk)
    l0 = k.sb("a_l0", [128, 512])
    l1 = k.sb("a_l1", [128, 512])
    k.dma(l0, lb_logits[0].partition_broadcast(128))
    k.dma(l1, lb_logits[1].partition_broadcast(128))
    lb = k.sb("a_lb", [128, 512])
    oml = k.sb("a_oml", [128, 512])
    k.tt(l0, l0, l1, ALU.subtract)
    k.act(lb, l0, AF.Sigmoid)
    k.ts(oml, lb, -1.0, 1.0, ALU.mult, ALU.add)
    gq = k.sb("a_gq", [128, 12, 64])
    for h in range(12):
        src = q_norm_g if h < 8 else (k_norm_g[1] if h < 10 else k_norm_g[2])
        k.dma(gq[:, h, :], src.partition_broadcast(128))
    ghg = k.sb("a_ghg", [128, 4, 128])
    for h in range(4):
        k.dma(ghg[:, h, :], hg_norm_g.partition_broadcast(128))
    Sf = k.sb("a_Sf", [128, 4, 128])
    Sb = [k.sb("a_Sb%d" % i, [128, 4, 128], BF16) for i in range(3)]
    k.memset(Sf, 0.0)
    k.memset(Sb[0], 0.0, eng="pool")

    xt = [k.sb("a_x%d" % i, [128, D]) for i in range(2)]
    junk = k.sb("a_junk", [128, D], BF16)
    ss = k.sb("a_ss", [128, 1])
    rstd = k.sb("a_rstd", [128, 1])
    xn = k.sb("a_xn", [128, D], BF16)
    xnT = k.sb("a_xnT", [128, 8, 128], BF16)
    silq = k.sb("a_silq", [128, 512])
    sig = k.sb("a_sig", [128, 512])
    logf = k.sb("a_logf", [128, 512])
    kk = k.sb("a_kk", [128, 512])
    enb = k.sb("a_enb", [128, 512])
    epb = k.sb("a_epb", [128, 512])
    kp = k.sb("a_kp", [128, 512], BF16)
    qp = k.sb("a_qp", [128, 512], BF16)
    vb = k.sb("a_vb", [128, 512], BF16)
    sg = k.sb("a_sg", [128, 512])
    ebl = k.sb("a_ebl", [128, 4, 2])
    qkT = k.sb("a_qkT", [128, 8, 128], BF16)
    ATm = k.sb("a_ATm", [128, 4, 128], BF16)
    tmpS = k.sb("a_tmpS", [128, 4, 128])
    osb = k.sb("a_osb", [128, 4, 128])
    osq = k.sb("a_osq", [128, 4, 128])
    ss4 = k.sb("a_ss4", [128, 4])
    rstd4 = k.sb("a_rstd4", [128, 4])
    onb = k.sb("a_onb", [128, 4, 128], BF16)
    mixs = k.sb("a_mixs", [128, 4, 128], BF16)
    qk = k.sb("a_qk", [128, 12, 64])
    qsq = k.sb("a_qsq", [128, 12, 64])
    ss12 = k.sb("a_ss12", [128, 12])
    rstd12 = k.sb("a_rstd12", [128, 12])
    qkb = k.sb("a_qkb", [128, 12, 64], BF16)
    r_a = k.sb("a_ra", [128, 12, 8])
    r_b = k.sb("a_rb", [128, 12, 8])
    r_c = k.sb("a_rc", [128, 12, 8])
    r_d = k.sb("a_rd", [128, 12, 8])
    kcvc = k.sb("a_kcvc", [128, 256], BF16)
    T16 = k.sb("a_T16", [64, 16, 128], BF16)
    vv = k.sb("a_vv", [128, 256], BF16)
    gsb = k.sb("a_gsb", [128, 24])

    pA = [k.ps("a_pA%d" % i, [128, 512]) for i in range(2)]
    pB = k.ps("a_pB", [128, 512])
    pC = k.ps("a_pC", [128, 8, 128], BF16)
    pD = k.ps("a_pD", [64, 16, 128], BF16)
    pF = k.ps("a_pF", [128, 4, 128])
    pG = k.ps("a_pG", [128, 4, 128])

    cols = [(0, 512), (512, 512), (1024, 512), (1536, 512), (2048, 512), (2560, 512), (3072, 280)]
    mixv = MIXT.rearrange("(c p) t -> p c t", p=128)
    ftv = FT.rearrange("n d t -> d n t")
    xnT2 = [xnT, k.sb("a_xnT1", [128, 8, 128], BF16)]

    def proj(g, dst, xT):
        c0, n = cols[g]
        k.mm(dst[:, 0:n], [(xT[:, c, :], w_sb[:, c, c0:c0 + n]) for c in range(8)])
        return dst

    def genH(it):
        t0 = it * 128
        xb = xt[it % 2]
        xT = xnT2[it % 2]
        pAh = pA[0]
        if it == 0:
            k.dma(xb, x[t0:t0 + 128, :])
        if it + 1 < NT:
            k.dma(xt[(it + 1) % 2], x[t0 + 128:t0 + 256, :])
        yield
        k.act(junk, xb, AF.Square, accum_out=ss)
        yield
        k.act(rstd, ss, AF.Ln, scale=1.0 / D, bias=EPS)
        yield
        k.act(rstd, rstd, AF.Exp, scale=-0.5)
        yield
        k.act(xn, xb, AF.Copy, scale=rstd)
        yield
        k.trs([(pC[:, c, :], xn[:, c * 128:(c + 1) * 128]) for c in range(8)], ident)
        yield
        k.tt(xT, pC, gT.unsqueeze(2).to_broadcast([128, 8, 128]), ALU.mult)
        yield
        d = proj(1, pAh, xT)
        yield
        k.act(sig, d, AF.Sigmoid)
        yield
        k.tt(sig, sig, oml, ALU.mult)
        yield
        k.tt(sig, sig, lb, ALU.add)
        yield
        k.act(logf, sig, AF.Ln)
        yield
        k.ts(kk, sig, -1.0, 1.0, ALU.mult, ALU.add)
        yield
        k.mm(pB, [(tri2, logf)])
        yield
        k.act(enb, pB, AF.Exp, scale=-1.0)
        yield
        k.act(epb, pB, AF.Exp)
        yield
        k.tt(kp, kk, enb, ALU.mult)
        yield
        k.mms([(pF[:, h, 0:2], [(logf[:, h * 128:(h + 1) * 128], chunk)]) for h in range(4)])
        yield
        k.act(ebl, pF[:, :, 0:2], AF.Exp)
        yield
        d = proj(0, pAh, xT)
        yield
        k.act(silq, d, AF.Silu)
        yield
        k.stt(qp, silq, 128 ** -0.5, epb, ALU.mult, ALU.mult)
        yield
        d = proj(2, pAh, xT)
        yield
        k.cp(vb, d, eng="act")
        yield
        d = proj(3, pAh, xT)
        yield
        k.act(sg, d, AF.Silu)
        yield
        k.tt(sg, sg, ghg.rearrange("p h v -> p (h v)"), ALU.mult, eng="pool")
        yield
        k.trs([(pC[:, h, :], qp[:, h * 128:(h + 1) * 128]) for h in range(4)] +
              [(pC[:, 4 + h, :], kp[:, h * 128:(h + 1) * 128]) for h in range(4)], ident)
        yield
        k.cp(qkT, pC)
        yield
        S0, S1, S2 = Sb[(2 * it) % 3], Sb[(2 * it + 1) % 3], Sb[(2 * it + 2) % 3]
        k.mms([(pF[:, h, :], [(qkT[:, 4 + h, :], qkT[:, h, :])]) for h in range(4)])
        yield
        k.tt(ATm, pF, tri2.unsqueeze(1).to_broadcast([128, 4, 128]), ALU.mult)
        yield
        for c in range(2):
            rs = slice(c * 64, (c + 1) * 64)
            k.mms([(pF[:, h, :], [(kp[rs, h * 128:(h + 1) * 128], vb[rs, h * 128:(h + 1) * 128])])
                   for h in range(4)])
            yield
            k.tt(tmpS, pF, Sf, ALU.add)
            yield
            k.tt(Sf, tmpS, ebl[:, :, c:c + 1].to_broadcast([128, 4, 128]), ALU.mult)
            yield
            k.cp(S1 if c == 0 else S2, Sf, eng="pool")
            yield
        groups = []
        for h in range(4):
            for c in range(2):
                rs = slice(c * 64, (c + 1) * 64)
                Sc = S0 if c == 0 else S1
                groups.append((pG[rs, h, :], [(ATm[rs, h, rs], vb[rs, h * 128:(h + 1) * 128]),
                                              (qkT[:, h, rs], Sc[:, h, :])]))
        k.mms(groups)
        yield
        k.cp(osb, pG, eng="act")
        yield
        k.tt(osq, osb, osb, ALU.mult)
        yield
        k.rsum(ss4, osq)
        yield
        k.act(rstd4, ss4, AF.Ln, scale=1.0 / 128, bias=EPS)
        yield
        k.act(rstd4, rstd4, AF.Exp, scale=-0.5)
        yield
        k.tt(osb, osb, rstd4.unsqueeze(2).to_broadcast([128, 4, 128]), ALU.mult)
        yield
        k.tt(onb, osb, sg.rearrange("p (h v) -> p h v", h=4), ALU.mult)
        yield
        k.trs([(pC[:, h, :], onb[:, h, :]) for h in range(4)], ident)
        yield
        k.cp(mixs, pC[:, 0:4, :])
        yield
        k.dma(mixv[:, 0:4, t0:t0 + 128], mixs)
        yield

    def genN(it):
        t0 = it * 128
        xT = xnT2[it % 2]
        pAn = pA[1]
        d = proj(4, pAn, xT)
        yield
        k.cp(qk[:, 0:8, :], d.rearrange("p (h d) -> p h d", d=64), eng="act")
        yield
        d = proj(5, pAn, xT)
        yield
        k.cp(kcvc, d[:, 0:256], eng="act")
        yield
        k.cp(qk[:, 8:10, :], d[:, 256:384].rearrange("p (h d) -> p h d", d=64), eng="act")
        yield
        k.cp(vv[:, 0:128], d[:, 384:512], eng="act")
        yield
        d = proj(6, pAn, xT)
        yield
        k.cp(qk[:, 10:12, :], d[:, 0:128].rearrange("p (h d) -> p h d", d=64), eng="act")
        yield
        k.cp(vv[:, 128:256], d[:, 128:256], eng="act")
        yield
        k.act(gsb, d[:, 256:280], AF.Sigmoid)
        yield
        k.dma(GT[t0:t0 + 128, :], gsb)
        k.dma(VT[t0:t0 + 128, :], vv)
        yield
        k.tt(qsq, qk, qk, ALU.mult, eng="pool")
        yield
        k.rsum(ss12, qsq)
        yield
        k.act(rstd12, ss12, AF.Ln, scale=1.0 / 64, bias=EPS)
        yield
        k.act(rstd12, rstd12, AF.Exp, scale=-0.5)
        yield
        k.tt(qk, qk, rstd12.unsqueeze(2).to_broadcast([128, 12, 64]), ALU.mult)
        yield
        k.tt(qk, qk, gq, ALU.mult, eng="pool")
        yield
        cosb = rope[:, it:it + 1, 0:8].to_broadcast([128, 12, 8])
        sinb = rope[:, it:it + 1, 8:16].to_broadcast([128, 12, 8])
        k.tt(r_a, qk[:, :, 0:8], cosb, ALU.mult, eng="pool")
        yield
        k.tt(r_b, qk[:, :, 8:16], sinb, ALU.mult, eng="pool")
        yield
        k.tt(r_c, qk[:, :, 8:16], cosb, ALU.mult, eng="pool")
        yield
        k.tt(r_d, qk[:, :, 0:8], sinb, ALU.mult, eng="pool")
        yield
        k.cp(qkb, qk, eng="pool")
        yield
        k.tt(qkb[:, :, 0:8], r_a, r_b, ALU.subtract, eng="pool")
        yield
        k.tt(qkb[:, :, 8:16], r_c, r_d, ALU.add, eng="pool")
        yield
        k.trs([(pD[:, n, :], qkb[:, n, :]) for n in range(12)] +
              [(pD[:, 12 + n, :], kcvc[:, n * 64:(n + 1) * 64]) for n in range(4)], ident)
        yield
        k.cp(T16, pD)
        yield
        k.dma(ftv[:, :, t0:t0 + 128], T16)
        yield

    for it in range(NT + 1):
        gens = []
        if it < NT:
            gens.append(genH(it))
        if it >= 1:
            gens.append(genN(it - 1))
        pipeline(iter(gens), 2)


def phase_b(nc, k, T, NT, FT, VT, GT, MIXT, k_norm_g, cmp_pe, cmp_w1, cmp_w2, out_norm_g,
            c_rope, c_cmask, c_tri, c_ntri, c_E, c_vis, c_cadd, c_ovl1, ident, identf):
    NQ = T // 512
    NCMP = T // 16 - 1
    NCT = (NCMP + 127) // 128

    def crow(ct):
        return min(128, NCMP - 128 * ct)

    KA = k.sb("b_KA", [128, 2, T], BF16)
    KW = k.sb("b_KW", [64, 2, T], BF16)
    VS1 = k.sb("b_VS1", [128, NT, 2, 65], BF16)
    VW1 = k.sb("b_VW1", [128, NT, 2, 65], BF16)
    VTv = VT.rearrange("(n p) c -> p n c", p=128)
    k.memset(VS1, 1.0)
    k.memset(VW1, 1.0, eng="pool")
    for g in range(2):
        k.dma(KA[0:64, g, :], FT[8 + g])
        k.dma(KA[64:128, g, :], c_E)
        k.dma(KW[:, g, :], FT[10 + g])
        k.dma(VS1[:, :, g, 0:64], VTv[:, :, g * 64:(g + 1) * 64])
        k.dma(VW1[:, :, g, 0:64], VTv[:, :, 128 + g * 64:128 + (g + 1) * 64])
    tri = k.sb("b_tri", [128, 128], BF16)
    ntri = k.sb("b_ntri", [128, 128], BF16)
    k.dma(tri, c_tri)
    k.dma(ntri, c_ntri)
    zer = k.sb("b_zer", [128, 65], BF16)
    k.memset(zer, 0.0)
    gout = k.sb("b_gout", [128, 64])
    k.dma(gout, out_norm_g.partition_broadcast(128))

    pS = [k.ps("b_pS%d" % i, [128, 512]) for i in range(2)]
    pOs = [k.ps("b_pO%d" % i, [128, 512]) for i in range(2)]
    pO = pOs[0]
    pCIs = [k.ps("b_pCI%d" % i, [128, 4, 128]) for i in range(2)]
    pT = k.ps("b_pT", [128, 4, 128])
    pTb = k.ps("b_pTb", [128, 4, 128], BF16)

    kcT = k.sb("b_kcT", [64, 2, NCT * 128], BF16)
    Rv = k.sb("b_R", [128, NCT, 2, 128], BF16)
    k.memset(kcT, 0.0)
    k.memset(Rv, 0.0, eng="pool")
    ovv = c_ovl1.rearrange("(ct p) c -> p ct c", p=128)
    for ct in range(NCT):
        for g in range(2):
            k.dma(Rv[:, ct, g, 64:128], ovv[:, ct, 1:65])
    with ExitStack() as es_c:
        es_prev = k.es
        k.es = es_c
        kc2 = k.sb("b_kc2", [128, T], BF16)
        hid = k.sb("b_hid", [128, NCT * 128], BF16)
        k.memset(hid, 0.0)
        kcn = k.sb("b_kcn", [128, NCT * 2, 64])
        k.memset(kcn, 0.0)
        gk0 = k.sb("b_gk0", [128, 64])
        k.dma(gk0, k_norm_g[0].partition_broadcast(128))
        ropec = k.sb("b_ropec", [128, NCT, 16])
        k.memset(ropec, 0.0)
        rv = c_rope.rearrange("(c s) f -> c s f", s=16)
        for ct in range(NCT):
            k.dma(ropec[0:crow(ct), ct, :], rv[1 + ct * 128:1 + ct * 128 + crow(ct), 15, :])
        w1 = [k.sb("b_w1%d" % j, [128, 16, 128], BF16) for j in range(2)]
        w2 = [k.sb("b_w2%d" % j, [128, 64], BF16) for j in range(2)]
        pes = [k.sb("b_pe%d" % j, [128, 16], BF16) for j in range(2)]
        bvec = [k.sb("b_bv%d" % j, [128, 1]) for j in range(2)]
        for j in range(2):
            k.dma(w1[j], cmp_w1[j].rearrange("(l p) h -> p l h", p=128), q="pool")
            k.dma(w2[j], cmp_w2[j], q="pool")
            k.dma(pes[j], cmp_pe[j].rearrange("(l two) d -> (two d) l", two=2), q="pool",
                  allow_slow_non_contiguous=True)
        v16 = kc2.rearrange("p (c s) -> p c s", s=16)
        for j in range(2):
            k.mm(pO[:, 0:1], [(w1[j][:, l2, :], pes[j][:, l2:l2 + 1]) for l2 in range(16)])
            k.cp(bvec[j], pO[:, 0:1])
            for g in range(2):
                n = 12 + 2 * j + g
                k.dma(kc2[0:64, :], FT[n])
                k.dma(kc2[64:128, 0:T - 1], FT[n][:, 1:T])
                pairs = []
                for l2 in range(16):
                    rhs = v16[:, 0:NCMP, 2 * l2] if l2 < 8 else v16[:, 1:NCMP + 1, 2 * l2 - 16]
                    pairs.append((w1[j][:, l2, :], rhs))
                k.mm(pS[0][:, 0:NCMP], pairs)
                k.act(hid[:, 0:NCMP], pS[0][:, 0:NCMP], AF.Silu, bias=bvec[j])
                for ct in range(NCT):
                    r = crow(ct)
                    k.mm(pT[0:r, ct, 0:64], [(hid[:, ct * 128:ct * 128 + r], w2[j])])
                    if j == 0:
                        k.cp(kcn[0:r, ct * 2 + g, :], pT[0:r, ct, 0:64])
                    else:
                        k.cp(Rv[0:r, ct, g, 0:64], pT[0:r, ct, 0:64])
        NS = NCT * 2
        ksq = k.sb("b_ksq", [128, NS, 64])
        kss = k.sb("b_kss", [128, NS])
        krs = k.sb("b_krs", [128, NS])
        kcb = k.sb("b_kcb", [128, NS, 64], BF16)
        ra = k.sb("b_ra", [128, 2, 8])
        rb = k.sb("b_rb", [128, 2, 8])
        k.tt(ksq, kcn, kcn, ALU.mult)
        k.rsum(kss, ksq)
        rms_rstd(k, krs, kss, 64)
        k.tt(kcn, kcn, krs.unsqueeze(2).to_broadcast([128, NS, 64]), ALU.mult)
        k.tt(kcn, kcn, gk0.unsqueeze(1).to_broadcast([128, NS, 64]), ALU.mult)
        k.cp(kcb, kcn)
        for ct in range(NCT):
            sl = slice(ct * 2, ct * 2 + 2)
            cosb = ropec[:, ct:ct + 1, 0:8].to_broadcast([128, 2, 8])
            sinb = ropec[:, ct:ct + 1, 8:16].to_broadcast([128, 2, 8])
            k.tt(ra, kcn[:, sl, 0:8], cosb, ALU.mult)
            k.tt(rb, kcn[:, sl, 8:16], sinb, ALU.mult)
            k.tt(kcb[:, sl, 0:8], ra, rb, ALU.subtract)
            k.tt(ra, kcn[:, sl, 8:16], cosb, ALU.mult)
            k.tt(rb, kcn[:, sl, 0:8], sinb, ALU.mult)
            k.tt(kcb[:, sl, 8:16], ra, rb, ALU.add)
        for ct in range(NCT):
            r = crow(ct)
            for g in range(2):
                k.trs([(pTb[0:64, 0, 0:r], kcb[0:r, ct * 2 + g, :])], ident[0:r, 0:r])
                k.cp(kcT[:, g, ct * 128:ct * 128 + r], pTb[0:64, 0, 0:r])
        k.barrier()
        k.es = es_prev

    if B_STAGE < 1:
        return
    QA = [k.sb("b_QA%d" % i, [128, 8, 512], BF16) for i in range(2)]
    cmT = k.sb("b_cmT", [128, NCT, 512], BF16)
    gts = k.sb("b_gts", [128, 4, 24])
    vis = k.sb("b_vis", [128, 4, 64])
    cadd = k.sb("b_cadd", [128, 4, 64])
    NP = 5
    P = [k.sb("b_P%d" % i, [128, 512], BF16) for i in range(NP)]
    ocmp = k.sb("b_ocmp", [128, 4, 8, 64])
    osel = k.sb("b_osel", [128, 4, 8, 64])
    owin = k.sb("b_owin", [128, 4, 8, 64])
    oTs = [k.sb("b_oT%d" % i, [65, 512]) for i in range(2)]
    recs = [k.sb("b_rec%d" % i, [128, 4, 1]) for i in range(2)]
    dens = [k.sb("b_den%d" % i, [128, 4]) for i in range(2)]
    imp = k.sb("b_imp", [128, 4, 2, 64])
    itmps = [k.sb("b_itmp%d" % i, [128, 4, 64]) for i in range(2)]
    NB = 3
    mxs = [k.sb("b_mx%d" % i, [128, 8]) for i in range(NB)]
    mx2s = [k.sb("b_mx2%d" % i, [128, 8]) for i in range(NB)]
    wks = [k.sb("b_wk%d" % i, [128, 64]) for i in range(NB)]
    mks = [k.sb("b_mk%d" % i, [128, 64]) for i in range(NB)]
    negm4 = k.sb("b_negm4", [128, 4, 128], BF16)
    k.memset(negm4, 0.0)
    osq = k.sb("b_osq", [128, 4, 8, 64])
    oss = k.sb("b_oss", [128, 32])
    ors = k.sb("b_ors", [128, 32])
    onb = k.sb("b_onb", [128, 4, 512], BF16)
    mixs = k.sb("b_mixs", [128, 4, 128], BF16)
    ftq = FT.rearrange("n d t -> d n t")
    gtv = GT.rearrange("(s p) c -> p s c", p=128)
    visv = c_vis.rearrange("(s p) c -> p s c", p=128)
    caddv = c_cadd.rearrange("(s p) c -> p s c", p=128)
    cmv = c_cmask.rearrange("(ct p) t -> p ct t", p=128)
    mixv = MIXT.rearrange("(c p) t -> p c t", p=128)
    cnt = [0]
    ocnt = [0]

    def kt_gen(Qa, h, g, kt, lo, hi, mcol, mtile, Ksrc, Vsrc, kdim, pOb, first, last, zero_first, evac):
        i = cnt[0]
        cnt[0] += 1
        ps, Pb = pS[i % 2], P[i % NP]
        if first and zero_first:
            k.mm1(pOb[0:65, :], zer, Qa[:, h, :], True, False)
        k.mm(ps[:, lo:hi], [(Ksrc[0:kdim, g, kt * 128:(kt + 1) * 128], Qa[0:kdim, h, lo:hi])])
        yield
        k.act(Pb[:, lo:hi], ps[:, lo:hi], AF.Exp, scale=0.125)
        yield
        if mtile is not None:
            k.tt(Pb[:, mcol:mcol + 128], Pb[:, mcol:mcol + 128], mtile, ALU.mult)
            yield
        k.mm1(pOb[0:65, lo:hi], Vsrc[:, kt, g, :], Pb[:, lo:hi], (first and not zero_first), last)
        yield
        if last:
            yield from evac_gen(*evac)

    def evac_gen(pOb, oTb, rc, h, odst):
        k.cp(oTb, pOb[0:65, :], eng="act")
        yield
        k.trs([(pT[:, s, 0:65], oTb[:, s * 128:(s + 1) * 128]) for s in range(4)], identf[0:65, 0:65])
        yield
        k.recip(rc, pT[:, :, 64:65])
        yield
        k.tt(odst[:, :, h, :], pT[:, :, 0:64], rc.to_broadcast([128, 4, 64]), ALU.mult)
        yield

    def attend_gens(Qa, h, g, kts, Ksrc, Vsrc, kdim, odst, zero_first):
        j = ocnt[0]
        ocnt[0] += 1
        pOb, oTb, rc = pOs[j % 2], oTs[j % 2], recs[j % 2]
        n = len(kts)
        for idx, (kt, lo, hi, mcol, mtile) in enumerate(kts):
            yield kt_gen(Qa, h, g, kt, lo, hi, mcol, mtile, Ksrc, Vsrc, kdim, pOb,
                         idx == 0, idx == n - 1, zero_first, (pOb, oTb, rc, h, odst))

    def cmp_gen(Qa, h, g, cts):
        pc, rc, dn, itmp = pCIs[h % 2], recs[h % 2], dens[h % 2], itmps[h % 2]
        for ci, ct in enumerate(cts):
            i = cnt[0]
            cnt[0] += 1
            ps, Pb = pS[i % 2], P[i % NP]
            k.mm(ps, [(kcT[0:64, g, ct * 128:(ct + 1) * 128], Qa[0:64, h, :])])
            yield
            k.act(Pb, ps, AF.Exp, scale=0.125)
            yield
            k.tt(Pb, Pb, cmT[:, ct, :], ALU.mult)
            yield
            for s in range(4):
                k.mm1(pc[:, s, :], Pb[:, s * 128:(s + 1) * 128], Rv[:, ct, g, :],
                      ci == 0 and s == 0, ci == len(cts) - 1)
            yield
        k.rsum(dn, pc[:, :, 64:128])
        yield
        k.ts(rc, dn.unsqueeze(2), 1e-30, None, ALU.add)
        yield
        k.recip(rc, rc)
        yield
        k.tt(ocmp[:, :, h, :], pc[:, :, 0:64], rc.to_broadcast([128, 4, 64]), ALU.mult)
        yield
        if h % 4 == 0:
            k.tt(imp[:, :, g, :], pc[:, :, 64:128], rc.to_broadcast([128, 4, 64]), ALU.mult)
        else:
            k.tt(itmp, pc[:, :, 64:128], rc.to_broadcast([128, 4, 64]), ALU.mult)
            yield
            k.tt(imp[:, :, g, :], imp[:, :, g, :], itmp, ALU.add, eng="pool")
        yield

    def topk_gen(g, s, j):
        iv = imp[:, s, g, :]
        mx, mx2, wk, mk = mxs[j % NB], mx2s[j % NB], wks[j % NB], mks[j % NB]
        k.vmax(mx, iv)
        yield
        k.vmatch(wk, mx, iv, -3.0e38)
        yield
        k.vmax(mx2, wk)
        yield
        k.tt(mk, iv, mx2[:, 7:8].to_broadcast([128, 64]), ALU.is_ge)
        yield
        k.ts(negm4[:, s, 64:128], mk, -NEGM, NEGM, ALU.mult, ALU.add)
        yield

    for Qi in range(NQ):
        t0 = Qi * 512
        Qa = QA[Qi % 2]
        k.dma(Qa[0:64, :, :], ftq[:, 0:8, t0:t0 + 512])
        k.dma(gts, gtv[:, Qi * 4:(Qi + 1) * 4, :])
        k.dma(vis, visv[:, Qi * 4:(Qi + 1) * 4, :])
        k.dma(cadd, caddv[:, Qi * 4:(Qi + 1) * 4, :])
        k.dma(cmT, cmv[:, 0:NCT, t0:t0 + 512])
        cts = [ct for ct in range(NCT) if 16 * 128 * ct + 31 <= t0 + 511]
        pipeline((cmp_gen(Qa, h, h // 4, cts) for h in range(8)), 2)
        for g in range(2):
            k.tt(imp[:, :, g, :], imp[:, :, g, :], vis, ALU.mult)
            k.tt(imp[:, :, g, :], imp[:, :, g, :], cadd, ALU.add)
        for g in range(2):
            pipeline((topk_gen(g, s, g * 4 + s) for s in range(4)), 3)
            k.trs([(pTb[:, s, :], negm4[:, s, :]) for s in range(4)], ident)
            k.cp(Qa[64:128, 4 * g:4 * g + 4, :].rearrange("p h (s t) -> p h s t", s=4),
                 pTb[64:128, :, :].unsqueeze(1).to_broadcast([64, 4, 4, 128]))
        def all_gens():
            for h in range(8):
                g = h // 4
                kts = []
                for kt in range(4 * Qi + 4):
                    m = kt - 4 * Qi
                    if m >= 0:
                        kts.append((kt, 128 * m, 512, 128 * m, tri))
                    else:
                        kts.append((kt, 0, 512, 0, None))
                yield from attend_gens(Qa, h, g, kts, KA, VS1, 128, osel, False)
                kts = []
                for kt in range(max(0, 4 * Qi - 4), 4 * Qi + 4):
                    m = kt - 4 * Qi
                    if m >= 0:
                        kts.append((kt, 128 * m, 512, 128 * m, tri))
                    else:
                        kts.append((kt, 0, 128 * (m + 5), 128 * (m + 4), ntri))
                yield from attend_gens(Qa, h, g, kts, KW, VW1, 64, owin, True)
        pipeline(all_gens(), 5)
        if B_STAGE < 4:
            continue
        for br, ob in enumerate((ocmp, osel, owin)):
            k.tt(ob, ob, gts[:, :, br * 8:(br + 1) * 8].unsqueeze(3).to_broadcast([128, 4, 8, 64]),
                 ALU.mult, eng=("pool" if br == 1 else "dve"))
        k.tt(ocmp, ocmp, osel, ALU.add)
        k.tt(ocmp, ocmp, owin, ALU.add, eng="pool")
        if B_STAGE < 5:
            continue
        k.tt(osq, ocmp, ocmp, ALU.mult)
        k.rsum(oss, osq.rearrange("p s h d -> p (s h) d"))
        rms_rstd(k, ors, oss, 64)
        k.tt(ocmp.rearrange("p s h d -> p (s h) d"), ocmp.rearrange("p s h d -> p (s h) d"),
             ors.unsqueeze(2).to_broadcast([128, 32, 64]), ALU.mult)
        k.tt(onb.rearrange("p s (h d) -> p (s h) d", d=64), ocmp.rearrange("p s h d -> p (s h) d"),
             gout.unsqueeze(1).to_broadcast([128, 32, 64]), ALU.mult, eng="pool")
        if B_STAGE < 6:
            continue
        for s in range(4):
            k.trs([(pTb[:, c, :], onb[:, s, c * 128:(c + 1) * 128]) for c in range(4)], ident)
            k.cp(mixs, pTb)
            if B_STAGE >= 7:
                k.dma(mixv[:, 4:8, t0 + s * 128:t0 + (s + 1) * 128], mixs)


def load_colvec(k, dst, src, n_chunks, identf, pTf, tmp):
    k.dma(tmp[0:n_chunks, :], src.rearrange("(c p) -> c p", p=128))
    k.trs([(pTf[:, 0:n_chunks], tmp[0:n_chunks, :])], identf[0:n_chunks, 0:n_chunks])
    k.cp(dst, pTf[:, 0:n_chunks])


def phase_c(nc, k, T, x, MIXT, w_out, ffn_g, w_up, conv_w, conv_b, w_down, H2, ident, identf):
    TT = 256
    NTT = T // TT
    NF = 22
    wo = k.sb("c_wo", [128, 8, D], BF16)
    wu = k.sb("c_wu", [128, 8, 2 * DFF], BF16)
    wd = k.sb("c_wd", [128, NF, D], BF16)
    wov = w_out.rearrange("(c p) n -> p c n", p=128)
    wuv = w_up.rearrange("(c p) n -> p c n", p=128)
    wdv = w_down.rearrange("(c p) n -> p c n", p=128)
    for c in range(8):
        k.dma(wo[:, c, :], wov[:, c, :], q="pool")
    for c in range(8):
        for hh in range(2):
            k.dma(wu[:, c, hh * DFF:(hh + 1) * DFF], wuv[:, c, hh * DFF:(hh + 1) * DFF], q="pool")
    for c in range(NF):
        k.dma(wd[:, c, :], wdv[:, c, :], q="pool")
    pY = [k.ps("c_pY%d" % i, [128, 512]) for i in range(2)]
    pC = k.ps("c_pC", [128, 8, 128], BF16)
    pUs = [k.ps("c_pU%d" % i, [128, 512]) for i in range(4)]
    tmpv = k.sb("c_tmpv", [44, 128])
    gfT = k.sb("c_gfT", [128, 8])
    load_colvec(k, gfT, ffn_g, 8, identf, pY[0], tmpv)
    cw = k.sb("c_cw", [128, 3, 44])
    cb = k.sb("c_cb", [128, 44])
    for j in range(3):
        load_colvec(k, cw[:, j, :], conv_w[j], 44, identf, pY[0], tmpv)
    load_colvec(k, cb, conv_b, 44, identf, pY[0], tmpv)
    carry = k.sb("c_carry", [128, 44, 2])
    k.memset(carry, 0.0)

    mts = [k.sb("c_mt%d" % i, [128, 8, TT], BF16) for i in range(2)]
    hsbs = [k.sb("c_h%d" % i, [128, 2, D]) for i in range(2)]
    junk1 = k.sb("c_junk", [128, D], BF16)
    junks = [junk1, junk1]
    sss = [k.sb("c_ss%d" % i, [128, 1]) for i in range(2)]
    rstds = [k.sb("c_rstd%d" % i, [128, 1]) for i in range(2)]
    hns = [k.sb("c_hn%d" % i, [128, D], BF16) for i in range(2)]
    hnT = k.sb("c_hnT", [128, 8, TT], BF16)
    actT = k.sb("c_actT", [128, NF, TT], BF16)
    NU = 5
    uraw = [k.sb("c_uraw%d" % i, [128, TT + 2]) for i in range(NU)]
    acc = [k.sb("c_acc%d" % i, [128, TT]) for i in range(NU)]
    sil = [k.sb("c_sil%d" % i, [128, TT]) for i in range(2)]
    psl = [p[:, 0:256] for p in pUs] + [pY[0][:, 0:256], pY[1][:, 0:256]]
    mixv = MIXT.rearrange("(c p) t -> p c t", p=128)
    xv = x.rearrange("(n p) d -> p n d", p=128)
    h2v = H2.rearrange("(n p) d -> p n d", p=128)

    def pro_gen(it, s):
        mt, hsb = mts[it % 2], hsbs[it % 2]
        junk, ss, rstd, hn = junks[s], sss[s], rstds[s], hns[s]
        for hh in range(2):
            py = pY[hh]
            k.mm(py, [(mt[:, c, s * 128:(s + 1) * 128], wo[:, c, hh * 512:(hh + 1) * 512])
                      for c in range(8)])
            yield
            k.tt(hsb[:, s, hh * 512:(hh + 1) * 512], py, hsb[:, s, hh * 512:(hh + 1) * 512], ALU.add)
            yield
        k.act(junk, hsb[:, s, :], AF.Square, accum_out=ss)
        yield
        k.act(rstd, ss, AF.Ln, scale=1.0 / D, bias=EPS)
        yield
        k.act(rstd, rstd, AF.Exp, scale=-0.5)
        yield
        k.act(hn, hsb[:, s, :], AF.Copy, scale=rstd)
        yield
        k.trs([(pC[:, c, :], hn[:, c * 128:(c + 1) * 128]) for c in range(8)], ident)
        yield
        k.tt(hnT[:, :, s * 128:(s + 1) * 128], pC, gfT.unsqueeze(2).to_broadcast([128, 8, 128]),
             ALU.mult)
        yield

    def down_gen(it):
        hsb = hsbs[it % 2]
        for s in range(2):
            for hh in range(2):
                py = pUs[(s * 2 + hh) % 2]
                k.mm(py, [(actT[:, i, s * 128:(s + 1) * 128], wd[:, i, hh * 512:(hh + 1) * 512])
                          for i in range(NF)])
                yield
                k.tt(hsb[:, s, hh * 512:(hh + 1) * 512], py, hsb[:, s, hh * 512:(hh + 1) * 512], ALU.add)
                yield
        k.dma(h2v[:, 2 * it:2 * it + 2, :], hsb)
        yield

    def half_gen(n):
        i, gu = divmod(n, 2)
        ch = i + gu * NF
        pu = psl[n % 6]
        ur, ac = uraw[n % NU], acc[n % NU]
        k.mm(pu, [(wu[:, c, ch * 128:(ch + 1) * 128], hnT[:, c, :]) for c in range(8)])
        yield
        k.cp(ur[:, 0:2], carry[:, ch, :], eng="pool")
        k.cp(ur[:, 2:TT + 2], pu, eng="act")
        yield
        k.act(ac, pu, AF.Identity, scale=cw[:, 2, ch:ch + 1], bias=cb[:, ch:ch + 1])
        yield
        k.stt(ac, ur[:, 1:TT + 1], cw[:, 1, ch:ch + 1], ac, ALU.mult, ALU.add)
        yield
        k.stt(ac, ur[:, 0:TT], cw[:, 0, ch:ch + 1], ac, ALU.mult, ALU.add)
        k.cp(carry[:, ch, :], ur[:, TT:TT + 2], eng="pool")
        yield
        if gu == 1:
            sl = sil[i % 2]
            k.act(sl, acc[(n - 1) % NU], AF.Silu)
            yield
            k.tt(actT[:, i, :], sl, ac, ALU.mult, eng="pool")
            yield

    for it in range(NTT + 1):
        gens = []
        if it >= 1:
            gens.append(down_gen(it - 1))
        if it < NTT:
            t0 = it * TT
            k.dma(mts[it % 2], mixv[:, :, t0:t0 + TT])
            k.dma(hsbs[it % 2], xv[:, 2 * it:2 * it + 2, :])
            gens += [pro_gen(it, 0), pro_gen(it, 1)]
        pipeline(iter(gens), 3)
        if it < NTT:
            pipeline((half_gen(n) for n in range(2 * NF)), 4)


def phase_d(nc, k, T, H2, p_in, pleg_g, w_pleg, w_ple, ple_g, out, ident, identf):
    NT = T // 128
    wg = k.sb("d_wg", [128, 8, D], BF16)
    wp = k.sb("d_wp", [128, 2, D], BF16)
    wgv = w_pleg.rearrange("(c p) n -> p c n", p=128)
    wpv = w_ple.rearrange("(c p) n -> p c n", p=128)
    for c in range(8):
        k.dma(wg[:, c, :], wgv[:, c, :], q="pool")
    for c in range(2):
        k.dma(wp[:, c, :], wpv[:, c, :], q="pool")
    pY = [k.ps("d_pY%d" % i, [128, 512]) for i in range(2)]
    pE4 = [k.ps("d_pE%d" % i, [128, 512]) for i in range(4)]
    pC = k.ps("d_pC", [128, 8, 128], BF16)
    tmpv = k.sb("d_tmpv", [8, 128])
    ggT = k.sb("d_ggT", [128, 8])
    load_colvec(k, ggT, pleg_g, 8, identf, pY[0], tmpv)
    gple = k.sb("d_gple", [128, D])
    k.dma(gple, ple_g.partition_broadcast(128))

    hb = [k.sb("d_h%d" % i, [128, D]) for i in range(2)]
    pb = [k.sb("d_p%d" % i, [128, 256]) for i in range(2)]
    junks = [k.sb("d_junk%d" % i, [128, D], BF16) for i in range(2)]
    sss = [k.sb("d_ss%d" % i, [128, 1]) for i in range(2)]
    rstds = [k.sb("d_rstd%d" % i, [128, 1]) for i in range(2)]
    ss2s = [k.sb("d_ss2%d" % i, [128, 2]) for i in range(2)]
    rstd2s = [k.sb("d_rstd2%d" % i, [128, 1]) for i in range(2)]
    hns = [k.sb("d_hn%d" % i, [128, D], BF16) for i in range(2)]
    hTs = [k.sb("d_hT%d" % i, [128, 8, 128], BF16) for i in range(2)]
    pbfs = [k.sb("d_pbf%d" % i, [128, 256], BF16) for i in range(2)]
    pTs = [k.sb("d_pT%d" % i, [128, 2, 128], BF16) for i in range(2)]
    gates = [k.sb("d_gate%d" % i, [128, D]) for i in range(2)]
    es = [k.sb("d_e%d" % i, [128, D]) for i in range(2)]
    ob = [k.sb("d_o%d" % i, [128, D]) for i in range(2)]

    def d_gen(it):
        t0 = it * 128
        b = it % 2
        h, pp, o = hb[b], pb[b], ob[b]
        junk, ss, rstd, ss2, rstd2 = junks[b], sss[b], rstds[b], ss2s[b], rstd2s[b]
        hn, hT, pbf, pT, gate, e = hns[b], hTs[b], pbfs[b], pTs[b], gates[b], es[b]
        pE = pE4[2 * b:2 * b + 2]
        k.dma(h, H2[t0:t0 + 128, :])
        k.dma(pp, p_in[t0:t0 + 128, :])
        yield
        k.act(junk, h, AF.Square, accum_out=ss)
        yield
        k.act(rstd, ss, AF.Ln, scale=1.0 / D, bias=EPS)
        yield
        k.act(rstd, rstd, AF.Exp, scale=-0.5)
        yield
        k.act(hn, h, AF.Copy, scale=rstd)
        yield
        k.trs([(pC[:, c, :], hn[:, c * 128:(c + 1) * 128]) for c in range(8)], ident)
        yield
        k.tt(hT, pC, ggT.unsqueeze(2).to_broadcast([128, 8, 128]), ALU.mult)
        yield
        k.cp(pbf, pp, eng="pool")
        yield
        k.trs([(pC[:, c, :], pbf[:, c * 128:(c + 1) * 128]) for c in range(2)], ident)
        yield
        k.cp(pT, pC[:, 0:2, :])
        yield
        for hh in range(2):
            k.mm(pY[hh], [(hT[:, c, :], wg[:, c, hh * 512:(hh + 1) * 512]) for c in range(8)])
            yield
            k.act(gate[:, hh * 512:(hh + 1) * 512], pY[hh], AF.Sigmoid)
            yield
        for hh in range(2):
            k.mm(pE[hh], [(pT[:, c, :], wp[:, c, hh * 512:(hh + 1) * 512]) for c in range(2)])
            yield
            k.act(junk[:, hh * 512:(hh + 1) * 512], pE[hh], AF.Square, accum_out=ss2[:, hh:hh + 1])
            yield
        k.tt(ss, ss2[:, 0:1], ss2[:, 1:2], ALU.add)
        yield
        k.act(rstd2, ss, AF.Ln, scale=1.0 / D, bias=EPS)
        yield
        k.act(rstd2, rstd2, AF.Exp, scale=-0.5)
        yield
        for hh in range(2):
            k.act(e[:, hh * 512:(hh + 1) * 512], pE[hh], AF.Copy, scale=rstd2)
            yield
        k.tt(e, e, gple, ALU.mult)
        yield
        k.tt(e, e, gate, ALU.mult, eng="pool")
        yield
        k.tt(o, e, h, ALU.add)
        yield
        tok = k.dma(out[t0:t0 + 128, :], o)
        k.out_toks.append(tok)
        yield

    pipeline((d_gen(it) for it in range(NT)), D_DEPTH)


def _consts(T):
    bf = ml_dtypes.bfloat16
    c = {}
    c["c_ident"] = np.eye(128).astype(bf)
    c["c_identf"] = np.eye(128, dtype=np.float32)
    half = 8
    inv = np.float32(500000.0) ** (-np.arange(half, dtype=np.float32) / half)
    ang = np.arange(T, dtype=np.float32)[:, None] * inv[None, :].astype(np.float32)
    c["c_rope"] = np.concatenate([np.cos(ang), np.sin(ang)], 1).astype(np.float32)
    s = np.arange(128)
    c["c_tri2"] = ((s[:, None] <= s[None, :]) & (s[:, None] // 64 == s[None, :] // 64)).astype(np.float32)
    c["c_chunk"] = (s[:, None] // 64 == np.arange(2)[None, :]).astype(np.float32)
    t = np.arange(T)
    cc = np.arange(256)
    ncmp = T // 16 - 1
    c["c_cmask"] = (((16 * cc[:, None] + 31) <= t[None, :]) & (cc[:, None] < ncmp)).astype(bf)
    c["c_tri"] = (s[:, None] <= s[None, :]).astype(bf)
    c["c_ntri"] = (s[None, :] < s[:, None]).astype(bf)
    n = np.arange(64)
    c["c_E"] = ((t[None, :] // 64) == n[:, None]).astype(bf)
    cur = t // 64
    vis = (n[None, :] * 64 <= t[:, None])
    bonus = np.zeros((T, 64), np.float32)
    bonus += (n[None, :] == 0) * 1.0e6
    bonus += (n[None, :] == cur[:, None]) * 2.0e6
    bonus += (n[None, :] == cur[:, None] - 1) * 4.0e6
    c["c_vis"] = vis.astype(np.float32)
    c["c_cadd"] = np.where(vis, bonus, np.float32(-1e30)).astype(np.float32)
    cs = cc * 16
    ssb = n * 64
    ov = np.clip(np.minimum(cs[:, None] + 32, ssb[None, :] + 64)
                 - np.maximum(cs[:, None], ssb[None, :]), 0, None) / 32.0
    o1 = np.zeros((256, 65), np.float32)
    o1[:, 0] = 1.0
    o1[:, 1:] = ov
    o1[ncmp:] = 0.0
    c["c_ovl1"] = o1.astype(bf)
    return c


_W_NAMES = ["attn_norm_g", "w_in", "hg_norm_g", "nsa_q_norm_g", "nsa_k_norm_g", "cmp_pe", "cmp_w1",
            "cmp_w2", "nsa_out_norm_g", "w_out", "ffn_norm_g", "w_up", "conv_w", "conv_b", "w_down",
            "ple_gate_norm_g", "w_ple_gate", "w_ple", "ple_norm_g"]


def kernel(**inputs):
    x = np.asarray(inputs["x"], np.float32)
    p = np.asarray(inputs["p"], np.float32)
    B, T, _ = x.shape
    nc = build(T=T, dbg=False, phases="ABCD")
    shared = {n: np.ascontiguousarray(np.asarray(inputs[n], np.float32)[0]) for n in _W_NAMES}
    shared["hg_lb_logits"] = np.ascontiguousarray(np.asarray(inputs["hg_lb_logits"], np.float32))
    shared.update(_consts(T))
    in_maps = []
    for b in range(B):
        m = dict(shared)
        m["x"] = np.ascontiguousarray(x[b])
        m["p"] = np.ascontiguousarray(p[0, b])
        in_maps.append(m)
    res = run_bass_kernel_spmd(nc, in_maps, core_ids=list(range(B)))
    return np.stack([np.asarray(r["out"], np.float32) for r in res.results], axis=0)
```

```python
from contextlib import ExitStack
import numpy as np
import ml_dtypes
import concourse.bass as bass
import concourse.mybir as mybir
from concourse.bass_utils import run_bass_kernel_spmd

F32 = mybir.dt.float32
BF16 = mybir.dt.bfloat16
ALU = mybir.AluOpType
AF = mybir.ActivationFunctionType
AX = mybir.AxisListType

D = 1024
IN_TOTAL = 3352
DFF = 2816
EPS = 1e-6
NEGM = -30000.0
B_STAGE = 9
FFN_DEPTH = 2
D_DEPTH = 2
PRO_DEPTH = 2


class Sched:
    def __init__(self, nc, n_dma_sems=48):
        self.nc = nc
        self.engs = {"pe": nc.tensor, "act": nc.scalar, "dve": nc.vector,
                     "pool": nc.gpsimd, "sp": nc.sync}
        self.sem = {}
        self.cnt = {}
        for k in ("pe", "act", "dve", "pool"):
            self.sem[k] = nc.alloc_semaphore("s_" + k)
            self.cnt[k] = 0
        self.dma_sems = [nc.alloc_semaphore("s_dma%d" % i) for i in range(n_dma_sems)]
        self.dma_val = [0] * n_dma_sems
        self.dma_rr = 0
        self.waited = {}
        self.last_w = {}
        self.readers = {}
        self.nwaits = 0
        self.inflight = {}
        self.max_desc = 600

    def _wait(self, eng, tok):
        sem, val, key = tok
        if key == eng and eng == "pe":
            return
        k = (eng, key)
        if self.waited.get(k, 0) >= val:
            return
        self.engs[eng].wait_ge(sem, val)
        self.nwaits += 1
        self.waited[k] = val

    def deps(self, eng, reads, writes):
        toks = []
        for r in reads:
            t = self.last_w.get(r)
            if t is not None:
                toks.append(t)
        for w in writes:
            t = self.last_w.get(w)
            if t is not None:
                toks.append(t)
            toks.extend(self.readers.get(w, ()))
        for t in toks:
            self._wait(eng, t)

    def commit(self, tok, reads, writes):
        for w in writes:
            self.last_w[w] = tok
            self.readers[w] = []
        for r in reads:
            if r in writes:
                continue
            lst = self.readers.setdefault(r, [])
            lst[:] = [t for t in lst if t[2] != tok[2]]
            lst.append(tok)

    def op(self, eng, reads, writes, fn):
        self.deps(eng, reads, writes)
        ins = fn(self.engs[eng])
        self.cnt[eng] += 1
        ins.then_inc(self.sem[eng], 1)
        tok = (self.sem[eng], self.cnt[eng], eng)
        self.commit(tok, reads, writes)
        return tok

    @staticmethod
    def _ndesc(ap):
        dims = list(ap.ap)
        total = 1
        for st, n in dims:
            total *= n
        run = 1
        for st, n in reversed(dims[1:]):
            if st == run:
                run *= n
            else:
                break
        return max(1, total // max(run, 1))

    def dma(self, out, in_, reads, writes, q="sp", **kw):
        nd = max(self._ndesc(out), self._ndesc(in_))
        fifo = self.inflight.setdefault(q, [])
        while fifo and sum(d for _, d in fifo) + nd > self.max_desc:
            tok0, _ = fifo.pop(0)
            self._wait(q, tok0)
        tok = self._dma(out, in_, reads, writes, q, **kw)
        fifo.append((tok, nd))
        return tok

    def _dma(self, out, in_, reads, writes, q="sp", **kw):
        i = self.dma_rr
        self.dma_rr = (self.dma_rr + 1) % len(self.dma_sems)
        sem = self.dma_sems[i]
        key = "dma%d" % i
        if self.dma_val[i] > 0:
            self._wait(q, (sem, self.dma_val[i], key))
        self.deps(q, reads, writes)
        self.dma_val[i] += 16
        self.engs[q].dma_start(out=out, in_=in_, **kw).then_inc(sem, 16)
        tok = (sem, self.dma_val[i], key)
        self.commit(tok, reads, writes)
        return tok


_TAGS = {}
_KEEP = []


def tag(ap, name):
    _TAGS[id(ap)] = name
    _KEEP.append(ap)
    return ap


def sub(ap, fn):
    r = fn(ap)
    if id(ap) in _TAGS:
        tag(r, _TAGS[id(ap)])
    return r


def pipeline(gens, depth):
    gens = iter(gens)
    active = []
    exhausted = False
    while True:
        if not exhausted and len(active) < depth:
            g = next(gens, None)
            if g is None:
                exhausted = True
            else:
                active.append(g)
        if not active:
            if exhausted:
                break
            continue
        for g in list(active):
            try:
                next(g)
            except StopIteration:
                active.remove(g)


def _names(aps):
    out = []
    for a in aps:
        if a is None or isinstance(a, (int, float)):
            continue
        n = _TAGS.get(id(a)) or a.name
        if n not in out:
            out.append(n)
    return out


class K:
    def __init__(self, nc):
        self.nc = nc
        self.S = Sched(nc)
        self.out_toks = []
        self.es = None

    def sb(self, name, shape, dt=F32):
        return self.es.enter_context(self.nc.sbuf_tensor(name, list(shape), dt))[:]

    def ps(self, name, shape, dt=F32):
        return self.es.enter_context(self.nc.psum_tensor(name, list(shape), dt))[:]

    def barrier(self):
        S = self.S
        toks = [(S.sem[e], S.cnt[e], e) for e in ("pe", "act", "dve", "pool") if S.cnt[e] > 0]
        toks += [(S.dma_sems[i], S.dma_val[i], "dma%d" % i) for i in range(len(S.dma_sems))
                 if S.dma_val[i] > 0]
        for eng in ("sp", "pe", "act", "dve", "pool"):
            for t in toks:
                S._wait(eng, t)
        S.last_w.clear()
        S.readers.clear()

    def mm1(self, out, lhsT, rhs, start, stop):
        return self.S.op("pe", _names([lhsT, rhs]), _names([out]),
                         lambda e: e.matmul(out, lhsT=lhsT, rhs=rhs, start=start, stop=stop))

    def vmax(self, out, in_):
        return self.S.op("dve", _names([in_]), _names([out]), lambda e: e.max(out=out, in_=in_))

    def vmatch(self, out, mx, vals, imm):
        return self.S.op("dve", _names([mx, vals]), _names([out]),
                         lambda e: e.match_replace(out=out, in_to_replace=mx, in_values=vals,
                                                   imm_value=imm))

    def recip(self, out, in_):
        return self.S.op("dve", _names([in_]), _names([out]), lambda e: e.reciprocal(out=out, in_=in_))

    def act(self, out, in_, func, bias=None, scale=None, accum_out=None, eng="act"):
        kw = {}
        if bias is not None:
            kw["bias"] = bias
        if scale is not None:
            kw["scale"] = scale
        if accum_out is not None:
            kw["accum_out"] = accum_out
        rd = _names([in_, bias, scale])
        wr = _names([out, accum_out])
        return self.S.op(eng, rd, wr, lambda e: e.activation(out=out, in_=in_, func=func, **kw))

    def tt(self, out, in0, in1, op, eng="dve"):
        return self.S.op(eng, _names([in0, in1]), _names([out]),
                         lambda e: e.tensor_tensor(out=out, in0=in0, in1=in1, op=op))

    def ts(self, out, in0, s1, s2, op0, op1=None, eng="dve"):
        def f(e):
            if op1 is None:
                return e.tensor_scalar(out=out, in0=in0, scalar1=s1, scalar2=None, op0=op0)
            return e.tensor_scalar(out=out, in0=in0, scalar1=s1, scalar2=s2, op0=op0, op1=op1)
        return self.S.op(eng, _names([in0, s1, s2]), _names([out]), f)

    def stt(self, out, in0, scalar, in1, op0, op1, eng="dve"):
        return self.S.op(eng, _names([in0, scalar, in1]), _names([out]),
                         lambda e: e.scalar_tensor_tensor(out=out, in0=in0, scalar=scalar, in1=in1,
                                                          op0=op0, op1=op1))

    def cp(self, out, in_, eng="dve"):
        if eng == "act":
            return self.S.op("act", _names([in_]), _names([out]), lambda e: e.copy(out=out, in_=in_))
        return self.S.op(eng, _names([in_]), _names([out]), lambda e: e.tensor_copy(out=out, in_=in_))

    def rsum(self, out, in_, eng="dve"):
        return self.S.op(eng, _names([in_]), _names([out]),
                         lambda e: e.reduce_sum(out=out, in_=in_, axis=AX.X))

    def memset(self, out, val, eng="dve"):
        return self.S.op(eng, [], _names([out]), lambda e: e.memset(out, val))

    def mm(self, out, pairs, extra_w=()):
        rd = _names([a for p in pairs for a in p])
        n = len(pairs)

        def f(e):
            for i, (l, r) in enumerate(pairs):
                ins = e.matmul(out, lhsT=l, rhs=r, start=(i == 0), stop=(i == n - 1))
            return ins
        return self.S.op("pe", rd, _names([out]) + list(extra_w), f)

    def mms(self, groups):
        rd, wr = [], []
        for out, pairs in groups:
            wr += _names([out])
            rd += _names([a for p in pairs for a in p])

        def f(e):
            for out, pairs in groups:
                n = len(pairs)
                for i, (l, r) in enumerate(pairs):
                    ins = e.matmul(out, lhsT=l, rhs=r, start=(i == 0), stop=(i == n - 1))
            return ins
        return self.S.op("pe", list(dict.fromkeys(rd)), list(dict.fromkeys(wr)), f)

    def trs(self, items, ident):
        rd = _names([i for _, i in items] + [ident])
        wr = _names([o for o, _ in items])

        def f(e):
            for o, i in items:
                ins = e.transpose(out=o, in_=i, identity=ident)
            return ins
        return self.S.op("pe", rd, wr, f)

    def dma(self, out, in_, q="sp", **kw):
        return self.S.dma(out, in_, _names([in_]), _names([out]), q=q, **kw)

    def finish(self):
        for t in self.out_toks:
            self.S._wait("sp", t)


def rms_rstd(k, out, ss, n):
    k.act(out, ss, AF.Ln, scale=1.0 / n, bias=EPS)
    k.act(out, out, AF.Exp, scale=-0.5)


def build(T=4096, dbg=False, phases="ABCD"):
    NT = T // 128
    nc = bass.Bass("TRN2", target_bir_lowering=False)
    k = K(nc)

    def din(name, shape, dt=F32):
        return nc.dram_tensor(name, list(shape), dt, kind="ExternalInput").ap()

    def dscr(name, shape, dt):
        return nc.dram_tensor(name, list(shape), dt, kind=("ExternalOutput" if dbg else "Internal")).ap()

    x = din("x", [T, D])
    p_in = din("p", [T, 256])
    attn_g = din("attn_norm_g", [D])
    w_in = din("w_in", [D, IN_TOTAL])
    lb_logits = din("hg_lb_logits", [2, 512])
    hg_norm_g = din("hg_norm_g", [128])
    q_norm_g = din("nsa_q_norm_g", [64])
    k_norm_g = din("nsa_k_norm_g", [3, 64])
    cmp_pe = din("cmp_pe", [2, 32, 64])
    cmp_w1 = din("cmp_w1", [2, 2048, 128])
    cmp_w2 = din("cmp_w2", [2, 128, 64])
    out_norm_g = din("nsa_out_norm_g", [64])
    w_out = din("w_out", [D, D])
    ffn_g = din("ffn_norm_g", [D])
    w_up = din("w_up", [D, 2 * DFF])
    conv_w = din("conv_w", [3, 2 * DFF])
    conv_b = din("conv_b", [2 * DFF])
    w_down = din("w_down", [DFF, D])
    pleg_g = din("ple_gate_norm_g", [D])
    w_pleg = din("w_ple_gate", [D, D])
    w_ple = din("w_ple", [256, D])
    ple_g = din("ple_norm_g", [D])
    c_ident = din("c_ident", [128, 128], BF16)
    c_rope = din("c_rope", [T, 16])
    c_tri2 = din("c_tri2", [128, 128])
    c_chunk = din("c_chunk", [128, 2])

    out = nc.dram_tensor("out", [T, D], F32, kind="ExternalOutput").ap()

    FT = dscr("FT", [16, 64, T], BF16)
    VT = dscr("VT", [T, 256], BF16)
    GT = dscr("GT", [T, 24], F32)
    MIXT = dscr("MIXT", [D, T], BF16)

    c_identf = din("c_identf", [128, 128])
    c_cmask = din("c_cmask", [256, T], BF16)
    c_tri = din("c_tri", [128, 128], BF16)
    c_ntri = din("c_ntri", [128, 128], BF16)
    c_E = din("c_E", [64, T], BF16)
    c_vis = din("c_vis", [T, 64])
    c_cadd = din("c_cadd", [T, 64])
    c_ovl1 = din("c_ovl1", [256, 65], BF16)
    H2 = dscr("H2", [T, D], F32)

    with ExitStack() as es0:
        k.es = es0
        ident = k.sb("ident", [128, 128], BF16)
        k.dma(ident, c_ident)
        identf = k.sb("identf", [128, 128])
        k.dma(identf, c_identf)
        if "A" in phases:
            with ExitStack() as es:
                k.es = es
                phase_a(nc, k, T, NT, x, attn_g, w_in, lb_logits, hg_norm_g, q_norm_g, k_norm_g,
                        c_rope, c_tri2, c_chunk, ident, FT, VT, GT, MIXT)
                k.barrier()
        if "B" in phases:
            with ExitStack() as es:
                k.es = es
                phase_b(nc, k, T, NT, FT, VT, GT, MIXT, k_norm_g, cmp_pe, cmp_w1, cmp_w2, out_norm_g,
                        c_rope, c_cmask, c_tri, c_ntri, c_E, c_vis, c_cadd, c_ovl1, ident, identf)
                k.barrier()
        if "A" not in phases and dbg:
            mi = din("MIXT_in", [D, T], BF16)
            with ExitStack() as es:
                k.es = es
                tb = k.sb("dbg_mix", [128, 8, T], BF16)
                k.dma(tb, mi.rearrange("(c p) t -> p c t", p=128))
                k.dma(MIXT.rearrange("(c p) t -> p c t", p=128), tb)
                k.barrier()
        if "C" in phases:
            with ExitStack() as es:
                k.es = es
                phase_c(nc, k, T, x, MIXT, w_out, ffn_g, w_up, conv_w, conv_b, w_down, H2, ident, identf)
                k.barrier()
        if "D" in phases:
            with ExitStack() as es:
                k.es = es
                phase_d(nc, k, T, H2, p_in, pleg_g, w_pleg, w_ple, ple_g, out, ident, identf)
                k.barrier()
        k.finish()
    return nc


def phase_a(nc, k, T, NT, x, attn_g, w_in, lb_logits, hg_norm_g, q_norm_g, k_norm_g,
            c_rope, c_tri2, c_chunk, ident, FT, VT, GT, MIXT):
    S = k.S
    w_sb = k.sb("a_w", [128, 8, IN_TOTAL], BF16)
    w_v = w_in.rearrange("(c p) n -> p c n", p=128)
    for c in range(8):
        k.dma(w_sb[:, c, :], w_v[:, c, :], q="pool")
    gT = k.sb("a_gT", [128, 8])
    k.dma(gT, attn_g.rearrange("(c p) -> p c", p=128), allow_slow_non_contiguous=True)
    rope = k.sb("a_rope", [128, NT, 16])
    k.dma(rope, c_rope.rearrange("(n p) c -> p n c", p=128))
    tri2 = k.sb("a_tri2", [128, 128])
    k.dma(tri2, c_tri2)
    chunk = k.sb("a_chunk", [128, 2])
    k.dma(chunk, c_chunk)
    l0 = k.sb("a_l0", [128, 512])
    l1 = k.sb("a_l1", [128, 512])
    k.dma(l0, lb_logits[0].partition_broadcast(128))
    k.dma(l1, lb_logits[1].partition_broadcast(128))
    lb = k.sb("a_lb", [128, 512])
    oml = k.sb("a_oml", [128, 512])
    k.tt(l0, l0, l1, ALU.subtract)
    k.act(lb, l0, AF.Sigmoid)
    k.ts(oml, lb, -1.0, 1.0, ALU.mult, ALU.add)
    gq = k.sb("a_gq", [128, 12, 64])
    for h in range(12):
        src = q_norm_g if h < 8 else (k_norm_g[1] if h < 10 else k_norm_g[2])
        k.dma(gq[:, h, :], src.partition_broadcast(128))
    ghg = k.sb("a_ghg", [128, 4, 128])
    for h in range(4):
        k.dma(ghg[:, h, :], hg_norm_g.partition_broadcast(128))
    Sf = k.sb("a_Sf", [128, 4, 128])
    Sb = [k.sb("a_Sb%d" % i, [128, 4, 128], BF16) for i in range(3)]
    k.memset(Sf, 0.0)
    k.memset(Sb[0], 0.0, eng="pool")

    xt = [k.sb("a_x%d" % i, [128, D]) for i in range(2)]
    junk = k.sb("a_junk", [128, D], BF16)
    ss = k.sb("a_ss", [128, 1])
    rstd = k.sb("a_rstd", [128, 1])
    xn = k.sb("a_xn", [128, D], BF16)
    xnT = k.sb("a_xnT", [128, 8, 128], BF16)
    silq = k.sb("a_silq", [128, 512])
    sig = k.sb("a_sig", [128, 512])
    logf = k.sb("a_logf", [128, 512])
    kk = k.sb("a_kk", [128, 512])
    enb = k.sb("a_enb", [128, 512])
    epb = k.sb("a_epb", [128, 512])
    kp = k.sb("a_kp", [128, 512], BF16)
    qp = k.sb("a_qp", [128, 512], BF16)
    vb = k.sb("a_vb", [128, 512], BF16)
    sg = k.sb("a_sg", [128, 512])
    ebl = k.sb("a_ebl", [128, 4, 2])
    qkT = k.sb("a_qkT", [128, 8, 128], BF16)
    ATm = k.sb("a_ATm", [128, 4, 128], BF16)
    tmpS = k.sb("a_tmpS", [128, 4, 128])
    osb = k.sb("a_osb", [128, 4, 128])
    osq = k.sb("a_osq", [128, 4, 128])
    ss4 = k.sb("a_ss4", [128, 4])
    rstd4 = k.sb("a_rstd4", [128, 4])
    onb = k.sb("a_onb", [128, 4, 128], BF16)
    mixs = k.sb("a_mixs", [128, 4, 128], BF16)
    qk = k.sb("a_qk", [128, 12, 64])
    qsq = k.sb("a_qsq", [128, 12, 64])
    ss12 = k.sb("a_ss12", [128, 12])
    rstd12 = k.sb("a_rstd12", [128, 12])
    qkb = k.sb("a_qkb", [128, 12, 64], BF16)
    r_a = k.sb("a_ra", [128, 12, 8])
    r_b = k.sb("a_rb", [128, 12, 8])
    r_c = k.sb("a_rc", [128, 12, 8])
    r_d = k.sb("a_rd", [128, 12, 8])
    kcvc = k.sb("a_kcvc", [128, 256], BF16)
    T16 = k.sb("a_T16", [64, 16, 128], BF16)
    vv = k.sb("a_vv", [128, 256], BF16)
    gsb = k.sb("a_gsb", [128, 24])

    pA = [k.ps("a_pA%d" % i, [128, 512]) for i in range(2)]
    pB = k.ps("a_pB", [128, 512])
    pC = k.ps("a_pC", [128, 8, 128], BF16)
    pD = k.ps("a_pD", [64, 16, 128], BF16)
    pF = k.ps("a_pF", [128, 4, 128])
    pG = k.ps("a_pG", [128, 4, 128])

    cols = [(0, 512), (512, 512), (1024, 512), (1536, 512), (2048, 512), (2560, 512), (3072, 280)]
    mixv = MIXT.rearrange("(c p) t -> p c t", p=128)
    ftv = FT.rearrange("n d t -> d n t")
    xnT2 = [xnT, k.sb("a_xnT1", [128, 8, 128], BF16)]

    def proj(g, dst, xT):
        c0, n = cols[g]
        k.mm(dst[:, 0:n], [(xT[:, c, :], w_sb[:, c, c0:c0 + n]) for c in range(8)])
        return dst

    def genH(it):
        t0 = it * 128
        xb = xt[it % 2]
        xT = xnT2[it % 2]
        pAh = pA[0]
        if it == 0:
            k.dma(xb, x[t0:t0 + 128, :])
        if it + 1 < NT:
            k.dma(xt[(it + 1) % 2], x[t0 + 128:t0 + 256, :])
        yield
        k.act(junk, xb, AF.Square, accum_out=ss)
        yield
        k.act(rstd, ss, AF.Ln, scale=1.0 / D, bias=EPS)
        yield
        k.act(rstd, rstd, AF.Exp, scale=-0.5)
        yield
        k.act(xn, xb, AF.Copy, scale=rstd)
        yield
        k.trs([(pC[:, c, :], xn[:, c * 128:(c + 1) * 128]) for c in range(8)], ident)
        yield
        k.tt(xT, pC, gT.unsqueeze(2).to_broadcast([128, 8, 128]), ALU.mult)
        yield
        d = proj(1, pAh, xT)
        yield
        k.act(sig, d, AF.Sigmoid)
        yield
        k.tt(sig, sig, oml, ALU.mult)
        yield
        k.tt(sig, sig, lb, ALU.add)
        yield
        k.act(logf, sig, AF.Ln)
        yield
        k.ts(kk, sig, -1.0, 1.0, ALU.mult, ALU.add)
        yield
        k.mm(pB, [(tri2, logf)])
        yield
        k.act(enb, pB, AF.Exp, scale=-1.0)
        yield
        k.act(epb, pB, AF.Exp)
        yield
        k.tt(kp, kk, enb, ALU.mult)
        yield
        k.mms([(pF[:, h, 0:2], [(logf[:, h * 128:(h + 1) * 128], chunk)]) for h in range(4)])
        yield
        k.act(ebl, pF[:, :, 0:2], AF.Exp)
        yield
        d = proj(0, pAh, xT)
        yield
        k.act(silq, d, AF.Silu)
        yield
        k.stt(qp, silq, 128 ** -0.5, epb, ALU.mult, ALU.mult)
        yield
        d = proj(2, pAh, xT)
        yield
        k.cp(vb, d, eng="act")
        yield
        d = proj(3, pAh, xT)
        yield
        k.act(sg, d, AF.Silu)
        yield
        k.tt(sg, sg, ghg.rearrange("p h v -> p (h v)"), ALU.mult, eng="pool")
        yield
        k.trs([(pC[:, h, :], qp[:, h * 128:(h + 1) * 128]) for h in range(4)] +
              [(pC[:, 4 + h, :], kp[:, h * 128:(h + 1) * 128]) for h in range(4)], ident)
        yield
        k.cp(qkT, pC)
        yield
        S0, S1, S2 = Sb[(2 * it) % 3], Sb[(2 * it + 1) % 3], Sb[(2 * it + 2) % 3]
        k.mms([(pF[:, h, :], [(qkT[:, 4 + h, :], qkT[:, h, :])]) for h in range(4)])
        yield
        k.tt(ATm, pF, tri2.unsqueeze(1).to_broadcast([128, 4, 128]), ALU.mult)
        yield
        for c in range(2):
            rs = slice(c * 64, (c + 1) * 64)
            k.mms([(pF[:, h, :], [(kp[rs, h * 128:(h + 1) * 128], vb[rs, h * 128:(h + 1) * 128])])
                   for h in range(4)])
            yield
            k.tt(tmpS, pF, Sf, ALU.add)
            yield
            k.tt(Sf, tmpS, ebl[:, :, c:c + 1].to_broadcast([128, 4, 128]), ALU.mult)
            yield
            k.cp(S1 if c == 0 else S2, Sf, eng="act")
            yield
        groups = []
        for h in range(4):
            for c in range(2):
                rs = slice(c * 64, (c + 1) * 64)
                Sc = S0 if c == 0 else S1
                groups.append((pG[rs, h, :], [(ATm[rs, h, rs], vb[rs, h * 128:(h + 1) * 128]),
                                              (qkT[:, h, rs], Sc[:, h, :])]))
        k.mms(groups)
        yield
        k.cp(osb, pG, eng="act")
        yield
        k.tt(osq, osb, osb, ALU.mult)
        yield
        k.rsum(ss4, osq)
        yield
        k.act(rstd4, ss4, AF.Ln, scale=1.0 / 128, bias=EPS)
        yield
        k.act(rstd4, rstd4, AF.Exp, scale=-0.5)
        yield
        k.tt(osb, osb, rstd4.unsqueeze(2).to_broadcast([128, 4, 128]), ALU.mult)
        yield
        k.tt(onb, osb, sg.rearrange("p (h v) -> p h v", h=4), ALU.mult)
        yield
        k.trs([(pC[:, h, :], onb[:, h, :]) for h in range(4)], ident)
        yield
        k.cp(mixs, pC[:, 0:4, :])
        yield
        k.dma(mixv[:, 0:4, t0:t0 + 128], mixs)
        yield

    def genN(it):
        t0 = it * 128
        xT = xnT2[it % 2]
        pAn = pA[1]
        d = proj(4, pAn, xT)
        yield
        k.cp(qk[:, 0:8, :], d.rearrange("p (h d) -> p h d", d=64), eng="act")
        yield
        d = proj(5, pAn, xT)
        yield
        k.cp(kcvc, d[:, 0:256], eng="act")
        yield
        k.cp(qk[:, 8:10, :], d[:, 256:384].rearrange("p (h d) -> p h d", d=64), eng="act")
        yield
        k.cp(vv[:, 0:128], d[:, 384:512], eng="act")
        yield
        d = proj(6, pAn, xT)
        yield
        k.cp(qk[:, 10:12, :], d[:, 0:128].rearrange("p (h d) -> p h d", d=64), eng="act")
        yield
        k.cp(vv[:, 128:256], d[:, 128:256], eng="act")
        yield
        k.act(gsb, d[:, 256:280], AF.Sigmoid)
        yield
        k.dma(GT[t0:t0 + 128, :], gsb)
        k.dma(VT[t0:t0 + 128, :], vv)
        yield
        k.tt(qsq, qk, qk, ALU.mult, eng="pool")
        yield
        k.rsum(ss12, qsq)
        yield
        k.act(rstd12, ss12, AF.Ln, scale=1.0 / 64, bias=EPS)
        yield
        k.act(rstd12, rstd12, AF.Exp, scale=-0.5)
        yield
        k.tt(qk, qk, rstd12.unsqueeze(2).to_broadcast([128, 12, 64]), ALU.mult)
        yield
        k.tt(qk, qk, gq, ALU.mult, eng="pool")
        yield
        cosb = rope[:, it:it + 1, 0:8].to_broadcast([128, 12, 8])
        sinb = rope[:, it:it + 1, 8:16].to_broadcast([128, 12, 8])
        k.tt(r_a, qk[:, :, 0:8], cosb, ALU.mult, eng="pool")
        yield
        k.tt(r_b, qk[:, :, 8:16], sinb, ALU.mult, eng="pool")
        yield
        k.tt(r_c, qk[:, :, 8:16], cosb, ALU.mult, eng="pool")
        yield
        k.tt(r_d, qk[:, :, 0:8], sinb, ALU.mult, eng="pool")
        yield
        k.cp(qkb, qk, eng="pool")
        yield
        k.tt(qkb[:, :, 0:8], r_a, r_b, ALU.subtract, eng="pool")
        yield
        k.tt(qkb[:, :, 8:16], r_c, r_d, ALU.add, eng="pool")
        yield
        k.trs([(pD[:, n, :], qkb[:, n, :]) for n in range(12)] +
              [(pD[:, 12 + n, :], kcvc[:, n * 64:(n + 1) * 64]) for n in range(4)], ident)
        yield
        k.cp(T16, pD)
        yield
        k.dma(ftv[:, :, t0:t0 + 128], T16)
        yield

    for it in range(NT + 1):
        gens = []
        if it < NT:
            gens.append(genH(it))
        if it >= 1:
            gens.append(genN(it - 1))
        pipeline(iter(gens), 2)


def phase_b(nc, k, T, NT, FT, VT, GT, MIXT, k_norm_g, cmp_pe, cmp_w1, cmp_w2, out_norm_g,
            c_rope, c_cmask, c_tri, c_ntri, c_E, c_vis, c_cadd, c_ovl1, ident, identf):
    NQ = T // 512
    NCMP = T // 16 - 1
    NCT = (NCMP + 127) // 128

    def crow(ct):
        return min(128, NCMP - 128 * ct)

    KA = k.sb("b_KA", [128, 2, T], BF16)
    KW = k.sb("b_KW", [64, 2, T], BF16)
    VS1 = k.sb("b_VS1", [128, NT, 2, 65], BF16)
    VW1 = k.sb("b_VW1", [128, NT, 2, 65], BF16)
    VTv = VT.rearrange("(n p) c -> p n c", p=128)
    k.memset(VS1, 1.0)
    k.memset(VW1, 1.0, eng="pool")
    for g in range(2):
        k.dma(KA[0:64, g, :], FT[8 + g])
        k.dma(KA[64:128, g, :], c_E)
        k.dma(KW[:, g, :], FT[10 + g])
        k.dma(VS1[:, :, g, 0:64], VTv[:, :, g * 64:(g + 1) * 64])
        k.dma(VW1[:, :, g, 0:64], VTv[:, :, 128 + g * 64:128 + (g + 1) * 64])
    tri = k.sb("b_tri", [128, 128], BF16)
    ntri = k.sb("b_ntri", [128, 128], BF16)
    k.dma(tri, c_tri)
    k.dma(ntri, c_ntri)
    zer = k.sb("b_zer", [128, 65], BF16)
    k.memset(zer, 0.0)
    gout = k.sb("b_gout", [128, 64])
    k.dma(gout, out_norm_g.partition_broadcast(128))

    pS = [k.ps("b_pS%d" % i, [128, 512]) for i in range(2)]
    pOs = [k.ps("b_pO%d" % i, [128, 512]) for i in range(2)]
    pO = pOs[0]
    pCIs = [k.ps("b_pCI%d" % i, [128, 4, 128]) for i in range(2)]
    pT = k.ps("b_pT", [128, 4, 128])
    pTb = k.ps("b_pTb", [128, 4, 128], BF16)

    kcT = k.sb("b_kcT", [64, 2, NCT * 128], BF16)
    Rv = k.sb("b_R", [128, NCT, 2, 128], BF16)
    k.memset(kcT, 0.0)
    k.memset(Rv, 0.0, eng="pool")
    ovv = c_ovl1.rearrange("(ct p) c -> p ct c", p=128)
    for ct in range(NCT):
        for g in range(2):
            k.dma(Rv[:, ct, g, 64:128], ovv[:, ct, 1:65])
    with ExitStack() as es_c:
        es_prev = k.es
        k.es = es_c
        kc2 = k.sb("b_kc2", [128, T], BF16)
        hid = k.sb("b_hid", [128, NCT * 128], BF16)
        k.memset(hid, 0.0)
        kcn = k.sb("b_kcn", [128, NCT * 2, 64])
        k.memset(kcn, 0.0)
        gk0 = k.sb("b_gk0", [128, 64])
        k.dma(gk0, k_norm_g[0].partition_broadcast(128))
        ropec = k.sb("b_ropec", [128, NCT, 16])
        k.memset(ropec, 0.0)
        rv = c_rope.rearrange("(c s) f -> c s f", s=16)
        for ct in range(NCT):
            k.dma(ropec[0:crow(ct), ct, :], rv[1 + ct * 128:1 + ct * 128 + crow(ct), 15, :])
        w1 = [k.sb("b_w1%d" % j, [128, 16, 128], BF16) for j in range(2)]
        w2 = [k.sb("b_w2%d" % j, [128, 64], BF16) for j in range(2)]
        pes = [k.sb("b_pe%d" % j, [128, 16], BF16) for j in range(2)]
        bvec = [k.sb("b_bv%d" % j, [128, 1]) for j in range(2)]
        for j in range(2):
            k.dma(w1[j], cmp_w1[j].rearrange("(l p) h -> p l h", p=128), q="pool")
            k.dma(w2[j], cmp_w2[j], q="pool")
            k.dma(pes[j], cmp_pe[j].rearrange("(l two) d -> (two d) l", two=2), q="pool",
                  allow_slow_non_contiguous=True)
        v16 = kc2.rearrange("p (c s) -> p c s", s=16)
        for j in range(2):
            k.mm(pO[:, 0:1], [(w1[j][:, l2, :], pes[j][:, l2:l2 + 1]) for l2 in range(16)])
            k.cp(bvec[j], pO[:, 0:1])
            for g in range(2):
                n = 12 + 2 * j + g
                k.dma(kc2[0:64, :], FT[n])
                k.dma(kc2[64:128, 0:T - 1], FT[n][:, 1:T])
                pairs = []
                for l2 in range(16):
                    rhs = v16[:, 0:NCMP, 2 * l2] if l2 < 8 else v16[:, 1:NCMP + 1, 2 * l2 - 16]
                    pairs.append((w1[j][:, l2, :], rhs))
                k.mm(pS[0][:, 0:NCMP], pairs)
                k.act(hid[:, 0:NCMP], pS[0][:, 0:NCMP], AF.Silu, bias=bvec[j])
                for ct in range(NCT):
                    r = crow(ct)
                    k.mm(pT[0:r, ct, 0:64], [(hid[:, ct * 128:ct * 128 + r], w2[j])])
                    if j == 0:
                        k.cp(kcn[0:r, ct * 2 + g, :], pT[0:r, ct, 0:64])
                    else:
                        k.cp(Rv[0:r, ct, g, 0:64], pT[0:r, ct, 0:64])
        NS = NCT * 2
        ksq = k.sb("b_ksq", [128, NS, 64])
        kss = k.sb("b_kss", [128, NS])
        krs = k.sb("b_krs", [128, NS])
        kcb = k.sb("b_kcb", [128, NS, 64], BF16)
        ra = k.sb("b_ra", [128, 2, 8])
        rb = k.sb("b_rb", [128, 2, 8])
        k.tt(ksq, kcn, kcn, ALU.mult)
        k.rsum(kss, ksq)
        rms_rstd(k, krs, kss, 64)
        k.tt(kcn, kcn, krs.unsqueeze(2).to_broadcast([128, NS, 64]), ALU.mult)
        k.tt(kcn, kcn, gk0.unsqueeze(1).to_broadcast([128, NS, 64]), ALU.mult)
        k.cp(kcb, kcn)
        for ct in range(NCT):
            sl = slice(ct * 2, ct * 2 + 2)
            cosb = ropec[:, ct:ct + 1, 0:8].to_broadcast([128, 2, 8])
            sinb = ropec[:, ct:ct + 1, 8:16].to_broadcast([128, 2, 8])
            k.tt(ra, kcn[:, sl, 0:8], cosb, ALU.mult)
            k.tt(rb, kcn[:, sl, 8:16], sinb, ALU.mult)
            k.tt(kcb[:, sl, 0:8], ra, rb, ALU.subtract)
            k.tt(ra, kcn[:, sl, 8:16], cosb, ALU.mult)
            k.tt(rb, kcn[:, sl, 0:8], sinb, ALU.mult)
            k.tt(kcb[:, sl, 8:16], ra, rb, ALU.add)
        for ct in range(NCT):
            r = crow(ct)
            for g in range(2):
                k.trs([(pTb[0:64, 0, 0:r], kcb[0:r, ct * 2 + g, :])], ident[0:r, 0:r])
                k.cp(kcT[:, g, ct * 128:ct * 128 + r], pTb[0:64, 0, 0:r])
        k.barrier()
        k.es = es_prev

    if B_STAGE < 1:
        return
    QA = [k.sb("b_QA%d" % i, [128, 8, 512], BF16) for i in range(2)]
    cmT = k.sb("b_cmT", [128, NCT, 512], BF16)
    gts = k.sb("b_gts", [128, 4, 24])
    vis = k.sb("b_vis", [128, 4, 64])
    cadd = k.sb("b_cadd", [128, 4, 64])
    NP = 5
    P = [k.sb("b_P%d" % i, [128, 512], BF16) for i in range(NP)]
    ocmp = k.sb("b_ocmp", [128, 4, 8, 64])
    osel = k.sb("b_osel", [128, 4, 8, 64])
    owin = k.sb("b_owin", [128, 4, 8, 64])
    oTs = [k.sb("b_oT%d" % i, [65, 512]) for i in range(2)]
    recs = [k.sb("b_rec%d" % i, [128, 4, 1]) for i in range(2)]
    dens = [k.sb("b_den%d" % i, [128, 4]) for i in range(2)]
    imp = k.sb("b_imp", [128, 4, 2, 64])
    itmps = [k.sb("b_itmp%d" % i, [128, 4, 64]) for i in range(2)]
    NB = 3
    mxs = [k.sb("b_mx%d" % i, [128, 8]) for i in range(NB)]
    mx2s = [k.sb("b_mx2%d" % i, [128, 8]) for i in range(NB)]
    wks = [k.sb("b_wk%d" % i, [128, 64]) for i in range(NB)]
    mks = [k.sb("b_mk%d" % i, [128, 64]) for i in range(NB)]
    negm4 = k.sb("b_negm4", [128, 4, 128], BF16)
    k.memset(negm4, 0.0)
    osq = k.sb("b_osq", [128, 4, 8, 64])
    oss = k.sb("b_oss", [128, 32])
    ors = k.sb("b_ors", [128, 32])
    onb = k.sb("b_onb", [128, 4, 512], BF16)
    mixs = k.sb("b_mixs", [128, 4, 128], BF16)
    ftq = FT.rearrange("n d t -> d n t")
    gtv = GT.rearrange("(s p) c -> p s c", p=128)
    visv = c_vis.rearrange("(s p) c -> p s c", p=128)
    caddv = c_cadd.rearrange("(s p) c -> p s c", p=128)
    cmv = c_cmask.rearrange("(ct p) t -> p ct t", p=128)
    mixv = MIXT.rearrange("(c p) t -> p c t", p=128)
    cnt = [0]
    ocnt = [0]

    def kt_gen(Qa, h, g, kt, lo, hi, mcol, mtile, Ksrc, Vsrc, kdim, pOb, first, last, zero_first, evac):
        i = cnt[0]
        cnt[0] += 1
        ps, Pb = pS[i % 2], P[i % NP]
        if first and zero_first:
            k.mm1(pOb[0:65, :], zer, Qa[:, h, :], True, False)
        k.mm(ps[:, lo:hi], [(Ksrc[0:kdim, g, kt * 128:(kt + 1) * 128], Qa[0:kdim, h, lo:hi])])
        yield
        k.act(Pb[:, lo:hi], ps[:, lo:hi], AF.Exp, scale=0.125)
        yield
        if mtile is not None:
            k.tt(Pb[:, mcol:mcol + 128], Pb[:, mcol:mcol + 128], mtile, ALU.mult)
            yield
        k.mm1(pOb[0:65, lo:hi], Vsrc[:, kt, g, :], Pb[:, lo:hi], (first and not zero_first), last)
        yield
        if last:
            yield from evac_gen(*evac)

    def evac_gen(pOb, oTb, rc, h, odst):
        k.cp(oTb, pOb[0:65, :], eng="act")
        yield
        k.trs([(pT[:, s, 0:65], oTb[:, s * 128:(s + 1) * 128]) for s in range(4)], identf[0:65, 0:65])
        yield
        k.recip(rc, pT[:, :, 64:65])
        yield
        k.tt(odst[:, :, h, :], pT[:, :, 0:64], rc.to_broadcast([128, 4, 64]), ALU.mult)
        yield

    def attend_gens(Qa, h, g, kts, Ksrc, Vsrc, kdim, odst, zero_first):
        j = ocnt[0]
        ocnt[0] += 1
        pOb, oTb, rc = pOs[j % 2], oTs[j % 2], recs[j % 2]
        n = len(kts)
        for idx, (kt, lo, hi, mcol, mtile) in enumerate(kts):
            yield kt_gen(Qa, h, g, kt, lo, hi, mcol, mtile, Ksrc, Vsrc, kdim, pOb,
                         idx == 0, idx == n - 1, zero_first, (pOb, oTb, rc, h, odst))

    def cmp_gen(Qa, h, g, cts):
        pc, rc, dn, itmp = pCIs[h % 2], recs[h % 2], dens[h % 2], itmps[h % 2]
        for ci, ct in enumerate(cts):
            i = cnt[0]
            cnt[0] += 1
            ps, Pb = pS[i % 2], P[i % NP]
            k.mm(ps, [(kcT[0:64, g, ct * 128:(ct + 1) * 128], Qa[0:64, h, :])])
            yield
            k.act(Pb, ps, AF.Exp, scale=0.125)
            yield
            k.tt(Pb, Pb, cmT[:, ct, :], ALU.mult)
            yield
            for s in range(4):
                k.mm1(pc[:, s, :], Pb[:, s * 128:(s + 1) * 128], Rv[:, ct, g, :],
                      ci == 0 and s == 0, ci == len(cts) - 1)
            yield
        k.rsum(dn, pc[:, :, 64:128])
        yield
        k.ts(rc, dn.unsqueeze(2), 1e-30, None, ALU.add)
        yield
        k.recip(rc, rc)
        yield
        k.tt(ocmp[:, :, h, :], pc[:, :, 0:64], rc.to_broadcast([128, 4, 64]), ALU.mult)
        yield
        if h % 4 == 0:
            k.tt(imp[:, :, g, :], pc[:, :, 64:128], rc.to_broadcast([128, 4, 64]), ALU.mult)
        else:
            k.tt(itmp, pc[:, :, 64:128], rc.to_broadcast([128, 4, 64]), ALU.mult)
            yield
            k.tt(imp[:, :, g, :], imp[:, :, g, :], itmp, ALU.add, eng="pool")
        yield

    def topk_gen(g, s, j):
        iv = imp[:, s, g, :]
        mx, mx2, wk, mk = mxs[j % NB], mx2s[j % NB], wks[j % NB], mks[j % NB]
        k.vmax(mx, iv)
        yield
        k.vmatch(wk, mx, iv, -3.0e38)
        yield
        k.vmax(mx2, wk)
        yield
        k.tt(mk, iv, mx2[:, 7:8].to_broadcast([128, 64]), ALU.is_ge)
        yield
        k.ts(negm4[:, s, 64:128], mk, -NEGM, NEGM, ALU.mult, ALU.add)
        yield

    for Qi in range(NQ):
        t0 = Qi * 512
        Qa = QA[Qi % 2]
        k.dma(Qa[0:64, :, :], ftq[:, 0:8, t0:t0 + 512])
        k.dma(gts, gtv[:, Qi * 4:(Qi + 1) * 4, :])
        k.dma(vis, visv[:, Qi * 4:(Qi + 1) * 4, :])
        k.dma(cadd, caddv[:, Qi * 4:(Qi + 1) * 4, :])
        k.dma(cmT, cmv[:, 0:NCT, t0:t0 + 512])
        cts = [ct for ct in range(NCT) if 16 * 128 * ct + 31 <= t0 + 511]
        pipeline((cmp_gen(Qa, h, h // 4, cts) for h in range(8)), 2)
        for g in range(2):
            k.tt(imp[:, :, g, :], imp[:, :, g, :], vis, ALU.mult)
            k.tt(imp[:, :, g, :], imp[:, :, g, :], cadd, ALU.add)
        for g in range(2):
            pipeline((topk_gen(g, s, g * 4 + s) for s in range(4)), 3)
            k.trs([(pTb[:, s, :], negm4[:, s, :]) for s in range(4)], ident)
            k.cp(Qa[64:128, 4 * g:4 * g + 4, :].rearrange("p h (s t) -> p h s t", s=4),
                 pTb[64:128, :, :].unsqueeze(1).to_broadcast([64, 4, 4, 128]))
        def all_gens():
            for h in range(8):
                g = h // 4
                kts = []
                for kt in range(4 * Qi + 4):
                    m = kt - 4 * Qi
                    if m >= 0:
                        kts.append((kt, 128 * m, 512, 128 * m, tri))
                    else:
                        kts.append((kt, 0, 512, 0, None))
                yield from attend_gens(Qa, h, g, kts, KA, VS1, 128, osel, False)
                kts = []
                for kt in range(max(0, 4 * Qi - 4), 4 * Qi + 4):
                    m = kt - 4 * Qi
                    if m >= 0:
                        kts.append((kt, 128 * m, 512, 128 * m, tri))
                    else:
                        kts.append((kt, 0, 128 * (m + 5), 128 * (m + 4), ntri))
                yield from attend_gens(Qa, h, g, kts, KW, VW1, 64, owin, True)
        pipeline(all_gens(), 5)
        if B_STAGE < 4:
            continue
        for br, ob in enumerate((ocmp, osel, owin)):
            k.tt(ob, ob, gts[:, :, br * 8:(br + 1) * 8].unsqueeze(3).to_broadcast([128, 4, 8, 64]),
                 ALU.mult, eng=("pool" if br == 1 else "dve"))
        k.tt(ocmp, ocmp, osel, ALU.add)
        k.tt(ocmp, ocmp, owin, ALU.add, eng="pool")
        if B_STAGE < 5:
            continue
        k.tt(osq, ocmp, ocmp, ALU.mult)
        k.rsum(oss, osq.rearrange("p s h d -> p (s h) d"))
        rms_rstd(k, ors, oss, 64)
        k.tt(ocmp.rearrange("p s h d -> p (s h) d"), ocmp.rearrange("p s h d -> p (s h) d"),
             ors.unsqueeze(2).to_broadcast([128, 32, 64]), ALU.mult)
        k.tt(onb.rearrange("p s (h d) -> p (s h) d", d=64), ocmp.rearrange("p s h d -> p (s h) d"),
             gout.unsqueeze(1).to_broadcast([128, 32, 64]), ALU.mult, eng="pool")
        if B_STAGE < 6:
            continue
        for s in range(4):
            k.trs([(pTb[:, c, :], onb[:, s, c * 128:(c + 1) * 128]) for c in range(4)], ident)
            k.cp(mixs, pTb)
            if B_STAGE >= 7:
                k.dma(mixv[:, 4:8, t0 + s * 128:t0 + (s + 1) * 128], mixs)


def load_colvec(k, dst, src, n_chunks, identf, pTf, tmp):
    k.dma(tmp[0:n_chunks, :], src.rearrange("(c p) -> c p", p=128))
    k.trs([(pTf[:, 0:n_chunks], tmp[0:n_chunks, :])], identf[0:n_chunks, 0:n_chunks])
    k.cp(dst, pTf[:, 0:n_chunks])


def phase_c(nc, k, T, x, MIXT, w_out, ffn_g, w_up, conv_w, conv_b, w_down, H2, ident, identf):
    TT = 256
    NTT = T // TT
    NF = 22
    wo = k.sb("c_wo", [128, 8, D], BF16)
    wu = k.sb("c_wu", [128, 8, 2 * DFF], BF16)
    wd = k.sb("c_wd", [128, NF, D], BF16)
    wov = w_out.rearrange("(c p) n -> p c n", p=128)
    wuv = w_up.rearrange("(c p) n -> p c n", p=128)
    wdv = w_down.rearrange("(c p) n -> p c n", p=128)
    for c in range(8):
        k.dma(wo[:, c, :], wov[:, c, :], q="pool")
    for c in range(8):
        for hh in range(2):
            k.dma(wu[:, c, hh * DFF:(hh + 1) * DFF], wuv[:, c, hh * DFF:(hh + 1) * DFF], q="pool")
    for c in range(NF):
        k.dma(wd[:, c, :], wdv[:, c, :], q="pool")
    pY = [k.ps("c_pY%d" % i, [128, 512]) for i in range(2)]
    pC = k.ps("c_pC", [128, 8, 128], BF16)
    pUs = [k.ps("c_pU%d" % i, [128, 512]) for i in range(4)]
    tmpv = k.sb("c_tmpv", [44, 128])
    gfT = k.sb("c_gfT", [128, 8])
    load_colvec(k, gfT, ffn_g, 8, identf, pY[0], tmpv)
    cw = k.sb("c_cw", [128, 3, 44])
    cb = k.sb("c_cb", [128, 44])
    for j in range(3):
        load_colvec(k, cw[:, j, :], conv_w[j], 44, identf, pY[0], tmpv)
    load_colvec(k, cb, conv_b, 44, identf, pY[0], tmpv)
    carry = k.sb("c_carry", [128, 44, 2])
    k.memset(carry, 0.0)

    mts = [k.sb("c_mt%d" % i, [128, 8, TT], BF16) for i in range(2)]
    hsbs = [k.sb("c_h%d" % i, [128, 2, D]) for i in range(2)]
    junk1 = k.sb("c_junk", [128, D], BF16)
    junks = [junk1, junk1]
    sss = [k.sb("c_ss%d" % i, [128, 1]) for i in range(2)]
    rstds = [k.sb("c_rstd%d" % i, [128, 1]) for i in range(2)]
    hns = [k.sb("c_hn%d" % i, [128, D], BF16) for i in range(2)]
    hnT = k.sb("c_hnT", [128, 8, TT], BF16)
    actT = k.sb("c_actT", [128, NF, TT], BF16)
    NU = 5
    uraw = [k.sb("c_uraw%d" % i, [128, TT + 2]) for i in range(NU)]
    acc = [k.sb("c_acc%d" % i, [128, TT]) for i in range(NU)]
    sil = [k.sb("c_sil%d" % i, [128, TT]) for i in range(2)]
    psl = [p[:, 0:256] for p in pUs] + [pY[0][:, 0:256], pY[1][:, 0:256]]
    mixv = MIXT.rearrange("(c p) t -> p c t", p=128)
    xv = x.rearrange("(n p) d -> p n d", p=128)
    h2v = H2.rearrange("(n p) d -> p n d", p=128)

    def pro_gen(it, s):
        mt, hsb = mts[it % 2], hsbs[it % 2]
        junk, ss, rstd, hn = junks[s], sss[s], rstds[s], hns[s]
        for hh in range(2):
            py = pY[hh]
            k.mm(py, [(mt[:, c, s * 128:(s + 1) * 128], wo[:, c, hh * 512:(hh + 1) * 512])
                      for c in range(8)])
            yield
            k.tt(hsb[:, s, hh * 512:(hh + 1) * 512], py, hsb[:, s, hh * 512:(hh + 1) * 512], ALU.add)
            yield
        k.act(junk, hsb[:, s, :], AF.Square, accum_out=ss)
        yield
        k.act(rstd, ss, AF.Ln, scale=1.0 / D, bias=EPS)
        yield
        k.act(rstd, rstd, AF.Exp, scale=-0.5)
        yield
        k.act(hn, hsb[:, s, :], AF.Copy, scale=rstd)
        yield
        k.trs([(pC[:, c, :], hn[:, c * 128:(c + 1) * 128]) for c in range(8)], ident)
        yield
        k.tt(hnT[:, :, s * 128:(s + 1) * 128], pC, gfT.unsqueeze(2).to_broadcast([128, 8, 128]),
             ALU.mult)
        yield

    def down_gen(it):
        hsb = hsbs[it % 2]
        for s in range(2):
            for hh in range(2):
                py = pUs[(s * 2 + hh) % 2]
                k.mm(py, [(actT[:, i, s * 128:(s + 1) * 128], wd[:, i, hh * 512:(hh + 1) * 512])
                          for i in range(NF)])
                yield
                k.tt(hsb[:, s, hh * 512:(hh + 1) * 512], py, hsb[:, s, hh * 512:(hh + 1) * 512], ALU.add)
                yield
        k.dma(h2v[:, 2 * it:2 * it + 2, :], hsb)
        yield

    def half_gen(n):
        i, gu = divmod(n, 2)
        ch = i + gu * NF
        pu = psl[n % 6]
        ur, ac = uraw[n % NU], acc[n % NU]
        k.mm(pu, [(wu[:, c, ch * 128:(ch + 1) * 128], hnT[:, c, :]) for c in range(8)])
        yield
        k.cp(ur[:, 0:2], carry[:, ch, :], eng="pool")
        k.cp(ur[:, 2:TT + 2], pu, eng="act")
        yield
        k.act(ac, pu, AF.Identity, scale=cw[:, 2, ch:ch + 1], bias=cb[:, ch:ch + 1])
        yield
        k.stt(ac, ur[:, 1:TT + 1], cw[:, 1, ch:ch + 1], ac, ALU.mult, ALU.add)
        yield
        k.stt(ac, ur[:, 0:TT], cw[:, 0, ch:ch + 1], ac, ALU.mult, ALU.add)
        k.cp(carry[:, ch, :], ur[:, TT:TT + 2], eng="pool")
        yield
        if gu == 1:
            sl = sil[i % 2]
            k.act(sl, acc[(n - 1) % NU], AF.Silu)
            yield
            k.tt(actT[:, i, :], sl, ac, ALU.mult, eng="pool")
            yield

    for it in range(NTT + 1):
        gens = []
        if it >= 1:
            gens.append(down_gen(it - 1))
        if it < NTT:
            t0 = it * TT
            k.dma(mts[it % 2], mixv[:, :, t0:t0 + TT])
            k.dma(hsbs[it % 2], xv[:, 2 * it:2 * it + 2, :])
            gens += [pro_gen(it, 0), pro_gen(it, 1)]
        pipeline(iter(gens), 3)
        if it < NTT:
            pipeline((half_gen(n) for n in range(2 * NF)), 4)


def phase_d(nc, k, T, H2, p_in, pleg_g, w_pleg, w_ple, ple_g, out, ident, identf):
    NT = T // 128
    wg = k.sb("d_wg", [128, 8, D], BF16)
    wp = k.sb("d_wp", [128, 2, D], BF16)
    wgv = w_pleg.rearrange("(c p) n -> p c n", p=128)
    wpv = w_ple.rearrange("(c p) n -> p c n", p=128)
    for c in range(8):
        k.dma(wg[:, c, :], wgv[:, c, :], q="pool")
    for c in range(2):
        k.dma(wp[:, c, :], wpv[:, c, :], q="pool")
    pY = [k.ps("d_pY%d" % i, [128, 512]) for i in range(2)]
    pE4 = [k.ps("d_pE%d" % i, [128, 512]) for i in range(4)]
    pC = k.ps("d_pC", [128, 8, 128], BF16)
    tmpv = k.sb("d_tmpv", [8, 128])
    ggT = k.sb("d_ggT", [128, 8])
    load_colvec(k, ggT, pleg_g, 8, identf, pY[0], tmpv)
    gple = k.sb("d_gple", [128, D])
    k.dma(gple, ple_g.partition_broadcast(128))

    hb = [k.sb("d_h%d" % i, [128, D]) for i in range(2)]
    pb = [k.sb("d_p%d" % i, [128, 256]) for i in range(2)]
    junks = [k.sb("d_junk%d" % i, [128, D], BF16) for i in range(2)]
    sss = [k.sb("d_ss%d" % i, [128, 1]) for i in range(2)]
    rstds = [k.sb("d_rstd%d" % i, [128, 1]) for i in range(2)]
    ss2s = [k.sb("d_ss2%d" % i, [128, 2]) for i in range(2)]
    rstd2s = [k.sb("d_rstd2%d" % i, [128, 1]) for i in range(2)]
    hns = [k.sb("d_hn%d" % i, [128, D], BF16) for i in range(2)]
    hTs = [k.sb("d_hT%d" % i, [128, 8, 128], BF16) for i in range(2)]
    pbfs = [k.sb("d_pbf%d" % i, [128, 256], BF16) for i in range(2)]
    pTs = [k.sb("d_pT%d" % i, [128, 2, 128], BF16) for i in range(2)]
    gates = [k.sb("d_gate%d" % i, [128, D]) for i in range(2)]
    es = [k.sb("d_e%d" % i, [128, D]) for i in range(2)]
    ob = [k.sb("d_o%d" % i, [128, D]) for i in range(2)]

    def d_gen(it):
        t0 = it * 128
        b = it % 2
        h, pp, o = hb[b], pb[b], ob[b]
        junk, ss, rstd, ss2, rstd2 = junks[b], sss[b], rstds[b], ss2s[b], rstd2s[b]
        hn, hT, pbf, pT, gate, e = hns[b], hTs[b], pbfs[b], pTs[b], gates[b], es[b]
        pE = pE4[2 * b:2 * b + 2]
        k.dma(h, H2[t0:t0 + 128, :])
        k.dma(pp, p_in[t0:t0 + 128, :])
        yield
        k.act(junk, h, AF.Square, accum_out=ss)
        yield
        k.act(rstd, ss, AF.Ln, scale=1.0 / D, bias=EPS)
        yield
        k.act(rstd, rstd, AF.Exp, scale=-0.5)
        yield
        k.act(hn, h, AF.Copy, scale=rstd)
        yield
        k.trs([(pC[:, c, :], hn[:, c * 128:(c + 1) * 128]) for c in range(8)], ident)
        yield
        k.tt(hT, pC, ggT.unsqueeze(2).to_broadcast([128, 8, 128]), ALU.mult)
        yield
        k.cp(pbf, pp, eng="pool")
        yield
        k.trs([(pC[:, c, :], pbf[:, c * 128:(c + 1) * 128]) for c in range(2)], ident)
        yield
        k.cp(pT, pC[:, 0:2, :])
        yield
        for hh in range(2):
            k.mm(pY[hh], [(hT[:, c, :], wg[:, c, hh * 512:(hh + 1) * 512]) for c in range(8)])
            yield
            k.act(gate[:, hh * 512:(hh + 1) * 512], pY[hh], AF.Sigmoid)
            yield
        for hh in range(2):
            k.mm(pE[hh], [(pT[:, c, :], wp[:, c, hh * 512:(hh + 1) * 512]) for c in range(2)])
            yield
            k.act(junk[:, hh * 512:(hh + 1) * 512], pE[hh], AF.Square, accum_out=ss2[:, hh:hh + 1])
            yield
        k.tt(ss, ss2[:, 0:1], ss2[:, 1:2], ALU.add)
        yield
        k.act(rstd2, ss, AF.Ln, scale=1.0 / D, bias=EPS)
        yield
        k.act(rstd2, rstd2, AF.Exp, scale=-0.5)
        yield
        for hh in range(2):
            k.act(e[:, hh * 512:(hh + 1) * 512], pE[hh], AF.Copy, scale=rstd2)
            yield
        k.tt(e, e, gple, ALU.mult)
        yield
        k.tt(e, e, gate, ALU.mult, eng="pool")
        yield
        k.tt(o, e, h, ALU.add)
        yield
        tok = k.dma(out[t0:t0 + 128, :], o)
        k.out_toks.append(tok)
        yield

    pipeline((d_gen(it) for it in range(NT)), D_DEPTH)


def _consts(T):
    bf = ml_dtypes.bfloat16
    c = {}
    c["c_ident"] = np.eye(128).astype(bf)
    c["c_identf"] = np.eye(128, dtype=np.float32)
    half = 8
    inv = np.float32(500000.0) ** (-np.arange(half, dtype=np.float32) / half)
    ang = np.arange(T, dtype=np.float32)[:, None] * inv[None, :].astype(np.float32)
    c["c_rope"] = np.concatenate([np.cos(ang), np.sin(ang)], 1).astype(np.float32)
    s = np.arange(128)
    c["c_tri2"] = ((s[:, None] <= s[None, :]) & (s[:, None] // 64 == s[None, :] // 64)).astype(np.float32)
    c["c_chunk"] = (s[:, None] // 64 == np.arange(2)[None, :]).astype(np.float32)
    t = np.arange(T)
    cc = np.arange(256)
    ncmp = T // 16 - 1
    c["c_cmask"] = (((16 * cc[:, None] + 31) <= t[None, :]) & (cc[:, None] < ncmp)).astype(bf)
    c["c_tri"] = (s[:, None] <= s[None, :]).astype(bf)
    c["c_ntri"] = (s[None, :] < s[:, None]).astype(bf)
    n = np.arange(64)
    c["c_E"] = ((t[None, :] // 64) == n[:, None]).astype(bf)
    cur = t // 64
    vis = (n[None, :] * 64 <= t[:, None])
    bonus = np.zeros((T, 64), np.float32)
    bonus += (n[None, :] == 0) * 1.0e6
    bonus += (n[None, :] == cur[:, None]) * 2.0e6
    bonus += (n[None, :] == cur[:, None] - 1) * 4.0e6
    c["c_vis"] = vis.astype(np.float32)
    c["c_cadd"] = np.where(vis, bonus, np.float32(-1e30)).astype(np.float32)
    cs = cc * 16
    ssb = n * 64
    ov = np.clip(np.minimum(cs[:, None] + 32, ssb[None, :] + 64)
                 - np.maximum(cs[:, None], ssb[None, :]), 0, None) / 32.0
    o1 = np.zeros((256, 65), np.float32)
    o1[:, 0] = 1.0
    o1[:, 1:] = ov
    o1[ncmp:] = 0.0
    c["c_ovl1"] = o1.astype(bf)
    return c


_W_NAMES = ["attn_norm_g", "w_in", "hg_norm_g", "nsa_q_norm_g", "nsa_k_norm_g", "cmp_pe", "cmp_w1",
            "cmp_w2", "nsa_out_norm_g", "w_out", "ffn_norm_g", "w_up", "conv_w", "conv_b", "w_down",
            "ple_gate_norm_g", "w_ple_gate", "w_ple", "ple_norm_g"]


def kernel(**inputs):
    x = np.asarray(inputs["x"], np.float32)
    p = np.asarray(inputs["p"], np.float32)
    B, T, _ = x.shape
    nc = build(T=T, dbg=False, phases="ABCD")
    shared = {n: np.ascontiguousarray(np.asarray(inputs[n], np.float32)[0]) for n in _W_NAMES}
    shared["hg_lb_logits"] = np.ascontiguousarray(np.asarray(inputs["hg_lb_logits"], np.float32))
    shared.update(_consts(T))
    in_maps = []
    for b in range(B):
        m = dict(shared)
        m["x"] = np.ascontiguousarray(x[b])
        m["p"] = np.ascontiguousarray(p[0, b])
        in_maps.append(m)
    res = run_bass_kernel_spmd(nc, in_maps, core_ids=list(range(B)))
    return np.stack([np.asarray(r["out"], np.float32) for r in res.results], axis=0)
```

```python
from contextlib import ExitStack
import numpy as np
import ml_dtypes
import concourse.bass as bass
import concourse.mybir as mybir
from concourse.bass_utils import run_bass_kernel_spmd

F32 = mybir.dt.float32
BF16 = mybir.dt.bfloat16
ALU = mybir.AluOpType
AF = mybir.ActivationFunctionType
AX = mybir.AxisListType

D = 1024
IN_TOTAL = 3352
DFF = 2816
EPS = 1e-6
NEGM = -30000.0
B_STAGE = 9
FFN_DEPTH = 2
D_DEPTH = 2
PRO_DEPTH = 2


class Sched:
    def __init__(self, nc, n_dma_sems=48):
        self.nc = nc
        self.engs = {"pe": nc.tensor, "act": nc.scalar, "dve": nc.vector,
                     "pool": nc.gpsimd, "sp": nc.sync}
        self.sem = {}
        self.cnt = {}
        for k in ("pe", "act", "dve", "pool"):
            self.sem[k] = nc.alloc_semaphore("s_" + k)
            self.cnt[k] = 0
        self.dma_sems = [nc.alloc_semaphore("s_dma%d" % i) for i in range(n_dma_sems)]
        self.dma_val = [0] * n_dma_sems
        self.dma_rr = 0
        self.waited = {}
        self.last_w = {}
        self.readers = {}
        self.nwaits = 0
        self.inflight = {}
        self.max_desc = 600

    def _wait(self, eng, tok):
        sem, val, key = tok
        if key == eng and eng == "pe":
            return
        k = (eng, key)
        if self.waited.get(k, 0) >= val:
            return
        self.engs[eng].wait_ge(sem, val)
        self.nwaits += 1
        self.waited[k] = val

    def deps(self, eng, reads, writes):
        toks = []
        for r in reads:
            t = self.last_w.get(r)
            if t is not None:
                toks.append(t)
        for w in writes:
            t = self.last_w.get(w)
            if t is not None:
                toks.append(t)
            toks.extend(self.readers.get(w, ()))
        for t in toks:
            self._wait(eng, t)

    def commit(self, tok, reads, writes):
        for w in writes:
            self.last_w[w] = tok
            self.readers[w] = []
        for r in reads:
            if r in writes:
                continue
            lst = self.readers.setdefault(r, [])
            lst[:] = [t for t in lst if t[2] != tok[2]]
            lst.append(tok)

    def op(self, eng, reads, writes, fn):
        self.deps(eng, reads, writes)
        ins = fn(self.engs[eng])
        self.cnt[eng] += 1
        ins.then_inc(self.sem[eng], 1)
        tok = (self.sem[eng], self.cnt[eng], eng)
        self.commit(tok, reads, writes)
        return tok

    @staticmethod
    def _ndesc(ap):
        dims = list(ap.ap)
        total = 1
        for st, n in dims:
            total *= n
        run = 1
        for st, n in reversed(dims[1:]):
            if st == run:
                run *= n
            else:
                break
        return max(1, total // max(run, 1))

    def dma(self, out, in_, reads, writes, q="sp", **kw):
        nd = max(self._ndesc(out), self._ndesc(in_))
        fifo = self.inflight.setdefault(q, [])
        while fifo and sum(d for _, d in fifo) + nd > self.max_desc:
            tok0, _ = fifo.pop(0)
            self._wait(q, tok0)
        tok = self._dma(out, in_, reads, writes, q, **kw)
        fifo.append((tok, nd))
        return tok

    def _dma(self, out, in_, reads, writes, q="sp", **kw):
        i = self.dma_rr
        self.dma_rr = (self.dma_rr + 1) % len(self.dma_sems)
        sem = self.dma_sems[i]
        key = "dma%d" % i
        if self.dma_val[i] > 0:
            self._wait(q, (sem, self.dma_val[i], key))
        self.deps(q, reads, writes)
        self.dma_val[i] += 16
        self.engs[q].dma_start(out=out, in_=in_, **kw).then_inc(sem, 16)
        tok = (sem, self.dma_val[i], key)
        self.commit(tok, reads, writes)
        return tok


_TAGS = {}
_KEEP = []


def tag(ap, name):
    _TAGS[id(ap)] = name
    _KEEP.append(ap)
    return ap


def sub(ap, fn):
    r = fn(ap)
    if id(ap) in _TAGS:
        tag(r, _TAGS[id(ap)])
    return r


def pipeline(gens, depth):
    gens = iter(gens)
    active = []
    exhausted = False
    while True:
        if not exhausted and len(active) < depth:
            g = next(gens, None)
            if g is None:
                exhausted = True
            else:
                active.append(g)
        if not active:
            if exhausted:
                break
            continue
        for g in list(active):
            try:
                next(g)
            except StopIteration:
                active.remove(g)


def _names(aps):
    out = []
    for a in aps:
        if a is None or isinstance(a, (int, float)):
            continue
        n = _TAGS.get(id(a)) or a.name
        if n not in out:
            out.append(n)
    return out


class K:
    def __init__(self, nc):
        self.nc = nc
        self.S = Sched(nc)
        self.out_toks = []
        self.es = None

    def sb(self, name, shape, dt=F32):
        return self.es.enter_context(self.nc.sbuf_tensor(name, list(shape), dt))[:]

    def ps(self, name, shape, dt=F32):
        return self.es.enter_context(self.nc.psum_tensor(name, list(shape), dt))[:]

    def barrier(self):
        S = self.S
        toks = [(S.sem[e], S.cnt[e], e) for e in ("pe", "act", "dve", "pool") if S.cnt[e] > 0]
        toks += [(S.dma_sems[i], S.dma_val[i], "dma%d" % i) for i in range(len(S.dma_sems))
                 if S.dma_val[i] > 0]
        for eng in ("sp", "pe", "act", "dve", "pool"):
            for t in toks:
                S._wait(eng, t)
        S.last_w.clear()
        S.readers.clear()

    def mm1(self, out, lhsT, rhs, start, stop):
        return self.S.op("pe", _names([lhsT, rhs]), _names([out]),
                         lambda e: e.matmul(out, lhsT=lhsT, rhs=rhs, start=start, stop=stop))

    def vmax(self, out, in_):
        return self.S.op("dve", _names([in_]), _names([out]), lambda e: e.max(out=out, in_=in_))

    def vmatch(self, out, mx, vals, imm):
        return self.S.op("dve", _names([mx, vals]), _names([out]),
                         lambda e: e.match_replace(out=out, in_to_replace=mx, in_values=vals,
                                                   imm_value=imm))

    def recip(self, out, in_):
        return self.S.op("dve", _names([in_]), _names([out]), lambda e: e.reciprocal(out=out, in_=in_))

    def act(self, out, in_, func, bias=None, scale=None, accum_out=None, eng="act"):
        kw = {}
        if bias is not None:
            kw["bias"] = bias
        if scale is not None:
            kw["scale"] = scale
        if accum_out is not None:
            kw["accum_out"] = accum_out
        rd = _names([in_, bias, scale])
        wr = _names([out, accum_out])
        return self.S.op(eng, rd, wr, lambda e: e.activation(out=out, in_=in_, func=func, **kw))

    def tt(self, out, in0, in1, op, eng="dve"):
        return self.S.op(eng, _names([in0, in1]), _names([out]),
                         lambda e: e.tensor_tensor(out=out, in0=in0, in1=in1, op=op))

    def ts(self, out, in0, s1, s2, op0, op1=None, eng="dve"):
        def f(e):
            if op1 is None:
                return e.tensor_scalar(out=out, in0=in0, scalar1=s1, scalar2=None, op0=op0)
            return e.tensor_scalar(out=out, in0=in0, scalar1=s1, scalar2=s2, op0=op0, op1=op1)
        return self.S.op(eng, _names([in0, s1, s2]), _names([out]), f)

    def stt(self, out, in0, scalar, in1, op0, op1, eng="dve"):
        return self.S.op(eng, _names([in0, scalar, in1]), _names([out]),
                         lambda e: e.scalar_tensor_tensor(out=out, in0=in0, scalar=scalar, in1=in1,
                                                          op0=op0, op1=op1))

    def cp(self, out, in_, eng="dve"):
        if eng == "act":
            return self.S.op("act", _names([in_]), _names([out]), lambda e: e.copy(out=out, in_=in_))
        return self.S.op(eng, _names([in_]), _names([out]), lambda e: e.tensor_copy(out=out, in_=in_))

    def rsum(self, out, in_, eng="dve"):
        return self.S.op(eng, _names([in_]), _names([out]),
                         lambda e: e.reduce_sum(out=out, in_=in_, axis=AX.X))

    def memset(self, out, val, eng="dve"):
        return self.S.op(eng, [], _names([out]), lambda e: e.memset(out, val))

    def mm(self, out, pairs, extra_w=()):
        rd = _names([a for p in pairs for a in p])
        n = len(pairs)

        def f(e):
            for i, (l, r) in enumerate(pairs):
                ins = e.matmul(out, lhsT=l, rhs=r, start=(i == 0), stop=(i == n - 1))
            return ins
        return self.S.op("pe", rd, _names([out]) + list(extra_w), f)

    def mms(self, groups):
        rd, wr = [], []
        for out, pairs in groups:
            wr += _names([out])
            rd += _names([a for p in pairs for a in p])

        def f(e):
            for out, pairs in groups:
                n = len(pairs)
                for i, (l, r) in enumerate(pairs):
                    ins = e.matmul(out, lhsT=l, rhs=r, start=(i == 0), stop=(i == n - 1))
            return ins
        return self.S.op("pe", list(dict.fromkeys(rd)), list(dict.fromkeys(wr)), f)

    def trs(self, items, ident):
        rd = _names([i for _, i in items] + [ident])
        wr = _names([o for o, _ in items])

        def f(e):
            for o, i in items:
                ins = e.transpose(out=o, in_=i, identity=ident)
            return ins
        return self.S.op("pe", rd, wr, f)

    def dma(self, out, in_, q="sp", **kw):
        return self.S.dma(out, in_, _names([in_]), _names([out]), q=q, **kw)

    def finish(self):
        for t in self.out_toks:
            self.S._wait("sp", t)


def rms_rstd(k, out, ss, n):
    k.act(out, ss, AF.Ln, scale=1.0 / n, bias=EPS)
    k.act(out, out, AF.Exp, scale=-0.5)


def build(T=4096, dbg=False, phases="ABCD"):
    NT = T // 128
    nc = bass.Bass("TRN2", target_bir_lowering=False)
    k = K(nc)

    def din(name, shape, dt=F32):
        return nc.dram_tensor(name, list(shape), dt, kind="ExternalInput").ap()

    def dscr(name, shape, dt):
        return nc.dram_tensor(name, list(shape), dt, kind=("ExternalOutput" if dbg else "Internal")).ap()

    x = din("x", [T, D])
    p_in = din("p", [T, 256])
    attn_g = din("attn_norm_g", [D])
    w_in = din("w_in", [D, IN_TOTAL])
    lb_logits = din("hg_lb_logits", [2, 512])
    hg_norm_g = din("hg_norm_g", [128])
    q_norm_g = din("nsa_q_norm_g", [64])
    k_norm_g = din("nsa_k_norm_g", [3, 64])
    cmp_pe = din("cmp_pe", [2, 32, 64])
    cmp_w1 = din("cmp_w1", [2, 2048, 128])
    cmp_w2 = din("cmp_w2", [2, 128, 64])
    out_norm_g = din("nsa_out_norm_g", [64])
    w_out = din("w_out", [D, D])
    ffn_g = din("ffn_norm_g", [D])
    w_up = din("w_up", [D, 2 * DFF])
    conv_w = din("conv_w", [3, 2 * DFF])
    conv_b = din("conv_b", [2 * DFF])
    w_down = din("w_down", [DFF, D])
    pleg_g = din("ple_gate_norm_g", [D])
    w_pleg = din("w_ple_gate", [D, D])
    w_ple = din("w_ple", [256, D])
    ple_g = din("ple_norm_g", [D])
    c_ident = din("c_ident", [128, 128], BF16)
    c_rope = din("c_rope", [T, 16])
    c_tri2 = din("c_tri2", [128, 128])
    c_chunk = din("c_chunk", [128, 2])

    out = nc.dram_tensor("out", [T, D], F32, kind="ExternalOutput").ap()

    FT = dscr("FT", [16, 64, T], BF16)
    VT = dscr("VT", [T, 256], BF16)
    GT = dscr("GT", [T, 24], F32)
    MIXT = dscr("MIXT", [D, T], BF16)

    c_identf = din("c_identf", [128, 128])
    c_cmask = din("c_cmask", [256, T], BF16)
    c_tri = din("c_tri", [128, 128], BF16)
    c_ntri = din("c_ntri", [128, 128], BF16)
    c_E = din("c_E", [64, T], BF16)
    c_vis = din("c_vis", [T, 64])
    c_cadd = din("c_cadd", [T, 64])
    c_ovl1 = din("c_ovl1", [256, 65], BF16)
    H2 = dscr("H2", [T, D], F32)

    with ExitStack() as es0:
        k.es = es0
        ident = k.sb("ident", [128, 128], BF16)
        k.dma(ident, c_ident)
        identf = k.sb("identf", [128, 128])
        k.dma(identf, c_identf)
        if "A" in phases:
            with ExitStack() as es:
                k.es = es
                phase_a(nc, k, T, NT, x, attn_g, w_in, lb_logits, hg_norm_g, q_norm_g, k_norm_g,
                        c_rope, c_tri2, c_chunk, ident, FT, VT, GT, MIXT)
                k.barrier()
        if "B" in phases:
            with ExitStack() as es:
                k.es = es
                phase_b(nc, k, T, NT, FT, VT, GT, MIXT, k_norm_g, cmp_pe, cmp_w1, cmp_w2, out_norm_g,
                        c_rope, c_cmask, c_tri, c_ntri, c_E, c_vis, c_cadd, c_ovl1, ident, identf)
                k.barrier()
        if "A" not in phases and dbg:
            mi = din("MIXT_in", [D, T], BF16)
            with ExitStack() as es:
                k.es = es
                tb = k.sb("dbg_mix", [128, 8, T], BF16)
                k.dma(tb, mi.rearrange("(c p) t -> p c t", p=128))
                k.dma(MIXT.rearrange("(c p) t -> p c t", p=128), tb)
                k.barrier()
        if "C" in phases:
            with ExitStack() as es:
                k.es = es
                phase_c(nc, k, T, x, MIXT, w_out, ffn_g, w_up, conv_w, conv_b, w_down, H2, ident, identf)
                k.barrier()
        if "D" in phases:
            with ExitStack() as es:
                k.es = es
                phase_d(nc, k, T, H2, p_in, pleg_g, w_pleg, w_ple, ple_g, out, ident, identf)
                k.barrier()
        k.finish()
    return nc


def phase_a(nc, k, T, NT, x, attn_g, w_in, lb_logits, hg_norm_g, q_norm_g, k_norm_g,
            c_rope, c_tri2, c_chunk, ident, FT, VT, GT, MIXT):
    S = k.S
    w_sb = k.sb("a_w", [128, 8, IN_TOTAL], BF16)
    w_v = w_in.rearrange("(c p) n -> p c n", p=128)
    for c in range(8):
        k.dma(w_sb[:, c, :], w_v[:, c, :], q="pool")
    gT = k.sb("a_gT", [128, 8])
    k.dma(gT, attn_g.rearrange("(c p) -> p c", p=128), allow_slow_non_contiguous=True)
    rope = k.sb("a_rope", [128, NT, 16])
    k.dma(rope, c_rope.rearrange("(n p) c -> p n c", p=128))
    tri2 = k.sb("a_tri2", [128, 128])
    k.dma(tri2, c_tri2)
    chunk = k.sb("a_chunk", [128, 2])
    k.dma(chunk, c_chunk)
    l0 = k.sb("a_l0", [128, 512])
    l1 = k.sb("a_l1", [128, 512])
    k.dma(l0, lb_logits[0].partition_broadcast(128))
    k.dma(l1, lb_logits[1].partition_broadcast(128))
    lb = k.sb("a_lb", [128, 512])
    oml = k.sb("a_oml", [128, 512])
    k.tt(l0, l0, l1, ALU.subtract)
    k.act(lb, l0, AF.Sigmoid)
    k.ts(oml, lb, -1.0, 1.0, ALU.mult, ALU.add)
    gq = k.sb("a_gq", [128, 12, 64])
    for h in range(12):
        src = q_norm_g if h < 8 else (k_norm_g[1] if h < 10 else k_norm_g[2])
        k.dma(gq[:, h, :], src.partition_broadcast(128))
    ghg = k.sb("a_ghg", [128, 4, 128])
    for h in range(4):
        k.dma(ghg[:, h, :], hg_norm_g.partition_broadcast(128))
    Sf = k.sb("a_Sf", [128, 4, 128])
    Sb = [k.sb("a_Sb%d" % i, [128, 4, 128], BF16) for i in range(3)]
    k.memset(Sf, 0.0)
    k.memset(Sb[0], 0.0, eng="pool")

    xt = [k.sb("a_x%d" % i, [128, D]) for i in range(2)]
    junk = k.sb("a_junk", [128, D], BF16)
    ss = k.sb("a_ss", [128, 1])
    rstd = k.sb("a_rstd", [128, 1])
    xn = k.sb("a_xn", [128, D], BF16)
    xnT = k.sb("a_xnT", [128, 8, 128], BF16)
    silq = k.sb("a_silq", [128, 512])
    sig = k.sb("a_sig", [128, 512])
    logf = k.sb("a_logf", [128, 512])
    kk = k.sb("a_kk", [128, 512])
    enb = k.sb("a_enb", [128, 512])
    epb = k.sb("a_epb", [128, 512])
    kp = k.sb("a_kp", [128, 512], BF16)
    qp = k.sb("a_qp", [128, 512], BF16)
    vb = k.sb("a_vb", [128, 512], BF16)
    sg = k.sb("a_sg", [128, 512])
    ebl = k.sb("a_ebl", [128, 4, 2])
    qkT = k.sb("a_qkT", [128, 8, 128], BF16)
    ATm = k.sb("a_ATm", [128, 4, 128], BF16)
    tmpS = k.sb("a_tmpS", [128, 4, 128])
    osb = k.sb("a_osb", [128, 4, 128])
    osq = k.sb("a_osq", [128, 4, 128])
    ss4 = k.sb("a_ss4", [128, 4])
    rstd4 = k.sb("a_rstd4", [128, 4])
    onb = k.sb("a_onb", [128, 4, 128], BF16)
    mixs = k.sb("a_mixs", [128, 4, 128], BF16)
    qk = k.sb("a_qk", [128, 12, 64])
    qsq = k.sb("a_qsq", [128, 12, 64])
    ss12 = k.sb("a_ss12", [128, 12])
    rstd12 = k.sb("a_rstd12", [128, 12])
    qkb = k.sb("a_qkb", [128, 12, 64], BF16)
    r_a = k.sb("a_ra", [128, 12, 8])
    r_b = k.sb("a_rb", [128, 12, 8])
    r_c = k.sb("a_rc", [128, 12, 8])
    r_d = k.sb("a_rd", [128, 12, 8])
    kcvc = k.sb("a_kcvc", [128, 256], BF16)
    T16 = k.sb("a_T16", [64, 16, 128], BF16)
    vv = k.sb("a_vv", [128, 256], BF16)
    gsb = k.sb("a_gsb", [128, 24])

    pA = [k.ps("a_pA%d" % i, [128, 512]) for i in range(2)]
    pB = k.ps("a_pB", [128, 512])
    pC = k.ps("a_pC", [128, 8, 128], BF16)
    pD = k.ps("a_pD", [64, 16, 128], BF16)
    pF = k.ps("a_pF", [128, 4, 128])
    pG = k.ps("a_pG", [128, 4, 128])

    cols = [(0, 512), (512, 512), (1024, 512), (1536, 512), (2048, 512), (2560, 512), (3072, 280)]
    mixv = MIXT.rearrange("(c p) t -> p c t", p=128)
    ftv = FT.rearrange("n d t -> d n t")
    xnT2 = [xnT, k.sb("a_xnT1", [128, 8, 128], BF16)]

    def proj(g, dst, xT):
        c0, n = cols[g]
        k.mm(dst[:, 0:n], [(xT[:, c, :], w_sb[:, c, c0:c0 + n]) for c in range(8)])
        return dst

    def genH(it):
        t0 = it * 128
        xb = xt[it % 2]
        xT = xnT2[it % 2]
        pAh = pA[0]
        if it == 0:
            k.dma(xb, x[t0:t0 + 128, :])
        if it + 1 < NT:
            k.dma(xt[(it + 1) % 2], x[t0 + 128:t0 + 256, :])
        yield
        k.act(junk, xb, AF.Square, accum_out=ss)
        yield
        k.act(rstd, ss, AF.Ln, scale=1.0 / D, bias=EPS)
        yield
        k.act(rstd, rstd, AF.Exp, scale=-0.5)
        yield
        k.act(xn, xb, AF.Copy, scale=rstd)
        yield
        k.trs([(pC[:, c, :], xn[:, c * 128:(c + 1) * 128]) for c in range(8)], ident)
        yield
        k.tt(xT, pC, gT.unsqueeze(2).to_broadcast([128, 8, 128]), ALU.mult)
        yield
        d = proj(1, pAh, xT)
        yield
        k.act(sig, d, AF.Sigmoid)
        yield
        k.tt(sig, sig, oml, ALU.mult)
        yield
        k.tt(sig, sig, lb, ALU.add)
        yield
        k.act(logf, sig, AF.Ln)
        yield
        k.ts(kk, sig, -1.0, 1.0, ALU.mult, ALU.add)
        yield
        k.mm(pB, [(tri2, logf)])
        yield
        k.act(enb, pB, AF.Exp, scale=-1.0)
        yield
        k.act(epb, pB, AF.Exp)
        yield
        k.tt(kp, kk, enb, ALU.mult)
        yield
        k.mms([(pF[:, h, 0:2], [(logf[:, h * 128:(h + 1) * 128], chunk)]) for h in range(4)])
        yield
        k.act(ebl, pF[:, :, 0:2], AF.Exp)
        yield
        d = proj(0, pAh, xT)
        yield
        k.act(silq, d, AF.Silu)
        yield
        k.stt(qp, silq, 128 ** -0.5, epb, ALU.mult, ALU.mult)
        yield
        d = proj(2, pAh, xT)
        yield
        k.cp(vb, d, eng="act")
        yield
        d = proj(3, pAh, xT)
        yield
        k.act(sg, d, AF.Silu)
        yield
        k.tt(sg, sg, ghg.rearrange("p h v -> p (h v)"), ALU.mult, eng="pool")
        yield
        k.trs([(pC[:, h, :], qp[:, h * 128:(h + 1) * 128]) for h in range(4)] +
              [(pC[:, 4 + h, :], kp[:, h * 128:(h + 1) * 128]) for h in range(4)], ident)
        yield
        k.cp(qkT, pC)
        yield
        S0, S1, S2 = Sb[(2 * it) % 3], Sb[(2 * it + 1) % 3], Sb[(2 * it + 2) % 3]
        k.mms([(pF[:, h, :], [(qkT[:, 4 + h, :], qkT[:, h, :])]) for h in range(4)])
        yield
        k.tt(ATm, pF, tri2.unsqueeze(1).to_broadcast([128, 4, 128]), ALU.mult)
        yield
        for c in range(2):
            rs = slice(c * 64, (c + 1) * 64)
            k.mms([(pF[:, h, :], [(kp[rs, h * 128:(h + 1) * 128], vb[rs, h * 128:(h + 1) * 128])])
                   for h in range(4)])
            yield
            k.tt(tmpS, pF, Sf, ALU.add)
            yield
            k.tt(Sf, tmpS, ebl[:, :, c:c + 1].to_broadcast([128, 4, 128]), ALU.mult)
            yield
            k.cp(S1 if c == 0 else S2, Sf, eng="act")
            yield
        groups = []
        for h in range(4):
            for c in range(2):
                rs = slice(c * 64, (c + 1) * 64)
                Sc = S0 if c == 0 else S1
                groups.append((pG[rs, h, :], [(ATm[rs, h, rs], vb[rs, h * 128:(h + 1) * 128]),
                                              (qkT[:, h, rs], Sc[:, h, :])]))
        k.mms(groups)
        yield
        k.cp(osb, pG, eng="act")
        yield
        k.tt(osq, osb, osb, ALU.mult)
        yield
        k.rsum(ss4, osq)
        yield
        k.act(rstd4, ss4, AF.Ln, scale=1.0 / 128, bias=EPS)
        yield
        k.act(rstd4, rstd4, AF.Exp, scale=-0.5)
        yield
        k.tt(osb, osb, rstd4.unsqueeze(2).to_broadcast([128, 4, 128]), ALU.mult)
        yield
        k.tt(onb, osb, sg.rearrange("p (h v) -> p h v", h=4), ALU.mult)
        yield
        k.trs([(pC[:, h, :], onb[:, h, :]) for h in range(4)], ident)
        yield
        k.cp(mixs, pC[:, 0:4, :])
        yield
        k.dma(mixv[:, 0:4, t0:t0 + 128], mixs)
        yield

    def genN(it):
        t0 = it * 128
        xT = xnT2[it % 2]
        pAn = pA[1]
        d = proj(4, pAn, xT)
        yield
        k.cp(qk[:, 0:8, :], d.rearrange("p (h d) -> p h d", d=64), eng="act")
        yield
        d = proj(5, pAn, xT)
        yield
        k.cp(kcvc, d[:, 0:256], eng="act")
        yield
        k.cp(qk[:, 8:10, :], d[:, 256:384].rearrange("p (h d) -> p h d", d=64), eng="act")
        yield
        k.cp(vv[:, 0:128], d[:, 384:512], eng="act")
        yield
        d = proj(6, pAn, xT)
        yield
        k.cp(qk[:, 10:12, :], d[:, 0:128].rearrange("p (h d) -> p h d", d=64), eng="act")
        yield
        k.cp(vv[:, 128:256], d[:, 128:256], eng="act")
        yield
        k.act(gsb, d[:, 256:280], AF.Sigmoid)
        yield
        k.dma(GT[t0:t0 + 128, :], gsb)
        k.dma(VT[t0:t0 + 128, :], vv)
        yield
        k.tt(qsq, qk, qk, ALU.mult, eng="pool")
        yield
        k.rsum(ss12, qsq)
        yield
        k.act(rstd12, ss12, AF.Ln, scale=1.0 / 64, bias=EPS)
        yield
        k.act(rstd12, rstd12, AF.Exp, scale=-0.5)
        yield
        k.tt(qk, qk, rstd12.unsqueeze(2).to_broadcast([128, 12, 64]), ALU.mult)
        yield
        k.tt(qk, qk, gq, ALU.mult, eng="pool")
        yield
        cosb = rope[:, it:it + 1, 0:8].to_broadcast([128, 12, 8])
        sinb = rope[:, it:it + 1, 8:16].to_broadcast([128, 12, 8])
        k.tt(r_a, qk[:, :, 0:8], cosb, ALU.mult, eng="pool")
        yield
        k.tt(r_b, qk[:, :, 8:16], sinb, ALU.mult, eng="pool")
        yield
        k.tt(r_c, qk[:, :, 8:16], cosb, ALU.mult, eng="pool")
        yield
        k.tt(r_d, qk[:, :, 0:8], sinb, ALU.mult, eng="pool")
        yield
        k.cp(qkb, qk, eng="pool")
        yield
        k.tt(qkb[:, :, 0:8], r_a, r_b, ALU.subtract, eng="pool")
        yield
        k.tt(qkb[:, :, 8:16], r_c, r_d, ALU.add, eng="pool")
        yield
        k.trs([(pD[:, n, :], qkb[:, n, :]) for n in range(12)] +
              [(pD[:, 12 + n, :], kcvc[:, n * 64:(n + 1) * 64]) for n in range(4)], ident)
        yield
        k.cp(T16, pD)
        yield
        k.dma(ftv[:, :, t0:t0 + 128], T16)
        yield

    for it in range(NT + 1):
        gens = []
        if it < NT:
            gens.append(genH(it))
        if it >= 1:
            gens.append(genN(it - 1))
        pipeline(iter(gens), 2)


def phase_b(nc, k, T, NT, FT, VT, GT, MIXT, k_norm_g, cmp_pe, cmp_w1, cmp_w2, out_norm_g,
            c_rope, c_cmask, c_tri, c_ntri, c_E, c_vis, c_cadd, c_ovl1, ident, identf):
    NQ = T // 512
    NCMP = T // 16 - 1
    NCT = (NCMP + 127) // 128

    def crow(ct):
        return min(128, NCMP - 128 * ct)

    KA = k.sb("b_KA", [128, 2, T], BF16)
    KW = k.sb("b_KW", [64, 2, T], BF16)
    VS1 = k.sb("b_VS1", [128, NT, 2, 65], BF16)
    VW1 = k.sb("b_VW1", [128, NT, 2, 65], BF16)
    VTv = VT.rearrange("(n p) c -> p n c", p=128)
    k.memset(VS1, 1.0)
    k.memset(VW1, 1.0, eng="pool")
    for g in range(2):
        k.dma(KA[0:64, g, :], FT[8 + g])
        k.dma(KA[64:128, g, :], c_E)
        k.dma(KW[:, g, :], FT[10 + g])
        k.dma(VS1[:, :, g, 0:64], VTv[:, :, g * 64:(g + 1) * 64])
        k.dma(VW1[:, :, g, 0:64], VTv[:, :, 128 + g * 64:128 + (g + 1) * 64])
    tri = k.sb("b_tri", [128, 128], BF16)
    ntri = k.sb("b_ntri", [128, 128], BF16)
    k.dma(tri, c_tri)
    k.dma(ntri, c_ntri)
    zer = k.sb("b_zer", [128, 65], BF16)
    k.memset(zer, 0.0)
    gout = k.sb("b_gout", [128, 64])
    k.dma(gout, out_norm_g.partition_broadcast(128))

    pS = [k.ps("b_pS%d" % i, [128, 512]) for i in range(2)]
    pOs = [k.ps("b_pO%d" % i, [128, 512]) for i in range(2)]
    pO = pOs[0]
    pCIs = [k.ps("b_pCI%d" % i, [128, 4, 128]) for i in range(2)]
    pT = k.ps("b_pT", [128, 4, 128])
    pTb = k.ps("b_pTb", [128, 4, 128], BF16)

    kcT = k.sb("b_kcT", [64, 2, NCT * 128], BF16)
    Rv = k.sb("b_R", [128, NCT, 2, 128], BF16)
    k.memset(kcT, 0.0)
    k.memset(Rv, 0.0, eng="pool")
    ovv = c_ovl1.rearrange("(ct p) c -> p ct c", p=128)
    for ct in range(NCT):
        for g in range(2):
            k.dma(Rv[:, ct, g, 64:128], ovv[:, ct, 1:65])
    with ExitStack() as es_c:
        es_prev = k.es
        k.es = es_c
        kc2 = k.sb("b_kc2", [128, T], BF16)
        hid = k.sb("b_hid", [128, NCT * 128], BF16)
        k.memset(hid, 0.0)
        kcn = k.sb("b_kcn", [128, NCT * 2, 64])
        k.memset(kcn, 0.0)
        gk0 = k.sb("b_gk0", [128, 64])
        k.dma(gk0, k_norm_g[0].partition_broadcast(128))
        ropec = k.sb("b_ropec", [128, NCT, 16])
        k.memset(ropec, 0.0)
        rv = c_rope.rearrange("(c s) f -> c s f", s=16)
        for ct in range(NCT):
            k.dma(ropec[0:crow(ct), ct, :], rv[1 + ct * 128:1 + ct * 128 + crow(ct), 15, :])
        w1 = [k.sb("b_w1%d" % j, [128, 16, 128], BF16) for j in range(2)]
        w2 = [k.sb("b_w2%d" % j, [128, 64], BF16) for j in range(2)]
        pes = [k.sb("b_pe%d" % j, [128, 16], BF16) for j in range(2)]
        bvec = [k.sb("b_bv%d" % j, [128, 1]) for j in range(2)]
        for j in range(2):
            k.dma(w1[j], cmp_w1[j].rearrange("(l p) h -> p l h", p=128), q="pool")
            k.dma(w2[j], cmp_w2[j], q="pool")
            k.dma(pes[j], cmp_pe[j].rearrange("(l two) d -> (two d) l", two=2), q="pool",
                  allow_slow_non_contiguous=True)
        v16 = kc2.rearrange("p (c s) -> p c s", s=16)
        for j in range(2):
            k.mm(pO[:, 0:1], [(w1[j][:, l2, :], pes[j][:, l2:l2 + 1]) for l2 in range(16)])
            k.cp(bvec[j], pO[:, 0:1])
            for g in range(2):
                n = 12 + 2 * j + g
                k.dma(kc2[0:64, :], FT[n])
                k.dma(kc2[64:128, 0:T - 1], FT[n][:, 1:T])
                pairs = []
                for l2 in range(16):
                    rhs = v16[:, 0:NCMP, 2 * l2] if l2 < 8 else v16[:, 1:NCMP + 1, 2 * l2 - 16]
                    pairs.append((w1[j][:, l2, :], rhs))
                k.mm(pS[0][:, 0:NCMP], pairs)
                k.act(hid[:, 0:NCMP], pS[0][:, 0:NCMP], AF.Silu, bias=bvec[j])
                for ct in range(NCT):
                    r = crow(ct)
                    k.mm(pT[0:r, ct, 0:64], [(hid[:, ct * 128:ct * 128 + r], w2[j])])
                    if j == 0:
                        k.cp(kcn[0:r, ct * 2 + g, :], pT[0:r, ct, 0:64])
                    else:
                        k.cp(Rv[0:r, ct, g, 0:64], pT[0:r, ct, 0:64])
        NS = NCT * 2
        ksq = k.sb("b_ksq", [128, NS, 64])
        kss = k.sb("b_kss", [128, NS])
        krs = k.sb("b_krs", [128, NS])
        kcb = k.sb("b_kcb", [128, NS, 64], BF16)
        ra = k.sb("b_ra", [128, 2, 8])
        rb = k.sb("b_rb", [128, 2, 8])
        k.tt(ksq, kcn, kcn, ALU.mult)
        k.rsum(kss, ksq)
        rms_rstd(k, krs, kss, 64)
        k.tt(kcn, kcn, krs.unsqueeze(2).to_broadcast([128, NS, 64]), ALU.mult)
        k.tt(kcn, kcn, gk0.unsqueeze(1).to_broadcast([128, NS, 64]), ALU.mult)
        k.cp(kcb, kcn)
        for ct in range(NCT):
            sl = slice(ct * 2, ct * 2 + 2)
            cosb = ropec[:, ct:ct + 1, 0:8].to_broadcast([128, 2, 8])
            sinb = ropec[:, ct:ct + 1, 8:16].to_broadcast([128, 2, 8])
            k.tt(ra, kcn[:, sl, 0:8], cosb, ALU.mult)
            k.tt(rb, kcn[:, sl, 8:16], sinb, ALU.mult)
            k.tt(kcb[:, sl, 0:8], ra, rb, ALU.subtract)
            k.tt(ra, kcn[:, sl, 8:16], cosb, ALU.mult)
            k.tt(rb, kcn[:, sl, 0:8], sinb, ALU.mult)
            k.tt(kcb[:, sl, 8:16], ra, rb, ALU.add)
        for ct in range(NCT):
            r = crow(ct)
            for g in range(2):
                k.trs([(pTb[0:64, 0, 0:r], kcb[0:r, ct * 2 + g, :])], ident[0:r, 0:r])
                k.cp(kcT[:, g, ct * 128:ct * 128 + r], pTb[0:64, 0, 0:r])
        k.barrier()
        k.es = es_prev

    if B_STAGE < 1:
        return
    QA = [k.sb("b_QA%d" % i, [128, 8, 512], BF16) for i in range(2)]
    cmT = k.sb("b_cmT", [128, NCT, 512], BF16)
    gts = k.sb("b_gts", [128, 4, 24])
    vis = k.sb("b_vis", [128, 4, 64])
    cadd = k.sb("b_cadd", [128, 4, 64])
    NP = 5
    P = [k.sb("b_P%d" % i, [128, 512], BF16) for i in range(NP)]
    ocmp = k.sb("b_ocmp", [128, 4, 8, 64])
    osel = k.sb("b_osel", [128, 4, 8, 64])
    owin = k.sb("b_owin", [128, 4, 8, 64])
    oTs = [k.sb("b_oT%d" % i, [65, 512]) for i in range(2)]
    recs = [k.sb("b_rec%d" % i, [128, 4, 1]) for i in range(2)]
    dens = [k.sb("b_den%d" % i, [128, 4]) for i in range(2)]
    imp = k.sb("b_imp", [128, 4, 2, 64])
    itmps = [k.sb("b_itmp%d" % i, [128, 4, 64]) for i in range(2)]
    NB = 3
    mxs = [k.sb("b_mx%d" % i, [128, 8]) for i in range(NB)]
    mx2s = [k.sb("b_mx2%d" % i, [128, 8]) for i in range(NB)]
    wks = [k.sb("b_wk%d" % i, [128, 64]) for i in range(NB)]
    mks = [k.sb("b_mk%d" % i, [128, 64]) for i in range(NB)]
    negm4 = k.sb("b_negm4", [128, 4, 128], BF16)
    k.memset(negm4, 0.0)
    osq = k.sb("b_osq", [128, 4, 8, 64])
    oss = k.sb("b_oss", [128, 32])
    ors = k.sb("b_ors", [128, 32])
    onb = k.sb("b_onb", [128, 4, 512], BF16)
    mixs = k.sb("b_mixs", [128, 4, 128], BF16)
    ftq = FT.rearrange("n d t -> d n t")
    gtv = GT.rearrange("(s p) c -> p s c", p=128)
    visv = c_vis.rearrange("(s p) c -> p s c", p=128)
    caddv = c_cadd.rearrange("(s p) c -> p s c", p=128)
    cmv = c_cmask.rearrange("(ct p) t -> p ct t", p=128)
    mixv = MIXT.rearrange("(c p) t -> p c t", p=128)
    cnt = [0]
    ocnt = [0]

    def kt_gen(Qa, h, g, kt, lo, hi, mcol, mtile, Ksrc, Vsrc, kdim, pOb, first, last, zero_first, evac):
        i = cnt[0]
        cnt[0] += 1
        ps, Pb = pS[i % 2], P[i % NP]
        if first and zero_first:
            k.mm1(pOb[0:65, :], zer, Qa[:, h, :], True, False)
        k.mm(ps[:, lo:hi], [(Ksrc[0:kdim, g, kt * 128:(kt + 1) * 128], Qa[0:kdim, h, lo:hi])])
        yield
        k.act(Pb[:, lo:hi], ps[:, lo:hi], AF.Exp, scale=0.125)
        yield
        if mtile is not None:
            k.tt(Pb[:, mcol:mcol + 128], Pb[:, mcol:mcol + 128], mtile, ALU.mult)
            yield
        k.mm1(pOb[0:65, lo:hi], Vsrc[:, kt, g, :], Pb[:, lo:hi], (first and not zero_first), last)
        yield
        if last:
            yield from evac_gen(*evac)

    def evac_gen(pOb, oTb, rc, h, odst):
        k.cp(oTb, pOb[0:65, :], eng="act")
        yield
        k.trs([(pT[:, s, 0:65], oTb[:, s * 128:(s + 1) * 128]) for s in range(4)], identf[0:65, 0:65])
        yield
        k.recip(rc, pT[:, :, 64:65])
        yield
        k.tt(odst[:, :, h, :], pT[:, :, 0:64], rc.to_broadcast([128, 4, 64]), ALU.mult)
        yield

    def attend_gens(Qa, h, g, kts, Ksrc, Vsrc, kdim, odst, zero_first):
        j = ocnt[0]
        ocnt[0] += 1
        pOb, oTb, rc = pOs[j % 2], oTs[j % 2], recs[j % 2]
        n = len(kts)
        for idx, (kt, lo, hi, mcol, mtile) in enumerate(kts):
            yield kt_gen(Qa, h, g, kt, lo, hi, mcol, mtile, Ksrc, Vsrc, kdim, pOb,
                         idx == 0, idx == n - 1, zero_first, (pOb, oTb, rc, h, odst))

    def cmp_gen(Qa, h, g, cts):
        pc, rc, dn, itmp = pCIs[h % 2], recs[h % 2], dens[h % 2], itmps[h % 2]
        for ci, ct in enumerate(cts):
            i = cnt[0]
            cnt[0] += 1
            ps, Pb = pS[i % 2], P[i % NP]
            k.mm(ps, [(kcT[0:64, g, ct * 128:(ct + 1) * 128], Qa[0:64, h, :])])
            yield
            k.act(Pb, ps, AF.Exp, scale=0.125)
            yield
            k.tt(Pb, Pb, cmT[:, ct, :], ALU.mult)
            yield
            for s in range(4):
                k.mm1(pc[:, s, :], Pb[:, s * 128:(s + 1) * 128], Rv[:, ct, g, :],
                      ci == 0 and s == 0, ci == len(cts) - 1)
            yield
        k.rsum(dn, pc[:, :, 64:128])
        yield
        k.ts(rc, dn.unsqueeze(2), 1e-30, None, ALU.add)
        yield
        k.recip(rc, rc)
        yield
        k.tt(ocmp[:, :, h, :], pc[:, :, 0:64], rc.to_broadcast([128, 4, 64]), ALU.mult)
        yield
        if h % 4 == 0:
            k.tt(imp[:, :, g, :], pc[:, :, 64:128], rc.to_broadcast([128, 4, 64]), ALU.mult)
        else:
            k.tt(itmp, pc[:, :, 64:128], rc.to_broadcast([128, 4, 64]), ALU.mult)
            yield
            k.tt(imp[:, :, g, :], imp[:, :, g, :], itmp, ALU.add, eng="pool")
        yield

    def topk_gen(g, s, j):
        iv = imp[:, s, g, :]
        mx, mx2, wk, mk = mxs[j % NB], mx2s[j % NB], wks[j % NB], mks[j % NB]
        k.vmax(mx, iv)
        yield
        k.vmatch(wk, mx, iv, -3.0e38)
        yield
        k.vmax(mx2, wk)
        yield
        k.tt(mk, iv, mx2[:, 7:8].to_broadcast([128, 64]), ALU.is_ge)
        yield
        k.ts(negm4[:, s, 64:128], mk, -NEGM, NEGM, ALU.mult, ALU.add)
        yield

    for Qi in range(NQ):
        t0 = Qi * 512
        Qa = QA[Qi % 2]
        k.dma(Qa[0:64, :, :], ftq[:, 0:8, t0:t0 + 512])
        k.dma(gts, gtv[:, Qi * 4:(Qi + 1) * 4, :])
        k.dma(vis, visv[:, Qi * 4:(Qi + 1) * 4, :])
        k.dma(cadd, caddv[:, Qi * 4:(Qi + 1) * 4, :])
        k.dma(cmT, cmv[:, 0:NCT, t0:t0 + 512])
        cts = [ct for ct in range(NCT) if 16 * 128 * ct + 31 <= t0 + 511]
        pipeline((cmp_gen(Qa, h, h // 4, cts) for h in range(8)), 2)
        for g in range(2):
            k.tt(imp[:, :, g, :], imp[:, :, g, :], vis, ALU.mult)
            k.tt(imp[:, :, g, :], imp[:, :, g, :], cadd, ALU.add)
        for g in range(2):
            pipeline((topk_gen(g, s, g * 4 + s) for s in range(4)), 3)
            k.trs([(pTb[:, s, :], negm4[:, s, :]) for s in range(4)], ident)
            k.cp(Qa[64:128, 4 * g:4 * g + 4, :].rearrange("p h (s t) -> p h s t", s=4),
                 pTb[64:128, :, :].unsqueeze(1).to_broadcast([64, 4, 4, 128]))
        def all_gens():
            for h in range(8):
                g = h // 4
                kts = []
                for kt in range(4 * Qi + 4):
                    m = kt - 4 * Qi
                    if m >= 0:
                        kts.append((kt, 128 * m, 512, 128 * m, tri))
                    else:
                        kts.append((kt, 0, 512, 0, None))
                yield from attend_gens(Qa, h, g, kts, KA, VS1, 128, osel, False)
                kts = []
                for kt in range(max(0, 4 * Qi - 4), 4 * Qi + 4):
                    m = kt - 4 * Qi
                    if m >= 0:
                        kts.append((kt, 128 * m, 512, 128 * m, tri))
                    else:
                        kts.append((kt, 0, 128 * (m + 5), 128 * (m + 4), ntri))
                yield from attend_gens(Qa, h, g, kts, KW, VW1, 64, owin, True)
        pipeline(all_gens(), 5)
        if B_STAGE < 4:
            continue
        for br, ob in enumerate((ocmp, osel, owin)):
            k.tt(ob, ob, gts[:, :, br * 8:(br + 1) * 8].unsqueeze(3).to_broadcast([128, 4, 8, 64]),
                 ALU.mult, eng=("pool" if br == 1 else "dve"))
        k.tt(ocmp, ocmp, osel, ALU.add)
        k.tt(ocmp, ocmp, owin, ALU.add)
        if B_STAGE < 5:
            continue
        k.tt(osq, ocmp, ocmp, ALU.mult)
        k.rsum(oss, osq.rearrange("p s h d -> p (s h) d"))
        rms_rstd(k, ors, oss, 64)
        k.tt(ocmp.rearrange("p s h d -> p (s h) d"), ocmp.rearrange("p s h d -> p (s h) d"),
             ors.unsqueeze(2).to_broadcast([128, 32, 64]), ALU.mult)
        k.tt(onb.rearrange("p s (h d) -> p (s h) d", d=64), ocmp.rearrange("p s h d -> p (s h) d"),
             gout.unsqueeze(1).to_broadcast([128, 32, 64]), ALU.mult)
        if B_STAGE < 6:
            continue
        for s in range(4):
            k.trs([(pTb[:, c, :], onb[:, s, c * 128:(c + 1) * 128]) for c in range(4)], ident)
            k.cp(mixs, pTb)
            if B_STAGE >= 7:
                k.dma(mixv[:, 4:8, t0 + s * 128:t0 + (s + 1) * 128], mixs)


def load_colvec(k, dst, src, n_chunks, identf, pTf, tmp):
    k.dma(tmp[0:n_chunks, :], src.rearrange("(c p) -> c p", p=128))
    k.trs([(pTf[:, 0:n_chunks], tmp[0:n_chunks, :])], identf[0:n_chunks, 0:n_chunks])
    k.cp(dst, pTf[:, 0:n_chunks])


def phase_c(nc, k, T, x, MIXT, w_out, ffn_g, w_up, conv_w, conv_b, w_down, H2, ident, identf):
    TT = 256
    NTT = T // TT
    NF = 22
    wo = k.sb("c_wo", [128, 8, D], BF16)
    wu = k.sb("c_wu", [128, 8, 2 * DFF], BF16)
    wd = k.sb("c_wd", [128, NF, D], BF16)
    wov = w_out.rearrange("(c p) n -> p c n", p=128)
    wuv = w_up.rearrange("(c p) n -> p c n", p=128)
    wdv = w_down.rearrange("(c p) n -> p c n", p=128)
    for c in range(8):
        k.dma(wo[:, c, :], wov[:, c, :], q="pool")
    for c in range(8):
        for hh in range(2):
            k.dma(wu[:, c, hh * DFF:(hh + 1) * DFF], wuv[:, c, hh * DFF:(hh + 1) * DFF], q="pool")
    for c in range(NF):
        k.dma(wd[:, c, :], wdv[:, c, :], q="pool")
    pY = [k.ps("c_pY%d" % i, [128, 512]) for i in range(2)]
    pC = k.ps("c_pC", [128, 8, 128], BF16)
    pUs = [k.ps("c_pU%d" % i, [128, 512]) for i in range(4)]
    tmpv = k.sb("c_tmpv", [44, 128])
    gfT = k.sb("c_gfT", [128, 8])
    load_colvec(k, gfT, ffn_g, 8, identf, pY[0], tmpv)
    cw = k.sb("c_cw", [128, 3, 44])
    cb = k.sb("c_cb", [128, 44])
    for j in range(3):
        load_colvec(k, cw[:, j, :], conv_w[j], 44, identf, pY[0], tmpv)
    load_colvec(k, cb, conv_b, 44, identf, pY[0], tmpv)
    carry = k.sb("c_carry", [128, 44, 2])
    k.memset(carry, 0.0)

    mts = [k.sb("c_mt%d" % i, [128, 8, TT], BF16) for i in range(2)]
    hsbs = [k.sb("c_h%d" % i, [128, 2, D]) for i in range(2)]
    junk1 = k.sb("c_junk", [128, D], BF16)
    junks = [junk1, junk1]
    sss = [k.sb("c_ss%d" % i, [128, 1]) for i in range(2)]
    rstds = [k.sb("c_rstd%d" % i, [128, 1]) for i in range(2)]
    hns = [k.sb("c_hn%d" % i, [128, D], BF16) for i in range(2)]
    hnT = k.sb("c_hnT", [128, 8, TT], BF16)
    actT = k.sb("c_actT", [128, NF, TT], BF16)
    NU = 5
    uraw = [k.sb("c_uraw%d" % i, [128, TT + 2]) for i in range(NU)]
    acc = [k.sb("c_acc%d" % i, [128, TT]) for i in range(NU)]
    sil = [k.sb("c_sil%d" % i, [128, TT]) for i in range(2)]
    psl = [p[:, 0:256] for p in pUs] + [pY[0][:, 0:256], pY[1][:, 0:256]]
    mixv = MIXT.rearrange("(c p) t -> p c t", p=128)
    xv = x.rearrange("(n p) d -> p n d", p=128)
    h2v = H2.rearrange("(n p) d -> p n d", p=128)

    def pro_gen(it, s):
        mt, hsb = mts[it % 2], hsbs[it % 2]
        junk, ss, rstd, hn = junks[s], sss[s], rstds[s], hns[s]
        for hh in range(2):
            py = pY[hh]
            k.mm(py, [(mt[:, c, s * 128:(s + 1) * 128], wo[:, c, hh * 512:(hh + 1) * 512])
                      for c in range(8)])
            yield
            k.tt(hsb[:, s, hh * 512:(hh + 1) * 512], py, hsb[:, s, hh * 512:(hh + 1) * 512], ALU.add)
            yield
        k.act(junk, hsb[:, s, :], AF.Square, accum_out=ss)
        yield
        k.act(rstd, ss, AF.Ln, scale=1.0 / D, bias=EPS)
        yield
        k.act(rstd, rstd, AF.Exp, scale=-0.5)
        yield
        k.act(hn, hsb[:, s, :], AF.Copy, scale=rstd)
        yield
        k.trs([(pC[:, c, :], hn[:, c * 128:(c + 1) * 128]) for c in range(8)], ident)
        yield
        k.tt(hnT[:, :, s * 128:(s + 1) * 128], pC, gfT.unsqueeze(2).to_broadcast([128, 8, 128]),
             ALU.mult)
        yield

    def down_gen(it):
        hsb = hsbs[it % 2]
        for s in range(2):
            for hh in range(2):
                py = pUs[(s * 2 + hh) % 2]
                k.mm(py, [(actT[:, i, s * 128:(s + 1) * 128], wd[:, i, hh * 512:(hh + 1) * 512])
                          for i in range(NF)])
                yield
                k.tt(hsb[:, s, hh * 512:(hh + 1) * 512], py, hsb[:, s, hh * 512:(hh + 1) * 512], ALU.add)
                yield
        k.dma(h2v[:, 2 * it:2 * it + 2, :], hsb)
        yield

    def half_gen(n):
        i, gu = divmod(n, 2)
        ch = i + gu * NF
        pu = psl[n % 6]
        ur, ac = uraw[n % NU], acc[n % NU]
        k.mm(pu, [(wu[:, c, ch * 128:(ch + 1) * 128], hnT[:, c, :]) for c in range(8)])
        yield
        k.cp(ur[:, 0:2], carry[:, ch, :], eng="pool")
        k.cp(ur[:, 2:TT + 2], pu, eng="act")
        yield
        k.act(ac, pu, AF.Identity, scale=cw[:, 2, ch:ch + 1], bias=cb[:, ch:ch + 1])
        yield
        k.stt(ac, ur[:, 1:TT + 1], cw[:, 1, ch:ch + 1], ac, ALU.mult, ALU.add)
        yield
        k.stt(ac, ur[:, 0:TT], cw[:, 0, ch:ch + 1], ac, ALU.mult, ALU.add)
        k.cp(carry[:, ch, :], ur[:, TT:TT + 2], eng="pool")
        yield
        if gu == 1:
            sl = sil[i % 2]
            k.act(sl, acc[(n - 1) % NU], AF.Silu)
            yield
            k.tt(actT[:, i, :], sl, ac, ALU.mult, eng="pool")
            yield

    for it in range(NTT + 1):
        gens = []
        if it >= 1:
            gens.append(down_gen(it - 1))
        if it < NTT:
            t0 = it * TT
            k.dma(mts[it % 2], mixv[:, :, t0:t0 + TT])
            k.dma(hsbs[it % 2], xv[:, 2 * it:2 * it + 2, :])
            gens += [pro_gen(it, 0), pro_gen(it, 1)]
        pipeline(iter(gens), 3)
        if it < NTT:
            pipeline((half_gen(n) for n in range(2 * NF)), 4)


def phase_d(nc, k, T, H2, p_in, pleg_g, w_pleg, w_ple, ple_g, out, ident, identf):
    NT = T // 128
    wg = k.sb("d_wg", [128, 8, D], BF16)
    wp = k.sb("d_wp", [128, 2, D], BF16)
    wgv = w_pleg.rearrange("(c p) n -> p c n", p=128)
    wpv = w_ple.rearrange("(c p) n -> p c n", p=128)
    for c in range(8):
        k.dma(wg[:, c, :], wgv[:, c, :], q="pool")
    for c in range(2):
        k.dma(wp[:, c, :], wpv[:, c, :], q="pool")
    pY = [k.ps("d_pY%d" % i, [128, 512]) for i in range(2)]
    pE4 = [k.ps("d_pE%d" % i, [128, 512]) for i in range(4)]
    pC = k.ps("d_pC", [128, 8, 128], BF16)
    tmpv = k.sb("d_tmpv", [8, 128])
    ggT = k.sb("d_ggT", [128, 8])
    load_colvec(k, ggT, pleg_g, 8, identf, pY[0], tmpv)
    gple = k.sb("d_gple", [128, D])
    k.dma(gple, ple_g.partition_broadcast(128))

    hb = [k.sb("d_h%d" % i, [128, D]) for i in range(2)]
    pb = [k.sb("d_p%d" % i, [128, 256]) for i in range(2)]
    junks = [k.sb("d_junk%d" % i, [128, D], BF16) for i in range(2)]
    sss = [k.sb("d_ss%d" % i, [128, 1]) for i in range(2)]
    rstds = [k.sb("d_rstd%d" % i, [128, 1]) for i in range(2)]
    ss2s = [k.sb("d_ss2%d" % i, [128, 2]) for i in range(2)]
    rstd2s = [k.sb("d_rstd2%d" % i, [128, 1]) for i in range(2)]
    hns = [k.sb("d_hn%d" % i, [128, D], BF16) for i in range(2)]
    hTs = [k.sb("d_hT%d" % i, [128, 8, 128], BF16) for i in range(2)]
    pbfs = [k.sb("d_pbf%d" % i, [128, 256], BF16) for i in range(2)]
    pTs = [k.sb("d_pT%d" % i, [128, 2, 128], BF16) for i in range(2)]
    gates = [k.sb("d_gate%d" % i, [128, D]) for i in range(2)]
    es = [k.sb("d_e%d" % i, [128, D]) for i in range(2)]
    ob = [k.sb("d_o%d" % i, [128, D]) for i in range(2)]

    def d_gen(it):
        t0 = it * 128
        b = it % 2
        h, pp, o = hb[b], pb[b], ob[b]
        junk, ss, rstd, ss2, rstd2 = junks[b], sss[b], rstds[b], ss2s[b], rstd2s[b]
        hn, hT, pbf, pT, gate, e = hns[b], hTs[b], pbfs[b], pTs[b], gates[b], es[b]
        pE = pE4[2 * b:2 * b + 2]
        k.dma(h, H2[t0:t0 + 128, :])
        k.dma(pp, p_in[t0:t0 + 128, :])
        yield
        k.act(junk, h, AF.Square, accum_out=ss)
        yield
        k.act(rstd, ss, AF.Ln, scale=1.0 / D, bias=EPS)
        yield
        k.act(rstd, rstd, AF.Exp, scale=-0.5)
        yield
        k.act(hn, h, AF.Copy, scale=rstd)
        yield
        k.trs([(pC[:, c, :], hn[:, c * 128:(c + 1) * 128]) for c in range(8)], ident)
        yield
        k.tt(hT, pC, ggT.unsqueeze(2).to_broadcast([128, 8, 128]), ALU.mult)
        yield
        k.cp(pbf, pp, eng="pool")
        yield
        k.trs([(pC[:, c, :], pbf[:, c * 128:(c + 1) * 128]) for c in range(2)], ident)
        yield
        k.cp(pT, pC[:, 0:2, :])
        yield
        for hh in range(2):
            k.mm(pY[hh], [(hT[:, c, :], wg[:, c, hh * 512:(hh + 1) * 512]) for c in range(8)])
            yield
            k.act(gate[:, hh * 512:(hh + 1) * 512], pY[hh], AF.Sigmoid)
            yield
        for hh in range(2):
            k.mm(pE[hh], [(pT[:, c, :], wp[:, c, hh * 512:(hh + 1) * 512]) for c in range(2)])
            yield
            k.act(junk[:, hh * 512:(hh + 1) * 512], pE[hh], AF.Square, accum_out=ss2[:, hh:hh + 1])
            yield
        k.tt(ss, ss2[:, 0:1], ss2[:, 1:2], ALU.add)
        yield
        k.act(rstd2, ss, AF.Ln, scale=1.0 / D, bias=EPS)
        yield
        k.act(rstd2, rstd2, AF.Exp, scale=-0.5)
        yield
        for hh in range(2):
            k.act(e[:, hh * 512:(hh + 1) * 512], pE[hh], AF.Copy, scale=rstd2)
            yield
        k.tt(e, e, gple, ALU.mult)
        yield
        k.tt(e, e, gate, ALU.mult)
        yield
        k.tt(o, e, h, ALU.add)
        yield
        tok = k.dma(out[t0:t0 + 128, :], o)
        k.out_toks.append(tok)
        yield

    pipeline((d_gen(it) for it in range(NT)), D_DEPTH)


def _consts(T):
    bf = ml_dtypes.bfloat16
    c = {}
    c["c_ident"] = np.eye(128).astype(bf)
    c["c_identf"] = np.eye(128, dtype=np.float32)
    half = 8
    inv = np.float32(500000.0) ** (-np.arange(half, dtype=np.float32) / half)
    ang = np.arange(T, dtype=np.float32)[:, None] * inv[None, :].astype(np.float32)
    c["c_rope"] = np.concatenate([np.cos(ang), np.sin(ang)], 1).astype(np.float32)
    s = np.arange(128)
    c["c_tri2"] = ((s[:, None] <= s[None, :]) & (s[:, None] // 64 == s[None, :] // 64)).astype(np.float32)
    c["c_chunk"] = (s[:, None] // 64 == np.arange(2)[None, :]).astype(np.float32)
    t = np.arange(T)
    cc = np.arange(256)
    ncmp = T // 16 - 1
    c["c_cmask"] = (((16 * cc[:, None] + 31) <= t[None, :]) & (cc[:, None] < ncmp)).astype(bf)
    c["c_tri"] = (s[:, None] <= s[None, :]).astype(bf)
    c["c_ntri"] = (s[None, :] < s[:, None]).astype(bf)
    n = np.arange(64)
    c["c_E"] = ((t[None, :] // 64) == n[:, None]).astype(bf)
    cur = t // 64
    vis = (n[None, :] * 64 <= t[:, None])
    bonus = np.zeros((T, 64), np.float32)
    bonus += (n[None, :] == 0) * 1.0e6
    bonus += (n[None, :] == cur[:, None]) * 2.0e6
    bonus += (n[None, :] == cur[:, None] - 1) * 4.0e6
    c["c_vis"] = vis.astype(np.float32)
    c["c_cadd"] = np.where(vis, bonus, np.float32(-1e30)).astype(np.float32)
    cs = cc * 16
    ssb = n * 64
    ov = np.clip(np.minimum(cs[:, None] + 32, ssb[None, :] + 64)
                 - np.maximum(cs[:, None], ssb[None, :]), 0, None) / 32.0
    o1 = np.zeros((256, 65), np.float32)
    o1[:, 0] = 1.0
    o1[:, 1:] = ov
    o1[ncmp:] = 0.0
    c["c_ovl1"] = o1.astype(bf)
    return c


_W_NAMES = ["attn_norm_g", "w_in", "hg_norm_g", "nsa_q_norm_g", "nsa_k_norm_g", "cmp_pe", "cmp_w1",
            "cmp_w2", "nsa_out_norm_g", "w_out", "ffn_norm_g", "w_up", "conv_w", "conv_b", "w_down",
            "ple_gate_norm_g", "w_ple_gate", "w_ple", "ple_norm_g"]


def kernel(**inputs):
    x = np.asarray(inputs["x"], np.float32)
    p = np.asarray(inputs["p"], np.float32)
    B, T, _ = x.shape
    nc = build(T=T, dbg=False, phases="ABCD")
    shared = {n: np.ascontiguousarray(np.asarray(inputs[n], np.float32)[0]) for n in _W_NAMES}
    shared["hg_lb_logits"] = np.ascontiguousarray(np.asarray(inputs["hg_lb_logits"], np.float32))
    shared.update(_consts(T))
    in_maps = []
    for b in range(B):
        m = dict(shared)
        m["x"] = np.ascontiguousarray(x[b])
        m["p"] = np.ascontiguousarray(p[0, b])
        in_maps.append(m)
    res = run_bass_kernel_spmd(nc, in_maps, core_ids=list(range(B)))
    return np.stack([np.asarray(r["out"], np.float32) for r in res.results], axis=0)
```
